# Optimizing a Trainium2 kernel written in Bass

```python
import math
import jax, jax.numpy as jnp
from jax import lax
import numpy as np

D_MODEL = 1024
BATCH = 32
SEQ = 2048
DEPTH = 2

CTX_LEN = 256
GRID_W = 64
EPS = 1e-6

N_MIX_GROUPS = 4
GROUP_W = D_MODEL // N_MIX_GROUPS
D_MIX = N_MIX_GROUPS * GROUP_W

MLA_HEADS = 4
MLA_NOPE = 64
MLA_ROPE = 32
MLA_QK = MLA_NOPE + MLA_ROPE
MLA_V = GROUP_W // MLA_HEADS
MLA_Q_RANK = 3 * D_MODEL // 16
MLA_KV_RANK = D_MODEL // 8
ROPE_BASE = 10000.0
ATTN_BLOCK = 128

RET_HEADS = 4
RET_DK = 64
RET_DV = GROUP_W // RET_HEADS
RET_CHUNK = 128

HY_CH = GROUP_W
HY_CONV = 3
HY_BANDS = 16
HY_EMB = 1 + 2 * HY_BANDS
HY_FFN = 64
HY_FAST_PCT = 0.3
HY_SLOW_PCT = 1.5
HY_TARGET = 1e-2

LRU_W = GROUP_W
LRU_BLOCKS = 4
LRU_BW = LRU_W // LRU_BLOCKS
LRU_CONV = 4
LRU_C = 8.0

MOE_GROUPS = 4
MOE_PER_GROUP = 8
N_EXPERTS = MOE_GROUPS * MOE_PER_GROUP
TOP_K = 2
EXPERT_FF = D_MODEL // 2
MOE_BLOCK = 128

IN_SPLITS = (MLA_Q_RANK, MLA_KV_RANK, MLA_ROPE,
             RET_HEADS * RET_DK, RET_HEADS * RET_DK, GROUP_W, GROUP_W,
             3 * HY_CH, LRU_W, LRU_W)
IN_COLS = sum(IN_SPLITS)

kernel_name = 'hybrid_mla_retnet_hyena_rglru_hmoe_dit'


def rms_norm(x, g):
    xf = x.astype(jnp.float32)
    y = xf * lax.rsqrt(jnp.mean(xf * xf, axis=-1, keepdims=True) + EPS)
    return (y * g.astype(jnp.float32)).astype(x.dtype)


def modulate(h, shift, scale):
    return h * (1 + scale) + shift


def split_cols(z):
    out, o = [], 0
    for w in IN_SPLITS:
        out.append(z[..., o:o + w])
        o += w
    return out


def dwconv(x, w, b, pad_left):
    width, ch = w.shape
    y = lax.conv_general_dilated(x, w[:, None, :].astype(x.dtype), window_strides=(1,),
                                 padding=[(pad_left, width - 1 - pad_left)],
                                 dimension_numbers=('NWC', 'WIO', 'NWC'), feature_group_count=ch)
    return y + b.astype(x.dtype)


def axial_rope_angles(n_tokens):
    rows = n_tokens // GRID_W
    row = jnp.repeat(jnp.arange(rows), GRID_W).astype(jnp.float32)
    col = jnp.tile(jnp.arange(GRID_W), rows).astype(jnp.float32)
    half = MLA_ROPE // 4
    inv_freq = ROPE_BASE ** (-jnp.arange(half, dtype=jnp.float32) / half)
    ang = jnp.stack([row[:, None] * inv_freq, col[:, None] * inv_freq], axis=1)
    return jnp.cos(ang), jnp.sin(ang)


def apply_axial_rope(x, cos, sin):
    b, l, h, _ = x.shape
    xr = x.astype(jnp.float32).reshape(b, l, h, 2, 2, MLA_ROPE // 4)
    x1, x2 = xr[..., 0, :], xr[..., 1, :]
    cc, ss = cos[None, :, None], sin[None, :, None]
    out = jnp.stack([x1 * cc - x2 * ss, x2 * cc + x1 * ss], axis=-2)
    return out.reshape(b, l, h, MLA_ROPE).astype(x.dtype)


def rope_tail(t, cos, sin):
    return jnp.concatenate([t[..., :MLA_NOPE], apply_axial_rope(t[..., MLA_NOPE:], cos, sin)], axis=-1)


def mla_qkv(cq, ckv, kr, p):
    b, l, _ = cq.shape
    q = (rms_norm(cq, p['mla_q_norm_g']) @ p['mla_w_uq']).reshape(b, l, MLA_HEADS, MLA_QK)
    kv = (rms_norm(ckv, p['mla_kv_norm_g']) @ p['mla_w_ukv']).reshape(b, l, MLA_HEADS, MLA_NOPE + MLA_V)
    k_nope, v = kv[..., :MLA_NOPE], kv[..., MLA_NOPE:]
    k = jnp.concatenate([k_nope, jnp.broadcast_to(kr[:, :, None, :], (b, l, MLA_HEADS, MLA_ROPE))], axis=-1)
    return rms_norm(q, p['mla_qn_g']), rms_norm(k, p['mla_kn_g']), v


def softmax_attend(q, k, v):
    s = jnp.einsum('bqhd,bkhd->bhqk', q, k).astype(jnp.float32) * (MLA_QK ** -0.5)
    pr = jax.nn.softmax(s, axis=-1).astype(v.dtype)
    return jnp.einsum('bhqk,bkhd->bqhd', pr, v)


def blocked_attend(q, k, v):
    b, l, h, d = q.shape
    nb = l // ATTN_BLOCK
    qb = jnp.moveaxis(q.reshape(b, nb, ATTN_BLOCK, h, d), 1, 0)
    ob = lax.map(lambda qq: softmax_attend(qq, k, v), qb)
    return jnp.moveaxis(ob, 0, 1).reshape(b, l, h * v.shape[-1])


def ret_heads(zq, zk, zv):
    b, l, _ = zq.shape
    q = zq.astype(jnp.float32).reshape(b, l, RET_HEADS, RET_DK)
    k = zk.astype(jnp.float32).reshape(b, l, RET_HEADS, RET_DK) * (RET_DK ** -0.5)
    v = zv.astype(jnp.float32).reshape(b, l, RET_HEADS, RET_DV)
    return q, k, v


def retention_state(k, v, log_gamma):
    l = k.shape[1]
    w = jnp.exp((l - 1 - jnp.arange(l, dtype=jnp.float32))[:, None] * log_gamma)
    return jnp.einsum('blhk,blhv,lh->bhkv', k, v, w)


def retention_chunkwise(q, k, v, log_gamma, s0):
    b, l, h, dk = q.shape
    dv = v.shape[-1]
    n = l // RET_CHUNK
    j = jnp.arange(RET_CHUNK, dtype=jnp.float32)
    qc = q.reshape(b, n, RET_CHUNK, h, dk)
    kc = k.reshape(b, n, RET_CHUNK, h, dk)
    vc = v.reshape(b, n, RET_CHUNK, h, dv)
    rel = j[:, None] - j[None, :]
    dmask = jnp.where(rel[None] >= 0,
                      jnp.exp(jnp.maximum(rel, 0.0)[None] * log_gamma[:, None, None]), 0.0)
    inner = jnp.einsum('bnihk,bnjhk->bnhij', qc, kc) * dmask
    inner = jnp.einsum('bnhij,bnjhv->bnihv', inner, vc)
    k_dec = kc * jnp.exp((RET_CHUNK - 1 - j)[:, None] * log_gamma)[:, :, None]
    kv = jnp.einsum('bnjhk,bnjhv->nbhkv', k_dec, vc)
    chunk_decay = jnp.exp(RET_CHUNK * log_gamma)[None, :, None, None]

    def step(state, kv_i):
        return state * chunk_decay + kv_i, state

    _, prev = lax.scan(step, s0, kv)
    q_dec = qc * jnp.exp((j + 1)[:, None] * log_gamma)[:, :, None]
    cross = jnp.einsum('bnihk,nbhkv->bnihv', q_dec, prev)
    return (inner + cross).reshape(b, l, h, dv)


def retention_bidir(q, k, v, lg, s_fwd, s_bwd):
    fwd = retention_chunkwise(q, k, v, lg[0], s_fwd)
    bwd = retention_chunkwise(q[:, ::-1], k[:, ::-1], v[:, ::-1], lg[1], s_bwd)
    return fwd + bwd[:, ::-1]


def retention_out(o, zg, p):
    b, l = o.shape[0], o.shape[1]
    o = o * lax.rsqrt(jnp.mean(o * o, axis=-1, keepdims=True) + EPS)
    o = o.reshape(b, l, GROUP_W) * p['ret_norm_g'].astype(jnp.float32)
    return (o * jax.nn.silu(zg.astype(jnp.float32))).astype(zg.dtype)


def hyena_filter(n, p):
    f32 = jnp.float32
    t = jnp.linspace(0.0, 1.0, n, dtype=f32)[:, None]
    bands = jnp.linspace(1e-4, HY_BANDS - 1, HY_BANDS, dtype=f32)
    w = 2.0 * math.pi * jnp.arange(n, dtype=f32)[:, None] / n
    z = jnp.concatenate([t, jnp.cos(bands * w), -jnp.sin(bands * w)], axis=-1)
    freq = p['hy_freq'].astype(f32)
    hdn = jnp.sin(freq * (z @ p['hy_w1'].astype(f32) + p['hy_b1'].astype(f32)))
    hdn = jnp.sin(freq * (hdn @ p['hy_w2'].astype(f32) + p['hy_b2'].astype(f32)))
    filt = hdn @ p['hy_w3'].astype(f32)
    max_decay = math.log(HY_TARGET) / HY_FAST_PCT
    min_decay = math.log(HY_TARGET) / HY_SLOW_PCT
    deltas = jnp.abs(jnp.linspace(min_decay, max_decay, HY_CH, dtype=f32))
    window = jnp.exp(-t * deltas)
    h_fwd = filt[:, :HY_CH] * window
    h_bwd = filt[:, HY_CH:] * window
    return jnp.concatenate([h_fwd, jnp.zeros((1, HY_CH), f32), h_bwd[1:][::-1]], axis=0)


def hyena_mix(zh, p):
    l = zh.shape[1]
    u = dwconv(zh, p['hy_conv_w'], p['hy_conv_b'], HY_CONV // 2)
    x0, x1, v = jnp.split(u, 3, axis=-1)
    s = (x1 * v).astype(jnp.float32)
    hf = jnp.fft.rfft(hyena_filter(l, p), axis=0)
    y = jnp.fft.irfft(jnp.fft.rfft(s, n=2 * l, axis=1) * hf[None], n=2 * l, axis=1)[:, :l]
    y = y + s * p['hy_d'].astype(jnp.float32)
    return (x0.astype(jnp.float32) * y).astype(zh.dtype)


def rglru_scan(x, wa, ba, wx, bx, lam, h0):
    b, l, _ = x.shape
    xb = x.reshape(b, l, LRU_BLOCKS, LRU_BW)
    r = jax.nn.sigmoid(jnp.einsum('blnd,nde->blne', xb, wa).reshape(b, l, LRU_W) + ba)
    i = jax.nn.sigmoid(jnp.einsum('blnd,nde->blne', xb, wx).reshape(b, l, LRU_W) + bx)
    log_a = -LRU_C * r * jax.nn.softplus(-lam)
    a = jnp.exp(log_a)
    bt = jnp.sqrt(-jnp.expm1(2.0 * log_a)) * (i * x)
    bt = bt.at[:, 0].add(a[:, 0] * h0)

    def combine(lhs, rhs):
        a_l, b_l = lhs
        a_r, b_r = rhs
        return a_l * a_r, a_r * b_l + b_r

    _, h = lax.associative_scan(combine, (a, bt), axis=1)
    return h


def lru_dir_params(p, d):
    f = lambda t: t[d].astype(jnp.float32)
    return f(p['lru_wa']), f(p['lru_ba']), f(p['lru_wx']), f(p['lru_bx']), f(p['lru_lambda'])


def lru_out(h, zg):
    return (h * jax.nn.gelu(zg.astype(jnp.float32))).astype(zg.dtype)


def merge_groups(parts, p):
    y = jnp.concatenate(parts, axis=-1)
    shp = y.shape
    y = rms_norm(y.reshape(shp[:-1] + (N_MIX_GROUPS, GROUP_W)),
                 p['group_norm_g'].reshape(N_MIX_GROUPS, GROUP_W)).reshape(shp)
    return y @ p['w_out']


def parallel_mixer(a_lat, a_ctx, p, rope_cs, ctx_out):
    f32 = jnp.float32
    b = a_lat.shape[0]
    zl = split_cols(a_lat @ p['w_in'])
    zc = split_cols(a_ctx @ p['w_in'])
    cos, sin = rope_cs
    q_l, k_l, v_l = mla_qkv(zl[0], zl[1], zl[2], p)
    q_l, k_l = rope_tail(q_l, cos, sin), rope_tail(k_l, cos, sin)
    q_c, k_c, v_c = mla_qkv(zc[0], zc[1], zc[2], p)
    y_lat = [blocked_attend(q_l, jnp.concatenate([k_l, k_c], axis=1), jnp.concatenate([v_l, v_c], axis=1))]
    lg = p['ret_log_gamma'].astype(f32)
    rq_l, rk_l, rv_l = ret_heads(zl[3], zl[4], zl[5])
    rq_c, rk_c, rv_c = ret_heads(zc[3], zc[4], zc[5])
    s_f = retention_state(rk_c, rv_c, lg[0])
    s_b = retention_state(rk_c[:, ::-1], rv_c[:, ::-1], lg[1])
    y_lat.append(retention_out(retention_bidir(rq_l, rk_l, rv_l, lg, s_f, s_b), zl[6], p))
    y_lat.append(hyena_mix(zl[7], p))
    pf, pb = lru_dir_params(p, 0), lru_dir_params(p, 1)
    xc = dwconv(zc[8], p['lru_conv_w'], p['lru_conv_b'], LRU_CONV // 2).astype(f32)
    xl = dwconv(zl[8], p['lru_conv_w'], p['lru_conv_b'], LRU_CONV // 2).astype(f32)
    h0 = jnp.zeros((b, LRU_W), f32)
    hc_f = rglru_scan(xc, *pf, h0)
    hc_b = rglru_scan(xc[:, ::-1], *pb, h0)
    h_l = rglru_scan(xl, *pf, hc_f[:, -1]) + rglru_scan(xl[:, ::-1], *pb, hc_b[:, -1])[:, ::-1]
    y_lat.append(lru_out(h_l, zl[9]))
    out_lat = merge_groups(y_lat, p)
    if not ctx_out:
        return out_lat, None
    zero = jnp.zeros_like(s_f)
    n_ctx = a_ctx.shape[1]
    y_ctx = [softmax_attend(q_c, k_c, v_c).reshape(b, n_ctx, GROUP_W),
             retention_out(retention_bidir(rq_c, rk_c, rv_c, lg, zero, zero), zc[6], p),
             hyena_mix(zc[7], p),
             lru_out(hc_f + hc_b[:, ::-1], zc[9])]
    return out_lat, merge_groups(y_ctx, p)


def grouped_experts(h, eid, w, p):
    t, d = h.shape
    n_assign = t * TOP_K
    e_flat = eid.reshape(-1)
    w_flat = w.reshape(-1)
    tok = jnp.arange(n_assign, dtype=jnp.int32) // TOP_K
    counts = jnp.zeros((N_EXPERTS,), jnp.int32).at[e_flat].add(1)
    padded = (counts + MOE_BLOCK - 1) // MOE_BLOCK * MOE_BLOCK
    pad_end = jnp.cumsum(padded)
    pad_start = pad_end - padded
    raw_start = jnp.cumsum(counts) - counts
    order = jnp.argsort(e_flat)
    e_sorted = e_flat[order]
    dest = pad_start[e_sorted] + jnp.arange(n_assign, dtype=jnp.int32) - raw_start[e_sorted]
    n_blocks = -(-n_assign // MOE_BLOCK) + N_EXPERTS
    n_slots = n_blocks * MOE_BLOCK
    slot_tok = jnp.full((n_slots,), t, jnp.int32).at[dest].set(tok[order])
    slot_w = jnp.zeros((n_slots,), jnp.float32).at[dest].set(w_flat[order])
    block_exp = jnp.minimum(jnp.searchsorted(pad_end, jnp.arange(n_blocks, dtype=jnp.int32) * MOE_BLOCK,
                                             side='right'), N_EXPERTS - 1)
    h_pad = jnp.concatenate([h, jnp.zeros((1, d), h.dtype)], axis=0)
    xs = h_pad[slot_tok].reshape(n_blocks, MOE_BLOCK, d)

    def expert_block(args):
        xb, e = args
        return (jax.nn.silu(xb @ p['moe_w1'][e]) * (xb @ p['moe_w3'][e])) @ p['moe_w2'][e]

    ys = lax.map(expert_block, (xs, block_exp)).reshape(n_slots, d)
    out = jnp.zeros((t + 1, d), jnp.float32).at[slot_tok].add(ys.astype(jnp.float32) * slot_w[:, None])
    return out[:t].astype(h.dtype)


def hier_moe(h, p):
    t = h.shape[0]
    g_prob = jax.nn.softmax((h @ p['moe_w_group']).astype(jnp.float32), axis=-1)
    g_val, g_idx = lax.top_k(g_prob, 1)
    e_logits = (h @ p['moe_w_expert']).astype(jnp.float32).reshape(t, MOE_GROUPS, MOE_PER_GROUP)
    e_in_group = e_logits[jnp.arange(t), g_idx[:, 0]]
    e_val, e_idx = lax.top_k(e_in_group, TOP_K)
    w = g_val * jax.nn.softmax(e_val, axis=-1)
    eid = g_idx * MOE_PER_GROUP + e_idx
    return grouped_experts(h, eid, w, p)


def trunk_layer(x, ctx, c, c_ctx, p, rope_cs, ctx_out):
    b, s, d = x.shape
    n_ctx = ctx.shape[1]
    mod_l = (jax.nn.silu(c) @ p['w_mod'] + p['b_mod'])[:, None, :]
    mod_c = jax.nn.silu(c_ctx) @ p['w_mod'] + p['b_mod']
    sh1_l, sc1_l, g1_l, sh2_l, sc2_l, g2_l = jnp.split(mod_l, 6, axis=-1)
    sh1_c, sc1_c, g1_c, sh2_c, sc2_c, g2_c = jnp.split(mod_c, 6, axis=-1)
    a_l = modulate(rms_norm(x, p['norm1_g']), sh1_l, sc1_l)
    a_c = modulate(rms_norm(ctx, p['norm1_g']), sh1_c, sc1_c)
    m_l, m_c = parallel_mixer(a_l, a_c, p, rope_cs, ctx_out)
    x = x + g1_l * m_l
    f_l = modulate(rms_norm(x, p['norm2_g']), sh2_l, sc2_l).reshape(b * s, d)
    if ctx_out:
        ctx = ctx + g1_c * m_c
        f_c = modulate(rms_norm(ctx, p['norm2_g']), sh2_c, sc2_c).reshape(b * n_ctx, d)
        ff = hier_moe(jnp.concatenate([f_l, f_c], axis=0), p)
        x = x + g2_l * ff[:b * s].reshape(b, s, d)
        ctx = ctx + g2_c * ff[b * s:].reshape(b, n_ctx, d)
    else:
        x = x + g2_l * hier_moe(f_l, p).reshape(b, s, d)
    return x, ctx


def setup_inputs(seed: int = 0) -> dict:
    key = jax.random.key(seed)
    keys = list(jax.random.split(key, 48))

    def nrm(shape, scale):
        return scale * jax.random.normal(keys.pop(), shape, jnp.float32)

    def gain(shape):
        return 1.0 + 0.05 * jax.random.normal(keys.pop(), shape, jnp.float32)

    L = DEPTH
    x = nrm((BATCH, SEQ, D_MODEL), 1.0)
    c = nrm((BATCH, D_MODEL), 1.0)
    ctx = nrm((BATCH, CTX_LEN, D_MODEL), 1.0)
    c_ctx = nrm((D_MODEL,), 1.0)
    w_mod = nrm((L, D_MODEL, 6 * D_MODEL), 0.3 * D_MODEL ** -0.5)
    b_mod = nrm((L, 6 * D_MODEL), 0.02)
    norm1_g = gain((L, D_MODEL))
    norm2_g = gain((L, D_MODEL))
    w_in = nrm((L, D_MODEL, IN_COLS), D_MODEL ** -0.5)
    mla_q_norm_g = gain((L, MLA_Q_RANK))
    mla_w_uq = nrm((L, MLA_Q_RANK, MLA_HEADS * MLA_QK), MLA_Q_RANK ** -0.5)
    mla_kv_norm_g = gain((L, MLA_KV_RANK))
    mla_w_ukv = nrm((L, MLA_KV_RANK, MLA_HEADS * (MLA_NOPE + MLA_V)), MLA_KV_RANK ** -0.5)
    mla_qn_g = gain((L, MLA_QK))
    mla_kn_g = gain((L, MLA_QK))
    base_lg = jnp.log1p(-jnp.exp2(-5.0 - jnp.arange(RET_HEADS, dtype=jnp.float32)))
    ret_log_gamma = base_lg * gain((L, 2, RET_HEADS))
    ret_norm_g = gain((L, GROUP_W))
    hy_conv_w = nrm((L, HY_CONV, 3 * HY_CH), HY_CONV ** -0.5)
    hy_conv_b = nrm((L, 3 * HY_CH), 0.02)
    hy_w1 = nrm((L, HY_EMB, HY_FFN), HY_EMB ** -0.5)
    hy_b1 = nrm((L, HY_FFN), 0.02)
    hy_w2 = nrm((L, HY_FFN, HY_FFN), HY_FFN ** -0.5)
    hy_b2 = nrm((L, HY_FFN), 0.02)
    hy_w3 = nrm((L, HY_FFN, 2 * HY_CH), 0.1 * HY_FFN ** -0.5)
    hy_freq = gain((L, HY_FFN))
    hy_d = nrm((L, HY_CH), 0.5)
    lru_conv_w = nrm((L, LRU_CONV, LRU_W), LRU_CONV ** -0.5)
    lru_conv_b = nrm((L, LRU_W), 0.02)
    lru_wa = nrm((L, 2, LRU_BLOCKS, LRU_BW, LRU_BW), LRU_BW ** -0.5)
    lru_ba = nrm((L, 2, LRU_W), 0.02)
    lru_wx = nrm((L, 2, LRU_BLOCKS, LRU_BW, LRU_BW), LRU_BW ** -0.5)
    lru_bx = nrm((L, 2, LRU_W), 0.02)
    u = jax.random.uniform(keys.pop(), (L, 2, LRU_W), jnp.float32, 0.9, 0.999)
    a0 = u ** (1.0 / LRU_C)
    lru_lambda = jnp.log(a0) - jnp.log1p(-a0)
    group_norm_g = gain((L, D_MIX))
    w_out = nrm((L, D_MIX, D_MODEL), D_MIX ** -0.5)
    moe_w_group = nrm((L, D_MODEL, MOE_GROUPS), D_MODEL ** -0.5)
    moe_w_expert = nrm((L, D_MODEL, N_EXPERTS), D_MODEL ** -0.5)
    moe_w1 = nrm((L, N_EXPERTS, D_MODEL, EXPERT_FF), D_MODEL ** -0.5)
    moe_w3 = nrm((L, N_EXPERTS, D_MODEL, EXPERT_FF), D_MODEL ** -0.5)
    moe_w2 = nrm((L, N_EXPERTS, EXPERT_FF, D_MODEL), EXPERT_FF ** -0.5)
    return {'x': x, 'c': c, 'ctx': ctx, 'c_ctx': c_ctx, 'w_mod': w_mod, 'b_mod': b_mod,
            'norm1_g': norm1_g, 'norm2_g': norm2_g, 'w_in': w_in,
            'mla_q_norm_g': mla_q_norm_g, 'mla_w_uq': mla_w_uq, 'mla_kv_norm_g': mla_kv_norm_g,
            'mla_w_ukv': mla_w_ukv, 'mla_qn_g': mla_qn_g, 'mla_kn_g': mla_kn_g,
            'ret_log_gamma': ret_log_gamma, 'ret_norm_g': ret_norm_g,
            'hy_conv_w': hy_conv_w, 'hy_conv_b': hy_conv_b, 'hy_w1': hy_w1, 'hy_b1': hy_b1,
            'hy_w2': hy_w2, 'hy_b2': hy_b2, 'hy_w3': hy_w3, 'hy_freq': hy_freq, 'hy_d': hy_d,
            'lru_conv_w': lru_conv_w, 'lru_conv_b': lru_conv_b, 'lru_wa': lru_wa, 'lru_ba': lru_ba,
            'lru_wx': lru_wx, 'lru_bx': lru_bx, 'lru_lambda': lru_lambda,
            'group_norm_g': group_norm_g, 'w_out': w_out,
            'moe_w_group': moe_w_group, 'moe_w_expert': moe_w_expert,
            'moe_w1': moe_w1, 'moe_w3': moe_w3, 'moe_w2': moe_w2}


def reference(x, c, ctx, c_ctx, w_mod, b_mod, norm1_g, norm2_g, w_in,
              mla_q_norm_g, mla_w_uq, mla_kv_norm_g, mla_w_ukv, mla_qn_g, mla_kn_g,
              ret_log_gamma, ret_norm_g,
              hy_conv_w, hy_conv_b, hy_w1, hy_b1, hy_w2, hy_b2, hy_w3, hy_freq, hy_d,
              lru_conv_w, lru_conv_b, lru_wa, lru_ba, lru_wx, lru_bx, lru_lambda,
              group_norm_g, w_out, moe_w_group, moe_w_expert, moe_w1, moe_w3, moe_w2):
    rope_cs = axial_rope_angles(x.shape[1])
    for l in range(DEPTH):
        p = dict(w_mod=w_mod[l], b_mod=b_mod[l], norm1_g=norm1_g[l], norm2_g=norm2_g[l], w_in=w_in[l],
                 mla_q_norm_g=mla_q_norm_g[l], mla_w_uq=mla_w_uq[l], mla_kv_norm_g=mla_kv_norm_g[l],
                 mla_w_ukv=mla_w_ukv[l], mla_qn_g=mla_qn_g[l], mla_kn_g=mla_kn_g[l],
                 ret_log_gamma=ret_log_gamma[l], ret_norm_g=ret_norm_g[l],
                 hy_conv_w=hy_conv_w[l], hy_conv_b=hy_conv_b[l], hy_w1=hy_w1[l], hy_b1=hy_b1[l],
                 hy_w2=hy_w2[l], hy_b2=hy_b2[l], hy_w3=hy_w3[l], hy_freq=hy_freq[l], hy_d=hy_d[l],
                 lru_conv_w=lru_conv_w[l], lru_conv_b=lru_conv_b[l], lru_wa=lru_wa[l], lru_ba=lru_ba[l],
                 lru_wx=lru_wx[l], lru_bx=lru_bx[l], lru_lambda=lru_lambda[l],
                 group_norm_g=group_norm_g[l], w_out=w_out[l],
                 moe_w_group=moe_w_group[l], moe_w_expert=moe_w_expert[l],
                 moe_w1=moe_w1[l], moe_w3=moe_w3[l], moe_w2=moe_w2[l])
        x, ctx = trunk_layer(x, ctx, c, c_ctx, p, rope_cs, l < DEPTH - 1)
    return x
```

```python
import math
from contextlib import ExitStack
import numpy as np
import ml_dtypes
import concourse.bass as bass
import concourse.mybir as mybir
from concourse.bass_utils import run_bass_kernel_spmd

F32 = mybir.dt.float32
BF16 = mybir.dt.bfloat16
AF = mybir.ActivationFunctionType
ALU = mybir.AluOpType
AX = mybir.AxisListType
ENGS = ("pe", "act", "dve", "pool", "sp")

D = 1024
SEQ = 2048
NCTX = 256
TB = SEQ + NCTX
DEPTH = 2
EPS = 1e-6
INC = 2656
SPARSE_MOE = True
STRICT_SCATTER = False


class Prog:
    def __init__(self, nc):
        self.nc = nc
        self.q = {e: [] for e in ENGS}
        self.cnt = {e: 0 for e in ENGS}
        self.known = {e: {} for e in ENGS}
        self.w = {}
        self.r = {}
        self.dsem = {}
        self.semobj = {}
        for e in ENGS:
            self.semobj[("c", e)] = nc.alloc_semaphore(name="c_" + e)
        self.out_waits = {}
        self.n_ins = 0
        self.n_wait = 0
        self.free_hw = []
        self.free_sw = []
        self.nsem = 0

    def _deps(self, e, reads, writes, skip_self):
        deps = {}
        for k in reads:
            for s, v in self.w.get(k, {}).items():
                if deps.get(s, -1) < v:
                    deps[s] = v
        for k in writes:
            for d in (self.w.get(k, {}), self.r.get(k, {})):
                for s, v in d.items():
                    if deps.get(s, -1) < v:
                        deps[s] = v
        waits = []
        kn = self.known[e]
        for s, v in deps.items():
            if skip_self and s == ("c", e):
                continue
            if kn.get(s, 0) >= v:
                continue
            kn[s] = v
            waits.append((s, v))
        return waits

    def _update(self, me, reads, writes):
        s, v = me
        for k in writes:
            if self.r.get(k):
                self.w[k] = {s: v}
                self.r[k] = {}
            else:
                self.w.setdefault(k, {})[s] = v
        for k in reads:
            self.r.setdefault(k, {})[s] = v

    def op(self, e, fn, reads=(), writes=(), skip_self=False):
        waits = self._deps(e, reads, writes, skip_self)
        self.cnt[e] += 1
        self._update((("c", e), self.cnt[e]), reads, writes)
        self.q[e].append((waits, fn, (("c", e), 1)))
        self.n_ins += 1
        self.n_wait += len(waits)

    def dma(self, fn, reads=(), writes=(), dkey=None, q="sp", is_output=False):
        if dkey is None:
            dkey = writes[0]
        waits = self._deps(q, reads, writes, False)
        if dkey not in self.dsem:
            pool = self.free_sw if q == "pool" else self.free_hw
            if pool:
                ent = pool.pop()
            else:
                self.nsem += 1
                ent = [("d", self.nsem), 0, q == "pool"]
                self.semobj[ent[0]] = self.nc.alloc_semaphore(name="d%d" % self.nsem)
            self.dsem[dkey] = ent
        ent = self.dsem[dkey]
        assert ent[2] == (q == "pool"), "DMA semaphore shared between software and hardware DGE: %r" % (dkey,)
        if ent[2] and ent[1] > 0 and self.known[q].get(ent[0], 0) < ent[1]:
            self.known[q][ent[0]] = ent[1]
            waits.append((ent[0], ent[1]))
        ent[1] += 16
        self._update((ent[0], ent[1]), reads, writes)
        self.q[q].append((waits, fn, (ent[0], 16)))
        if is_output:
            self.out_waits[ent[0]] = ent[1]
        self.n_ins += 1
        self.n_wait += len(waits)

    def barrier(self):
        allv = {("c", e): self.cnt[e] for e in ENGS if self.cnt[e] > 0}
        for ent in list(self.dsem.values()) + self.free_hw + self.free_sw:
            if ent[1] > 0:
                allv[ent[0]] = ent[1]
        for e in ENGS:
            kn = self.known[e]
            waits = []
            for s, v in allv.items():
                if kn.get(s, 0) < v:
                    kn[s] = v
                    waits.append((s, v))
            if waits:
                self.q[e].append((waits, None, None))
        self.w = {}
        self.r = {}
        for ent in self.dsem.values():
            (self.free_sw if ent[2] else self.free_hw).append(ent)
        self.dsem = {}

    def emit(self):
        nc = self.nc
        final = list(self.out_waits.items())
        with nc.Block() as block:
            def run(e):
                def body(eng):
                    for waits, fn, inc in self.q[e]:
                        for s, v in waits:
                            eng.wait_ge(self.semobj[s], v)
                        if fn is not None:
                            fn(eng).then_inc(self.semobj[inc[0]], inc[1])
                    if e == "sp":
                        for s, v in final:
                            eng.wait_ge(self.semobj[s], v)
                return body
            block.tensor(run("pe"))
            block.scalar(run("act"))
            block.vector(run("dve"))
            block.gpsimd(run("pool"))
            block.sync(run("sp"))
        self.q = {e: [] for e in ENGS}
        self.out_waits = {}


class Rec:
    def __init__(self, sfx, local):
        self.ops, self.sfx, self.local = [], sfx, local

    def _k(self, keys):
        return [(x + self.sfx) if (isinstance(x, str) and x in self.local) else x for x in keys]

    def op(self, e, fn, reads=(), writes=(), skip_self=False):
        self.ops.append((0, e, fn, self._k(reads), self._k(writes), skip_self))

    def dma(self, fn, reads=(), writes=(), dkey=None, q="sp", is_output=False):
        if dkey is None:
            dkey = writes[0]
        self.ops.append((1, fn, self._k(reads), self._k(writes), self._k([dkey])[0], q, is_output))


def replay(P, recs):
    idx = [0] * len(recs)
    live = True
    while live:
        live = False
        for j, r in enumerate(recs):
            if idx[j] < len(r.ops):
                o = r.ops[idx[j]]
                idx[j] += 1
                live = True
                if o[0] == 0:
                    P.op(o[1], o[2], reads=o[3], writes=o[4], skip_self=o[5])
                else:
                    P.dma(o[1], reads=o[2], writes=o[3], dkey=o[4], q=o[5], is_output=o[6])


class K:
    pass


_UID = [0]


def _u(name):
    _UID[0] += 1
    return "%s_%d" % (name, _UID[0])


def _tiles(es, nc, specs):
    out = {}
    for name, shape, dt in specs:
        out[name] = es.enter_context(nc.sbuf_tensor(_u(name), list(shape), dt))
    return out


def _psum(es, nc, name, shape, dt):
    return es.enter_context(nc.psum_tensor(_u(name), list(shape), dt))


def stage_mod(k):
    nc, P, NB = k.nc, k.P, k.NB
    R = NB + 1
    with ExitStack() as es:
        t = _tiles(es, nc, [("crow", (R, D), F32), ("srow", (R, D), F32), ("scT", (128, 8, R), F32),
                            ("wm0", (128, 8, 512), F32), ("wm1", (128, 8, 512), F32),
                            ("brow", (R, 6 * D), F32), ("mrow", (R, 6 * D), F32), ("idf", (128, 128), F32)])
        pt = _psum(es, nc, "pt0", [128, 512], F32)
        pm = [_psum(es, nc, "pm%d" % i, [128, 512], F32) for i in range(2)]
        P.dma(lambda e: e.dma_start(out=t["idf"][:], in_=k.identf), writes=["idf"])
        P.dma(lambda e: e.dma_start(out=t["crow"][0:NB, :], in_=k.c), writes=["crow"])
        P.dma(lambda e: e.dma_start(out=t["crow"][NB:R, :], in_=k.c_ctx.rearrange("(o d) -> o d", o=1)), writes=["crow"])
        P.op("act", lambda e: e.activation(out=t["srow"][:], in_=t["crow"][:], func=AF.Silu), reads=["crow"], writes=["srow"])
        for kk in range(8):
            P.op("pe", lambda e, kk=kk: e.transpose(out=pt[:, 0:R], in_=t["srow"][:, kk * 128:(kk + 1) * 128], identity=t["idf"][0:R, 0:R]),
                 reads=["srow", "idf"], writes=["pt"])
            P.op("dve", lambda e, kk=kk: e.tensor_copy(out=t["scT"][:, kk, :], in_=pt[:, 0:R]), reads=["pt"], writes=["scT"])
        for l in range(DEPTH):
            P.dma(lambda e, l=l: e.dma_start(out=t["brow"][:], in_=k.b_mod[l].partition_broadcast(R)), writes=["brow"])
            for n in range(12):
                wt = t["wm%d" % (n % 2)]
                wk = "wm%d" % (n % 2)
                P.dma(lambda e, l=l, n=n, wt=wt: e.dma_start(out=wt[:], in_=k.w_mod[l][:, n * 512:(n + 1) * 512].rearrange("(k p) n -> p k n", p=128)),
                      writes=[wk], q="sp")
                ps = pm[n % 2]
                pk = "pm%d" % (n % 2)
                for kk in range(8):
                    P.op("pe", lambda e, kk=kk, wt=wt, ps=ps: e.matmul(ps[0:R, :], lhsT=t["scT"][:, kk, :], rhs=wt[:, kk, :], start=(kk == 0), stop=(kk == 7)),
                         reads=["scT", wk], writes=[pk], skip_self=(kk > 0))
                P.op("dve", lambda e, n=n, ps=ps: e.tensor_tensor(out=t["mrow"][:, n * 512:(n + 1) * 512], in0=ps[0:R, :], in1=t["brow"][:, n * 512:(n + 1) * 512], op=ALU.add),
                     reads=[pk, "brow"], writes=["mrow"])
            P.dma(lambda e, l=l: e.dma_start(out=k.modrow[l], in_=t["mrow"][:]), reads=["mrow"], writes=[("modrow", l)])
        P.barrier()
        P.emit()


def load_mod_cols(k, es, l, which):
    nc, P, NB = k.nc, k.P, k.NB
    R = NB + 1
    A = es.enter_context(nc.sbuf_tensor(_u("modA"), [128, 8, R], F32))
    B = es.enter_context(nc.sbuf_tensor(_u("modB"), [128, 8, R], F32))
    with ExitStack() as e2:
        t = _tiles(e2, nc, [("mr", (R + 1, 2 * D), F32), ("gT", (128, 8, R + 1), F32), ("idf2", (128, 128), F32)])
        pt = _psum(e2, nc, "ptm", [128, 512], F32)
        base = 0 if which == 0 else 3 * D
        g = k.norm1_g if which == 0 else k.norm2_g
        P.dma(lambda e: e.dma_start(out=t["idf2"][:], in_=k.identf), writes=["idf2"])
        P.dma(lambda e: e.dma_start(out=t["mr"][0:R, :], in_=k.modrow[l][:, base:base + 2 * D]), reads=[("modrow", l)], writes=["mr"])
        P.dma(lambda e: e.dma_start(out=t["mr"][R:R + 1, 0:D], in_=g[l].rearrange("(o d) -> o d", o=1)), writes=["mr"])
        P.dma(lambda e: e.dma_start(out=t["mr"][R:R + 1, D:2 * D], in_=g[l].rearrange("(o d) -> o d", o=1)), writes=["mr"])
        for half, dst in ((0, B), (1, A)):
            for kk in range(8):
                c0 = half * D + kk * 128
                P.op("pe", lambda e, c0=c0: e.transpose(out=pt[:, 0:R + 1], in_=t["mr"][:, c0:c0 + 128], identity=t["idf2"][0:R + 1, 0:R + 1]),
                     reads=["mr", "idf2"], writes=["ptm"])
                if half == 0:
                    P.op("dve", lambda e, kk=kk: e.tensor_copy(out=B[:, kk, :], in_=pt[:, 0:R]), reads=["ptm"], writes=["modB"])
                else:
                    P.op("dve", lambda e, kk=kk: e.tensor_copy(out=t["gT"][:, kk, :], in_=pt[:, 0:R + 1]), reads=["ptm"], writes=["gT"])
                    P.op("dve", lambda e, kk=kk: e.tensor_scalar(out=A[:, kk, :], in0=t["gT"][:, kk, 0:R], scalar1=1.0, scalar2=t["gT"][:, kk, R:R + 1],
                                                               op0=ALU.add, op1=ALU.mult), reads=["gT"], writes=["modA"])
        P.barrier()
    return A, B


def xsrc(k, l, b, i):
    if l == 0:
        if i < 16:
            return k.x[b, i * 128:(i + 1) * 128, :]
        return k.ctx[b, (i - 16) * 128:(i - 15) * 128, :]
    t0 = b * TB + i * 128
    return k.x2[t0:t0 + 128, :]


def norm_mod_T(k, t, pst, xt_ap, xkey, A, B, b, dstT, dkeyT, col0, tag):
    P = k.P
    ss, rstd, xn = t["ss" + tag], t["rstd" + tag], t["xn" + tag]
    P.op("act", lambda e: e.activation(out=t["junk" + tag][:], in_=xt_ap, func=AF.Square, accum_out=ss[:]), reads=[xkey], writes=["junk" + tag, "ss" + tag])
    P.op("act", lambda e: e.activation(out=ss[:], in_=ss[:], func=AF.Sqrt, scale=1.0 / D, bias=EPS), reads=["ss" + tag], writes=["ss" + tag])
    P.op("dve", lambda e: e.reciprocal(out=rstd[:], in_=ss[:]), reads=["ss" + tag], writes=["rstd" + tag])
    P.op("dve", lambda e: e.tensor_scalar(out=xn[:], in0=xt_ap, scalar1=rstd[:], scalar2=None, op0=ALU.mult), reads=[xkey, "rstd" + tag], writes=["xn" + tag])
    for kk in range(8):
        P.op("pe", lambda e, kk=kk: e.transpose(out=pst[:, kk, :], in_=xn[:, kk * 128:(kk + 1) * 128], identity=t["idb"][:]),
             reads=["xn" + tag, "idb"], writes=["pst"])
    for kk in range(8):
        eng = "act" if kk % 2 == 0 else "dve"
        if eng == "act":
            P.op("act", lambda e, kk=kk: e.activation(out=dstT[:, kk, col0:col0 + 128], in_=pst[:, kk, :], func=AF.Identity,
                                                     scale=A[:, kk, b:b + 1], bias=B[:, kk, b:b + 1]), reads=["pst", "modA", "modB"], writes=[dkeyT])
        else:
            P.op("dve", lambda e, kk=kk: e.tensor_scalar(out=dstT[:, kk, col0:col0 + 128], in0=pst[:, kk, :], scalar1=A[:, kk, b:b + 1], scalar2=B[:, kk, b:b + 1],
                                                        op0=ALU.mult, op1=ALU.add), reads=["pst", "modA", "modB"], writes=[dkeyT])


TM_BLOCKS = ((0, 352, 0), (608, 512, 352), (1120, 256, 864))
ZT_W = 1120
FM_COLS = [352, 480, 608, 736] + [1376 + 128 * i for i in range(6)] + [2144, 2272, 2400, 2528]
ZF_ROWS = 128 * len(FM_COLS)
ZF_RQ, ZF_RK, ZF_ZH, ZF_LX, ZF_LG = 0, 256, 512, 1280, 1536


def stage_inproj(k, l):
    nc, P, NB = k.nc, k.P, k.NB
    with ExitStack() as es:
        A, B = load_mod_cols(k, es, l, 0)
        t = _tiles(es, nc, [("win", (128, 8, INC), BF16), ("wst0", (128, INC), F32), ("wst1", (128, INC), F32),
                            ("idf", (128, 128), F32), ("idb", (128, 128), BF16),
                            ("xt0", (128, D), F32), ("xt1", (128, D), F32),
                            ("junk0", (128, D), BF16), ("junk1", (128, D), BF16),
                            ("ss0", (128, 1), F32), ("ss1", (128, 1), F32), ("rstd0", (128, 1), F32), ("rstd1", (128, 1), F32),
                            ("xn0", (128, D), BF16), ("xn1", (128, D), BF16),
                            ("aT0", (128, 8, 512), BF16), ("aT1", (128, 8, 512), BF16),
                            ("zf0", (128, 14, 512), BF16), ("zf1", (128, 14, 512), BF16),
                            ("zt0", (128, ZT_W), BF16), ("zt1", (128, ZT_W), BF16)])
        pst = _psum(es, nc, "pst", [128, 8, 128], BF16)
        pf = [_psum(es, nc, "pf%d" % i, [128, 512], F32) for i in range(3)]
        pq = [_psum(es, nc, "pq%d" % i, [128, 512], F32) for i in range(3)]
        P.dma(lambda e: e.dma_start(out=t["idf"][:], in_=k.identf), writes=["idf"])
        P.op("dve", lambda e: e.tensor_copy(out=t["idb"][:], in_=t["idf"][:]), reads=["idf"], writes=["idb"])
        for kk in range(8):
            ws = t["wst%d" % (kk % 2)]
            wk = "wst%d" % (kk % 2)
            P.dma(lambda e, kk=kk, ws=ws: e.dma_start(out=ws[:], in_=k.w_in[l][kk * 128:(kk + 1) * 128, :]), writes=[wk], q="sp")
            P.op("pool", lambda e, kk=kk, ws=ws: e.tensor_copy(out=t["win"][:, kk, :], in_=ws[:]), reads=[wk], writes=["win"])
        zf_v = k.zf.rearrange("(c p) t -> p c t", p=128)
        gi = 0
        ti = 0
        for b in range(NB):
            for (g0, W) in ((0, 512), (512, 512), (1024, 512), (1536, 512), (2048, 256)):
                aT = t["aT%d" % (gi % 2)]
                ak = "aT%d" % (gi % 2)
                nt = W // 128
                for j in range(nt):
                    tag = str(ti % 2)
                    i = g0 // 128 + j
                    P.dma(lambda e, b=b, i=i, tag=tag: e.dma_start(out=t["xt" + tag][:], in_=xsrc(k, l, b, i)),
                          reads=([("x2", b, i)] if l > 0 else []), writes=["xt" + tag], q="sp")
                    norm_mod_T(k, t, pst, t["xt" + tag][:], "xt" + tag, A, B, (b if i < 16 else NB), aT, ak, j * 128, tag)
                    zt = t["zt" + tag]
                    for bi, (c0, w, d0) in enumerate(TM_BLOCKS):
                        for kk in range(8):
                            P.op("pe", lambda e, kk=kk, c0=c0, w=w, j=j, bi=bi, aT=aT: e.matmul(pq[bi][:, 0:w], lhsT=aT[:, kk, j * 128:(j + 1) * 128], rhs=t["win"][:, kk, c0:c0 + w],
                                                                                  start=(kk == 0), stop=(kk == 7)),
                                 reads=[ak, "win"], writes=[("pq", bi)], skip_self=(kk > 0))
                        if bi == 1:
                            P.op("act", lambda e, zt=zt: e.activation(out=zt[:, 352:608], in_=pq[1][:, 0:256], func=AF.Copy, scale=0.125), reads=[("pq", 1)], writes=["zt" + tag])
                            P.op("dve", lambda e, zt=zt: e.tensor_copy(out=zt[:, 608:864], in_=pq[1][:, 256:512]), reads=[("pq", 1)], writes=["zt" + tag])
                        elif bi == 0:
                            P.op("act", lambda e, zt=zt: e.copy(out=zt[:, 0:352], in_=pq[0][:, 0:352]), reads=[("pq", 0)], writes=["zt" + tag])
                        else:
                            P.op("dve", lambda e, zt=zt: e.tensor_copy(out=zt[:, 864:1120], in_=pq[2][:, 0:256]), reads=[("pq", 2)], writes=["zt" + tag])
                    t0 = b * TB + g0 + j * 128
                    P.dma(lambda e, zt=zt, t0=t0: e.dma_start(out=k.zt[t0:t0 + 128, :], in_=zt[:]), reads=["zt" + tag], writes=[("ztd", l, b, i)], dkey=("ztd", tag))
                    ti += 1
                zfs = t["zf%d" % (gi % 2)]
                zk = "zf%d" % (gi % 2)
                for ci, c0 in enumerate(FM_COLS):
                    ps = pf[ci % 3]
                    for kk in range(8):
                        P.op("pe", lambda e, kk=kk, c0=c0, ps=ps, aT=aT, W=W: e.matmul(ps[:, 0:W], lhsT=t["win"][:, kk, c0:c0 + 128], rhs=aT[:, kk, 0:W], start=(kk == 0), stop=(kk == 7)),
                             reads=[ak, "win"], writes=[("pf", ci % 3)], skip_self=(kk > 0))
                    if ci in (2, 3):
                        P.op("act", lambda e, ci=ci, ps=ps, W=W, zfs=zfs: e.activation(out=zfs[:, ci, 0:W], in_=ps[:, 0:W], func=AF.Copy, scale=0.125), reads=[("pf", ci % 3)], writes=[zk])
                    elif ci % 2 == 0:
                        P.op("act", lambda e, ci=ci, ps=ps, W=W, zfs=zfs: e.copy(out=zfs[:, ci, 0:W], in_=ps[:, 0:W]), reads=[("pf", ci % 3)], writes=[zk])
                    else:
                        P.op("dve", lambda e, ci=ci, ps=ps, W=W, zfs=zfs: e.tensor_copy(out=zfs[:, ci, 0:W], in_=ps[:, 0:W]), reads=[("pf", ci % 3)], writes=[zk])
                t0 = b * TB + g0
                for ci in range(len(FM_COLS)):
                    P.dma(lambda e, zfs=zfs, t0=t0, W=W, ci=ci: e.dma_start(out=zf_v[:, ci, t0:t0 + W], in_=zfs[:, ci, 0:W]), reads=[zk], writes=[("zfd", l, b, g0)],
                          dkey=("zfd", gi % 2))
                gi += 1
        P.barrier()
        P.emit()


def attn_core(k, pfx, QT_h, KT_h, V_h, q0, W, key_tiles, transform, ps_s, ps_o, pt, dv, rkeys, okey):
    P = k.P
    nt = W // 128
    for n, kt in enumerate(key_tiles):
        sb = k.cc % 2
        k.cc += 1
        P.op("pe", lambda e, kt=kt, sb=sb: e.matmul(ps_s[sb][:, 0:W], lhsT=KT_h[:, kt * 128:(kt + 1) * 128], rhs=QT_h[:, q0:q0 + W], start=True, stop=True),
             reads=rkeys, writes=[pfx + "ps_s%d" % sb])
        transform(kt, ps_s[sb], pfx + "ps_s%d" % sb, pt[sb], pfx + "pt%d" % sb)
        for sidx in range(nt):
            P.op("pe", lambda e, kt=kt, sb=sb, sidx=sidx, n=n: e.matmul(ps_o[sidx][:, 0:dv], lhsT=pt[sb][:, sidx * 128:(sidx + 1) * 128], rhs=V_h(kt),
                                                                  start=(n == 0), stop=(n == len(key_tiles) - 1)),
                 reads=[pfx + "pt%d" % sb] + rkeys, writes=[okey + str(sidx)], skip_self=(n > 0))


def rms_rstd(k, t, src_ap, srckey, junk, jkey, ss, sskey, n):
    P = k.P
    P.op("act", lambda e: e.activation(out=junk, in_=src_ap, func=AF.Square, accum_out=ss), reads=[srckey], writes=[jkey, sskey])
    P.op("act", lambda e: e.activation(out=ss, in_=ss, func=AF.Sqrt, scale=1.0 / n, bias=EPS), reads=[sskey], writes=[sskey])
    P.op("dve", lambda e: e.reciprocal(out=ss, in_=ss), reads=[sskey], writes=[sskey])


def transpose_to(k, t, pstile, pskey, src_blocks, dst_ap, dstkey, srckeys, npart, eng="act"):
    P = k.P
    for i, blk in enumerate(src_blocks):
        w = blk.shape[-1]
        P.op("pe", lambda e, i=i, blk=blk, w=w: e.transpose(out=pstile[0:w, i, :], in_=blk, identity=t["idb"][:]), reads=srckeys + ["idb"], writes=[pskey])
    n = len(src_blocks)
    if eng == "act":
        P.op("act", lambda e: e.copy(out=dst_ap, in_=pstile[0:npart, 0:n, :]), reads=[pskey], writes=[dstkey])
    else:
        P.op("dve", lambda e: e.tensor_copy(out=dst_ap, in_=pstile[0:npart, 0:n, :]), reads=[pskey], writes=[dstkey])


def store_yT(k, t, ytm, ykey, nt, row0, tcol0, pstile, pskey, l):
    P = k.P
    W = nt * 128
    yts = t["yts%d" % (k.yc % 2)]
    ytk = "yts%d" % (k.yc % 2)
    k.yc += 1
    for c in range(2):
        for sidx in range(nt):
            P.op("pe", lambda e, c=c, sidx=sidx: e.transpose(out=pstile[:, sidx, :], in_=ytm[:, sidx, c * 128:(c + 1) * 128], identity=t["idb"][:]),
                 reads=[ykey, "idb"], writes=[pskey])
        P.op("act", lambda e, c=c: e.copy(out=yts[:, c, 0:W], in_=pstile[:, 0:nt, :]), reads=[pskey], writes=[ytk])
        P.dma(lambda e, c=c: e.dma_start(out=k.yT[row0 + c * 128:row0 + (c + 1) * 128, tcol0:tcol0 + W], in_=yts[:, c, 0:W]), reads=[ytk],
              writes=[("yTd", l, row0, c, tcol0)], dkey=("yTd", ytk))


def stage_mla(k, l):
    nc, P, NB = k.nc, k.P, k.NB
    with ExitStack() as es:
        t = _tiles(es, nc, [("idf", (128, 128), F32), ("idb", (128, 128), BF16),
                            ("wuqf", (128, 2, 384), F32), ("wuq", (128, 2, 384), BF16), ("gq", (128, 2), F32),
                            ("wukvf", (128, 512), F32), ("wukv", (128, 512), BF16), ("gkv", (128, 1), F32),
                            ("qng", (128, 96), F32), ("kng", (128, 96), F32),
                            ("cos", (128, 16, 16), F32), ("sin", (128, 16, 16), F32),
                            ("QT", (96, 4, TB), BF16), ("KT", (96, 4, TB), BF16), ("V", (128, 18, 4, 65), BF16),
                            ("zin0", (128, 352), BF16), ("zin1", (128, 352), BF16),
                            ("junk", (128, 384), F32), ("ssq", (128, 1), F32), ("ssk", (128, 1), F32),
                            ("cqn", (128, 192), BF16), ("ckvn", (128, 128), BF16),
                            ("cqT", (128, 2, 128), BF16), ("ckvT", (128, 1, 128), BF16),
                            ("sq", (128, 4, 96), F32), ("ssh", (128, 4), F32),
                            ("qn", (128, 4, 96), F32), ("kfull", (128, 4, 96), F32), ("qf", (128, 4, 96), BF16),
                            ("r1", (128, 4, 2, 8), F32), ("r2", (128, 4, 2, 8), F32),
                            ("pt0", (128, 512), BF16), ("pt1", (128, 512), BF16),
                            ("rec", (128, 4), F32), ("ytm0", (128, 4, 256), BF16), ("ytm1", (128, 4, 256), BF16),
                            ("yts0", (128, 2, 512), BF16), ("yts1", (128, 2, 512), BF16)])
        pstb = _psum(es, nc, "pstb", [128, 8, 128], BF16)
        pskv = _psum(es, nc, "pskv", [128, 512], F32)
        psq = pskv
        ps_s = [_psum(es, nc, "pss%d" % i, [128, 512], F32) for i in range(2)]
        ps_o = [_psum(es, nc, "pso%d" % i, [128, 512], F32) for i in range(4)]
        P.dma(lambda e: e.dma_start(out=t["idf"][:], in_=k.identf), writes=["idf"])
        P.op("dve", lambda e: e.tensor_copy(out=t["idb"][:], in_=t["idf"][:]), reads=["idf"], writes=["idb"])
        P.op("pool", lambda e: e.memset(t["wuqf"][:], 0.0), writes=["wuqf"])
        P.op("pool", lambda e: e.memset(t["gq"][:], 0.0), writes=["gq"])
        P.dma(lambda e: e.dma_start(out=t["wuqf"][:, 0, :], in_=k.mla_w_uq[l][0:128, :]), writes=["wuqf"])
        P.dma(lambda e: e.dma_start(out=t["wuqf"][0:64, 1, :], in_=k.mla_w_uq[l][128:192, :]), writes=["wuqf"])
        P.dma(lambda e: e.dma_start(out=t["gq"][:, 0:1], in_=k.mla_q_norm_g[l][0:128].rearrange("(p o) -> p o", o=1)), writes=["gq"])
        P.dma(lambda e: e.dma_start(out=t["gq"][0:64, 1:2], in_=k.mla_q_norm_g[l][128:192].rearrange("(p o) -> p o", o=1)), writes=["gq"])
        for c in range(2):
            P.op("dve", lambda e, c=c: e.tensor_scalar(out=t["wuq"][:, c, :], in0=t["wuqf"][:, c, :], scalar1=t["gq"][:, c:c + 1], scalar2=None, op0=ALU.mult),
                 reads=["wuqf", "gq"], writes=["wuq"])
        P.dma(lambda e: e.dma_start(out=t["wukvf"][:], in_=k.mla_w_ukv[l]), writes=["wukvf"])
        P.dma(lambda e: e.dma_start(out=t["gkv"][:], in_=k.mla_kv_norm_g[l].rearrange("(p o) -> p o", o=1)), writes=["gkv"])
        P.op("dve", lambda e: e.tensor_scalar(out=t["wukv"][:], in0=t["wukvf"][:], scalar1=t["gkv"][:, 0:1], scalar2=None, op0=ALU.mult), reads=["wukvf", "gkv"], writes=["wukv"])
        P.dma(lambda e: e.dma_start(out=t["qng"][:], in_=k.mla_qn_g[l].partition_broadcast(128)), writes=["qng"])
        P.dma(lambda e: e.dma_start(out=t["kng"][:], in_=k.mla_kn_g[l].partition_broadcast(128)), writes=["kng"])
        P.op("dve", lambda e: e.tensor_scalar(out=t["qng"][:], in0=t["qng"][:], scalar1=96.0 ** -0.5, scalar2=None, op0=ALU.mult), reads=["qng"], writes=["qng"])
        P.dma(lambda e: e.dma_start(out=t["cos"][:], in_=k.rope_cos.rearrange("(n p) f -> p n f", p=128)), writes=["cos"])
        P.dma(lambda e: e.dma_start(out=t["sin"][:], in_=k.rope_sin.rearrange("(n p) f -> p n f", p=128)), writes=["sin"])
        P.op("pool", lambda e: e.memset(t["V"][:], 1.0), writes=["V"])

        def head_norm_rope(src_ps_or_sb, srckey, gains, dstT, dstkey, i, col0, is_lat):
            P.op("act", lambda e: e.activation(out=t["sq"][:], in_=src_ps_or_sb, func=AF.Square), reads=[srckey], writes=["sq"])
            P.op("dve", lambda e: e.tensor_reduce(out=t["ssh"][:], in_=t["sq"][:], axis=AX.X, op=ALU.add), reads=["sq"], writes=["ssh"])
            P.op("act", lambda e: e.activation(out=t["ssh"][:], in_=t["ssh"][:], func=AF.Sqrt, scale=1.0 / 96, bias=EPS), reads=["ssh"], writes=["ssh"])
            P.op("dve", lambda e: e.reciprocal(out=t["ssh"][:], in_=t["ssh"][:]), reads=["ssh"], writes=["ssh"])
            P.op("dve", lambda e: e.tensor_tensor(out=t["qn"][:], in0=src_ps_or_sb, in1=t["ssh"][:].unsqueeze(2).to_broadcast([128, 4, 96]), op=ALU.mult),
                 reads=[srckey, "ssh"], writes=["qn"])
            if is_lat:
                P.op("dve", lambda e: e.tensor_tensor(out=t["qn"][:], in0=t["qn"][:], in1=gains[:].unsqueeze(1).to_broadcast([128, 4, 96]), op=ALU.mult),
                     reads=["qn", "qng", "kng"], writes=["qn"])
                P.op("act", lambda e: e.copy(out=t["qf"][:, :, 0:64], in_=t["qn"][:, :, 0:64]), reads=["qn"], writes=["qf"])
                rv = t["qn"][:, :, 64:96].rearrange("p h (a b f) -> p h a b f", a=2, b=2)
                ov = t["qf"][:, :, 64:96].rearrange("p h (a b f) -> p h a b f", a=2, b=2)
                x1, x2 = rv[:, :, :, 0, :], rv[:, :, :, 1, :]
                cs = t["cos"][:, i, :].rearrange("p (a f) -> p a f", a=2).unsqueeze(1).to_broadcast([128, 4, 2, 8])
                sn = t["sin"][:, i, :].rearrange("p (a f) -> p a f", a=2).unsqueeze(1).to_broadcast([128, 4, 2, 8])
                P.op("dve", lambda e: e.tensor_tensor(out=t["r1"][:], in0=x1, in1=cs, op=ALU.mult), reads=["qn", "cos"], writes=["r1"])
                P.op("dve", lambda e: e.tensor_tensor(out=t["r2"][:], in0=x2, in1=sn, op=ALU.mult), reads=["qn", "sin"], writes=["r2"])
                P.op("dve", lambda e: e.tensor_tensor(out=ov[:, :, :, 0, :], in0=t["r1"][:], in1=t["r2"][:], op=ALU.subtract), reads=["r1", "r2"], writes=["qf"])
                P.op("dve", lambda e: e.tensor_tensor(out=t["r1"][:], in0=x2, in1=cs, op=ALU.mult), reads=["qn", "cos", "qf"], writes=["r1"])
                P.op("dve", lambda e: e.tensor_tensor(out=t["r2"][:], in0=x1, in1=sn, op=ALU.mult), reads=["qn", "sin", "qf"], writes=["r2"])
                P.op("dve", lambda e: e.tensor_tensor(out=ov[:, :, :, 1, :], in0=t["r1"][:], in1=t["r2"][:], op=ALU.add), reads=["r1", "r2"], writes=["qf"])
            else:
                P.op("dve", lambda e: e.tensor_tensor(out=t["qf"][:], in0=t["qn"][:], in1=gains[:].unsqueeze(1).to_broadcast([128, 4, 96]), op=ALU.mult),
                     reads=["qn", "qng", "kng"], writes=["qf"])
            transpose_to(k, t, pstb, "pstb", [t["qf"][:, h, :] for h in range(4)], dstT[0:96, :, col0:col0 + 128], dstkey, ["qf"], 96)

        for b in range(NB):
            for i in range(18):
                zin = t["zin%d" % (i % 2)]
                zk = "zin%d" % (i % 2)
                t0 = b * TB + i * 128
                is_lat = i < 16
                P.dma(lambda e, zin=zin, t0=t0: e.dma_start(out=zin[:], in_=k.zt[t0:t0 + 128, 0:352]), reads=[("ztd", l, b, i)], writes=[zk])
                rms_rstd(k, t, zin[:, 0:192], zk, t["junk"][:, 0:192], "junk", t["ssq"][:], "ssq", 192)
                P.op("dve", lambda e, zin=zin: e.tensor_scalar(out=t["cqn"][:], in0=zin[:, 0:192], scalar1=t["ssq"][:, 0:1], scalar2=None, op0=ALU.mult), reads=[zk, "ssq"], writes=["cqn"])
                P.op("pe", lambda e: e.transpose(out=pstb[:, 0, :], in_=t["cqn"][:, 0:128], identity=t["idb"][:]), reads=["cqn", "idb"], writes=["pstb"])
                P.op("pe", lambda e: e.transpose(out=pstb[0:64, 1, :], in_=t["cqn"][:, 128:192], identity=t["idb"][:]), reads=["cqn", "idb"], writes=["pstb"])
                P.op("act", lambda e: e.copy(out=t["cqT"][:, 0, :], in_=pstb[:, 0, :]), reads=["pstb"], writes=["cqT"])
                P.op("act", lambda e: e.copy(out=t["cqT"][0:64, 1, :], in_=pstb[0:64, 1, :]), reads=["pstb"], writes=["cqT"])
                P.op("pe", lambda e: e.matmul(psq[:, 0:384], lhsT=t["cqT"][:, 0, :], rhs=t["wuq"][:, 0, :], start=True, stop=False), reads=["cqT", "wuq"], writes=["pskv"])
                P.op("pe", lambda e: e.matmul(psq[:, 0:384], lhsT=t["cqT"][0:64, 1, :], rhs=t["wuq"][0:64, 1, :], start=False, stop=True), reads=["cqT", "wuq"], writes=["pskv"], skip_self=True)
                head_norm_rope(psq[:, 0:384].rearrange("p (h d) -> p h d", h=4), "pskv", t["qng"], t["QT"], "QT", i, i * 128, is_lat)
                rms_rstd(k, t, zin[:, 192:320], zk, t["junk"][:, 0:128], "junk", t["ssk"][:], "ssk", 128)
                P.op("dve", lambda e, zin=zin: e.tensor_scalar(out=t["ckvn"][:], in0=zin[:, 192:320], scalar1=t["ssk"][:, 0:1], scalar2=None, op0=ALU.mult), reads=[zk, "ssk"], writes=["ckvn"])
                transpose_to(k, t, pstb, "pstb", [t["ckvn"][:, :]], t["ckvT"][:, :, :], "ckvT", ["ckvn"], 128)
                P.op("pe", lambda e: e.matmul(pskv[:, :], lhsT=t["ckvT"][:, 0, :], rhs=t["wukv"][:], start=True, stop=True), reads=["ckvT", "wukv"], writes=["pskv"])
                kvv = pskv[:, :].rearrange("p (h d) -> p h d", h=4)
                P.op("act", lambda e, kvv=kvv: e.copy(out=t["kfull"][:, :, 0:64], in_=kvv[:, :, 0:64]), reads=["pskv"], writes=["kfull"])
                P.op("dve", lambda e, zin=zin: e.tensor_copy(out=t["kfull"][:, :, 64:96], in_=zin[:, 320:352].unsqueeze(1).to_broadcast([128, 4, 32])), reads=[zk], writes=["kfull"])
                P.op("act", lambda e, kvv=kvv, i=i: e.copy(out=t["V"][:, i, :, 0:64], in_=kvv[:, :, 64:128]), reads=["pskv"], writes=["V"])
                head_norm_rope(t["kfull"][:], "kfull", t["kng"], t["KT"], "KT", i, i * 128, is_lat)
            qblocks = [(0, 512, list(range(18))), (512, 512, list(range(18))), (1024, 512, list(range(18))), (1536, 512, list(range(18)))]
            if l < DEPTH - 1:
                qblocks.append((2048, 256, [16, 17]))
            for (q0, W, kts) in qblocks:
                nt = W // 128
                ytm = t["ytm%d" % (k.yc % 2)]
                ykey = "ytm%d" % (k.yc % 2)
                for h in range(4):
                    okey = "mla_pso"

                    def tf(kt, pss, psk, ptile, ptk, W=W):
                        P.op("act", lambda e: e.activation(out=ptile[:, 0:W], in_=pss[:, 0:W], func=AF.Exp), reads=[psk], writes=[ptk])
                    attn_core(k, "mla_", t["QT"][0:96, h, :], t["KT"][0:96, h, :], lambda kt, h=h: t["V"][:, kt, h, :], q0, W, kts, tf,
                              ps_s, ps_o, [t["pt0"], t["pt1"]], 65, ["QT", "KT", "V"], okey)
                    for sidx in range(nt):
                        P.op("dve", lambda e, sidx=sidx: e.reciprocal(out=t["rec"][:, sidx:sidx + 1], in_=ps_o[sidx][:, 64:65]), reads=[okey + str(sidx)], writes=["rec"])
                        P.op("dve", lambda e, sidx=sidx, h=h, ytm=ytm: e.tensor_scalar(out=ytm[:, sidx, h * 64:(h + 1) * 64], in0=ps_o[sidx][:, 0:64],
                                                                                     scalar1=t["rec"][:, sidx:sidx + 1], scalar2=None, op0=ALU.mult),
                             reads=[okey + str(sidx), "rec"], writes=[ykey])
                store_yT(k, t, ytm, ykey, nt, 0, b * TB + q0, pstb, "pstb", l)
        P.barrier()
        P.emit()


def stage_ret(k, l):
    nc, P, NB = k.nc, k.P, k.NB
    with ExitStack() as es:
        t = _tiles(es, nc, [("idf", (128, 128), F32), ("idb", (128, 128), BF16),
                            ("rel0", (128, 128), F32), ("mge", (128, 128), F32), ("mle", (128, 128), F32), ("dvals", (128, 18), F32),
                            ("lg", (128, 8), F32), ("nlg", (128, 8), F32), ("biasF", (128, 4, 18), F32), ("biasB", (128, 4, 18), F32),
                            ("e1", (128, 128), F32), ("e2", (128, 128), F32),
                            ("arr", (128, 4, 35, 128), BF16), ("Cx", (128, 4, 2, 16, 128), BF16),
                            ("QTr", (128, 2, TB), BF16), ("KTr", (128, 2, TB), BF16), ("Vr", (128, 18, 256), BF16),
                            ("pt0", (128, 512), BF16), ("pt1", (128, 512), BF16),
                            ("yo", (128, 4, 256), F32), ("sq", (128, 4, 256), F32), ("ssh", (128, 16), F32),
                            ("rng", (128, 256), F32), ("zg0", (128, 4, 256), BF16), ("zg1", (128, 4, 256), BF16), ("gs", (128, 4, 256), F32),
                            ("ytm0", (128, 4, 256), BF16), ("ytm1", (128, 4, 256), BF16),
                            ("yts0", (128, 2, 512), BF16), ("yts1", (128, 2, 512), BF16)])
        pstb = _psum(es, nc, "pstb", [128, 8, 128], BF16)
        ps_s = [_psum(es, nc, "pss%d" % i, [128, 512], F32) for i in range(2)]
        ps_o = [_psum(es, nc, "pso%d" % i, [128, 512], F32) for i in range(4)]
        P.dma(lambda e: e.dma_start(out=t["idf"][:], in_=k.identf), writes=["idf"])
        P.op("dve", lambda e: e.tensor_copy(out=t["idb"][:], in_=t["idf"][:]), reads=["idf"], writes=["idb"])
        for nm, src in (("rel0", k.c_rel0), ("mge", k.c_mge), ("mle", k.c_mle), ("dvals", k.c_dvals)):
            P.dma(lambda e, nm=nm, src=src: e.dma_start(out=t[nm][:], in_=src), writes=[nm])
        P.dma(lambda e: e.dma_start(out=t["lg"][:], in_=k.ret_log_gamma[l].rearrange("a h -> (a h)").partition_broadcast(128)), writes=["lg"])
        P.dma(lambda e: e.dma_start(out=t["rng"][:], in_=k.ret_norm_g[l].partition_broadcast(128)), writes=["rng"])
        P.op("dve", lambda e: e.tensor_scalar(out=t["nlg"][:], in0=t["lg"][:], scalar1=-1.0, scalar2=None, op0=ALU.mult), reads=["lg"], writes=["nlg"])
        for h in range(4):
            P.op("dve", lambda e, h=h: e.tensor_scalar(out=t["biasF"][:, h, :], in0=t["dvals"][:], scalar1=t["lg"][:, h:h + 1], scalar2=None, op0=ALU.mult),
                 reads=["dvals", "lg"], writes=["biasF"])
            P.op("dve", lambda e, h=h: e.tensor_scalar(out=t["biasB"][:, h, :], in0=t["dvals"][:], scalar1=t["lg"][:, 4 + h:5 + h], scalar2=None, op0=ALU.mult),
                 reads=["dvals", "lg"], writes=["biasB"])
        for h in range(4):
            for d in range(1, 18):
                P.op("act", lambda e, h=h, d=d: e.activation(out=t["arr"][:, h, 17 + d, :], in_=t["rel0"][:], func=AF.Exp, scale=t["lg"][:, h:h + 1], bias=t["biasF"][:, h, d:d + 1]),
                     reads=["rel0", "lg", "biasF"], writes=["arr"])
                P.op("act", lambda e, h=h, d=d: e.activation(out=t["arr"][:, h, 17 - d, :], in_=t["rel0"][:], func=AF.Exp, scale=t["nlg"][:, 4 + h:5 + h], bias=t["biasB"][:, h, d:d + 1]),
                     reads=["rel0", "nlg", "biasB"], writes=["arr"])
            P.op("act", lambda e, h=h: e.activation(out=t["e1"][:], in_=t["rel0"][:], func=AF.Exp, scale=t["lg"][:, h:h + 1]), reads=["rel0", "lg"], writes=["e1"])
            P.op("act", lambda e, h=h: e.activation(out=t["e2"][:], in_=t["rel0"][:], func=AF.Exp, scale=t["nlg"][:, 4 + h:5 + h]), reads=["rel0", "nlg"], writes=["e2"])
            P.op("dve", lambda e: e.tensor_tensor(out=t["e1"][:], in0=t["e1"][:], in1=t["mge"][:], op=ALU.mult), reads=["e1", "mge"], writes=["e1"])
            P.op("dve", lambda e: e.tensor_tensor(out=t["e2"][:], in0=t["e2"][:], in1=t["mle"][:], op=ALU.mult), reads=["e2", "mle"], writes=["e2"])
            P.op("dve", lambda e, h=h: e.tensor_tensor(out=t["arr"][:, h, 17, :], in0=t["e1"][:], in1=t["e2"][:], op=ALU.add), reads=["e1", "e2"], writes=["arr"])
            for mi in range(2):
                for qi in range(16):
                    P.op("pool", lambda e, h=h, mi=mi, qi=qi: e.tensor_tensor(out=t["Cx"][:, h, mi, qi, :], in0=t["arr"][:, h, 17 + qi - mi + 2, :],
                                                                              in1=t["arr"][:, h, 17 + qi - 16 - mi, :], op=ALU.add), reads=["arr"], writes=["Cx"])
        zt_v = k.zt.rearrange("(n p) c -> p n c", p=128)
        for b in range(NB):
            for c in range(2):
                P.dma(lambda e, c=c, b=b: e.dma_start(out=t["QTr"][:, c, :], in_=k.zf[ZF_RQ + c * 128:ZF_RQ + (c + 1) * 128, b * TB:(b + 1) * TB]),
                      reads=[("zfd", l, b, g0) for g0 in (0, 512, 1024, 1536, 2048)], writes=["QTr"])
                P.dma(lambda e, c=c, b=b: e.dma_start(out=t["KTr"][:, c, :], in_=k.zf[ZF_RK + c * 128:ZF_RK + (c + 1) * 128, b * TB:(b + 1) * TB]),
                      reads=[("zfd", l, b, g0) for g0 in (0, 512, 1024, 1536, 2048)], writes=["KTr"])
            P.dma(lambda e, b=b: e.dma_start(out=t["Vr"][:], in_=zt_v[:, b * 18:(b + 1) * 18, 608:864]), reads=[("ztd", l, b, i) for i in range(18)], writes=["Vr"])
            qblocks = [(0, 512, list(range(18))), (512, 512, list(range(18))), (1024, 512, list(range(18))), (1536, 512, list(range(18)))]
            if l < DEPTH - 1:
                qblocks.append((2048, 256, [16, 17]))
            for (q0, W, kts) in qblocks:
                nt = W // 128
                qi0 = q0 // 128
                zg = t["zg%d" % (k.yc % 2)]
                zgk = "zg%d" % (k.yc % 2)
                ytm = t["ytm%d" % (k.yc % 2)]
                ykey = "ytm%d" % (k.yc % 2)
                P.dma(lambda e, zg=zg, b=b, qi0=qi0, nt=nt: e.dma_start(out=zg[:, 0:nt, :], in_=zt_v[:, b * 18 + qi0:b * 18 + qi0 + nt, 864:1120]),
                      reads=[("ztd", l, b, i) for i in range(qi0, qi0 + nt)], writes=[zgk])
                for h in range(4):
                    okey = "ret_pso"
                    c, p0 = h // 2, 64 * (h % 2)

                    def tf(kt, pss, psk, ptile, ptk, W=W, h=h, qi0=qi0, nt=nt):
                        if kt < 16 and qi0 < 16:
                            mv = t["arr"][:, h, qi0 - kt + 17:qi0 - kt + 17 + nt, :]
                        elif kt >= 16 and qi0 < 16:
                            mv = t["Cx"][:, h, kt - 16, qi0:qi0 + nt, :]
                        else:
                            mv = t["arr"][:, h, qi0 - kt + 17:qi0 - kt + 17 + nt, :]
                        P.op("dve", lambda e: e.tensor_tensor(out=ptile[:, 0:W].rearrange("p (n c) -> p n c", c=128), in0=pss[:, 0:W].rearrange("p (n c) -> p n c", c=128),
                                                              in1=mv, op=ALU.mult), reads=[psk, "arr", "Cx"], writes=[ptk])
                    attn_core(k, "ret_", t["QTr"][p0:p0 + 64, c, :], t["KTr"][p0:p0 + 64, c, :], lambda kt, h=h: t["Vr"][:, kt, h * 64:(h + 1) * 64], q0, W, kts, tf,
                              ps_s, ps_o, [t["pt0"], t["pt1"]], 64, ["QTr", "KTr", "Vr"], okey)
                    for sidx in range(nt):
                        P.op("act", lambda e, sidx=sidx, h=h: e.copy(out=t["yo"][:, sidx, h * 64:(h + 1) * 64], in_=ps_o[sidx][:, 0:64]), reads=[okey + str(sidx)], writes=["yo"])
                P.op("act", lambda e, nt=nt: e.activation(out=t["sq"][:, 0:nt, :], in_=t["yo"][:, 0:nt, :], func=AF.Square), reads=["yo"], writes=["sq"])
                P.op("dve", lambda e, nt=nt: e.tensor_reduce(out=t["ssh"][:, 0:nt * 4], in_=t["sq"][:, 0:nt, :].rearrange("p n (h d) -> p (n h) d", h=4), axis=AX.X, op=ALU.add),
                     reads=["sq"], writes=["ssh"])
                P.op("act", lambda e, nt=nt: e.activation(out=t["ssh"][:, 0:nt * 4], in_=t["ssh"][:, 0:nt * 4], func=AF.Sqrt, scale=1.0 / 64, bias=EPS), reads=["ssh"], writes=["ssh"])
                P.op("dve", lambda e, nt=nt: e.reciprocal(out=t["ssh"][:, 0:nt * 4], in_=t["ssh"][:, 0:nt * 4]), reads=["ssh"], writes=["ssh"])
                P.op("dve", lambda e, nt=nt: e.tensor_tensor(out=t["yo"][:, 0:nt, :].rearrange("p n (h d) -> p (n h) d", h=4), in0=t["yo"][:, 0:nt, :].rearrange("p n (h d) -> p (n h) d", h=4),
                                                             in1=t["ssh"][:, 0:nt * 4].unsqueeze(2).to_broadcast([128, nt * 4, 64]), op=ALU.mult), reads=["yo", "ssh"], writes=["yo"])
                P.op("dve", lambda e, nt=nt: e.tensor_tensor(out=t["yo"][:, 0:nt, :], in0=t["yo"][:, 0:nt, :], in1=t["rng"][:].unsqueeze(1).to_broadcast([128, nt, 256]), op=ALU.mult),
                     reads=["yo", "rng"], writes=["yo"])
                P.op("act", lambda e, nt=nt, zg=zg: e.activation(out=t["gs"][:, 0:nt, :], in_=zg[:, 0:nt, :], func=AF.Silu), reads=[zgk], writes=["gs"])
                P.op("dve", lambda e, nt=nt, ytm=ytm: e.tensor_tensor(out=ytm[:, 0:nt, :], in0=t["yo"][:, 0:nt, :], in1=t["gs"][:, 0:nt, :], op=ALU.mult), reads=["yo", "gs"], writes=[ykey])
                store_yT(k, t, ytm, ykey, nt, 256, b * TB + q0, pstb, "pstb", l)
        P.barrier()
        P.emit()


BLK5 = ((0, 512), (512, 512), (1024, 512), (1536, 512), (2048, 256))


def stage_lru(k, l):
    nc, P, NB = k.nc, k.P, k.NB
    with ExitStack() as es:
        t = _tiles(es, nc, [("lx", (128, TB), BF16), ("lgz", (128, TB), BF16), ("xc", (128, TB), F32), ("xcb", (128, TB), BF16),
                            ("wconv", (128, 2, 4), F32), ("bconv", (128, 2), F32), ("wst", (128, 128), F32),
                            ("Wbd", (128, 8, 128), BF16), ("bgate", (128, 8), F32), ("lam", (128, 4), F32), ("coef", (128, 4), F32), ("coef2", (128, 4), F32),
                            ("rg", (128, 512), F32), ("ig", (128, 512), F32), ("a2", (128, 512), F32),
                            ("af", (128, TB), F32), ("bf", (128, TB), F32), ("hf", (128, TB), F32), ("hb", (128, TB), F32),
                            ("g1", (128, TB), F32), ("g2", (128, TB), F32), ("yo", (128, TB), BF16)])
        psg = [_psum(es, nc, "psg%d" % i, [128, 512], F32) for i in range(2)]
        for c in range(2):
            for j in range(4):
                P.dma(lambda e, c=c, j=j: e.dma_start(out=t["wconv"][:, c, j:j + 1], in_=k.lru_conv_w[l][j, c * 128:(c + 1) * 128].rearrange("(p o) -> p o", o=1)), writes=["wconv"])
        P.dma(lambda e: e.dma_start(out=t["bconv"][:], in_=k.lru_conv_b[l].rearrange("(c p) -> p c", p=128)), writes=["bconv"])
        for d in range(2):
            P.dma(lambda e, d=d: e.dma_start(out=t["lam"][:, d * 2:d * 2 + 2], in_=k.lru_lambda[l][d].rearrange("(c p) -> p c", p=128)), writes=["lam"])
        for d in range(2):
            for gi, (wsrc, bsrc) in enumerate(((k.lru_wa, k.lru_ba), (k.lru_wx, k.lru_bx))):
                P.dma(lambda e, d=d, gi=gi, bsrc=bsrc: e.dma_start(out=t["bgate"][:, (d * 2 + gi) * 2:(d * 2 + gi) * 2 + 2], in_=bsrc[l][d].rearrange("(c p) -> p c", p=128)), writes=["bgate"])
                for c in range(2):
                    P.op("pool", lambda e: e.memset(t["wst"][:], 0.0), writes=["wst"])
                    for j in range(2):
                        P.dma(lambda e, d=d, c=c, j=j, wsrc=wsrc: e.dma_start(out=t["wst"][64 * j:64 * j + 64, 64 * j:64 * j + 64], in_=wsrc[l][d][2 * c + j]), writes=["wst"])
                    P.op("dve", lambda e, d=d, gi=gi, c=c: e.tensor_copy(out=t["Wbd"][:, (d * 2 + gi) * 2 + c, :], in_=t["wst"][:]), reads=["wst"], writes=["Wbd"])
        P.op("act", lambda e: e.activation(out=t["coef"][:], in_=t["lam"][:], func=AF.Exp, scale=-1.0), reads=["lam"], writes=["coef"])
        P.op("act", lambda e: e.activation(out=t["coef"][:], in_=t["coef"][:], func=AF.Ln, bias=1.0), reads=["coef"], writes=["coef"])
        P.op("dve", lambda e: e.tensor_scalar(out=t["coef2"][:], in0=t["coef"][:], scalar1=-16.0, scalar2=None, op0=ALU.mult), reads=["coef"], writes=["coef2"])
        P.op("dve", lambda e: e.tensor_scalar(out=t["coef"][:], in0=t["coef"][:], scalar1=-8.0, scalar2=None, op0=ALU.mult), reads=["coef"], writes=["coef"])
        for b in range(NB):
            for c in range(2):
                rds = [("zfd", l, b, g0) for g0 in (0, 512, 1024, 1536, 2048)]
                P.dma(lambda e, b=b, c=c: e.dma_start(out=t["lx"][:], in_=k.zf[ZF_LX + c * 128:ZF_LX + (c + 1) * 128, b * TB:(b + 1) * TB]), reads=rds, writes=["lx"])
                P.dma(lambda e, b=b, c=c: e.dma_start(out=t["lgz"][:], in_=k.zf[ZF_LG + c * 128:ZF_LG + (c + 1) * 128, b * TB:(b + 1) * TB]), reads=rds, writes=["lgz"])
                for (r0, r1) in ((0, SEQ), (SEQ, TB)):
                    P.op("dve", lambda e, r0=r0, r1=r1, c=c: e.tensor_scalar(out=t["xc"][:, r0:r1], in0=t["lx"][:, r0:r1], scalar1=t["wconv"][:, c, 2:3], scalar2=t["bconv"][:, c:c + 1],
                                                                          op0=ALU.mult, op1=ALU.add), reads=["lx", "wconv", "bconv"], writes=["xc"])
                    for j, o in ((0, -2), (1, -1), (3, 1)):
                        a0, a1 = max(r0, r0 - o), min(r1, r1 - o)
                        P.op("dve", lambda e, a0=a0, a1=a1, o=o, j=j, c=c: e.scalar_tensor_tensor(out=t["xc"][:, a0:a1], in0=t["lx"][:, a0 + o:a1 + o], scalar=t["wconv"][:, c, j:j + 1],
                                                                                              in1=t["xc"][:, a0:a1], op0=ALU.mult, op1=ALU.add), reads=["lx", "wconv", "xc"], writes=["xc"])
                P.op("act", lambda e: e.copy(out=t["xcb"][:], in_=t["xc"][:]), reads=["xc"], writes=["xcb"])
                for d in range(2):
                    hd = t["hf"] if d == 0 else t["hb"]
                    hk = "hf" if d == 0 else "hb"
                    for (g0, W) in BLK5:
                        for gi, dst in ((0, "rg"), (1, "ig")):
                            ps = psg[gi]
                            P.op("pe", lambda e, ps=ps, d=d, gi=gi, c=c, g0=g0, W=W: e.matmul(ps[:, 0:W], lhsT=t["Wbd"][:, (d * 2 + gi) * 2 + c, :], rhs=t["xcb"][:, g0:g0 + W], start=True, stop=True),
                                 reads=["Wbd", "xcb"], writes=["psg%d" % gi])
                            P.op("act", lambda e, ps=ps, d=d, gi=gi, c=c, W=W, dst=dst: e.activation(out=t[dst][:, 0:W], in_=ps[:, 0:W], func=AF.Sigmoid,
                                                                                                   bias=t["bgate"][:, (d * 2 + gi) * 2 + c:(d * 2 + gi) * 2 + c + 1]),
                                 reads=["psg%d" % gi, "bgate"], writes=[dst])
                        ci = d * 2 + c
                        P.op("act", lambda e, g0=g0, W=W, ci=ci: e.activation(out=t["af"][:, g0:g0 + W], in_=t["rg"][:, 0:W], func=AF.Exp, scale=t["coef"][:, ci:ci + 1]), reads=["rg", "coef"], writes=["af"])
                        P.op("act", lambda e, W=W, ci=ci: e.activation(out=t["a2"][:, 0:W], in_=t["rg"][:, 0:W], func=AF.Exp, scale=t["coef2"][:, ci:ci + 1]), reads=["rg", "coef2"], writes=["a2"])
                        P.op("act", lambda e, W=W: e.activation(out=t["a2"][:, 0:W], in_=t["a2"][:, 0:W], func=AF.Sqrt, scale=-1.0, bias=1.0), reads=["a2"], writes=["a2"])
                        P.op("dve", lambda e, g0=g0, W=W: e.tensor_tensor(out=t["ig"][:, 0:W], in0=t["ig"][:, 0:W], in1=t["xc"][:, g0:g0 + W], op=ALU.mult), reads=["ig", "xc"], writes=["ig"])
                        P.op("dve", lambda e, g0=g0, W=W: e.tensor_tensor(out=t["bf"][:, g0:g0 + W], in0=t["ig"][:, 0:W], in1=t["a2"][:, 0:W], op=ALU.mult), reads=["ig", "a2"], writes=["bf"])
                    if d == 0:
                        P.op("dve", lambda e, hd=hd: e.tensor_tensor_scan(out=hd[:, SEQ:TB], data0=t["af"][:, SEQ:TB], data1=t["bf"][:, SEQ:TB], initial=0.0, op0=ALU.mult, op1=ALU.add),
                             reads=["af", "bf"], writes=[hk])
                        P.op("dve", lambda e, hd=hd: e.tensor_tensor_scan(out=hd[:, 0:SEQ], data0=t["af"][:, 0:SEQ], data1=t["bf"][:, 0:SEQ], initial=hd[:, TB - 1:TB], op0=ALU.mult, op1=ALU.add),
                             reads=["af", "bf", hk], writes=[hk])
                    else:
                        P.op("dve", lambda e, hd=hd: e.tensor_tensor_scan(out=hd[:, SEQ:TB][:, ::-1], data0=t["af"][:, SEQ:TB][:, ::-1], data1=t["bf"][:, SEQ:TB][:, ::-1], initial=0.0,
                                                                         op0=ALU.mult, op1=ALU.add), reads=["af", "bf"], writes=[hk])
                        P.op("dve", lambda e, hd=hd: e.tensor_tensor_scan(out=hd[:, 0:SEQ][:, ::-1], data0=t["af"][:, 0:SEQ][:, ::-1], data1=t["bf"][:, 0:SEQ][:, ::-1], initial=hd[:, SEQ:SEQ + 1],
                                                                         op0=ALU.mult, op1=ALU.add), reads=["af", "bf", hk], writes=[hk])
                P.op("pool", lambda e: e.tensor_tensor(out=t["g1"][:], in0=t["lgz"][:], in1=t["lgz"][:], op=ALU.mult), reads=["lgz"], writes=["g1"])
                P.op("pool", lambda e: e.tensor_scalar(out=t["g1"][:], in0=t["g1"][:], scalar1=0.044715, scalar2=1.0, op0=ALU.mult, op1=ALU.add), reads=["g1"], writes=["g1"])
                P.op("pool", lambda e: e.tensor_tensor(out=t["g1"][:], in0=t["g1"][:], in1=t["lgz"][:], op=ALU.mult), reads=["g1", "lgz"], writes=["g1"])
                P.op("act", lambda e: e.activation(out=t["g2"][:], in_=t["g1"][:], func=AF.Sigmoid, scale=1.5957691216057308), reads=["g1"], writes=["g2"])
                P.op("pool", lambda e: e.tensor_tensor(out=t["g2"][:], in0=t["g2"][:], in1=t["lgz"][:], op=ALU.mult), reads=["g2", "lgz"], writes=["g2"])
                P.op("dve", lambda e: e.tensor_tensor(out=t["hf"][:], in0=t["hf"][:], in1=t["hb"][:], op=ALU.add), reads=["hf", "hb"], writes=["hf"])
                P.op("dve", lambda e: e.tensor_tensor(out=t["yo"][:], in0=t["hf"][:], in1=t["g2"][:], op=ALU.mult), reads=["hf", "g2"], writes=["yo"])
                P.dma(lambda e, b=b, c=c: e.dma_start(out=k.yT[768 + c * 128:768 + (c + 1) * 128, b * TB:(b + 1) * TB], in_=t["yo"][:]), reads=["yo"],
                      writes=[("yTd", l, 768, c, b)], dkey="yo_store")
        P.barrier()
        P.emit()


TWO_PI = 2.0 * math.pi


def stage_hyena(k, l, L, off):
    nc, P, NB = k.nc, k.P, k.NB
    hc = k.hc[L]
    nT = L // 128
    nS = 2 * nT
    W = min(512, L)
    BG = 2 if NB % 2 == 0 else 1
    with ExitStack() as es:
        t = _tiles(es, nc, [("idf", (128, 128), F32), ("idb", (128, 128), BF16),
                            ("HA", (128, nT, 256), BF16), ("HBm", (128, nT, 256), BF16), ("nHB", (128, nT, 256), BF16), ("P40", (128, 256), BF16),
                            ("m0", (128, 2), F32), ("wc", (128, 6, 3), F32), ("bc", (128, 6), F32), ("dcol", (128, 2), F32)])
        P.dma(lambda e: e.dma_start(out=t["idf"][:], in_=k.identf), writes=["idf"])
        P.op("dve", lambda e: e.tensor_copy(out=t["idb"][:], in_=t["idf"][:]), reads=["idf"], writes=["idb"])
        P.dma(lambda e: e.dma_start(out=t["m0"][:], in_=k.c_m0), writes=["m0"])
        for c in range(6):
            for j in range(3):
                P.dma(lambda e, c=c, j=j: e.dma_start(out=t["wc"][:, c, j:j + 1], in_=k.hy_conv_w[l][j, c * 128:(c + 1) * 128].rearrange("(p o) -> p o", o=1)), writes=["wc"])
        P.dma(lambda e: e.dma_start(out=t["bc"][:], in_=k.hy_conv_b[l].rearrange("(c p) -> p c", p=128)), writes=["bc"])
        P.dma(lambda e: e.dma_start(out=t["dcol"][:], in_=k.hy_d[l].rearrange("(c p) -> p c", p=128)), writes=["dcol"])
        with ExitStack() as e2:
            f = _tiles(e2, nc, [("w1", (33, 64), F32), ("w2", (64, 64), F32), ("w3", (64, 512), F32), ("fq", (64, 1), F32), ("b1", (64, 1), F32), ("b2", (64, 1), F32),
                                ("ze", (33, 2, L), F32), ("arg", (64, 512), F32), ("ni", (64, 512), mybir.dt.int32), ("nf", (64, 512), F32), ("npi", (64, 1), F32), ("h1", (64, 512), F32), ("h2", (64, 2, L), F32),
                                ("dl", (128, 256), F32), ("tneg", (128, 2, nT), F32), ("win", (128, 256), F32), ("g", (128, 2, nT, 256), BF16),
                                ("csl", (128, nS), F32), ("csh", (128, nS), F32), ("wf0", (128, nT, 128), BF16), ("wf1", (128, nT, 128), BF16),
                                ("Ht", (128, 256), F32), ("H", (128, nS, 256), F32)])
            pf = [_psum(e2, nc, "hpf%d" % i, [128, 512], F32) for i in range(4)]
            for nm, src in (("w1", k.hy_w1[l]), ("w2", k.hy_w2[l]), ("w3", k.hy_w3[l]), ("dl", hc["dl"]), ("csl", hc["csl"]), ("csh", hc["csh"])):
                P.dma(lambda e, nm=nm, src=src: e.dma_start(out=f[nm][:], in_=src), writes=[nm])
            for nm, src in (("fq", k.hy_freq[l]), ("b1", k.hy_b1[l]), ("b2", k.hy_b2[l])):
                P.dma(lambda e, nm=nm, src=src: e.dma_start(out=f[nm][:], in_=src.rearrange("(p o) -> p o", o=1)), writes=[nm])
            for gi, nm in enumerate(("zf", "zr")):
                P.dma(lambda e, gi=gi, nm=nm: e.dma_start(out=f["ze"][:, gi, :], in_=hc[nm]), writes=["ze"])
            for gi, nm in enumerate(("tf", "tr")):
                P.dma(lambda e, gi=gi, nm=nm: e.dma_start(out=f["tneg"][:, gi, :], in_=hc[nm]), writes=["tneg"])
            P.op("pool", lambda e: e.memset(f["npi"][:], -math.pi), writes=["npi"])
            P.op("dve", lambda e: e.tensor_tensor(out=f["b1"][:], in0=f["b1"][:], in1=f["fq"][:], op=ALU.mult), reads=["b1", "fq"], writes=["b1"])
            P.op("dve", lambda e: e.tensor_tensor(out=f["b2"][:], in0=f["b2"][:], in1=f["fq"][:], op=ALU.mult), reads=["b2", "fq"], writes=["b2"])

            def sin_layer(ps, pk, bias, dst_ap, dkey):
                P.op("dve", lambda e: e.tensor_scalar(out=f["arg"][:, 0:W], in0=ps[0:64, 0:W], scalar1=f["fq"][:, 0:1], scalar2=bias[:, 0:1], op0=ALU.mult, op1=ALU.add),
                     reads=[pk, "fq", "b1", "b2"], writes=["arg"])
                P.op("dve", lambda e: e.tensor_scalar(out=f["arg"][:, 0:W], in0=f["arg"][:, 0:W], scalar1=1.0 / TWO_PI, scalar2=4.5, op0=ALU.mult, op1=ALU.add), reads=["arg"], writes=["arg"])
                P.op("dve", lambda e: e.tensor_copy(out=f["ni"][:, 0:W], in_=f["arg"][:, 0:W]), reads=["arg"], writes=["ni"])
                P.op("dve", lambda e: e.tensor_copy(out=f["nf"][:, 0:W], in_=f["ni"][:, 0:W]), reads=["ni"], writes=["nf"])
                P.op("dve", lambda e: e.tensor_tensor(out=f["arg"][:, 0:W], in0=f["arg"][:, 0:W], in1=f["nf"][:, 0:W], op=ALU.subtract), reads=["arg", "nf"], writes=["arg"])
                P.op("dve", lambda e: e.tensor_scalar(out=f["nf"][:, 0:W], in0=f["arg"][:, 0:W], scalar1=0.0, scalar2=None, op0=ALU.is_lt), reads=["arg", "nf"], writes=["nf"])
                P.op("dve", lambda e: e.tensor_tensor(out=f["arg"][:, 0:W], in0=f["arg"][:, 0:W], in1=f["nf"][:, 0:W], op=ALU.add), reads=["arg", "nf"], writes=["arg"])
                P.op("act", lambda e: e.activation(out=dst_ap, in_=f["arg"][:, 0:W], func=AF.Sin, scale=TWO_PI, bias=f["npi"][:, 0:1]), reads=["arg", "npi"], writes=[dkey])

            for gi in range(2):
                for cb in range(L // W):
                    P.op("pe", lambda e, gi=gi, cb=cb: e.matmul(pf[0][0:64, 0:W], lhsT=f["w1"][:], rhs=f["ze"][:, gi, cb * W:(cb + 1) * W], start=True, stop=True), reads=["w1", "ze"], writes=["hpf0"])
                    sin_layer(pf[0], "hpf0", f["b1"], f["h1"][:, 0:W], "h1")
                    P.op("pe", lambda e: e.matmul(pf[1][0:64, 0:W], lhsT=f["w2"][:], rhs=f["h1"][:, 0:W], start=True, stop=True), reads=["w2", "h1"], writes=["hpf1"])
                    sin_layer(pf[1], "hpf1", f["b2"], f["h2"][:, gi, cb * W:(cb + 1) * W], "h2")
                for tt in range(nT):
                    P.op("pe", lambda e, gi=gi, tt=tt: e.matmul(pf[2][:, :], lhsT=f["h2"][:, gi, tt * 128:(tt + 1) * 128], rhs=f["w3"][:], start=True, stop=True), reads=["h2", "w3"], writes=["hpf2"])
                    P.op("act", lambda e, gi=gi, tt=tt: e.activation(out=f["win"][:], in_=f["dl"][:], func=AF.Exp, scale=f["tneg"][:, gi, tt:tt + 1]), reads=["dl", "tneg"], writes=["win"])
                    P.op("dve", lambda e, gi=gi, tt=tt: e.tensor_tensor(out=f["g"][:, gi, tt, :], in0=pf[2][:, gi * 256:(gi + 1) * 256], in1=f["win"][:], op=ALU.mult), reads=["hpf2", "win"], writes=["g"])
            P.op("dve", lambda e: e.memset(f["g"][0:1, 1, 0, :], 0.0), reads=["g"], writes=["g"])
            for j in range(nS):
                wf = f["wf%d" % (j % 2)]
                wk = "wf%d" % (j % 2)
                P.dma(lambda e, j=j, wf=wf: e.dma_start(out=wf[:], in_=hc["Wf"].rearrange("(t p) s -> p t s", p=128)[:, :, j * 128:(j + 1) * 128]), writes=[wk])
                for gi in range(2):
                    for tt in range(nT):
                        P.op("pe", lambda e, gi=gi, tt=tt, wf=wf: e.matmul(pf[gi][:, 0:256], lhsT=wf[:, tt, :], rhs=f["g"][:, gi, tt, :], start=(tt == 0), stop=(tt == nT - 1)),
                             reads=[wk, "g"], writes=["hpf%d" % gi], skip_self=(tt > 0))
                P.op("dve", lambda e, j=j: e.tensor_scalar(out=f["Ht"][:], in0=pf[0][:, 0:256], scalar1=f["csl"][:, j:j + 1], scalar2=None, op0=ALU.mult), reads=["hpf0", "csl"], writes=["Ht"])
                P.op("dve", lambda e, j=j: e.scalar_tensor_tensor(out=f["H"][:, j, :], in0=pf[1][:, 0:256], scalar=f["csh"][:, j:j + 1], in1=f["Ht"][:], op0=ALU.mult, op1=ALU.add),
                     reads=["hpf1", "csh", "Ht"], writes=["H"])
            HAf, HBf = f["H"][:, 0:nT, :], f["H"][:, nT:nS, :]
            P.op("act", lambda e: e.copy(out=t["HA"][:], in_=HAf), reads=["H"], writes=["HA"])
            P.op("act", lambda e: e.copy(out=t["HBm"][:], in_=HBf), reads=["H"], writes=["HBm"])
            P.op("dve", lambda e: e.tensor_scalar(out=t["HBm"][:, 0, :], in0=f["H"][:, nT, :], scalar1=t["m0"][:, 0:1], scalar2=None, op0=ALU.mult), reads=["H", "m0", "HBm"], writes=["HBm"])
            P.op("dve", lambda e: e.tensor_scalar(out=t["nHB"][:], in0=t["HBm"][:], scalar1=-1.0, scalar2=None, op0=ALU.mult), reads=["HBm"], writes=["nHB"])
            P.op("dve", lambda e: e.tensor_scalar(out=f["Ht"][:], in0=f["H"][:, 0, :], scalar1=t["m0"][:, 0:1], scalar2=None, op0=ALU.mult), reads=["H", "m0"], writes=["Ht"])
            P.op("dve", lambda e: e.scalar_tensor_tensor(out=t["P40"][:], in0=f["H"][:, nT, :], scalar=t["m0"][:, 1:2], in1=f["Ht"][:], op0=ALU.mult, op1=ALU.add), reads=["H", "m0", "Ht"], writes=["P40"])
            P.barrier()
        with ExitStack() as e3:
            g = _tiles(e3, nc, [("zh", (128, 6, L), BF16), ("u1", (128, L), F32), ("u2", (128, L), F32),
                                ("x0c", (128, 2, BG, L), BF16), ("sT", (128, 2, BG, L), BF16), ("stm", (128, nT, BG * 256), BF16),
                                ("Y", (128, nS, BG * 256), BF16), ("wfa", (128, nT, 128), BF16), ("wfb", (128, nT, 128), BF16),
                                ("wiv", (128, nS, W), BF16), ("p1", (128, BG * 256), F32), ("p2", (128, BG * 256), F32),
                                ("tmp", (128, W), F32), ("yo0", (128, W), BF16), ("yo1", (128, W), BF16)])
            pstb = _psum(e3, nc, "hpst", [128, 8, 128], BF16)
            psA = _psum(e3, nc, "hpA", [128, 512], F32)
            psB = _psum(e3, nc, "hpB", [128, 512], F32)
            psI = [_psum(e3, nc, "hpI%d" % i, [128, 512], F32) for i in range(4)]
            wf_v = hc["Wf"].rearrange("(t p) s -> p t s", p=128)
            wi_v = hc["Winv"].rearrange("(s p) t -> p s t", p=128)
            for g0 in range(0, NB, BG):
                for bb in range(BG):
                    b = g0 + bb
                    rds = [("zfd", l, b, x) for x in (0, 512, 1024, 1536, 2048)]
                    for c in range(6):
                        P.dma(lambda e, c=c, b=b: e.dma_start(out=g["zh"][:, c, :], in_=k.zf[ZF_ZH + c * 128:ZF_ZH + (c + 1) * 128, b * TB + off:b * TB + off + L]), reads=rds, writes=["zh"])

                    def conv3(c, dst_ap, dkey):
                        P.op("dve", lambda e: e.tensor_scalar(out=dst_ap, in0=g["zh"][:, c, :], scalar1=t["wc"][:, c, 1:2], scalar2=t["bc"][:, c:c + 1], op0=ALU.mult, op1=ALU.add),
                             reads=["zh", "wc", "bc"], writes=[dkey])
                        P.op("dve", lambda e: e.scalar_tensor_tensor(out=dst_ap[:, 1:L], in0=g["zh"][:, c, 0:L - 1], scalar=t["wc"][:, c, 0:1], in1=dst_ap[:, 1:L], op0=ALU.mult, op1=ALU.add),
                             reads=["zh", "wc", dkey], writes=[dkey])
                        P.op("dve", lambda e: e.scalar_tensor_tensor(out=dst_ap[:, 0:L - 1], in0=g["zh"][:, c, 1:L], scalar=t["wc"][:, c, 2:3], in1=dst_ap[:, 0:L - 1], op0=ALU.mult, op1=ALU.add),
                             reads=["zh", "wc", dkey], writes=[dkey])
                    for c in range(2):
                        conv3(c, g["u1"][:, :], "u1")
                        P.op("act", lambda e, c=c, bb=bb: e.copy(out=g["x0c"][:, c, bb, :], in_=g["u1"][:]), reads=["u1"], writes=["x0c"])
                        conv3(2 + c, g["u1"][:, :], "u1")
                        conv3(4 + c, g["u2"][:, :], "u2")
                        P.op("dve", lambda e, c=c, bb=bb: e.tensor_tensor(out=g["sT"][:, c, bb, :], in0=g["u1"][:], in1=g["u2"][:], op=ALU.mult), reads=["u1", "u2"], writes=["sT"])
                    for c in range(2):
                        for t4 in range(0, nT, 8):
                            n8 = min(8, nT - t4)
                            for i in range(n8):
                                P.op("pe", lambda e, c=c, bb=bb, i=i, t4=t4: e.transpose(out=pstb[:, i, :], in_=g["sT"][:, c, bb, (t4 + i) * 128:(t4 + i + 1) * 128], identity=t["idb"][:]),
                                     reads=["sT", "idb"], writes=["hpst"])
                            P.op("act", lambda e, c=c, bb=bb, t4=t4, n8=n8: e.copy(out=g["stm"][:, t4:t4 + n8, bb * 256 + c * 128:bb * 256 + (c + 1) * 128], in_=pstb[:, 0:n8, :]),
                                 reads=["hpst"], writes=["stm"])
                for j in range(nT):
                    P.dma(lambda e, j=j: e.dma_start(out=g["wfa"][:], in_=wf_v[:, :, j * 128:(j + 1) * 128]), writes=["wfa"])
                    P.dma(lambda e, j=j: e.dma_start(out=g["wfb"][:], in_=wf_v[:, :, (nT + j) * 128:(nT + j + 1) * 128]), writes=["wfb"])
                    for tt in range(nT):
                        P.op("pe", lambda e, tt=tt: e.matmul(psA[:, 0:BG * 256], lhsT=g["wfa"][:, tt, :], rhs=g["stm"][:, tt, :], start=(tt == 0), stop=(tt == nT - 1)),
                             reads=["wfa", "stm"], writes=["hpA"], skip_self=(tt > 0))
                    for tt in range(nT):
                        P.op("pe", lambda e, tt=tt: e.matmul(psB[:, 0:BG * 256], lhsT=g["wfb"][:, tt, :], rhs=g["stm"][:, tt, :], start=(tt == 0), stop=(tt == nT - 1)),
                             reads=["wfb", "stm"], writes=["hpB"], skip_self=(tt > 0))
                    A3 = psA[:, 0:BG * 256].rearrange("p (b c) -> p b c", b=BG)
                    B3 = psB[:, 0:BG * 256].rearrange("p (b c) -> p b c", b=BG)
                    p1 = g["p1"][:].rearrange("p (b c) -> p b c", b=BG)
                    p2 = g["p2"][:].rearrange("p (b c) -> p b c", b=BG)

                    def bc3(tab):
                        return tab.unsqueeze(1).to_broadcast([128, BG, 256])
                    P4 = t["P40"][:, :] if j == 0 else t["HA"][:, j, :]
                    P.op("dve", lambda e, j=j: e.tensor_tensor(out=p1, in0=A3, in1=bc3(t["HA"][:, j, :]), op=ALU.mult), reads=["hpA", "HA"], writes=["p1"])
                    P.op("dve", lambda e, j=j: e.tensor_tensor(out=p2, in0=B3, in1=bc3(t["nHB"][:, j, :]), op=ALU.mult), reads=["hpB", "nHB"], writes=["p2"])
                    P.op("pool", lambda e, j=j: e.tensor_tensor(out=g["Y"][:, j, :], in0=g["p1"][:], in1=g["p2"][:], op=ALU.add), reads=["p1", "p2"], writes=["Y"])
                    P.op("dve", lambda e, j=j: e.tensor_tensor(out=p1, in0=A3, in1=bc3(t["HBm"][:, j, :]), op=ALU.mult), reads=["hpA", "HBm", "Y"], writes=["p1"])
                    P.op("dve", lambda e, j=j, P4=P4: e.tensor_tensor(out=p2, in0=B3, in1=bc3(P4), op=ALU.mult), reads=["hpB", "HA", "P40", "Y"], writes=["p2"])
                    P.op("pool", lambda e, j=j: e.tensor_tensor(out=g["Y"][:, nT + j, :], in0=g["p1"][:], in1=g["p2"][:], op=ALU.add), reads=["p1", "p2"], writes=["Y"])
                for tb in range(L // W):
                    P.dma(lambda e, tb=tb: e.dma_start(out=g["wiv"][:], in_=wi_v[:, :, tb * W:(tb + 1) * W]), writes=["wiv"])
                    for bb in range(BG):
                        for c in range(2):
                            ps = psI[bb * 2 + c]
                            pk = "hpI%d" % (bb * 2 + c)
                            for sc in range(nS):
                                P.op("pe", lambda e, ps=ps, sc=sc, bb=bb, c=c: e.matmul(ps[:, 0:W], lhsT=g["Y"][:, sc, bb * 256 + c * 128:bb * 256 + (c + 1) * 128], rhs=g["wiv"][:, sc, :],
                                                                                  start=(sc == 0), stop=(sc == nS - 1)), reads=["Y", "wiv"], writes=[pk], skip_self=(sc > 0))
                            yo = g["yo%d" % (k.yc % 2)]
                            yk = "yo%d" % (k.yc % 2)
                            k.yc += 1
                            P.op("dve", lambda e, ps=ps, bb=bb, c=c, tb=tb: e.scalar_tensor_tensor(out=g["tmp"][:], in0=g["sT"][:, c, bb, tb * W:(tb + 1) * W], scalar=t["dcol"][:, c:c + 1], in1=ps[:, 0:W],
                                                                                             op0=ALU.mult, op1=ALU.add), reads=["sT", "dcol", pk], writes=["tmp"])
                            P.op("dve", lambda e, bb=bb, c=c, tb=tb, yo=yo: e.tensor_tensor(out=yo[:], in0=g["tmp"][:], in1=g["x0c"][:, c, bb, tb * W:(tb + 1) * W], op=ALU.mult),
                                 reads=["tmp", "x0c"], writes=[yk])
                            b = g0 + bb
                            P.dma(lambda e, yo=yo, b=b, c=c, tb=tb: e.dma_start(out=k.yT[512 + c * 128:512 + (c + 1) * 128, b * TB + off + tb * W:b * TB + off + (tb + 1) * W], in_=yo[:]),
                                  reads=[yk], writes=[("yTd", l, 512, c, b, off, tb)], dkey=("hyo", yk))
            P.barrier()
        P.emit()


def hyena_consts(L):
    n = L
    t_lin = np.linspace(0.0, 1.0, n, dtype=np.float32)
    bands = np.linspace(1e-4, 15.0, 16, dtype=np.float32)
    w = (2.0 * np.float32(math.pi) * np.arange(n, dtype=np.float32) / np.float32(n)).astype(np.float32)
    z = np.concatenate([t_lin[:, None], np.cos(bands[None, :] * w[:, None]), -np.sin(bands[None, :] * w[:, None])], axis=-1).astype(np.float32)
    ridx = (n - np.arange(n)) % n
    out = {}
    out["zf"] = np.ascontiguousarray(z.T)
    out["zr"] = np.ascontiguousarray(z[ridx].T)
    tf = -t_lin
    tr = -t_lin[ridx]
    out["tf"] = np.ascontiguousarray(tf.reshape(n // 128, 128).T)
    out["tr"] = np.ascontiguousarray(tr.reshape(n // 128, 128).T)
    max_decay = math.log(1e-2) / 0.3
    min_decay = math.log(1e-2) / 1.5
    deltas = np.abs(np.linspace(min_decay, max_decay, 256, dtype=np.float32))
    out["dl"] = np.broadcast_to(deltas[None, :], (128, 256)).astype(np.float32).copy()
    N = 2 * n
    tt = np.arange(n, dtype=np.float64)[:, None]
    kk = np.arange(n, dtype=np.float64)[None, :]
    ang = 2.0 * np.pi * ((tt * kk) % N) / N
    C = np.cos(ang)
    S = np.sin(ang)
    S[:, 0] = (-1.0) ** np.arange(n)
    Wf = np.concatenate([C, S], axis=1)
    out["Wf"] = Wf.astype(ml_dtypes.bfloat16)
    out["Winv"] = np.ascontiguousarray(Wf.T).astype(ml_dtypes.bfloat16)
    cw = np.full(N, 2.0 / N)
    cw[0] = 1.0 / N
    cw[n] = 1.0 / N
    sgn = np.tile((-1.0) ** np.arange(n), 2)
    sgn[n] = 1.0
    out["csl"] = np.ascontiguousarray(cw.reshape(N // 128, 128).T).astype(np.float32)
    out["csh"] = np.ascontiguousarray((cw * sgn).reshape(N // 128, 128).T).astype(np.float32)
    return out


def stage_outproj(k, l):
    nc, P, NB = k.nc, k.P, k.NB
    ntile = 18 if l < DEPTH - 1 else 16
    with ExitStack() as es:
        A, B = load_mod_cols(k, es, l, 1)
        t = _tiles(es, nc, [("idf", (128, 128), F32), ("idb", (128, 128), BF16), ("wst0", (128, D), F32), ("wst1", (128, D), F32), ("gng", (128, 8), F32), ("wout", (128, 8, D), BF16), ("wrf", (128, 8, 36), F32), ("wr", (128, 8, 36), BF16), ("ones", (128, 1), BF16), ("g1b", (128, D), F32), ("ohrun", (128, 32), F32), ("iota32", (128, 32), F32), ("LTf", (128, 128), F32), ("ONESf", (128, 128), F32), ("jbv", (128, 128), F32), ("A2row", (128, D), F32), ("B2row", (128, D), F32), ("g2row", (128, D), F32), ("cnt", (128, 32), F32), ("nf", (128, 32), F32), ("ni", (128, 32), mybir.dt.int32), ("dd", (128, 32), F32), ("up", (128, 32), F32), ("pend", (128, 32), F32), ("zero32", (128, 32), F32), ("cmp", (128, MAXBLK, 32), F32), ("bexp", (128, MAXBLK), F32)])
        ts = []
        for s_ in range(2):
            d_ = dict(t)
            d_.update(_tiles(es, nc, [("yt", (128, 8, 128), BF16), ("ysq", (128, 8, 128), BF16), ("r", (128, 4), F32), ("m", (128, D), F32), ("xt0", (128, D), F32), ("junk0", (128, D), BF16), ("ss0", (128, 1), F32), ("rstd0", (128, 1), F32), ("xn0", (128, D), BF16), ("fT", (128, 8, 128), BF16), ("lg", (128, 36), F32), ("s1", (128, 8), F32), ("s2", (128, 8), F32), ("s3", (128, 8), F32), ("mg", (128, 4), F32), ("pr", (128, 4, 8), F32), ("es", (128, 8), F32), ("m1", (128, 8), F32), ("m2", (128, 8), F32), ("e2", (128, 8), F32), ("cw8", (128, 8), F32), ("cw", (128, 4, 8), F32), ("oh1", (128, 4, 8), F32), ("oh2", (128, 4, 8), F32), ("ohs", (128, 32), F32), ("ohp", (128, 32), F32), ("rt", (128, 8), F32), ("ft1", (128, D), F32), ("ftm", (128, D), BF16)]))
            d_["pst"] = _psum(es, nc, "pst", [128, 8, 128], BF16)
            d_["pm"] = [_psum(es, nc, "pm%d" % i, [128, 512], F32) for i in range(2)]
            d_["misc"] = _psum(es, nc, "misc", [128, 512], F32)
            ts.append(d_)
        for nm, src in (("iota32", k.c_iota32), ("LTf", k.c_LT), ("ONESf", k.c_ONES), ("jbv", k.c_jbv)):
            P.dma(lambda e, nm=nm, src=src: e.dma_start(out=t[nm][:], in_=src), writes=[nm])
        P.op("pool", lambda e: e.memset(t["ohrun"][:], 0.0), writes=["ohrun"])
        P.op("pool", lambda e: e.memset(t["zero32"][:], 0.0), writes=["zero32"])
        for d_ in ts:
            P.op("pool", lambda e, d_=d_: e.memset(d_["rt"][:], 0.0), writes=["rt_" + str(ts.index(d_))])
        P.dma(lambda e: e.dma_start(out=t["g2row"][:], in_=k.norm2_g[l].partition_broadcast(128)), writes=["g2row"])
        P.dma(lambda e: e.dma_start(out=t["idf"][:], in_=k.identf), writes=["idf"])
        P.op("dve", lambda e: e.tensor_copy(out=t["idb"][:], in_=t["idf"][:]), reads=["idf"], writes=["idb"])
        P.op("pool", lambda e: e.memset(t["ones"][:], 1.0), writes=["ones"])
        P.dma(lambda e: e.dma_start(out=t["gng"][:], in_=k.group_norm_g[l].rearrange("(c p) -> p c", p=128)), writes=["gng"])
        for kk in range(8):
            ws = t["wst%d" % (kk % 2)]
            wk = "wst%d" % (kk % 2)
            P.dma(lambda e, kk=kk, ws=ws: e.dma_start(out=ws[:], in_=k.w_out[l][kk * 128:(kk + 1) * 128, :]), writes=[wk])
            P.op("dve", lambda e, kk=kk, ws=ws: e.tensor_scalar(out=t["wout"][:, kk, :], in0=ws[:], scalar1=t["gng"][:, kk:kk + 1], scalar2=None, op0=ALU.mult), reads=[wk, "gng"], writes=["wout"])
        P.dma(lambda e: e.dma_start(out=t["wrf"][:, :, 0:4], in_=k.moe_w_group[l].rearrange("(c p) n -> p c n", p=128)), writes=["wrf"])
        P.dma(lambda e: e.dma_start(out=t["wrf"][:, :, 4:36], in_=k.moe_w_expert[l].rearrange("(c p) n -> p c n", p=128)), writes=["wrf"])
        P.op("dve", lambda e: e.tensor_copy(out=t["wr"][:], in_=t["wrf"][:]), reads=["wrf"], writes=["wr"])
        yT_v = k.yT.rearrange("(c p) t -> p c t", p=128)
        fT_v = k.fT.rearrange("(c p) t -> p c t", p=128)
        def tile_body(t, b, i, other=None):
            P = k.P
            if True:
                t0 = b * TB + i * 128
                row = b if i < 16 else NB
                P.dma(lambda e, t0=t0: e.dma_start(out=t["yt"][:], in_=yT_v[:, :, t0:t0 + 128]), writes=["yt"])
                P.dma(lambda e, b=b, i=i: e.dma_start(out=t["xt0"][:], in_=xsrc(k, l, b, i)), writes=["xt0"])
                P.op("pool", lambda e: e.tensor_tensor(out=t["ysq"][:], in0=t["yt"][:], in1=t["yt"][:], op=ALU.mult), reads=["yt"], writes=["ysq"])
                for g in range(4):
                    for kc in range(2):
                        P.op("pe", lambda e, g=g, kc=kc: e.matmul(t["misc"][:, g:g + 1], lhsT=t["ysq"][:, 2 * g + kc, :], rhs=t["ones"][:], start=(kc == 0), stop=(kc == 1)), reads=["ysq", "ones"], writes=["misc"],
                             skip_self=(kc > 0))
                P.op("act", lambda e: e.activation(out=t["r"][:], in_=t["misc"][:, 0:4], func=AF.Sqrt, scale=1.0 / 256, bias=EPS), reads=["misc"], writes=["r"])
                P.op("dve", lambda e: e.reciprocal(out=t["r"][:], in_=t["r"][:]), reads=["r"], writes=["r"])
                for half in range(2):
                    for g in range(4):
                        ps = t["pm"][g % 2]
                        pk = "pm%d" % (g % 2)
                        for kc in range(2):
                            P.op("pe", lambda e, ps=ps, g=g, kc=kc, half=half: e.matmul(ps[:, :], lhsT=t["yt"][:, 2 * g + kc, :], rhs=t["wout"][:, 2 * g + kc, half * 512:(half + 1) * 512], start=(kc == 0), stop=(kc == 1)),
                                 reads=["yt", "wout"], writes=[pk], skip_self=(kc > 0))
                        if g == 0:
                            P.op("dve", lambda e, ps=ps, half=half: e.tensor_scalar(out=t["m"][:, half * 512:(half + 1) * 512], in0=ps[:, :], scalar1=t["r"][:, 0:1], scalar2=None, op0=ALU.mult), reads=[pk, "r"], writes=["m"])
                        else:
                            P.op("dve", lambda e, ps=ps, half=half, g=g: e.scalar_tensor_tensor(out=t["m"][:, half * 512:(half + 1) * 512], in0=ps[:, :], scalar=t["r"][:, g:g + 1], in1=t["m"][:, half * 512:(half + 1) * 512],
                                                                                             op0=ALU.mult, op1=ALU.add), reads=[pk, "r", "m"], writes=["m"])
                P.op("pool", lambda e: e.tensor_tensor(out=t["m"][:], in0=t["m"][:], in1=t["g1b"][:], op=ALU.mult), reads=["m", "g1b"], writes=["m"])
                P.op("dve", lambda e: e.tensor_tensor(out=t["xt0"][:], in0=t["xt0"][:], in1=t["m"][:], op=ALU.add), reads=["xt0", "m"], writes=["xt0"])
                P.dma(lambda e, t0=t0: e.dma_start(out=k.x1[t0:t0 + 128, :], in_=t["xt0"][:]), reads=["xt0"], writes=[("x1d", t0)], dkey="x1st")
                norm_mod_T(k, t, t["pst"], t["xt0"][:], "xt0", A, B, row, t["fT"], "fT", 0, "0")
                P.dma(lambda e, t0=t0: e.dma_start(out=fT_v[:, :, t0:t0 + 128], in_=t["fT"][:]), reads=["fT"], writes=[("fTd", t0)], dkey="fTst")
                for kk in range(8):
                    P.op("pe", lambda e, kk=kk: e.matmul(t["misc"][:, 64:100], lhsT=t["fT"][:, kk, :], rhs=t["wr"][:, kk, :], start=(kk == 0), stop=(kk == 7)), reads=["fT", "wr"], writes=["misc"], skip_self=(kk > 0))
                P.op("act", lambda e: e.copy(out=t["lg"][:], in_=t["misc"][:, 64:100]), reads=["misc"], writes=["lg"])
                gl = t["lg"][:, 0:4]
                el = t["lg"][:, 4:36].rearrange("p (g e) -> p g e", g=4)
                s1, s2, s3 = t["s1"], t["s2"], t["s3"]
                P.op("dve", lambda e: e.tensor_reduce(out=s1[:, 0:1], in_=gl, axis=AX.X, op=ALU.max), reads=["lg"], writes=["s1"])
                P.op("dve", lambda e: e.tensor_scalar(out=s1[:, 1:2], in0=s1[:, 0:1], scalar1=-1.0, scalar2=None, op0=ALU.mult), reads=["s1"], writes=["s1"])
                P.op("act", lambda e: e.activation(out=t["mg"][:], in_=gl, func=AF.Exp, bias=s1[:, 1:2], accum_out=s1[:, 2:3]), reads=["lg", "s1"], writes=["mg", "s1"])
                P.op("dve", lambda e: e.reciprocal(out=s1[:, 3:4], in_=s1[:, 2:3]), reads=["s1"], writes=["s1"])
                P.op("dve", lambda e: e.tensor_scalar(out=t["mg"][:], in0=gl, scalar1=s1[:, 0:1], scalar2=None, op0=ALU.is_equal), reads=["lg", "s1", "mg"], writes=["mg"])
                P.op("dve", lambda e: e.tensor_tensor(out=t["pr"][:], in0=el, in1=t["mg"][:].unsqueeze(2).to_broadcast([128, 4, 8]), op=ALU.mult), reads=["lg", "mg"], writes=["pr"])
                P.op("dve", lambda e: e.tensor_reduce(out=t["es"][:], in_=t["pr"][:].rearrange("p g e -> p e g"), axis=AX.X, op=ALU.add), reads=["pr"], writes=["es"])
                P.op("dve", lambda e: e.tensor_reduce(out=s2[:, 0:1], in_=t["es"][:], axis=AX.X, op=ALU.max), reads=["es"], writes=["s2"])
                P.op("dve", lambda e: e.tensor_scalar(out=t["m1"][:], in0=t["es"][:], scalar1=s2[:, 0:1], scalar2=None, op0=ALU.is_equal), reads=["es", "s2"], writes=["m1"])
                P.op("dve", lambda e: e.scalar_tensor_tensor(out=t["e2"][:], in0=t["m1"][:], scalar=-1e30, in1=t["es"][:], op0=ALU.mult, op1=ALU.add), reads=["m1", "es"], writes=["e2"])
                P.op("dve", lambda e: e.tensor_reduce(out=s2[:, 1:2], in_=t["e2"][:], axis=AX.X, op=ALU.max), reads=["e2"], writes=["s2"])
                P.op("dve", lambda e: e.tensor_scalar(out=t["m2"][:], in0=t["e2"][:], scalar1=s2[:, 1:2], scalar2=None, op0=ALU.is_equal), reads=["e2", "s2"], writes=["m2"])
                P.op("dve", lambda e: e.tensor_tensor(out=s2[:, 2:3], in0=s2[:, 1:2], in1=s2[:, 0:1], op=ALU.subtract), reads=["s2"], writes=["s2"])
                P.op("act", lambda e: e.activation(out=s2[:, 3:4], in_=s2[:, 2:3], func=AF.Exp), reads=["s2"], writes=["s2"])
                P.op("dve", lambda e: e.tensor_scalar(out=s3[:, 0:1], in0=s2[:, 3:4], scalar1=1.0, scalar2=None, op0=ALU.add), reads=["s2"], writes=["s3"])
                P.op("dve", lambda e: e.reciprocal(out=s3[:, 1:2], in_=s3[:, 0:1]), reads=["s3"], writes=["s3"])
                P.op("dve", lambda e: e.tensor_tensor(out=s3[:, 2:3], in0=s3[:, 1:2], in1=s1[:, 3:4], op=ALU.mult), reads=["s3", "s1"], writes=["s3"])
                P.op("dve", lambda e: e.tensor_tensor(out=s3[:, 3:4], in0=s3[:, 2:3], in1=s2[:, 3:4], op=ALU.mult), reads=["s3", "s2"], writes=["s3"])
                P.op("dve", lambda e: e.tensor_scalar(out=t["cw8"][:], in0=t["m1"][:], scalar1=s3[:, 2:3], scalar2=None, op0=ALU.mult), reads=["m1", "s3"], writes=["cw8"])
                P.op("dve", lambda e: e.scalar_tensor_tensor(out=t["cw8"][:], in0=t["m2"][:], scalar=s3[:, 3:4], in1=t["cw8"][:], op0=ALU.mult, op1=ALU.add), reads=["m2", "s3", "cw8"], writes=["cw8"])
                P.op("dve", lambda e: e.tensor_tensor(out=t["cw"][:], in0=t["mg"][:].unsqueeze(2).to_broadcast([128, 4, 8]), in1=t["cw8"][:].unsqueeze(1).to_broadcast([128, 4, 8]), op=ALU.mult),
                     reads=["mg", "cw8"], writes=["cw"])
                P.dma(lambda e, t0=t0: e.dma_start(out=k.cw[t0:t0 + 128, :], in_=t["cw"][:].rearrange("p g e -> p (g e)")), reads=["cw"], writes=[("cwd", t0)], dkey="cwst")
                mgb = t["mg"][:].unsqueeze(2).to_broadcast([128, 4, 8])
                P.op("dve", lambda e: e.tensor_tensor(out=t["oh1"][:], in0=mgb, in1=t["m1"][:].unsqueeze(1).to_broadcast([128, 4, 8]), op=ALU.mult), reads=["mg", "m1"], writes=["oh1"])
                P.op("dve", lambda e: e.tensor_tensor(out=t["oh2"][:], in0=mgb, in1=t["m2"][:].unsqueeze(1).to_broadcast([128, 4, 8]), op=ALU.mult), reads=["mg", "m2"], writes=["oh2"])
                oh1f = t["oh1"][:].rearrange("p g e -> p (g e)")
                oh2f = t["oh2"][:].rearrange("p g e -> p (g e)")
                P.op("dve", lambda e: e.tensor_tensor(out=t["ohs"][:], in0=oh1f, in1=oh2f, op=ALU.add), reads=["oh1", "oh2"], writes=["ohs"])
                P.op("pe", lambda e: e.matmul(t["misc"][:, 128:160], lhsT=t["LTf"][:], rhs=t["ohs"][:], start=True, stop=False), reads=["LTf", "ohs"], writes=["misc"])
                P.op("pe", lambda e: e.matmul(t["misc"][:, 128:160], lhsT=t["ONESf"][:], rhs=t["ohrun"][:], start=False, stop=(other is None)), reads=["ONESf", "ohrun"], writes=["misc"], skip_self=True)
                if other is not None:
                    P.op("pe", lambda e: e.matmul(t["misc"][:, 128:160], lhsT=t["ONESf"][:], rhs=other["ohs"][:], start=False, stop=True), reads=["ONESf", "ohs_0"], writes=["misc"], skip_self=True)
                for j, ohf, okey in ((0, oh1f, "oh1"), (1, oh2f, "oh2")):
                    P.op("dve", lambda e, ohf=ohf: e.tensor_tensor(out=t["ohp"][:], in0=t["misc"][:, 128:160], in1=ohf, op=ALU.mult), reads=["misc", okey], writes=["ohp"])
                    P.op("dve", lambda e, j=j: e.tensor_reduce(out=t["rt"][:, 2 + j:3 + j], in_=t["ohp"][:], axis=AX.X, op=ALU.add), reads=["ohp"], writes=["rt"])
                    P.op("dve", lambda e, ohf=ohf: e.tensor_tensor(out=t["ohp"][:], in0=t["iota32"][:], in1=ohf, op=ALU.mult), reads=["iota32", okey, "rt"], writes=["ohp"])
                    P.op("dve", lambda e, j=j: e.tensor_reduce(out=t["rt"][:, j:j + 1], in_=t["ohp"][:], axis=AX.X, op=ALU.add), reads=["ohp"], writes=["rt"])
                P.op("dve", lambda e: e.tensor_copy(out=t["rt"][:, 4:6], in_=s3[:, 2:4]), reads=["s3"], writes=["rt"])
                P.op("dve", lambda e: e.tensor_tensor(out=t["ohrun"][:], in0=t["ohrun"][:], in1=t["ohs"][:], op=ALU.add), reads=["ohrun", "ohs"], writes=["ohrun"])
                P.dma(lambda e, t0=t0: e.dma_start(out=k.rt[t0:t0 + 128, :], in_=t["rt"][:]), reads=["rt"], writes=[("rtd", t0)], dkey="rtst")
                P.op("pool", lambda e: e.tensor_tensor(out=t["ft1"][:], in0=t["xn0"][:], in1=t["A2row"][:], op=ALU.mult), reads=["xn0", "A2row"], writes=["ft1"])
                P.op("pool", lambda e: e.tensor_tensor(out=t["ftm"][:], in0=t["ft1"][:], in1=t["B2row"][:], op=ALU.add), reads=["ft1", "B2row"], writes=["ftm"])
                P.dma(lambda e, t0=t0: e.dma_start(out=k.ftm[t0:t0 + 128, :], in_=t["ftm"][:]), reads=["ftm"], writes=[("ftmd", t0)], dkey="ftmst")

        LOCALK = set(['yt', 'ysq', 'r', 'm', 'xt0', 'junk0', 'ss0', 'rstd0', 'xn0', 'fT', 'lg', 's1', 's2', 's3', 'mg', 'pr', 'es', 'm1', 'm2', 'e2', 'cw8', 'cw', 'oh1', 'oh2', 'ohs', 'ohp', 'rt', 'ft1', 'ftm']) | {"pst", "pm0", "pm1", "misc", "x1st", "fTst", "cwst", "rtst", "ftmst"}
        for b in range(NB):
            for i0 in range(0, ntile, 2):
                if i0 == 0 or i0 == 16:
                    row = b if i0 < 16 else NB
                    P.dma(lambda e, row=row: e.dma_start(out=t["g1b"][:], in_=k.modrow[l][row, 2 * D:3 * D].partition_broadcast(128)), reads=[("modrow", l)], writes=["g1b"])
                    P.dma(lambda e, row=row: e.dma_start(out=t["B2row"][:], in_=k.modrow[l][row, 3 * D:4 * D].partition_broadcast(128)), reads=[("modrow", l)], writes=["B2row"])
                    P.dma(lambda e, row=row: e.dma_start(out=t["A2row"][:], in_=k.modrow[l][row, 4 * D:5 * D].partition_broadcast(128)), reads=[("modrow", l)], writes=["A2row"])
                    P.op("dve", lambda e: e.scalar_tensor_tensor(out=t["A2row"][:], in0=t["A2row"][:], scalar=1.0, in1=t["g2row"][:], op0=ALU.add, op1=ALU.mult), reads=["A2row", "g2row"], writes=["A2row"])
                recs = []
                for s_ in range(2):
                    if i0 + s_ < ntile:
                        rec = Rec("_%d" % s_, LOCALK)
                        k.P = rec
                        tile_body(ts[s_], b, i0 + s_, other=(ts[0] if s_ == 1 else None))
                        recs.append(rec)
                k.P = P
                replay(P, recs)
        NBLK = k.nblk[l]
        P.op("pe", lambda e: e.matmul(ts[0]["misc"][:, 128:160], lhsT=t["ONESf"][:], rhs=t["ohrun"][:], start=True, stop=True), reads=["ONESf", "ohrun"], writes=["misc_0"])
        P.op("act", lambda e: e.copy(out=t["cnt"][:], in_=ts[0]["misc"][:, 128:160]), reads=["misc_0"], writes=["cnt"])
        P.op("dve", lambda e: e.tensor_scalar(out=t["nf"][:], in0=t["cnt"][:], scalar1=1.0 / MOE_BS, scalar2=None, op0=ALU.mult), reads=["cnt"], writes=["nf"])
        P.op("dve", lambda e: e.tensor_copy(out=t["ni"][:], in_=t["nf"][:]), reads=["nf"], writes=["ni"])
        P.op("dve", lambda e: e.tensor_copy(out=t["nf"][:], in_=t["ni"][:]), reads=["ni"], writes=["nf"])
        P.op("dve", lambda e: e.scalar_tensor_tensor(out=t["dd"][:], in0=t["nf"][:], scalar=float(MOE_BS), in1=t["cnt"][:], op0=ALU.mult, op1=ALU.subtract), reads=["nf", "cnt"], writes=["dd"])
        P.op("dve", lambda e: e.tensor_scalar(out=t["up"][:], in0=t["dd"][:], scalar1=0.0, scalar2=None, op0=ALU.is_lt), reads=["dd"], writes=["up"])
        P.op("dve", lambda e: e.tensor_tensor(out=t["nf"][:], in0=t["nf"][:], in1=t["up"][:], op=ALU.add), reads=["nf", "up"], writes=["nf"])
        P.op("dve", lambda e: e.scalar_tensor_tensor(out=t["dd"][:], in0=t["up"][:], scalar=float(MOE_BS), in1=t["dd"][:], op0=ALU.mult, op1=ALU.add), reads=["up", "dd"], writes=["dd"])
        P.op("dve", lambda e: e.tensor_scalar(out=t["up"][:], in0=t["dd"][:], scalar1=float(MOE_BS), scalar2=None, op0=ALU.is_ge), reads=["dd"], writes=["up"])
        P.op("dve", lambda e: e.tensor_tensor(out=t["nf"][:], in0=t["nf"][:], in1=t["up"][:], op=ALU.subtract), reads=["nf", "up"], writes=["nf"])
        P.op("dve", lambda e: e.tensor_scalar(out=t["nf"][:], in0=t["nf"][:], scalar1=float(MOE_BS), scalar2=None, op0=ALU.mult), reads=["nf"], writes=["nf"])
        P.op("dve", lambda e: e.tensor_tensor_scan(out=t["pend"][:], data0=t["nf"][:], data1=t["zero32"][:], initial=0.0, op0=ALU.add, op1=ALU.add), reads=["nf", "zero32"], writes=["pend"])
        P.op("dve", lambda e: e.tensor_tensor(out=t["nf"][:], in0=t["pend"][:], in1=t["nf"][:], op=ALU.subtract), reads=["pend", "nf"], writes=["nf"])
        P.dma(lambda e: e.dma_start(out=k.pstart[l], in_=t["nf"][:]), reads=["nf"], writes=[("pstart", l)], dkey="pstst")
        P.op("dve", lambda e: e.tensor_tensor(out=t["cmp"][:, 0:NBLK, :], in0=t["pend"][:].unsqueeze(1).to_broadcast([128, NBLK, 32]),
                                              in1=t["jbv"][:, 0:NBLK].unsqueeze(2).to_broadcast([128, NBLK, 32]), op=ALU.is_le), reads=["pend", "jbv"], writes=["cmp"])
        P.op("dve", lambda e: e.tensor_reduce(out=t["bexp"][:, 0:NBLK], in_=t["cmp"][:, 0:NBLK, :], axis=AX.X, op=ALU.add), reads=["cmp"], writes=["bexp"])
        P.op("dve", lambda e: e.tensor_scalar(out=t["bexp"][:, 0:NBLK], in0=t["bexp"][:, 0:NBLK], scalar1=31.0, scalar2=None, op0=ALU.min), reads=["bexp"], writes=["bexp"])
        P.dma(lambda e: e.dma_start(out=k.bexp[l][:, 0:NBLK], in_=t["bexp"][:, 0:NBLK]), reads=["bexp"], writes=[("bexp", l)], dkey="bexst")
        P.barrier()
        P.emit()


MOE_BS = 512
MAXBLK = 72
I32 = mybir.dt.int32


def stage_moe_sparse(k, l):
    nc, P, NB = k.nc, k.P, k.NB
    last = (l == DEPTH - 1)
    ntile = 16 if last else 18
    NBLK = k.nblk[l]
    NSLOT = NBLK * MOE_BS
    with ExitStack() as es:
        t = _tiles(es, nc, [("idf", (128, 128), F32), ("idb", (128, 128), BF16), ("zeros", (128, 4, D), BF16),
                            ("pstart", (128, 32), F32), ("bexp", (128, MAXBLK), F32), ("iota32", (128, 32), F32), ("iotaA", (128, 8), F32), ("iotaB", (128, 4), F32),
                            ("e1k", (128, MAXBLK), F32), ("ixf", (128, MAXBLK, 8), F32), ("ix1", (128, MAXBLK, 8), I32), ("ix2", (128, MAXBLK, 4), I32),
                            ("rtt", (128, 8), F32), ("ohq", (128, 32), F32), ("dsf", (128, 2), F32),
                            ("dest", (128, NB * 18, 2), I32), ("wgt", (128, NB * 18, 2), F32),
                            ("ft0", (128, D), BF16), ("ft1", (128, D), BF16),
                            ("stA0", (128, 8, 512), F32), ("stA1", (128, 8, 512), F32), ("stB0", (128, 8, 512), F32), ("stB1", (128, 8, 512), F32),
                            ("stC0", (128, 4, D), F32), ("stC1", (128, 4, D), F32),
                            ("w1b", (128, 8, 512), BF16), ("w3b", (128, 8, 512), BF16), ("w2b", (128, 4, D), BF16),
                            ("xb0", (128, 4, D), BF16), ("xb1", (128, 4, D), BF16), ("xT", (128, 8, 512), BF16),
                            ("s1", (128, 512), F32), ("act", (128, 4, 512), BF16), ("yb0", (128, 4, D), BF16), ("yb1", (128, 4, D), BF16),
                            ("y1", (128, D), BF16), ("y2", (128, D), BF16), ("acc", (128, D), F32), ("g2b", (128, D), F32), ("xt", (128, D), F32)])
        pstb = _psum(es, nc, "mpst", [128, 8, 128], BF16)
        p1 = [_psum(es, nc, "mp1_%d" % i, [128, 512], F32) for i in range(2)]
        p3 = [_psum(es, nc, "mp3_%d" % i, [128, 512], F32) for i in range(2)]
        py = [_psum(es, nc, "mpy_%d" % i, [128, 512], F32) for i in range(2)]
        P.dma(lambda e: e.dma_start(out=t["idf"][:], in_=k.identf), writes=["idf"])
        P.op("dve", lambda e: e.tensor_copy(out=t["idb"][:], in_=t["idf"][:]), reads=["idf"], writes=["idb"])
        P.op("dve", lambda e: e.memset(t["zeros"][:], 0.0), writes=["zeros"])
        for nm, src in (("iota32", k.c_iota32), ("iotaA", k.c_iotaA), ("iotaB", k.c_iotaB), ("pstart", k.pstart[l])):
            P.dma(lambda e, nm=nm, src=src: e.dma_start(out=t[nm][:], in_=src), writes=[nm])
        P.dma(lambda e: e.dma_start(out=t["bexp"][:, 0:NBLK], in_=k.bexp[l][:, 0:NBLK]), writes=["bexp"])
        xs_v = k.xs.rearrange("(n p) d -> p n d", p=128)
        ys_v = k.ys.rearrange("(n p) d -> p n d", p=128)
        for n0 in range(0, NSLOT // 128, 4):
            P.dma(lambda e, n0=n0: e.dma_start(out=xs_v[:, n0:n0 + 4, :], in_=t["zeros"][:]), reads=["zeros"], writes=["xs"], dkey="xszero")
            P.dma(lambda e, n0=n0: e.dma_start(out=ys_v[:, n0:n0 + 4, :], in_=t["zeros"][:]), reads=["zeros"], writes=["ys"], dkey="yszero")
        P.op("dve", lambda e: e.tensor_scalar(out=t["e1k"][:, 0:NBLK], in0=t["bexp"][:, 0:NBLK], scalar1=128.0, scalar2=float(l * 32 * 128), op0=ALU.mult, op1=ALU.add), reads=["bexp"], writes=["e1k"])
        P.op("dve", lambda e: e.tensor_scalar(out=t["e1k"][:, 0:NBLK], in0=t["e1k"][:, 0:NBLK], scalar1=t["iotaA"][:, 0:1], scalar2=None, op0=ALU.add), reads=["e1k", "iotaA"], writes=["e1k"])
        P.op("dve", lambda e: e.tensor_copy(out=t["ix1"][:, 0:NBLK, 0], in_=t["e1k"][:, 0:NBLK]), reads=["e1k"], writes=["ix1"])
        tl = []
        for b in range(NB):
            for i in range(ntile):
                tl.append((b, i))
        for n, (b, i) in enumerate(tl):
            t0 = b * TB + i * 128
            ft = t["ft%d" % (n % 2)]
            fk = "ft%d" % (n % 2)
            P.dma(lambda e, t0=t0: e.dma_start(out=t["rtt"][:], in_=k.rt[t0:t0 + 128, :]), writes=["rtt"])
            P.dma(lambda e, t0=t0, ft=ft: e.dma_start(out=ft[:], in_=k.ftm[t0:t0 + 128, :]), writes=[fk])
            for j in range(2):
                P.op("dve", lambda e, j=j: e.tensor_scalar(out=t["ohq"][:], in0=t["iota32"][:], scalar1=t["rtt"][:, j:j + 1], scalar2=None, op0=ALU.is_equal), reads=["iota32", "rtt"], writes=["ohq"])
                P.op("dve", lambda e: e.tensor_tensor(out=t["ohq"][:], in0=t["ohq"][:], in1=t["pstart"][:], op=ALU.mult), reads=["ohq", "pstart"], writes=["ohq"])
                P.op("dve", lambda e, j=j: e.tensor_reduce(out=t["dsf"][:, j:j + 1], in_=t["ohq"][:], axis=AX.X, op=ALU.add), reads=["ohq"], writes=["dsf"])
            P.op("dve", lambda e: e.tensor_tensor(out=t["dsf"][:], in0=t["dsf"][:], in1=t["rtt"][:, 2:4], op=ALU.add), reads=["dsf", "rtt"], writes=["dsf"])
            P.op("dve", lambda e, n=n: e.tensor_copy(out=t["dest"][:, n, :], in_=t["dsf"][:]), reads=["dsf"], writes=[("dest", n)])
            P.op("dve", lambda e, n=n: e.tensor_copy(out=t["wgt"][:, n, :], in_=t["rtt"][:, 4:6]), reads=["rtt"], writes=[("wgt", n)])
            for j in range(2):
                P.dma(lambda e, n=n, j=j, ft=ft: e.indirect_dma_start(out=k.xs[:, :], out_offset=bass.IndirectOffsetOnAxis(ap=t["dest"][:, n, j:j + 1], axis=0), in_=ft[:, :], in_offset=None),
                      reads=[fk, ("dest", n), "xs"], writes=[("xsw", n, j)], q="pool", dkey=("sw_sc" if STRICT_SCATTER else ("sw_sc", j, n % 4)))
        xs_dep = [("xsw", n, j) for n in range(len(tl)) for j in range(2)]
        w1_rows = k.moe_w1.rearrange("l e (p j) n -> (l e p) (j n)", j=8)
        w3_rows = k.moe_w3.rearrange("l e (p j) n -> (l e p) (j n)", j=8)
        w2_rows = k.moe_w2.rearrange("l e (p j) n -> (l e p) (j n)", j=4)
        for jb in range(NBLK):
            sa, sb_, sc_ = t["stA%d" % (jb % 2)], t["stB%d" % (jb % 2)], t["stC%d" % (jb % 2)]
            ka, kb, kc = "stA%d" % (jb % 2), "stB%d" % (jb % 2), "stC%d" % (jb % 2)
            P.dma(lambda e, jb=jb, sa=sa: e.indirect_dma_start(out=sa[:].rearrange("p a b -> p (a b)"), out_offset=None, in_=w1_rows[:, :], in_offset=bass.IndirectOffsetOnAxis(ap=t["ix1"][:, jb, 0:1], axis=0)),
                  reads=["ix1"], writes=[ka], q="pool", dkey=("sw_w", ka))
            P.dma(lambda e, jb=jb, sb_=sb_: e.indirect_dma_start(out=sb_[:].rearrange("p a b -> p (a b)"), out_offset=None, in_=w3_rows[:, :], in_offset=bass.IndirectOffsetOnAxis(ap=t["ix1"][:, jb, 0:1], axis=0)),
                  reads=["ix1"], writes=[kb], q="pool", dkey=("sw_w", kb))
            P.dma(lambda e, jb=jb, sc_=sc_: e.indirect_dma_start(out=sc_[:].rearrange("p a b -> p (a b)"), out_offset=None, in_=w2_rows[:, :], in_offset=bass.IndirectOffsetOnAxis(ap=t["ix1"][:, jb, 0:1], axis=0)),
                  reads=["ix1"], writes=[kc], q="pool", dkey=("sw_w", kc))
            P.op("act", lambda e, sa=sa: e.copy(out=t["w1b"][:].rearrange("p k (c q) -> p k q c", c=4), in_=sa[:].rearrange("p k (q c) -> p k q c", c=4)), reads=[ka], writes=["w1b"])
            P.op("act", lambda e, sb_=sb_: e.copy(out=t["w3b"][:].rearrange("p k (c q) -> p k q c", c=4), in_=sb_[:].rearrange("p k (q c) -> p k q c", c=4)), reads=[kb], writes=["w3b"])
            P.op("dve", lambda e, sc_=sc_: e.tensor_copy(out=t["w2b"][:], in_=sc_[:]), reads=[kc], writes=["w2b"])
            xb = t["xb%d" % (jb % 2)]
            xk = "xb%d" % (jb % 2)
            P.dma(lambda e, jb=jb, xb=xb: e.dma_start(out=xb[:], in_=xs_v[:, jb * 4:(jb + 1) * 4, :]), reads=["xs"] + xs_dep, writes=[xk])
            for sidx in range(4):
                for kk in range(8):
                    P.op("pe", lambda e, sidx=sidx, kk=kk, xb=xb: e.transpose(out=pstb[:, kk, :], in_=xb[:, sidx, kk:D:8], identity=t["idb"][:]), reads=[xk, "idb"], writes=["mpst"])
                P.op("dve", lambda e, sidx=sidx: e.tensor_copy(out=t["xT"][:, :, sidx * 128:(sidx + 1) * 128], in_=pstb[:, :, :]), reads=["mpst"], writes=["xT"])
            W = MOE_BS
            for ffc in range(4):
                pa, pb = p1[ffc % 2], p3[ffc % 2]
                kpa, kpb = "mp1_%d" % (ffc % 2), "mp3_%d" % (ffc % 2)
                for kk in range(8):
                    P.op("pe", lambda e, pa=pa, kk=kk, ffc=ffc: e.matmul(pa[:, 0:W], lhsT=t["w1b"][:, kk, ffc * 128:(ffc + 1) * 128], rhs=t["xT"][:, kk, :], start=(kk == 0), stop=(kk == 7)),
                         reads=["w1b", "xT"], writes=[kpa], skip_self=(kk > 0))
                for kk in range(8):
                    P.op("pe", lambda e, pb=pb, kk=kk, ffc=ffc: e.matmul(pb[:, 0:W], lhsT=t["w3b"][:, kk, ffc * 128:(ffc + 1) * 128], rhs=t["xT"][:, kk, :], start=(kk == 0), stop=(kk == 7)),
                         reads=["w3b", "xT"], writes=[kpb], skip_self=(kk > 0))
                P.op("act", lambda e, pa=pa: e.activation(out=t["s1"][:, 0:W], in_=pa[:, 0:W], func=AF.Silu), reads=[kpa], writes=["s1"])
                P.op("dve", lambda e, pb=pb, ffc=ffc: e.tensor_tensor(out=t["act"][:, ffc, 0:W], in0=pb[:, 0:W], in1=t["s1"][:, 0:W], op=ALU.mult), reads=[kpb, "s1"], writes=["act"])
            yb = t["yb%d" % (jb % 2)]
            yk = "yb%d" % (jb % 2)
            for sidx in range(4):
                for half in range(2):
                    ps = py[half]
                    pk = "mpy_%d" % half
                    for ffc in range(4):
                        P.op("pe", lambda e, ps=ps, ffc=ffc, sidx=sidx, half=half: e.matmul(ps[:, :], lhsT=t["act"][:, ffc, sidx * 128:(sidx + 1) * 128], rhs=t["w2b"][:, ffc, half * 512:(half + 1) * 512],
                                                                                      start=(ffc == 0), stop=(ffc == 3)), reads=["act", "w2b"], writes=[pk], skip_self=(ffc > 0))
                    if half == 0:
                        P.op("act", lambda e, ps=ps, sidx=sidx, yb=yb: e.copy(out=yb[:, sidx, 0:512], in_=ps[:, :]), reads=[pk], writes=[yk])
                    else:
                        P.op("dve", lambda e, ps=ps, sidx=sidx, yb=yb: e.tensor_copy(out=yb[:, sidx, 512:1024], in_=ps[:, :]), reads=[pk], writes=[yk])
            P.dma(lambda e, jb=jb, yb=yb: e.dma_start(out=ys_v[:, jb * 4:(jb + 1) * 4, :], in_=yb[:]), reads=[yk, "ys"], writes=[("ysw", jb)], dkey=("ysst", jb % 2))
        ys_dep = [("ysw", jb) for jb in range(NBLK)]
        for n, (b, i) in enumerate(tl):
            t0 = b * TB + i * 128
            row = b if i < 16 else NB
            if i == 0 or i == 16:
                P.dma(lambda e, row=row: e.dma_start(out=t["g2b"][:], in_=k.modrow[l][row, 5 * D:6 * D].partition_broadcast(128)), writes=["g2b"])
            for j, yn in ((0, "y1"), (1, "y2")):
                P.dma(lambda e, n=n, j=j, yn=yn: e.indirect_dma_start(out=t[yn][:, :], out_offset=None, in_=k.ys[:, :], in_offset=bass.IndirectOffsetOnAxis(ap=t["dest"][:, n, j:j + 1], axis=0)), reads=[("dest", n), "ys"] + ys_dep, writes=[yn], q="pool", dkey=("sw_y", yn))
            P.dma(lambda e, t0=t0: e.dma_start(out=t["xt"][:], in_=k.x1[t0:t0 + 128, :]), writes=["xt"])
            P.op("dve", lambda e, n=n: e.tensor_scalar(out=t["acc"][:], in0=t["y1"][:], scalar1=t["wgt"][:, n, 0:1], scalar2=None, op0=ALU.mult), reads=["y1", ("wgt", n)], writes=["acc"])
            P.op("dve", lambda e, n=n: e.scalar_tensor_tensor(out=t["acc"][:], in0=t["y2"][:], scalar=t["wgt"][:, n, 1:2], in1=t["acc"][:], op0=ALU.mult, op1=ALU.add), reads=["y2", ("wgt", n), "acc"], writes=["acc"])
            P.op("dve", lambda e: e.tensor_tensor(out=t["acc"][:], in0=t["acc"][:], in1=t["g2b"][:], op=ALU.mult), reads=["acc", "g2b"], writes=["acc"])
            P.op("dve", lambda e: e.tensor_tensor(out=t["xt"][:], in0=t["xt"][:], in1=t["acc"][:], op=ALU.add), reads=["xt", "acc"], writes=["xt"])
            if last:
                P.dma(lambda e, b=b, i=i: e.dma_start(out=k.out[b, i * 128:(i + 1) * 128, :], in_=t["xt"][:]), reads=["xt"], writes=[("outd", b, i)], dkey="outst")
            else:
                P.dma(lambda e, t0=t0: e.dma_start(out=k.x2[t0:t0 + 128, :], in_=t["xt"][:]), reads=["xt"], writes=[("x2d", t0)], dkey="outst")
        P.barrier()
        P.emit()


def stage_moe(k, l):
    nc, P, NB = k.nc, k.P, k.NB
    last = (l == DEPTH - 1)
    ntile = 16 if last else 18
    blocks = BLK5[:4] if last else BLK5
    with ExitStack() as es:
        t = _tiles(es, nc, [("fTb", (128, 8, TB), BF16), ("acc", (128, 18, D), F32), ("cwb", (128, 18, 32), F32),
                            ("st0", (128, 8, 512), F32), ("st1", (128, 8, 512), F32),
                            ("w1b", (128, 8, 512), BF16), ("w3b", (128, 8, 512), BF16), ("w2b", (128, 4, D), BF16),
                            ("s1", (128, 512), F32), ("act", (128, 4, 512), BF16), ("g2b", (128, D), F32), ("xt", (128, D), F32)])
        p1 = [_psum(es, nc, "mp1_%d" % i, [128, 512], F32) for i in range(2)]
        p3 = [_psum(es, nc, "mp3_%d" % i, [128, 512], F32) for i in range(2)]
        py = [_psum(es, nc, "mpy_%d" % i, [128, 512], F32) for i in range(2)]
        fT_v = k.fT.rearrange("(c p) t -> p c t", p=128)
        cw_v = k.cw.rearrange("(n p) e -> p n e", p=128)
        sc = 0
        for b in range(NB):
            nb_tok = ntile * 128
            for c in range(8):
                P.dma(lambda e, b=b, c=c: e.dma_start(out=t["fTb"][:, c, 0:nb_tok], in_=fT_v[:, c, b * TB:b * TB + nb_tok]), writes=["fTb"])
            P.dma(lambda e, b=b: e.dma_start(out=t["cwb"][:, 0:ntile, :], in_=cw_v[:, b * 18:b * 18 + ntile, :]), writes=["cwb"])
            for ex in range(32):
                for nm, src, dst in (("w1", k.moe_w1, "w1b"), ("w3", k.moe_w3, "w3b")):
                    st = t["st%d" % (sc % 2)]
                    sk = "st%d" % (sc % 2)
                    sc += 1
                    P.dma(lambda e, st=st, src=src, ex=ex: e.dma_start(out=st[:], in_=src[l][ex].rearrange("(c p) n -> p c n", p=128)), writes=[sk])
                    P.op("pool", lambda e, st=st, dst=dst: e.tensor_copy(out=t[dst][:], in_=st[:]), reads=[sk], writes=[dst])
                st = t["st%d" % (sc % 2)]
                sk = "st%d" % (sc % 2)
                sc += 1
                stv = st[:].rearrange("p a b -> p (a b)").rearrange("p (c n) -> p c n", c=4)
                P.dma(lambda e, stv=stv, ex=ex: e.dma_start(out=stv, in_=k.moe_w2[l][ex].rearrange("(c p) n -> p c n", p=128)), writes=[sk])
                P.op("pool", lambda e, stv=stv: e.tensor_copy(out=t["w2b"][:], in_=stv), reads=[sk], writes=["w2b"])
                for (g0, W) in blocks:
                    for ffc in range(4):
                        pa, pb = p1[ffc % 2], p3[ffc % 2]
                        ka, kb = "mp1_%d" % (ffc % 2), "mp3_%d" % (ffc % 2)
                        for kk in range(8):
                            P.op("pe", lambda e, pa=pa, kk=kk, ffc=ffc, g0=g0, W=W: e.matmul(pa[:, 0:W], lhsT=t["w1b"][:, kk, ffc * 128:(ffc + 1) * 128], rhs=t["fTb"][:, kk, g0:g0 + W], start=(kk == 0), stop=(kk == 7)),
                                 reads=["w1b", "fTb"], writes=[ka], skip_self=(kk > 0))
                        for kk in range(8):
                            P.op("pe", lambda e, pb=pb, kk=kk, ffc=ffc, g0=g0, W=W: e.matmul(pb[:, 0:W], lhsT=t["w3b"][:, kk, ffc * 128:(ffc + 1) * 128], rhs=t["fTb"][:, kk, g0:g0 + W], start=(kk == 0), stop=(kk == 7)),
                                 reads=["w3b", "fTb"], writes=[kb], skip_self=(kk > 0))
                        P.op("act", lambda e, pa=pa, W=W: e.activation(out=t["s1"][:, 0:W], in_=pa[:, 0:W], func=AF.Silu), reads=[ka], writes=["s1"])
                        P.op("dve", lambda e, pb=pb, W=W, ffc=ffc: e.tensor_tensor(out=t["act"][:, ffc, 0:W], in0=pb[:, 0:W], in1=t["s1"][:, 0:W], op=ALU.mult), reads=[kb, "s1"], writes=["act"])
                    for sidx in range(W // 128):
                        ti = g0 // 128 + sidx
                        for half in range(2):
                            ps = py[half]
                            pk = "mpy_%d" % half
                            for ffc in range(4):
                                P.op("pe", lambda e, ps=ps, ffc=ffc, sidx=sidx, half=half: e.matmul(ps[:, :], lhsT=t["act"][:, ffc, sidx * 128:(sidx + 1) * 128], rhs=t["w2b"][:, ffc, half * 512:(half + 1) * 512],
                                                                                              start=(ffc == 0), stop=(ffc == 3)), reads=["act", "w2b"], writes=[pk], skip_self=(ffc > 0))
                            dst = t["acc"][:, ti, half * 512:(half + 1) * 512]
                            if ex == 0:
                                P.op("dve", lambda e, ps=ps, dst=dst, ti=ti, ex=ex: e.tensor_scalar(out=dst, in0=ps[:, :], scalar1=t["cwb"][:, ti, ex:ex + 1], scalar2=None, op0=ALU.mult), reads=[pk, "cwb"], writes=["acc"])
                            else:
                                P.op("dve", lambda e, ps=ps, dst=dst, ti=ti, ex=ex: e.scalar_tensor_tensor(out=dst, in0=ps[:, :], scalar=t["cwb"][:, ti, ex:ex + 1], in1=dst, op0=ALU.mult, op1=ALU.add),
                                     reads=[pk, "cwb", "acc"], writes=["acc"])
            for i in range(ntile):
                t0 = b * TB + i * 128
                row = b if i < 16 else NB
                if i == 0 or i == 16:
                    P.dma(lambda e, row=row: e.dma_start(out=t["g2b"][:], in_=k.modrow[l][row, 5 * D:6 * D].partition_broadcast(128)), writes=["g2b"])
                P.dma(lambda e, t0=t0: e.dma_start(out=t["xt"][:], in_=k.x1[t0:t0 + 128, :]), writes=["xt"])
                P.op("pool", lambda e, i=i: e.tensor_tensor(out=t["acc"][:, i, :], in0=t["acc"][:, i, :], in1=t["g2b"][:], op=ALU.mult), reads=["acc", "g2b"], writes=["acc"])
                P.op("dve", lambda e, i=i: e.tensor_tensor(out=t["xt"][:], in0=t["xt"][:], in1=t["acc"][:, i, :], op=ALU.add), reads=["xt", "acc"], writes=["xt"])
                if last:
                    P.dma(lambda e, b=b, i=i: e.dma_start(out=k.out[b, i * 128:(i + 1) * 128, :], in_=t["xt"][:]), reads=["xt"], writes=[("outd", b, i)], dkey="outst")
                else:
                    P.dma(lambda e, t0=t0: e.dma_start(out=k.x2[t0:t0 + 128, :], in_=t["xt"][:]), reads=["xt"], writes=[("x2d", t0)], dkey="outst")
        P.barrier()
        P.emit()


def build_program(NB, n_stages=99, dbg=()):
    nc = bass.Bass("TRN2", target_bir_lowering=False)
    k = K()
    k.nc, k.NB = nc, NB
    k.P = Prog(nc)
    T = NB * TB
    k.T = T

    def din(name, shape, dt=F32):
        return nc.dram_tensor(name, list(shape), dt, kind="ExternalInput").ap()

    def dscr(name, shape, dt, out=False):
        return nc.dram_tensor(name, list(shape), dt, kind=("ExternalOutput" if out else "Internal")).ap()

    k.x = din("x", (NB, SEQ, D)); k.ctx = din("ctx", (NB, NCTX, D)); k.c = din("c", (NB, D)); k.c_ctx = din("c_ctx", (D,))
    k.w_mod = din("w_mod", (DEPTH, D, 6 * D)); k.b_mod = din("b_mod", (DEPTH, 6 * D))
    k.norm1_g = din("norm1_g", (DEPTH, D)); k.norm2_g = din("norm2_g", (DEPTH, D))
    k.w_in = din("w_in", (DEPTH, D, INC))
    k.identf = din("identf", (128, 128))
    k.mla_q_norm_g = din("mla_q_norm_g", (DEPTH, 192)); k.mla_w_uq = din("mla_w_uq", (DEPTH, 192, 384))
    k.mla_kv_norm_g = din("mla_kv_norm_g", (DEPTH, 128)); k.mla_w_ukv = din("mla_w_ukv", (DEPTH, 128, 512))
    k.mla_qn_g = din("mla_qn_g", (DEPTH, 96)); k.mla_kn_g = din("mla_kn_g", (DEPTH, 96))
    k.ret_log_gamma = din("ret_log_gamma", (DEPTH, 2, 4)); k.ret_norm_g = din("ret_norm_g", (DEPTH, 256))
    k.lru_conv_w = din("lru_conv_w", (DEPTH, 4, 256)); k.lru_conv_b = din("lru_conv_b", (DEPTH, 256))
    k.lru_wa = din("lru_wa", (DEPTH, 2, 4, 64, 64)); k.lru_ba = din("lru_ba", (DEPTH, 2, 256))
    k.lru_wx = din("lru_wx", (DEPTH, 2, 4, 64, 64)); k.lru_bx = din("lru_bx", (DEPTH, 2, 256)); k.lru_lambda = din("lru_lambda", (DEPTH, 2, 256))
    k.hy_conv_w = din("hy_conv_w", (DEPTH, 3, 768)); k.hy_conv_b = din("hy_conv_b", (DEPTH, 768))
    k.hy_w1 = din("hy_w1", (DEPTH, 33, 64)); k.hy_b1 = din("hy_b1", (DEPTH, 64)); k.hy_w2 = din("hy_w2", (DEPTH, 64, 64)); k.hy_b2 = din("hy_b2", (DEPTH, 64))
    k.hy_w3 = din("hy_w3", (DEPTH, 64, 512)); k.hy_freq = din("hy_freq", (DEPTH, 64)); k.hy_d = din("hy_d", (DEPTH, 256))
    k.c_m0 = din("c_m0", (128, 2))
    k.hc = {}
    for Lh in (SEQ, NCTX):
        k.hc[Lh] = {"zf": din("hz_f%d" % Lh, (33, Lh)), "zr": din("hz_r%d" % Lh, (33, Lh)), "tf": din("ht_f%d" % Lh, (128, Lh // 128)), "tr": din("ht_r%d" % Lh, (128, Lh // 128)),
                    "dl": din("h_dl%d" % Lh, (128, 256)), "Wf": din("h_Wf%d" % Lh, (Lh, 2 * Lh), BF16), "Winv": din("h_Wi%d" % Lh, (2 * Lh, Lh), BF16),
                    "csl": din("h_csl%d" % Lh, (128, 2 * Lh // 128)), "csh": din("h_csh%d" % Lh, (128, 2 * Lh // 128))}
    k.group_norm_g = din("group_norm_g", (DEPTH, D)); k.w_out = din("w_out", (DEPTH, D, D))
    k.moe_w_group = din("moe_w_group", (DEPTH, D, 4)); k.moe_w_expert = din("moe_w_expert", (DEPTH, D, 32))
    k.moe_w1 = din("moe_w1", (DEPTH, 32, D, 512)); k.moe_w3 = din("moe_w3", (DEPTH, 32, D, 512)); k.moe_w2 = din("moe_w2", (DEPTH, 32, 512, D))
    k.x1 = dscr("x1", (T, D), F32, out=("x1" in dbg)); k.fT = dscr("fT", (D, T), BF16); k.cw = dscr("cw", (T, 32), F32, out=("cw" in dbg))
    k.out = nc.dram_tensor("out", [NB, SEQ, D], F32, kind="ExternalOutput").ap()
    k.c_iota32 = din("c_iota32", (128, 32)); k.c_LT = din("c_LT", (128, 128)); k.c_ONES = din("c_ONES", (128, 128)); k.c_jbv = din("c_jbv", (128, 128))
    k.c_iotaA = din("c_iotaA", (128, 8)); k.c_iotaB = din("c_iotaB", (128, 4))
    k.nblk = [-(-(2 * NB * nt_ * 128) // MOE_BS) + 32 for nt_ in (18, 16)]
    assert max(k.nblk) <= MAXBLK
    k.rt = dscr("rt", (T, 8), F32); k.ftm = dscr("ftm", (T, D), BF16)
    k.pstart = dscr("pstart", (DEPTH, 128, 32), F32); k.bexp = dscr("bexp", (DEPTH, 128, MAXBLK), F32)
    k.xs = dscr("xs", (max(k.nblk) * MOE_BS, D), BF16); k.ys = dscr("ys", (max(k.nblk) * MOE_BS, D), BF16)
    k.rope_cos = din("rope_cos", (SEQ, 16)); k.rope_sin = din("rope_sin", (SEQ, 16))
    k.c_rel0 = din("c_rel0", (128, 128)); k.c_mge = din("c_mge", (128, 128)); k.c_mle = din("c_mle", (128, 128)); k.c_dvals = din("c_dvals", (128, 18))
    k.yT = dscr("yT", (D, T), BF16, out=("yT" in dbg))
    k.cc = 0; k.oc = 0; k.yc = 0
    k.modrow = dscr("modrow", (DEPTH, NB + 1, 6 * D), F32, out=("modrow" in dbg))
    k.zt = dscr("zt", (T, ZT_W), BF16, out=("zt" in dbg))
    k.zf = dscr("zf", (ZF_ROWS, T), BF16, out=("zf" in dbg))
    k.x2 = dscr("x2", (T, D), F32)
    with nc.allow_low_precision("bf16 matmul operands, fp32 accumulation"), nc.allow_non_contiguous_dma("small strided loads"):
        stage_mod(k)
        stages = []
        for l in range(DEPTH):
            stages += [lambda l=l: stage_inproj(k, l), lambda l=l: stage_mla(k, l), lambda l=l: stage_ret(k, l), lambda l=l: stage_lru(k, l),
                       lambda l=l: stage_hyena(k, l, SEQ, 0)]
            if l < DEPTH - 1:
                stages.append(lambda l=l: stage_hyena(k, l, NCTX, SEQ))
            stages += [lambda l=l: stage_outproj(k, l), lambda l=l: (stage_moe_sparse(k, l) if SPARSE_MOE else stage_moe(k, l))]
        for si, f in enumerate(stages[:max(0, n_stages - 1)]):
            with nc.named_scope("st%02d" % si):
                f()
    return nc, k


def host_consts():
    c = {"identf": np.eye(128, dtype=np.float32)}
    rows = SEQ // 64
    row = np.repeat(np.arange(rows), 64).astype(np.float32)
    col = np.tile(np.arange(64), rows).astype(np.float32)
    inv_freq = (10000.0 ** (-np.arange(8, dtype=np.float32) / 8)).astype(np.float32)
    ang = np.stack([row[:, None] * inv_freq, col[:, None] * inv_freq], axis=1).astype(np.float32)
    c["rope_cos"] = np.cos(ang).reshape(SEQ, 16).astype(np.float32)
    c["rope_sin"] = np.sin(ang).reshape(SEQ, 16).astype(np.float32)
    jl = np.arange(128, dtype=np.float32)[:, None]
    cc = np.arange(128, dtype=np.float32)[None, :]
    c["c_rel0"] = (cc - jl).astype(np.float32)
    c["c_mge"] = (cc >= jl).astype(np.float32)
    c["c_mle"] = (cc <= jl).astype(np.float32)
    m0 = np.ones((128, 2), np.float32); m0[0, 0] = 0.0; m0[:, 1] = 1.0 - m0[:, 0]
    c["c_m0"] = m0
    names = {"zf": "hz_f", "zr": "hz_r", "tf": "ht_f", "tr": "ht_r", "dl": "h_dl", "Wf": "h_Wf", "Winv": "h_Wi", "csl": "h_csl", "csh": "h_csh"}
    for Lh in (SEQ, NCTX):
        hcn = hyena_consts(Lh)
        for kk2, v in hcn.items():
            c[names[kk2] + str(Lh)] = v
    c["c_iota32"] = np.broadcast_to(np.arange(32, dtype=np.float32)[None, :], (128, 32)).copy()
    tt_ = np.arange(128)
    c["c_LT"] = (tt_[:, None] < tt_[None, :]).astype(np.float32)
    c["c_ONES"] = np.ones((128, 128), np.float32)
    c["c_jbv"] = np.broadcast_to((512.0 * np.arange(128, dtype=np.float32))[None, :], (128, 128)).copy()
    c["c_iotaA"] = (np.arange(8, dtype=np.float32)[None, :] * 128 + np.arange(128, dtype=np.float32)[:, None]).astype(np.float32)
    c["c_iotaB"] = (np.arange(4, dtype=np.float32)[None, :] * 128 + np.arange(128, dtype=np.float32)[:, None]).astype(np.float32)
    c["c_dvals"] = np.broadcast_to(128.0 * np.arange(18, dtype=np.float32)[None, :], (128, 18)).astype(np.float32).copy()
    return c


IN_NAMES = ["w_mod", "b_mod", "norm1_g", "norm2_g", "w_in", "mla_q_norm_g", "mla_w_uq", "mla_kv_norm_g", "mla_w_ukv", "mla_qn_g", "mla_kn_g",
            "ret_log_gamma", "ret_norm_g", "lru_conv_w", "lru_conv_b", "lru_wa", "lru_ba", "lru_wx", "lru_bx", "lru_lambda",
            "hy_conv_w", "hy_conv_b", "hy_w1", "hy_b1", "hy_w2", "hy_b2", "hy_w3", "hy_freq", "hy_d",
            "group_norm_g", "w_out", "moe_w_group", "moe_w_expert", "moe_w1", "moe_w3", "moe_w2"]


def make_in_map(inp, b0, NB, consts=None):
    consts = consts if consts is not None else host_consts()
    m = {"x": np.ascontiguousarray(inp["x"][b0:b0 + NB]), "ctx": np.ascontiguousarray(inp["ctx"][b0:b0 + NB]),
         "c": np.ascontiguousarray(inp["c"][b0:b0 + NB]), "c_ctx": np.asarray(inp["c_ctx"])}
    for n in IN_NAMES:
        m[n] = np.asarray(inp[n])
    m.update(consts)
    return m


_CACHE = {}


def kernel(**inputs):
    NB = 4
    n_cores = 8
    if "nc" not in _CACHE:
        _CACHE["nc"] = build_program(NB)[0]
        _CACHE["consts"] = host_consts()
    nc = _CACHE["nc"]
    in_maps = [make_in_map(inputs, i * NB, NB, _CACHE["consts"]) for i in range(n_cores)]
    res = run_bass_kernel_spmd(nc, in_maps, core_ids=list(range(n_cores)))
    return np.concatenate([np.asarray(r["out"]) for r in res.results], axis=0).astype(np.float32)
```

```python
import math
from contextlib import ExitStack
import numpy as np
import ml_dtypes
import concourse.bass as bass
import concourse.mybir as mybir
from concourse.bass_utils import run_bass_kernel_spmd

F32 = mybir.dt.float32
BF16 = mybir.dt.bfloat16
AF = mybir.ActivationFunctionType
ALU = mybir.AluOpType
AX = mybir.AxisListType
ENGS = ("pe", "act", "dve", "pool", "sp")

D = 1024
SEQ = 2048
NCTX = 256
TB = SEQ + NCTX
DEPTH = 2
EPS = 1e-6
INC = 2656
SPARSE_MOE = True
STRICT_SCATTER = False


class Prog:
    def __init__(self, nc):
        self.nc = nc
        self.q = {e: [] for e in ENGS}
        self.cnt = {e: 0 for e in ENGS}
        self.known = {e: {} for e in ENGS}
        self.w = {}
        self.r = {}
        self.dsem = {}
        self.semobj = {}
        for e in ENGS:
            self.semobj[("c", e)] = nc.alloc_semaphore(name="c_" + e)
        self.out_waits = {}
        self.n_ins = 0
        self.n_wait = 0
        self.free_hw = []
        self.free_sw = []
        self.nsem = 0

    def _deps(self, e, reads, writes, skip_self):
        deps = {}
        for k in reads:
            for s, v in self.w.get(k, {}).items():
                if deps.get(s, -1) < v:
                    deps[s] = v
        for k in writes:
            for d in (self.w.get(k, {}), self.r.get(k, {})):
                for s, v in d.items():
                    if deps.get(s, -1) < v:
                        deps[s] = v
        waits = []
        kn = self.known[e]
        for s, v in deps.items():
            if skip_self and s == ("c", e):
                continue
            if kn.get(s, 0) >= v:
                continue
            kn[s] = v
            waits.append((s, v))
        return waits

    def _update(self, me, reads, writes):
        s, v = me
        for k in writes:
            if self.r.get(k):
                self.w[k] = {s: v}
                self.r[k] = {}
            else:
                self.w.setdefault(k, {})[s] = v
        for k in reads:
            self.r.setdefault(k, {})[s] = v

    def op(self, e, fn, reads=(), writes=(), skip_self=False):
        waits = self._deps(e, reads, writes, skip_self)
        self.cnt[e] += 1
        self._update((("c", e), self.cnt[e]), reads, writes)
        self.q[e].append((waits, fn, (("c", e), 1)))
        self.n_ins += 1
        self.n_wait += len(waits)

    def dma(self, fn, reads=(), writes=(), dkey=None, q="sp", is_output=False):
        if dkey is None:
            dkey = writes[0]
        waits = self._deps(q, reads, writes, False)
        if dkey not in self.dsem:
            pool = self.free_sw if q == "pool" else self.free_hw
            if pool:
                ent = pool.pop()
            else:
                self.nsem += 1
                ent = [("d", self.nsem), 0, q == "pool"]
                self.semobj[ent[0]] = self.nc.alloc_semaphore(name="d%d" % self.nsem)
            self.dsem[dkey] = ent
        ent = self.dsem[dkey]
        assert ent[2] == (q == "pool"), "DMA semaphore shared between software and hardware DGE: %r" % (dkey,)
        if ent[2] and ent[1] > 0 and self.known[q].get(ent[0], 0) < ent[1]:
            self.known[q][ent[0]] = ent[1]
            waits.append((ent[0], ent[1]))
        ent[1] += 16
        self._update((ent[0], ent[1]), reads, writes)
        self.q[q].append((waits, fn, (ent[0], 16)))
        if is_output:
            self.out_waits[ent[0]] = ent[1]
        self.n_ins += 1
        self.n_wait += len(waits)

    def barrier(self):
        allv = {("c", e): self.cnt[e] for e in ENGS if self.cnt[e] > 0}
        for ent in list(self.dsem.values()) + self.free_hw + self.free_sw:
            if ent[1] > 0:
                allv[ent[0]] = ent[1]
        for e in ENGS:
            kn = self.known[e]
            waits = []
            for s, v in allv.items():
                if kn.get(s, 0) < v:
                    kn[s] = v
                    waits.append((s, v))
            if waits:
                self.q[e].append((waits, None, None))
        self.w = {}
        self.r = {}
        for ent in self.dsem.values():
            (self.free_sw if ent[2] else self.free_hw).append(ent)
        self.dsem = {}

    def emit(self):
        nc = self.nc
        final = list(self.out_waits.items())
        with nc.Block() as block:
            def run(e):
                def body(eng):
                    for waits, fn, inc in self.q[e]:
                        for s, v in waits:
                            eng.wait_ge(self.semobj[s], v)
                        if fn is not None:
                            fn(eng).then_inc(self.semobj[inc[0]], inc[1])
                    if e == "sp":
                        for s, v in final:
                            eng.wait_ge(self.semobj[s], v)
                return body
            block.tensor(run("pe"))
            block.scalar(run("act"))
            block.vector(run("dve"))
            block.gpsimd(run("pool"))
            block.sync(run("sp"))
        self.q = {e: [] for e in ENGS}
        self.out_waits = {}


class Rec:
    def __init__(self, sfx, local):
        self.ops, self.sfx, self.local = [], sfx, local

    def _k(self, keys):
        return [(x + self.sfx) if (isinstance(x, str) and x in self.local) else x for x in keys]

    def op(self, e, fn, reads=(), writes=(), skip_self=False):
        self.ops.append((0, e, fn, self._k(reads), self._k(writes), skip_self))

    def dma(self, fn, reads=(), writes=(), dkey=None, q="sp", is_output=False):
        if dkey is None:
            dkey = writes[0]
        self.ops.append((1, fn, self._k(reads), self._k(writes), self._k([dkey])[0], q, is_output))


def replay(P, recs):
    idx = [0] * len(recs)
    live = True
    while live:
        live = False
        for j, r in enumerate(recs):
            if idx[j] < len(r.ops):
                o = r.ops[idx[j]]
                idx[j] += 1
                live = True
                if o[0] == 0:
                    P.op(o[1], o[2], reads=o[3], writes=o[4], skip_self=o[5])
                else:
                    P.dma(o[1], reads=o[2], writes=o[3], dkey=o[4], q=o[5], is_output=o[6])


class K:
    pass


_UID = [0]


def _u(name):
    _UID[0] += 1
    return "%s_%d" % (name, _UID[0])


def _tiles(es, nc, specs):
    out = {}
    for name, shape, dt in specs:
        out[name] = es.enter_context(nc.sbuf_tensor(_u(name), list(shape), dt))
    return out


def _psum(es, nc, name, shape, dt):
    return es.enter_context(nc.psum_tensor(_u(name), list(shape), dt))


def stage_mod(k):
    nc, P, NB = k.nc, k.P, k.NB
    R = NB + 1
    with ExitStack() as es:
        t = _tiles(es, nc, [("crow", (R, D), F32), ("srow", (R, D), F32), ("scT", (128, 8, R), F32),
                            ("wm0", (128, 8, 512), F32), ("wm1", (128, 8, 512), F32),
                            ("brow", (R, 6 * D), F32), ("mrow", (R, 6 * D), F32), ("idf", (128, 128), F32)])
        pt = _psum(es, nc, "pt0", [128, 512], F32)
        pm = [_psum(es, nc, "pm%d" % i, [128, 512], F32) for i in range(2)]
        P.dma(lambda e: e.dma_start(out=t["idf"][:], in_=k.identf), writes=["idf"])
        P.dma(lambda e: e.dma_start(out=t["crow"][0:NB, :], in_=k.c), writes=["crow"])
        P.dma(lambda e: e.dma_start(out=t["crow"][NB:R, :], in_=k.c_ctx.rearrange("(o d) -> o d", o=1)), writes=["crow"])
        P.op("act", lambda e: e.activation(out=t["srow"][:], in_=t["crow"][:], func=AF.Silu), reads=["crow"], writes=["srow"])
        for kk in range(8):
            P.op("pe", lambda e, kk=kk: e.transpose(out=pt[:, 0:R], in_=t["srow"][:, kk * 128:(kk + 1) * 128], identity=t["idf"][0:R, 0:R]),
                 reads=["srow", "idf"], writes=["pt"])
            P.op("dve", lambda e, kk=kk: e.tensor_copy(out=t["scT"][:, kk, :], in_=pt[:, 0:R]), reads=["pt"], writes=["scT"])
        for l in range(DEPTH):
            P.dma(lambda e, l=l: e.dma_start(out=t["brow"][:], in_=k.b_mod[l].partition_broadcast(R)), writes=["brow"])
            for n in range(12):
                wt = t["wm%d" % (n % 2)]
                wk = "wm%d" % (n % 2)
                P.dma(lambda e, l=l, n=n, wt=wt: e.dma_start(out=wt[:], in_=k.w_mod[l][:, n * 512:(n + 1) * 512].rearrange("(k p) n -> p k n", p=128)),
                      writes=[wk], q="sp")
                ps = pm[n % 2]
                pk = "pm%d" % (n % 2)
                for kk in range(8):
                    P.op("pe", lambda e, kk=kk, wt=wt, ps=ps: e.matmul(ps[0:R, :], lhsT=t["scT"][:, kk, :], rhs=wt[:, kk, :], start=(kk == 0), stop=(kk == 7)),
                         reads=["scT", wk], writes=[pk], skip_self=(kk > 0))
                P.op("dve", lambda e, n=n, ps=ps: e.tensor_tensor(out=t["mrow"][:, n * 512:(n + 1) * 512], in0=ps[0:R, :], in1=t["brow"][:, n * 512:(n + 1) * 512], op=ALU.add),
                     reads=[pk, "brow"], writes=["mrow"])
            P.dma(lambda e, l=l: e.dma_start(out=k.modrow[l], in_=t["mrow"][:]), reads=["mrow"], writes=[("modrow", l)])
        P.barrier()
        P.emit()


def load_mod_cols(k, es, l, which):
    nc, P, NB = k.nc, k.P, k.NB
    R = NB + 1
    A = es.enter_context(nc.sbuf_tensor(_u("modA"), [128, 8, R], F32))
    B = es.enter_context(nc.sbuf_tensor(_u("modB"), [128, 8, R], F32))
    with ExitStack() as e2:
        t = _tiles(e2, nc, [("mr", (R + 1, 2 * D), F32), ("gT", (128, 8, R + 1), F32), ("idf2", (128, 128), F32)])
        pt = _psum(e2, nc, "ptm", [128, 512], F32)
        base = 0 if which == 0 else 3 * D
        g = k.norm1_g if which == 0 else k.norm2_g
        P.dma(lambda e: e.dma_start(out=t["idf2"][:], in_=k.identf), writes=["idf2"])
        P.dma(lambda e: e.dma_start(out=t["mr"][0:R, :], in_=k.modrow[l][:, base:base + 2 * D]), reads=[("modrow", l)], writes=["mr"])
        P.dma(lambda e: e.dma_start(out=t["mr"][R:R + 1, 0:D], in_=g[l].rearrange("(o d) -> o d", o=1)), writes=["mr"])
        P.dma(lambda e: e.dma_start(out=t["mr"][R:R + 1, D:2 * D], in_=g[l].rearrange("(o d) -> o d", o=1)), writes=["mr"])
        for half, dst in ((0, B), (1, A)):
            for kk in range(8):
                c0 = half * D + kk * 128
                P.op("pe", lambda e, c0=c0: e.transpose(out=pt[:, 0:R + 1], in_=t["mr"][:, c0:c0 + 128], identity=t["idf2"][0:R + 1, 0:R + 1]),
                     reads=["mr", "idf2"], writes=["ptm"])
                if half == 0:
                    P.op("dve", lambda e, kk=kk: e.tensor_copy(out=B[:, kk, :], in_=pt[:, 0:R]), reads=["ptm"], writes=["modB"])
                else:
                    P.op("dve", lambda e, kk=kk: e.tensor_copy(out=t["gT"][:, kk, :], in_=pt[:, 0:R + 1]), reads=["ptm"], writes=["gT"])
                    P.op("dve", lambda e, kk=kk: e.tensor_scalar(out=A[:, kk, :], in0=t["gT"][:, kk, 0:R], scalar1=1.0, scalar2=t["gT"][:, kk, R:R + 1],
                                                               op0=ALU.add, op1=ALU.mult), reads=["gT"], writes=["modA"])
        P.barrier()
    return A, B


def xsrc(k, l, b, i):
    if l == 0:
        if i < 16:
            return k.x[b, i * 128:(i + 1) * 128, :]
        return k.ctx[b, (i - 16) * 128:(i - 15) * 128, :]
    t0 = b * TB + i * 128
    return k.x2[t0:t0 + 128, :]


def norm_mod_T(k, t, pst, xt_ap, xkey, A, B, b, dstT, dkeyT, col0, tag):
    P = k.P
    ss, rstd, xn = t["ss" + tag], t["rstd" + tag], t["xn" + tag]
    P.op("act", lambda e: e.activation(out=t["junk" + tag][:], in_=xt_ap, func=AF.Square, accum_out=ss[:]), reads=[xkey], writes=["junk" + tag, "ss" + tag])
    P.op("act", lambda e: e.activation(out=ss[:], in_=ss[:], func=AF.Sqrt, scale=1.0 / D, bias=EPS), reads=["ss" + tag], writes=["ss" + tag])
    P.op("dve", lambda e: e.reciprocal(out=rstd[:], in_=ss[:]), reads=["ss" + tag], writes=["rstd" + tag])
    P.op("dve", lambda e: e.tensor_scalar(out=xn[:], in0=xt_ap, scalar1=rstd[:], scalar2=None, op0=ALU.mult), reads=[xkey, "rstd" + tag], writes=["xn" + tag])
    for kk in range(8):
        P.op("pe", lambda e, kk=kk: e.transpose(out=pst[:, kk, :], in_=xn[:, kk * 128:(kk + 1) * 128], identity=t["idb"][:]),
             reads=["xn" + tag, "idb"], writes=["pst"])
    for kk in range(8):
        eng = "act" if kk % 2 == 0 else "dve"
        if eng == "act":
            P.op("act", lambda e, kk=kk: e.activation(out=dstT[:, kk, col0:col0 + 128], in_=pst[:, kk, :], func=AF.Identity,
                                                     scale=A[:, kk, b:b + 1], bias=B[:, kk, b:b + 1]), reads=["pst", "modA", "modB"], writes=[dkeyT])
        else:
            P.op("dve", lambda e, kk=kk: e.tensor_scalar(out=dstT[:, kk, col0:col0 + 128], in0=pst[:, kk, :], scalar1=A[:, kk, b:b + 1], scalar2=B[:, kk, b:b + 1],
                                                        op0=ALU.mult, op1=ALU.add), reads=["pst", "modA", "modB"], writes=[dkeyT])


TM_BLOCKS = ((0, 352, 0), (608, 512, 352), (1120, 256, 864))
ZT_W = 1120
FM_COLS = [352, 480, 608, 736] + [1376 + 128 * i for i in range(6)] + [2144, 2272, 2400, 2528]
ZF_ROWS = 128 * len(FM_COLS)
ZF_RQ, ZF_RK, ZF_ZH, ZF_LX, ZF_LG = 0, 256, 512, 1280, 1536


def stage_inproj(k, l):
    nc, P, NB = k.nc, k.P, k.NB
    with ExitStack() as es:
        A, B = load_mod_cols(k, es, l, 0)
        t = _tiles(es, nc, [("win", (128, 8, INC), BF16), ("wst0", (128, INC), F32), ("wst1", (128, INC), F32),
                            ("idf", (128, 128), F32), ("idb", (128, 128), BF16),
                            ("xt0", (128, D), F32), ("xt1", (128, D), F32),
                            ("junk0", (128, D), BF16), ("junk1", (128, D), BF16),
                            ("ss0", (128, 1), F32), ("ss1", (128, 1), F32), ("rstd0", (128, 1), F32), ("rstd1", (128, 1), F32),
                            ("xn0", (128, D), BF16), ("xn1", (128, D), BF16),
                            ("aT0", (128, 8, 512), BF16), ("aT1", (128, 8, 512), BF16),
                            ("zf0", (128, 14, 512), BF16), ("zf1", (128, 14, 512), BF16),
                            ("zt0", (128, ZT_W), BF16), ("zt1", (128, ZT_W), BF16)])
        pst = _psum(es, nc, "pst", [128, 8, 128], BF16)
        pf = [_psum(es, nc, "pf%d" % i, [128, 512], F32) for i in range(3)]
        pq = [_psum(es, nc, "pq%d" % i, [128, 512], F32) for i in range(3)]
        P.dma(lambda e: e.dma_start(out=t["idf"][:], in_=k.identf), writes=["idf"])
        P.op("dve", lambda e: e.tensor_copy(out=t["idb"][:], in_=t["idf"][:]), reads=["idf"], writes=["idb"])
        for kk in range(8):
            ws = t["wst%d" % (kk % 2)]
            wk = "wst%d" % (kk % 2)
            P.dma(lambda e, kk=kk, ws=ws: e.dma_start(out=ws[:], in_=k.w_in[l][kk * 128:(kk + 1) * 128, :]), writes=[wk], q="sp")
            P.op("pool", lambda e, kk=kk, ws=ws: e.tensor_copy(out=t["win"][:, kk, :], in_=ws[:]), reads=[wk], writes=["win"])
        zf_v = k.zf.rearrange("(c p) t -> p c t", p=128)
        gi = 0
        ti = 0
        for b in range(NB):
            for (g0, W) in ((0, 512), (512, 512), (1024, 512), (1536, 512), (2048, 256)):
                aT = t["aT%d" % (gi % 2)]
                ak = "aT%d" % (gi % 2)
                nt = W // 128
                for j in range(nt):
                    tag = str(ti % 2)
                    i = g0 // 128 + j
                    P.dma(lambda e, b=b, i=i, tag=tag: e.dma_start(out=t["xt" + tag][:], in_=xsrc(k, l, b, i)),
                          reads=([("x2", b, i)] if l > 0 else []), writes=["xt" + tag], q="sp")
                    norm_mod_T(k, t, pst, t["xt" + tag][:], "xt" + tag, A, B, (b if i < 16 else NB), aT, ak, j * 128, tag)
                    zt = t["zt" + tag]
                    for bi, (c0, w, d0) in enumerate(TM_BLOCKS):
                        for kk in range(8):
                            P.op("pe", lambda e, kk=kk, c0=c0, w=w, j=j, bi=bi, aT=aT: e.matmul(pq[bi][:, 0:w], lhsT=aT[:, kk, j * 128:(j + 1) * 128], rhs=t["win"][:, kk, c0:c0 + w],
                                                                                  start=(kk == 0), stop=(kk == 7)),
                                 reads=[ak, "win"], writes=[("pq", bi)], skip_self=(kk > 0))
                        if bi == 1:
                            P.op("act", lambda e, zt=zt: e.activation(out=zt[:, 352:608], in_=pq[1][:, 0:256], func=AF.Copy, scale=0.125), reads=[("pq", 1)], writes=["zt" + tag])
                            P.op("dve", lambda e, zt=zt: e.tensor_copy(out=zt[:, 608:864], in_=pq[1][:, 256:512]), reads=[("pq", 1)], writes=["zt" + tag])
                        elif bi == 0:
                            P.op("act", lambda e, zt=zt: e.copy(out=zt[:, 0:352], in_=pq[0][:, 0:352]), reads=[("pq", 0)], writes=["zt" + tag])
                        else:
                            P.op("dve", lambda e, zt=zt: e.tensor_copy(out=zt[:, 864:1120], in_=pq[2][:, 0:256]), reads=[("pq", 2)], writes=["zt" + tag])
                    t0 = b * TB + g0 + j * 128
                    P.dma(lambda e, zt=zt, t0=t0: e.dma_start(out=k.zt[t0:t0 + 128, :], in_=zt[:]), reads=["zt" + tag], writes=[("ztd", l, b, i)], dkey=("ztd", tag))
                    ti += 1
                zfs = t["zf%d" % (gi % 2)]
                zk = "zf%d" % (gi % 2)
                for ci, c0 in enumerate(FM_COLS):
                    ps = pf[ci % 3]
                    for kk in range(8):
                        P.op("pe", lambda e, kk=kk, c0=c0, ps=ps, aT=aT, W=W: e.matmul(ps[:, 0:W], lhsT=t["win"][:, kk, c0:c0 + 128], rhs=aT[:, kk, 0:W], start=(kk == 0), stop=(kk == 7)),
                             reads=[ak, "win"], writes=[("pf", ci % 3)], skip_self=(kk > 0))
                    if ci in (2, 3):
                        P.op("act", lambda e, ci=ci, ps=ps, W=W, zfs=zfs: e.activation(out=zfs[:, ci, 0:W], in_=ps[:, 0:W], func=AF.Copy, scale=0.125), reads=[("pf", ci % 3)], writes=[zk])
                    elif ci % 2 == 0:
                        P.op("act", lambda e, ci=ci, ps=ps, W=W, zfs=zfs: e.copy(out=zfs[:, ci, 0:W], in_=ps[:, 0:W]), reads=[("pf", ci % 3)], writes=[zk])
                    else:
                        P.op("dve", lambda e, ci=ci, ps=ps, W=W, zfs=zfs: e.tensor_copy(out=zfs[:, ci, 0:W], in_=ps[:, 0:W]), reads=[("pf", ci % 3)], writes=[zk])
                t0 = b * TB + g0
                for ci in range(len(FM_COLS)):
                    P.dma(lambda e, zfs=zfs, t0=t0, W=W, ci=ci: e.dma_start(out=zf_v[:, ci, t0:t0 + W], in_=zfs[:, ci, 0:W]), reads=[zk], writes=[("zfd", l, b, g0)],
                          dkey=("zfd", gi % 2))
                gi += 1
        P.barrier()
        P.emit()


def attn_core(k, pfx, QT_h, KT_h, V_h, q0, W, key_tiles, transform, ps_s, ps_o, pt, dv, rkeys, okey):
    P = k.P
    nt = W // 128
    for n, kt in enumerate(key_tiles):
        sb = k.cc % 2
        k.cc += 1
        P.op("pe", lambda e, kt=kt, sb=sb: e.matmul(ps_s[sb][:, 0:W], lhsT=KT_h[:, kt * 128:(kt + 1) * 128], rhs=QT_h[:, q0:q0 + W], start=True, stop=True),
             reads=rkeys, writes=[pfx + "ps_s%d" % sb])
        transform(kt, ps_s[sb], pfx + "ps_s%d" % sb, pt[sb], pfx + "pt%d" % sb)
        for sidx in range(nt):
            P.op("pe", lambda e, kt=kt, sb=sb, sidx=sidx, n=n: e.matmul(ps_o[sidx][:, 0:dv], lhsT=pt[sb][:, sidx * 128:(sidx + 1) * 128], rhs=V_h(kt),
                                                                  start=(n == 0), stop=(n == len(key_tiles) - 1)),
                 reads=[pfx + "pt%d" % sb] + rkeys, writes=[okey + str(sidx)], skip_self=(n > 0))


def rms_rstd(k, t, src_ap, srckey, junk, jkey, ss, sskey, n):
    P = k.P
    P.op("act", lambda e: e.activation(out=junk, in_=src_ap, func=AF.Square, accum_out=ss), reads=[srckey], writes=[jkey, sskey])
    P.op("act", lambda e: e.activation(out=ss, in_=ss, func=AF.Sqrt, scale=1.0 / n, bias=EPS), reads=[sskey], writes=[sskey])
    P.op("dve", lambda e: e.reciprocal(out=ss, in_=ss), reads=[sskey], writes=[sskey])


def transpose_to(k, t, pstile, pskey, src_blocks, dst_ap, dstkey, srckeys, npart, eng="act"):
    P = k.P
    for i, blk in enumerate(src_blocks):
        w = blk.shape[-1]
        P.op("pe", lambda e, i=i, blk=blk, w=w: e.transpose(out=pstile[0:w, i, :], in_=blk, identity=t["idb"][:]), reads=srckeys + ["idb"], writes=[pskey])
    n = len(src_blocks)
    if eng == "act":
        P.op("act", lambda e: e.copy(out=dst_ap, in_=pstile[0:npart, 0:n, :]), reads=[pskey], writes=[dstkey])
    else:
        P.op("dve", lambda e: e.tensor_copy(out=dst_ap, in_=pstile[0:npart, 0:n, :]), reads=[pskey], writes=[dstkey])


def store_yT(k, t, ytm, ykey, nt, row0, tcol0, pstile, pskey, l):
    P = k.P
    W = nt * 128
    yts = t["yts%d" % (k.yc % 2)]
    ytk = "yts%d" % (k.yc % 2)
    k.yc += 1
    for c in range(2):
        for sidx in range(nt):
            P.op("pe", lambda e, c=c, sidx=sidx: e.transpose(out=pstile[:, sidx, :], in_=ytm[:, sidx, c * 128:(c + 1) * 128], identity=t["idb"][:]),
                 reads=[ykey, "idb"], writes=[pskey])
        P.op("act", lambda e, c=c: e.copy(out=yts[:, c, 0:W], in_=pstile[:, 0:nt, :]), reads=[pskey], writes=[ytk])
        P.dma(lambda e, c=c: e.dma_start(out=k.yT[row0 + c * 128:row0 + (c + 1) * 128, tcol0:tcol0 + W], in_=yts[:, c, 0:W]), reads=[ytk],
              writes=[("yTd", l, row0, c, tcol0)], dkey=("yTd", ytk))


def stage_mla(k, l):
    nc, P, NB = k.nc, k.P, k.NB
    with ExitStack() as es:
        t = _tiles(es, nc, [("idf", (128, 128), F32), ("idb", (128, 128), BF16), ("wuqf", (128, 2, 384), F32), ("wuq", (128, 2, 384), BF16), ("gq", (128, 2), F32), ("wukvf", (128, 512), F32), ("wukv", (128, 512), BF16), ("gkv", (128, 1), F32), ("qng", (128, 96), F32), ("kng", (128, 96), F32), ("cos", (128, 16, 16), F32), ("sin", (128, 16, 16), F32), ("QT", (96, 4, TB), BF16), ("KT", (96, 4, TB), BF16), ("V", (128, 18, 4, 65), BF16), ("pt0", (128, 512), BF16), ("pt1", (128, 512), BF16), ("rec", (128, 4), F32), ("ytm0", (128, 4, 256), BF16), ("ytm1", (128, 4, 256), BF16), ("yts0", (128, 2, 512), BF16), ("yts1", (128, 2, 512), BF16)])
        ps_s = [_psum(es, nc, "pss%d" % i, [128, 512], F32) for i in range(2)]
        ts = []
        for s_ in range(2):
            d_ = dict(t)
            d_.update(_tiles(es, nc, [("junk", (128, 384), F32), ("ssq", (128, 1), F32), ("ssk", (128, 1), F32), ("cqn", (128, 192), BF16), ("ckvn", (128, 128), BF16), ("cqT", (128, 2, 128), BF16), ("ckvT", (128, 1, 128), BF16), ("sq", (128, 4, 96), F32), ("ssh", (128, 4), F32), ("qn", (128, 4, 96), F32), ("kfull", (128, 4, 96), F32), ("qf", (128, 4, 96), BF16), ("r1", (128, 4, 2, 8), F32), ("r2", (128, 4, 2, 8), F32), ("zin", (128, 352), BF16)]))
            d_["pstb"] = _psum(es, nc, "pstb", [128, 8, 128], BF16)
            d_["pskv"] = ps_s[s_]
            d_["pskey"] = "mla_ps_s%d" % s_
            ts.append(d_)
        pstb = ts[0]["pstb"]
        ps_o = [_psum(es, nc, "pso%d" % i, [128, 512], F32) for i in range(4)]
        P.dma(lambda e: e.dma_start(out=t["idf"][:], in_=k.identf), writes=["idf"])
        P.op("dve", lambda e: e.tensor_copy(out=t["idb"][:], in_=t["idf"][:]), reads=["idf"], writes=["idb"])
        P.op("pool", lambda e: e.memset(t["wuqf"][:], 0.0), writes=["wuqf"])
        P.op("pool", lambda e: e.memset(t["gq"][:], 0.0), writes=["gq"])
        P.dma(lambda e: e.dma_start(out=t["wuqf"][:, 0, :], in_=k.mla_w_uq[l][0:128, :]), writes=["wuqf"])
        P.dma(lambda e: e.dma_start(out=t["wuqf"][0:64, 1, :], in_=k.mla_w_uq[l][128:192, :]), writes=["wuqf"])
        P.dma(lambda e: e.dma_start(out=t["gq"][:, 0:1], in_=k.mla_q_norm_g[l][0:128].rearrange("(p o) -> p o", o=1)), writes=["gq"])
        P.dma(lambda e: e.dma_start(out=t["gq"][0:64, 1:2], in_=k.mla_q_norm_g[l][128:192].rearrange("(p o) -> p o", o=1)), writes=["gq"])
        for c in range(2):
            P.op("dve", lambda e, c=c: e.tensor_scalar(out=t["wuq"][:, c, :], in0=t["wuqf"][:, c, :], scalar1=t["gq"][:, c:c + 1], scalar2=None, op0=ALU.mult),
                 reads=["wuqf", "gq"], writes=["wuq"])
        P.dma(lambda e: e.dma_start(out=t["wukvf"][:], in_=k.mla_w_ukv[l]), writes=["wukvf"])
        P.dma(lambda e: e.dma_start(out=t["gkv"][:], in_=k.mla_kv_norm_g[l].rearrange("(p o) -> p o", o=1)), writes=["gkv"])
        P.op("dve", lambda e: e.tensor_scalar(out=t["wukv"][:], in0=t["wukvf"][:], scalar1=t["gkv"][:, 0:1], scalar2=None, op0=ALU.mult), reads=["wukvf", "gkv"], writes=["wukv"])
        P.dma(lambda e: e.dma_start(out=t["qng"][:], in_=k.mla_qn_g[l].partition_broadcast(128)), writes=["qng"])
        P.dma(lambda e: e.dma_start(out=t["kng"][:], in_=k.mla_kn_g[l].partition_broadcast(128)), writes=["kng"])
        P.op("dve", lambda e: e.tensor_scalar(out=t["qng"][:], in0=t["qng"][:], scalar1=96.0 ** -0.5, scalar2=None, op0=ALU.mult), reads=["qng"], writes=["qng"])
        P.dma(lambda e: e.dma_start(out=t["cos"][:], in_=k.rope_cos.rearrange("(n p) f -> p n f", p=128)), writes=["cos"])
        P.dma(lambda e: e.dma_start(out=t["sin"][:], in_=k.rope_sin.rearrange("(n p) f -> p n f", p=128)), writes=["sin"])
        P.op("pool", lambda e: e.memset(t["V"][:], 1.0), writes=["V"])

        def prep_tile(t, b, i):
            P = k.P
            pstb, pskv, pskey = t["pstb"], t["pskv"], t["pskey"]
            psq = pskv
            def head_norm_rope(src_ps_or_sb, srckey, gains, dstT, dstkey, i, col0, is_lat):
                P.op("act", lambda e: e.activation(out=t["sq"][:], in_=src_ps_or_sb, func=AF.Square), reads=[srckey], writes=["sq"])
                P.op("dve", lambda e: e.tensor_reduce(out=t["ssh"][:], in_=t["sq"][:], axis=AX.X, op=ALU.add), reads=["sq"], writes=["ssh"])
                P.op("act", lambda e: e.activation(out=t["ssh"][:], in_=t["ssh"][:], func=AF.Sqrt, scale=1.0 / 96, bias=EPS), reads=["ssh"], writes=["ssh"])
                P.op("dve", lambda e: e.reciprocal(out=t["ssh"][:], in_=t["ssh"][:]), reads=["ssh"], writes=["ssh"])
                P.op("dve", lambda e: e.tensor_tensor(out=t["qn"][:], in0=src_ps_or_sb, in1=t["ssh"][:].unsqueeze(2).to_broadcast([128, 4, 96]), op=ALU.mult),
                     reads=[srckey, "ssh"], writes=["qn"])
                if is_lat:
                    P.op("dve", lambda e: e.tensor_tensor(out=t["qn"][:], in0=t["qn"][:], in1=gains[:].unsqueeze(1).to_broadcast([128, 4, 96]), op=ALU.mult),
                         reads=["qn", "qng", "kng"], writes=["qn"])
                    P.op("act", lambda e: e.copy(out=t["qf"][:, :, 0:64], in_=t["qn"][:, :, 0:64]), reads=["qn"], writes=["qf"])
                    rv = t["qn"][:, :, 64:96].rearrange("p h (a b f) -> p h a b f", a=2, b=2)
                    ov = t["qf"][:, :, 64:96].rearrange("p h (a b f) -> p h a b f", a=2, b=2)
                    x1, x2 = rv[:, :, :, 0, :], rv[:, :, :, 1, :]
                    cs = t["cos"][:, i, :].rearrange("p (a f) -> p a f", a=2).unsqueeze(1).to_broadcast([128, 4, 2, 8])
                    sn = t["sin"][:, i, :].rearrange("p (a f) -> p a f", a=2).unsqueeze(1).to_broadcast([128, 4, 2, 8])
                    P.op("dve", lambda e: e.tensor_tensor(out=t["r1"][:], in0=x1, in1=cs, op=ALU.mult), reads=["qn", "cos"], writes=["r1"])
                    P.op("dve", lambda e: e.tensor_tensor(out=t["r2"][:], in0=x2, in1=sn, op=ALU.mult), reads=["qn", "sin"], writes=["r2"])
                    P.op("dve", lambda e: e.tensor_tensor(out=ov[:, :, :, 0, :], in0=t["r1"][:], in1=t["r2"][:], op=ALU.subtract), reads=["r1", "r2"], writes=["qf"])
                    P.op("dve", lambda e: e.tensor_tensor(out=t["r1"][:], in0=x2, in1=cs, op=ALU.mult), reads=["qn", "cos", "qf"], writes=["r1"])
                    P.op("dve", lambda e: e.tensor_tensor(out=t["r2"][:], in0=x1, in1=sn, op=ALU.mult), reads=["qn", "sin", "qf"], writes=["r2"])
                    P.op("dve", lambda e: e.tensor_tensor(out=ov[:, :, :, 1, :], in0=t["r1"][:], in1=t["r2"][:], op=ALU.add), reads=["r1", "r2"], writes=["qf"])
                else:
                    P.op("dve", lambda e: e.tensor_tensor(out=t["qf"][:], in0=t["qn"][:], in1=gains[:].unsqueeze(1).to_broadcast([128, 4, 96]), op=ALU.mult),
                         reads=["qn", "qng", "kng"], writes=["qf"])
                transpose_to(k, t, pstb, "pstb", [t["qf"][:, h, :] for h in range(4)], dstT[0:96, :, col0:col0 + 128], dstkey, ["qf"], 96)

            zin = t["zin"]
            zk = "zin"
            t0 = b * TB + i * 128
            is_lat = i < 16
            P.dma(lambda e, zin=zin, t0=t0: e.dma_start(out=zin[:], in_=k.zt[t0:t0 + 128, 0:352]), reads=[("ztd", l, b, i)], writes=[zk])
            rms_rstd(k, t, zin[:, 0:192], zk, t["junk"][:, 0:192], "junk", t["ssq"][:], "ssq", 192)
            P.op("dve", lambda e, zin=zin: e.tensor_scalar(out=t["cqn"][:], in0=zin[:, 0:192], scalar1=t["ssq"][:, 0:1], scalar2=None, op0=ALU.mult), reads=[zk, "ssq"], writes=["cqn"])
            P.op("pe", lambda e: e.transpose(out=pstb[:, 0, :], in_=t["cqn"][:, 0:128], identity=t["idb"][:]), reads=["cqn", "idb"], writes=["pstb"])
            P.op("pe", lambda e: e.transpose(out=pstb[0:64, 1, :], in_=t["cqn"][:, 128:192], identity=t["idb"][:]), reads=["cqn", "idb"], writes=["pstb"])
            P.op("act", lambda e: e.copy(out=t["cqT"][:, 0, :], in_=pstb[:, 0, :]), reads=["pstb"], writes=["cqT"])
            P.op("act", lambda e: e.copy(out=t["cqT"][0:64, 1, :], in_=pstb[0:64, 1, :]), reads=["pstb"], writes=["cqT"])
            P.op("pe", lambda e: e.matmul(psq[:, 0:384], lhsT=t["cqT"][:, 0, :], rhs=t["wuq"][:, 0, :], start=True, stop=False), reads=["cqT", "wuq"], writes=[pskey])
            P.op("pe", lambda e: e.matmul(psq[:, 0:384], lhsT=t["cqT"][0:64, 1, :], rhs=t["wuq"][0:64, 1, :], start=False, stop=True), reads=["cqT", "wuq"], writes=[pskey], skip_self=True)
            head_norm_rope(psq[:, 0:384].rearrange("p (h d) -> p h d", h=4), pskey, t["qng"], t["QT"], "QT", i, i * 128, is_lat)
            rms_rstd(k, t, zin[:, 192:320], zk, t["junk"][:, 0:128], "junk", t["ssk"][:], "ssk", 128)
            P.op("dve", lambda e, zin=zin: e.tensor_scalar(out=t["ckvn"][:], in0=zin[:, 192:320], scalar1=t["ssk"][:, 0:1], scalar2=None, op0=ALU.mult), reads=[zk, "ssk"], writes=["ckvn"])
            transpose_to(k, t, pstb, "pstb", [t["ckvn"][:, :]], t["ckvT"][:, :, :], "ckvT", ["ckvn"], 128)
            P.op("pe", lambda e: e.matmul(pskv[:, :], lhsT=t["ckvT"][:, 0, :], rhs=t["wukv"][:], start=True, stop=True), reads=["ckvT", "wukv"], writes=[pskey])
            kvv = pskv[:, :].rearrange("p (h d) -> p h d", h=4)
            P.op("act", lambda e, kvv=kvv: e.copy(out=t["kfull"][:, :, 0:64], in_=kvv[:, :, 0:64]), reads=[pskey], writes=["kfull"])
            P.op("dve", lambda e, zin=zin: e.tensor_copy(out=t["kfull"][:, :, 64:96], in_=zin[:, 320:352].unsqueeze(1).to_broadcast([128, 4, 32])), reads=[zk], writes=["kfull"])
            P.op("act", lambda e, kvv=kvv, i=i: e.copy(out=t["V"][:, i, :, 0:64], in_=kvv[:, :, 64:128]), reads=[pskey], writes=["V"])
            head_norm_rope(t["kfull"][:], "kfull", t["kng"], t["KT"], "KT", i, i * 128, is_lat)

        LOCALK = set(['zin', 'junk', 'ssq', 'ssk', 'cqn', 'ckvn', 'cqT', 'ckvT', 'sq', 'ssh', 'qn', 'kfull', 'qf', 'r1', 'r2']) | {"pstb"}
        for b in range(NB):
            for i0 in range(0, 18, 2):
                recs = []
                for s_ in range(2):
                    rec = Rec("_%d" % s_, LOCALK)
                    k.P = rec
                    prep_tile(ts[s_], b, i0 + s_)
                    recs.append(rec)
                k.P = P
                replay(P, recs)
            qblocks = [(0, 512, list(range(18))), (512, 512, list(range(18))), (1024, 512, list(range(18))), (1536, 512, list(range(18)))]
            if l < DEPTH - 1:
                qblocks.append((2048, 256, [16, 17]))
            for (q0, W, kts) in qblocks:
                nt = W // 128
                ytm = t["ytm%d" % (k.yc % 2)]
                ykey = "ytm%d" % (k.yc % 2)
                for h in range(4):
                    okey = "mla_pso"

                    def tf(kt, pss, psk, ptile, ptk, W=W):
                        P.op("act", lambda e: e.activation(out=ptile[:, 0:W], in_=pss[:, 0:W], func=AF.Exp), reads=[psk], writes=[ptk])
                    attn_core(k, "mla_", t["QT"][0:96, h, :], t["KT"][0:96, h, :], lambda kt, h=h: t["V"][:, kt, h, :], q0, W, kts, tf,
                              ps_s, ps_o, [t["pt0"], t["pt1"]], 65, ["QT", "KT", "V"], okey)
                    for sidx in range(nt):
                        P.op("dve", lambda e, sidx=sidx: e.reciprocal(out=t["rec"][:, sidx:sidx + 1], in_=ps_o[sidx][:, 64:65]), reads=[okey + str(sidx)], writes=["rec"])
                        P.op("dve", lambda e, sidx=sidx, h=h, ytm=ytm: e.tensor_scalar(out=ytm[:, sidx, h * 64:(h + 1) * 64], in0=ps_o[sidx][:, 0:64],
                                                                                     scalar1=t["rec"][:, sidx:sidx + 1], scalar2=None, op0=ALU.mult),
                             reads=[okey + str(sidx), "rec"], writes=[ykey])
                store_yT(k, t, ytm, ykey, nt, 0, b * TB + q0, pstb, "pstb_0", l)
        P.barrier()
        P.emit()


def stage_ret(k, l):
    nc, P, NB = k.nc, k.P, k.NB
    with ExitStack() as es:
        t = _tiles(es, nc, [("idf", (128, 128), F32), ("idb", (128, 128), BF16),
                            ("rel0", (128, 128), F32), ("mge", (128, 128), F32), ("mle", (128, 128), F32), ("dvals", (128, 18), F32),
                            ("lg", (128, 8), F32), ("nlg", (128, 8), F32), ("biasF", (128, 4, 18), F32), ("biasB", (128, 4, 18), F32),
                            ("e1", (128, 128), F32), ("e2", (128, 128), F32),
                            ("arr", (128, 4, 35, 128), BF16), ("Cx", (128, 4, 2, 16, 128), BF16),
                            ("QTr", (128, 2, TB), BF16), ("KTr", (128, 2, TB), BF16), ("Vr", (128, 18, 256), BF16),
                            ("pt0", (128, 512), BF16), ("pt1", (128, 512), BF16),
                            ("yo", (128, 4, 256), F32), ("sq", (128, 4, 256), F32), ("ssh", (128, 16), F32),
                            ("rng", (128, 256), F32), ("zg0", (128, 4, 256), BF16), ("zg1", (128, 4, 256), BF16), ("gs", (128, 4, 256), F32),
                            ("ytm0", (128, 4, 256), BF16), ("ytm1", (128, 4, 256), BF16),
                            ("yts0", (128, 2, 512), BF16), ("yts1", (128, 2, 512), BF16)])
        pstb = _psum(es, nc, "pstb", [128, 8, 128], BF16)
        ps_s = [_psum(es, nc, "pss%d" % i, [128, 512], F32) for i in range(2)]
        ps_o = [_psum(es, nc, "pso%d" % i, [128, 512], F32) for i in range(4)]
        P.dma(lambda e: e.dma_start(out=t["idf"][:], in_=k.identf), writes=["idf"])
        P.op("dve", lambda e: e.tensor_copy(out=t["idb"][:], in_=t["idf"][:]), reads=["idf"], writes=["idb"])
        for nm, src in (("rel0", k.c_rel0), ("mge", k.c_mge), ("mle", k.c_mle), ("dvals", k.c_dvals)):
            P.dma(lambda e, nm=nm, src=src: e.dma_start(out=t[nm][:], in_=src), writes=[nm])
        P.dma(lambda e: e.dma_start(out=t["lg"][:], in_=k.ret_log_gamma[l].rearrange("a h -> (a h)").partition_broadcast(128)), writes=["lg"])
        P.dma(lambda e: e.dma_start(out=t["rng"][:], in_=k.ret_norm_g[l].partition_broadcast(128)), writes=["rng"])
        P.op("dve", lambda e: e.tensor_scalar(out=t["nlg"][:], in0=t["lg"][:], scalar1=-1.0, scalar2=None, op0=ALU.mult), reads=["lg"], writes=["nlg"])
        for h in range(4):
            P.op("dve", lambda e, h=h: e.tensor_scalar(out=t["biasF"][:, h, :], in0=t["dvals"][:], scalar1=t["lg"][:, h:h + 1], scalar2=None, op0=ALU.mult),
                 reads=["dvals", "lg"], writes=["biasF"])
            P.op("dve", lambda e, h=h: e.tensor_scalar(out=t["biasB"][:, h, :], in0=t["dvals"][:], scalar1=t["lg"][:, 4 + h:5 + h], scalar2=None, op0=ALU.mult),
                 reads=["dvals", "lg"], writes=["biasB"])
        for h in range(4):
            for d in range(1, 18):
                P.op("act", lambda e, h=h, d=d: e.activation(out=t["arr"][:, h, 17 + d, :], in_=t["rel0"][:], func=AF.Exp, scale=t["lg"][:, h:h + 1], bias=t["biasF"][:, h, d:d + 1]),
                     reads=["rel0", "lg", "biasF"], writes=["arr"])
                P.op("act", lambda e, h=h, d=d: e.activation(out=t["arr"][:, h, 17 - d, :], in_=t["rel0"][:], func=AF.Exp, scale=t["nlg"][:, 4 + h:5 + h], bias=t["biasB"][:, h, d:d + 1]),
                     reads=["rel0", "nlg", "biasB"], writes=["arr"])
            P.op("act", lambda e, h=h: e.activation(out=t["e1"][:], in_=t["rel0"][:], func=AF.Exp, scale=t["lg"][:, h:h + 1]), reads=["rel0", "lg"], writes=["e1"])
            P.op("act", lambda e, h=h: e.activation(out=t["e2"][:], in_=t["rel0"][:], func=AF.Exp, scale=t["nlg"][:, 4 + h:5 + h]), reads=["rel0", "nlg"], writes=["e2"])
            P.op("dve", lambda e: e.tensor_tensor(out=t["e1"][:], in0=t["e1"][:], in1=t["mge"][:], op=ALU.mult), reads=["e1", "mge"], writes=["e1"])
            P.op("dve", lambda e: e.tensor_tensor(out=t["e2"][:], in0=t["e2"][:], in1=t["mle"][:], op=ALU.mult), reads=["e2", "mle"], writes=["e2"])
            P.op("dve", lambda e, h=h: e.tensor_tensor(out=t["arr"][:, h, 17, :], in0=t["e1"][:], in1=t["e2"][:], op=ALU.add), reads=["e1", "e2"], writes=["arr"])
            for mi in range(2):
                for qi in range(16):
                    P.op("pool", lambda e, h=h, mi=mi, qi=qi: e.tensor_tensor(out=t["Cx"][:, h, mi, qi, :], in0=t["arr"][:, h, 17 + qi - mi + 2, :],
                                                                              in1=t["arr"][:, h, 17 + qi - 16 - mi, :], op=ALU.add), reads=["arr"], writes=["Cx"])
        zt_v = k.zt.rearrange("(n p) c -> p n c", p=128)
        for b in range(NB):
            for c in range(2):
                P.dma(lambda e, c=c, b=b: e.dma_start(out=t["QTr"][:, c, :], in_=k.zf[ZF_RQ + c * 128:ZF_RQ + (c + 1) * 128, b * TB:(b + 1) * TB]),
                      reads=[("zfd", l, b, g0) for g0 in (0, 512, 1024, 1536, 2048)], writes=["QTr"])
                P.dma(lambda e, c=c, b=b: e.dma_start(out=t["KTr"][:, c, :], in_=k.zf[ZF_RK + c * 128:ZF_RK + (c + 1) * 128, b * TB:(b + 1) * TB]),
                      reads=[("zfd", l, b, g0) for g0 in (0, 512, 1024, 1536, 2048)], writes=["KTr"])
            P.dma(lambda e, b=b: e.dma_start(out=t["Vr"][:], in_=zt_v[:, b * 18:(b + 1) * 18, 608:864]), reads=[("ztd", l, b, i) for i in range(18)], writes=["Vr"])
            qblocks = [(0, 512, list(range(18))), (512, 512, list(range(18))), (1024, 512, list(range(18))), (1536, 512, list(range(18)))]
            if l < DEPTH - 1:
                qblocks.append((2048, 256, [16, 17]))
            for (q0, W, kts) in qblocks:
                nt = W // 128
                qi0 = q0 // 128
                zg = t["zg%d" % (k.yc % 2)]
                zgk = "zg%d" % (k.yc % 2)
                ytm = t["ytm%d" % (k.yc % 2)]
                ykey = "ytm%d" % (k.yc % 2)
                P.dma(lambda e, zg=zg, b=b, qi0=qi0, nt=nt: e.dma_start(out=zg[:, 0:nt, :], in_=zt_v[:, b * 18 + qi0:b * 18 + qi0 + nt, 864:1120]),
                      reads=[("ztd", l, b, i) for i in range(qi0, qi0 + nt)], writes=[zgk])
                for h in range(4):
                    okey = "ret_pso"
                    c, p0 = h // 2, 64 * (h % 2)

                    def tf(kt, pss, psk, ptile, ptk, W=W, h=h, qi0=qi0, nt=nt):
                        if kt < 16 and qi0 < 16:
                            mv = t["arr"][:, h, qi0 - kt + 17:qi0 - kt + 17 + nt, :]
                        elif kt >= 16 and qi0 < 16:
                            mv = t["Cx"][:, h, kt - 16, qi0:qi0 + nt, :]
                        else:
                            mv = t["arr"][:, h, qi0 - kt + 17:qi0 - kt + 17 + nt, :]
                        P.op("dve", lambda e: e.tensor_tensor(out=ptile[:, 0:W].rearrange("p (n c) -> p n c", c=128), in0=pss[:, 0:W].rearrange("p (n c) -> p n c", c=128),
                                                              in1=mv, op=ALU.mult), reads=[psk, "arr", "Cx"], writes=[ptk])
                    attn_core(k, "ret_", t["QTr"][p0:p0 + 64, c, :], t["KTr"][p0:p0 + 64, c, :], lambda kt, h=h: t["Vr"][:, kt, h * 64:(h + 1) * 64], q0, W, kts, tf,
                              ps_s, ps_o, [t["pt0"], t["pt1"]], 64, ["QTr", "KTr", "Vr"], okey)
                    for sidx in range(nt):
                        P.op("act", lambda e, sidx=sidx, h=h: e.copy(out=t["yo"][:, sidx, h * 64:(h + 1) * 64], in_=ps_o[sidx][:, 0:64]), reads=[okey + str(sidx)], writes=["yo"])
                P.op("act", lambda e, nt=nt: e.activation(out=t["sq"][:, 0:nt, :], in_=t["yo"][:, 0:nt, :], func=AF.Square), reads=["yo"], writes=["sq"])
                P.op("dve", lambda e, nt=nt: e.tensor_reduce(out=t["ssh"][:, 0:nt * 4], in_=t["sq"][:, 0:nt, :].rearrange("p n (h d) -> p (n h) d", h=4), axis=AX.X, op=ALU.add),
                     reads=["sq"], writes=["ssh"])
                P.op("act", lambda e, nt=nt: e.activation(out=t["ssh"][:, 0:nt * 4], in_=t["ssh"][:, 0:nt * 4], func=AF.Sqrt, scale=1.0 / 64, bias=EPS), reads=["ssh"], writes=["ssh"])
                P.op("dve", lambda e, nt=nt: e.reciprocal(out=t["ssh"][:, 0:nt * 4], in_=t["ssh"][:, 0:nt * 4]), reads=["ssh"], writes=["ssh"])
                P.op("dve", lambda e, nt=nt: e.tensor_tensor(out=t["yo"][:, 0:nt, :].rearrange("p n (h d) -> p (n h) d", h=4), in0=t["yo"][:, 0:nt, :].rearrange("p n (h d) -> p (n h) d", h=4),
                                                             in1=t["ssh"][:, 0:nt * 4].unsqueeze(2).to_broadcast([128, nt * 4, 64]), op=ALU.mult), reads=["yo", "ssh"], writes=["yo"])
                P.op("dve", lambda e, nt=nt: e.tensor_tensor(out=t["yo"][:, 0:nt, :], in0=t["yo"][:, 0:nt, :], in1=t["rng"][:].unsqueeze(1).to_broadcast([128, nt, 256]), op=ALU.mult),
                     reads=["yo", "rng"], writes=["yo"])
                P.op("act", lambda e, nt=nt, zg=zg: e.activation(out=t["gs"][:, 0:nt, :], in_=zg[:, 0:nt, :], func=AF.Silu), reads=[zgk], writes=["gs"])
                P.op("dve", lambda e, nt=nt, ytm=ytm: e.tensor_tensor(out=ytm[:, 0:nt, :], in0=t["yo"][:, 0:nt, :], in1=t["gs"][:, 0:nt, :], op=ALU.mult), reads=["yo", "gs"], writes=[ykey])
                store_yT(k, t, ytm, ykey, nt, 256, b * TB + q0, pstb, "pstb", l)
        P.barrier()
        P.emit()


BLK5 = ((0, 512), (512, 512), (1024, 512), (1536, 512), (2048, 256))


def stage_lru(k, l):
    nc, P, NB = k.nc, k.P, k.NB
    with ExitStack() as es:
        t = _tiles(es, nc, [("lx", (128, TB), BF16), ("lgz", (128, TB), BF16), ("xc", (128, TB), F32), ("xcb", (128, TB), BF16),
                            ("wconv", (128, 2, 4), F32), ("bconv", (128, 2), F32), ("wst", (128, 128), F32),
                            ("Wbd", (128, 8, 128), BF16), ("bgate", (128, 8), F32), ("lam", (128, 4), F32), ("coef", (128, 4), F32), ("coef2", (128, 4), F32),
                            ("rg", (128, 512), F32), ("ig", (128, 512), F32), ("a2", (128, 512), F32),
                            ("af", (128, TB), F32), ("bf", (128, TB), F32), ("hf", (128, TB), F32), ("hb", (128, TB), F32),
                            ("g1", (128, TB), F32), ("g2", (128, TB), F32), ("yo", (128, TB), BF16)])
        psg = [_psum(es, nc, "psg%d" % i, [128, 512], F32) for i in range(2)]
        for c in range(2):
            for j in range(4):
                P.dma(lambda e, c=c, j=j: e.dma_start(out=t["wconv"][:, c, j:j + 1], in_=k.lru_conv_w[l][j, c * 128:(c + 1) * 128].rearrange("(p o) -> p o", o=1)), writes=["wconv"])
        P.dma(lambda e: e.dma_start(out=t["bconv"][:], in_=k.lru_conv_b[l].rearrange("(c p) -> p c", p=128)), writes=["bconv"])
        for d in range(2):
            P.dma(lambda e, d=d: e.dma_start(out=t["lam"][:, d * 2:d * 2 + 2], in_=k.lru_lambda[l][d].rearrange("(c p) -> p c", p=128)), writes=["lam"])
        for d in range(2):
            for gi, (wsrc, bsrc) in enumerate(((k.lru_wa, k.lru_ba), (k.lru_wx, k.lru_bx))):
                P.dma(lambda e, d=d, gi=gi, bsrc=bsrc: e.dma_start(out=t["bgate"][:, (d * 2 + gi) * 2:(d * 2 + gi) * 2 + 2], in_=bsrc[l][d].rearrange("(c p) -> p c", p=128)), writes=["bgate"])
                for c in range(2):
                    P.op("pool", lambda e: e.memset(t["wst"][:], 0.0), writes=["wst"])
                    for j in range(2):
                        P.dma(lambda e, d=d, c=c, j=j, wsrc=wsrc: e.dma_start(out=t["wst"][64 * j:64 * j + 64, 64 * j:64 * j + 64], in_=wsrc[l][d][2 * c + j]), writes=["wst"])
                    P.op("dve", lambda e, d=d, gi=gi, c=c: e.tensor_copy(out=t["Wbd"][:, (d * 2 + gi) * 2 + c, :], in_=t["wst"][:]), reads=["wst"], writes=["Wbd"])
        P.op("act", lambda e: e.activation(out=t["coef"][:], in_=t["lam"][:], func=AF.Exp, scale=-1.0), reads=["lam"], writes=["coef"])
        P.op("act", lambda e: e.activation(out=t["coef"][:], in_=t["coef"][:], func=AF.Ln, bias=1.0), reads=["coef"], writes=["coef"])
        P.op("dve", lambda e: e.tensor_scalar(out=t["coef2"][:], in0=t["coef"][:], scalar1=-16.0, scalar2=None, op0=ALU.mult), reads=["coef"], writes=["coef2"])
        P.op("dve", lambda e: e.tensor_scalar(out=t["coef"][:], in0=t["coef"][:], scalar1=-8.0, scalar2=None, op0=ALU.mult), reads=["coef"], writes=["coef"])
        for b in range(NB):
            for c in range(2):
                rds = [("zfd", l, b, g0) for g0 in (0, 512, 1024, 1536, 2048)]
                P.dma(lambda e, b=b, c=c: e.dma_start(out=t["lx"][:], in_=k.zf[ZF_LX + c * 128:ZF_LX + (c + 1) * 128, b * TB:(b + 1) * TB]), reads=rds, writes=["lx"])
                P.dma(lambda e, b=b, c=c: e.dma_start(out=t["lgz"][:], in_=k.zf[ZF_LG + c * 128:ZF_LG + (c + 1) * 128, b * TB:(b + 1) * TB]), reads=rds, writes=["lgz"])
                for (r0, r1) in ((0, SEQ), (SEQ, TB)):
                    P.op("dve", lambda e, r0=r0, r1=r1, c=c: e.tensor_scalar(out=t["xc"][:, r0:r1], in0=t["lx"][:, r0:r1], scalar1=t["wconv"][:, c, 2:3], scalar2=t["bconv"][:, c:c + 1],
                                                                          op0=ALU.mult, op1=ALU.add), reads=["lx", "wconv", "bconv"], writes=["xc"])
                    for j, o in ((0, -2), (1, -1), (3, 1)):
                        a0, a1 = max(r0, r0 - o), min(r1, r1 - o)
                        P.op("dve", lambda e, a0=a0, a1=a1, o=o, j=j, c=c: e.scalar_tensor_tensor(out=t["xc"][:, a0:a1], in0=t["lx"][:, a0 + o:a1 + o], scalar=t["wconv"][:, c, j:j + 1],
                                                                                              in1=t["xc"][:, a0:a1], op0=ALU.mult, op1=ALU.add), reads=["lx", "wconv", "xc"], writes=["xc"])
                P.op("act", lambda e: e.copy(out=t["xcb"][:], in_=t["xc"][:]), reads=["xc"], writes=["xcb"])
                for d in range(2):
                    hd = t["hf"] if d == 0 else t["hb"]
                    hk = "hf" if d == 0 else "hb"
                    for (g0, W) in BLK5:
                        for gi, dst in ((0, "rg"), (1, "ig")):
                            ps = psg[gi]
                            P.op("pe", lambda e, ps=ps, d=d, gi=gi, c=c, g0=g0, W=W: e.matmul(ps[:, 0:W], lhsT=t["Wbd"][:, (d * 2 + gi) * 2 + c, :], rhs=t["xcb"][:, g0:g0 + W], start=True, stop=True),
                                 reads=["Wbd", "xcb"], writes=["psg%d" % gi])
                            P.op("act", lambda e, ps=ps, d=d, gi=gi, c=c, W=W, dst=dst: e.activation(out=t[dst][:, 0:W], in_=ps[:, 0:W], func=AF.Sigmoid,
                                                                                                   bias=t["bgate"][:, (d * 2 + gi) * 2 + c:(d * 2 + gi) * 2 + c + 1]),
                                 reads=["psg%d" % gi, "bgate"], writes=[dst])
                        ci = d * 2 + c
                        P.op("act", lambda e, g0=g0, W=W, ci=ci: e.activation(out=t["af"][:, g0:g0 + W], in_=t["rg"][:, 0:W], func=AF.Exp, scale=t["coef"][:, ci:ci + 1]), reads=["rg", "coef"], writes=["af"])
                        P.op("act", lambda e, W=W, ci=ci: e.activation(out=t["a2"][:, 0:W], in_=t["rg"][:, 0:W], func=AF.Exp, scale=t["coef2"][:, ci:ci + 1]), reads=["rg", "coef2"], writes=["a2"])
                        P.op("act", lambda e, W=W: e.activation(out=t["a2"][:, 0:W], in_=t["a2"][:, 0:W], func=AF.Sqrt, scale=-1.0, bias=1.0), reads=["a2"], writes=["a2"])
                        P.op("dve", lambda e, g0=g0, W=W: e.tensor_tensor(out=t["ig"][:, 0:W], in0=t["ig"][:, 0:W], in1=t["xc"][:, g0:g0 + W], op=ALU.mult), reads=["ig", "xc"], writes=["ig"])
                        P.op("dve", lambda e, g0=g0, W=W: e.tensor_tensor(out=t["bf"][:, g0:g0 + W], in0=t["ig"][:, 0:W], in1=t["a2"][:, 0:W], op=ALU.mult), reads=["ig", "a2"], writes=["bf"])
                    if d == 0:
                        P.op("dve", lambda e, hd=hd: e.tensor_tensor_scan(out=hd[:, SEQ:TB], data0=t["af"][:, SEQ:TB], data1=t["bf"][:, SEQ:TB], initial=0.0, op0=ALU.mult, op1=ALU.add),
                             reads=["af", "bf"], writes=[hk])
                        P.op("dve", lambda e, hd=hd: e.tensor_tensor_scan(out=hd[:, 0:SEQ], data0=t["af"][:, 0:SEQ], data1=t["bf"][:, 0:SEQ], initial=hd[:, TB - 1:TB], op0=ALU.mult, op1=ALU.add),
                             reads=["af", "bf", hk], writes=[hk])
                    else:
                        P.op("dve", lambda e, hd=hd: e.tensor_tensor_scan(out=hd[:, SEQ:TB][:, ::-1], data0=t["af"][:, SEQ:TB][:, ::-1], data1=t["bf"][:, SEQ:TB][:, ::-1], initial=0.0,
                                                                         op0=ALU.mult, op1=ALU.add), reads=["af", "bf"], writes=[hk])
                        P.op("dve", lambda e, hd=hd: e.tensor_tensor_scan(out=hd[:, 0:SEQ][:, ::-1], data0=t["af"][:, 0:SEQ][:, ::-1], data1=t["bf"][:, 0:SEQ][:, ::-1], initial=hd[:, SEQ:SEQ + 1],
                                                                         op0=ALU.mult, op1=ALU.add), reads=["af", "bf", hk], writes=[hk])
                P.op("pool", lambda e: e.tensor_tensor(out=t["g1"][:], in0=t["lgz"][:], in1=t["lgz"][:], op=ALU.mult), reads=["lgz"], writes=["g1"])
                P.op("pool", lambda e: e.tensor_scalar(out=t["g1"][:], in0=t["g1"][:], scalar1=0.044715, scalar2=1.0, op0=ALU.mult, op1=ALU.add), reads=["g1"], writes=["g1"])
                P.op("pool", lambda e: e.tensor_tensor(out=t["g1"][:], in0=t["g1"][:], in1=t["lgz"][:], op=ALU.mult), reads=["g1", "lgz"], writes=["g1"])
                P.op("act", lambda e: e.activation(out=t["g2"][:], in_=t["g1"][:], func=AF.Sigmoid, scale=1.5957691216057308), reads=["g1"], writes=["g2"])
                P.op("pool", lambda e: e.tensor_tensor(out=t["g2"][:], in0=t["g2"][:], in1=t["lgz"][:], op=ALU.mult), reads=["g2", "lgz"], writes=["g2"])
                P.op("dve", lambda e: e.tensor_tensor(out=t["hf"][:], in0=t["hf"][:], in1=t["hb"][:], op=ALU.add), reads=["hf", "hb"], writes=["hf"])
                P.op("dve", lambda e: e.tensor_tensor(out=t["yo"][:], in0=t["hf"][:], in1=t["g2"][:], op=ALU.mult), reads=["hf", "g2"], writes=["yo"])
                P.dma(lambda e, b=b, c=c: e.dma_start(out=k.yT[768 + c * 128:768 + (c + 1) * 128, b * TB:(b + 1) * TB], in_=t["yo"][:]), reads=["yo"],
                      writes=[("yTd", l, 768, c, b)], dkey="yo_store")
        P.barrier()
        P.emit()


TWO_PI = 2.0 * math.pi


def stage_hyena(k, l, L, off):
    nc, P, NB = k.nc, k.P, k.NB
    hc = k.hc[L]
    nT = L // 128
    nS = 2 * nT
    W = min(512, L)
    BG = 2 if NB % 2 == 0 else 1
    with ExitStack() as es:
        t = _tiles(es, nc, [("idf", (128, 128), F32), ("idb", (128, 128), BF16),
                            ("HA", (128, nT, 256), BF16), ("HBm", (128, nT, 256), BF16), ("nHB", (128, nT, 256), BF16), ("P40", (128, 256), BF16),
                            ("m0", (128, 2), F32), ("wc", (128, 6, 3), F32), ("bc", (128, 6), F32), ("dcol", (128, 2), F32)])
        P.dma(lambda e: e.dma_start(out=t["idf"][:], in_=k.identf), writes=["idf"])
        P.op("dve", lambda e: e.tensor_copy(out=t["idb"][:], in_=t["idf"][:]), reads=["idf"], writes=["idb"])
        P.dma(lambda e: e.dma_start(out=t["m0"][:], in_=k.c_m0), writes=["m0"])
        for c in range(6):
            for j in range(3):
                P.dma(lambda e, c=c, j=j: e.dma_start(out=t["wc"][:, c, j:j + 1], in_=k.hy_conv_w[l][j, c * 128:(c + 1) * 128].rearrange("(p o) -> p o", o=1)), writes=["wc"])
        P.dma(lambda e: e.dma_start(out=t["bc"][:], in_=k.hy_conv_b[l].rearrange("(c p) -> p c", p=128)), writes=["bc"])
        P.dma(lambda e: e.dma_start(out=t["dcol"][:], in_=k.hy_d[l].rearrange("(c p) -> p c", p=128)), writes=["dcol"])
        with ExitStack() as e2:
            f = _tiles(e2, nc, [("w1", (33, 64), F32), ("w2", (64, 64), F32), ("w3", (64, 512), F32), ("fq", (64, 1), F32), ("b1", (64, 1), F32), ("b2", (64, 1), F32),
                                ("ze", (33, 2, L), F32), ("arg", (64, 512), F32), ("ni", (64, 512), mybir.dt.int32), ("nf", (64, 512), F32), ("npi", (64, 1), F32), ("h1", (64, 512), F32), ("h2", (64, 2, L), F32),
                                ("dl", (128, 256), F32), ("tneg", (128, 2, nT), F32), ("win", (128, 256), F32), ("g", (128, 2, nT, 256), BF16),
                                ("csl", (128, nS), F32), ("csh", (128, nS), F32), ("wf0", (128, nT, 128), BF16), ("wf1", (128, nT, 128), BF16),
                                ("Ht", (128, 256), F32), ("H", (128, nS, 256), F32)])
            pf = [_psum(e2, nc, "hpf%d" % i, [128, 512], F32) for i in range(4)]
            for nm, src in (("w1", k.hy_w1[l]), ("w2", k.hy_w2[l]), ("w3", k.hy_w3[l]), ("dl", hc["dl"]), ("csl", hc["csl"]), ("csh", hc["csh"])):
                P.dma(lambda e, nm=nm, src=src: e.dma_start(out=f[nm][:], in_=src), writes=[nm])
            for nm, src in (("fq", k.hy_freq[l]), ("b1", k.hy_b1[l]), ("b2", k.hy_b2[l])):
                P.dma(lambda e, nm=nm, src=src: e.dma_start(out=f[nm][:], in_=src.rearrange("(p o) -> p o", o=1)), writes=[nm])
            for gi, nm in enumerate(("zf", "zr")):
                P.dma(lambda e, gi=gi, nm=nm: e.dma_start(out=f["ze"][:, gi, :], in_=hc[nm]), writes=["ze"])
            for gi, nm in enumerate(("tf", "tr")):
                P.dma(lambda e, gi=gi, nm=nm: e.dma_start(out=f["tneg"][:, gi, :], in_=hc[nm]), writes=["tneg"])
            P.op("pool", lambda e: e.memset(f["npi"][:], -math.pi), writes=["npi"])
            P.op("dve", lambda e: e.tensor_tensor(out=f["b1"][:], in0=f["b1"][:], in1=f["fq"][:], op=ALU.mult), reads=["b1", "fq"], writes=["b1"])
            P.op("dve", lambda e: e.tensor_tensor(out=f["b2"][:], in0=f["b2"][:], in1=f["fq"][:], op=ALU.mult), reads=["b2", "fq"], writes=["b2"])

            def sin_layer(ps, pk, bias, dst_ap, dkey):
                P.op("dve", lambda e: e.tensor_scalar(out=f["arg"][:, 0:W], in0=ps[0:64, 0:W], scalar1=f["fq"][:, 0:1], scalar2=bias[:, 0:1], op0=ALU.mult, op1=ALU.add),
                     reads=[pk, "fq", "b1", "b2"], writes=["arg"])
                P.op("dve", lambda e: e.tensor_scalar(out=f["arg"][:, 0:W], in0=f["arg"][:, 0:W], scalar1=1.0 / TWO_PI, scalar2=4.5, op0=ALU.mult, op1=ALU.add), reads=["arg"], writes=["arg"])
                P.op("dve", lambda e: e.tensor_copy(out=f["ni"][:, 0:W], in_=f["arg"][:, 0:W]), reads=["arg"], writes=["ni"])
                P.op("dve", lambda e: e.tensor_copy(out=f["nf"][:, 0:W], in_=f["ni"][:, 0:W]), reads=["ni"], writes=["nf"])
                P.op("dve", lambda e: e.tensor_tensor(out=f["arg"][:, 0:W], in0=f["arg"][:, 0:W], in1=f["nf"][:, 0:W], op=ALU.subtract), reads=["arg", "nf"], writes=["arg"])
                P.op("dve", lambda e: e.tensor_scalar(out=f["nf"][:, 0:W], in0=f["arg"][:, 0:W], scalar1=0.0, scalar2=None, op0=ALU.is_lt), reads=["arg", "nf"], writes=["nf"])
                P.op("dve", lambda e: e.tensor_tensor(out=f["arg"][:, 0:W], in0=f["arg"][:, 0:W], in1=f["nf"][:, 0:W], op=ALU.add), reads=["arg", "nf"], writes=["arg"])
                P.op("act", lambda e: e.activation(out=dst_ap, in_=f["arg"][:, 0:W], func=AF.Sin, scale=TWO_PI, bias=f["npi"][:, 0:1]), reads=["arg", "npi"], writes=[dkey])

            for gi in range(2):
                for cb in range(L // W):
                    P.op("pe", lambda e, gi=gi, cb=cb: e.matmul(pf[0][0:64, 0:W], lhsT=f["w1"][:], rhs=f["ze"][:, gi, cb * W:(cb + 1) * W], start=True, stop=True), reads=["w1", "ze"], writes=["hpf0"])
                    sin_layer(pf[0], "hpf0", f["b1"], f["h1"][:, 0:W], "h1")
                    P.op("pe", lambda e: e.matmul(pf[1][0:64, 0:W], lhsT=f["w2"][:], rhs=f["h1"][:, 0:W], start=True, stop=True), reads=["w2", "h1"], writes=["hpf1"])
                    sin_layer(pf[1], "hpf1", f["b2"], f["h2"][:, gi, cb * W:(cb + 1) * W], "h2")
                for tt in range(nT):
                    P.op("pe", lambda e, gi=gi, tt=tt: e.matmul(pf[2][:, :], lhsT=f["h2"][:, gi, tt * 128:(tt + 1) * 128], rhs=f["w3"][:], start=True, stop=True), reads=["h2", "w3"], writes=["hpf2"])
                    P.op("act", lambda e, gi=gi, tt=tt: e.activation(out=f["win"][:], in_=f["dl"][:], func=AF.Exp, scale=f["tneg"][:, gi, tt:tt + 1]), reads=["dl", "tneg"], writes=["win"])
                    P.op("dve", lambda e, gi=gi, tt=tt: e.tensor_tensor(out=f["g"][:, gi, tt, :], in0=pf[2][:, gi * 256:(gi + 1) * 256], in1=f["win"][:], op=ALU.mult), reads=["hpf2", "win"], writes=["g"])
            P.op("dve", lambda e: e.memset(f["g"][0:1, 1, 0, :], 0.0), reads=["g"], writes=["g"])
            for j in range(nS):
                wf = f["wf%d" % (j % 2)]
                wk = "wf%d" % (j % 2)
                P.dma(lambda e, j=j, wf=wf: e.dma_start(out=wf[:], in_=hc["Wf"].rearrange("(t p) s -> p t s", p=128)[:, :, j * 128:(j + 1) * 128]), writes=[wk])
                for gi in range(2):
                    for tt in range(nT):
                        P.op("pe", lambda e, gi=gi, tt=tt, wf=wf: e.matmul(pf[gi][:, 0:256], lhsT=wf[:, tt, :], rhs=f["g"][:, gi, tt, :], start=(tt == 0), stop=(tt == nT - 1)),
                             reads=[wk, "g"], writes=["hpf%d" % gi], skip_self=(tt > 0))
                P.op("dve", lambda e, j=j: e.tensor_scalar(out=f["Ht"][:], in0=pf[0][:, 0:256], scalar1=f["csl"][:, j:j + 1], scalar2=None, op0=ALU.mult), reads=["hpf0", "csl"], writes=["Ht"])
                P.op("dve", lambda e, j=j: e.scalar_tensor_tensor(out=f["H"][:, j, :], in0=pf[1][:, 0:256], scalar=f["csh"][:, j:j + 1], in1=f["Ht"][:], op0=ALU.mult, op1=ALU.add),
                     reads=["hpf1", "csh", "Ht"], writes=["H"])
            HAf, HBf = f["H"][:, 0:nT, :], f["H"][:, nT:nS, :]
            P.op("act", lambda e: e.copy(out=t["HA"][:], in_=HAf), reads=["H"], writes=["HA"])
            P.op("act", lambda e: e.copy(out=t["HBm"][:], in_=HBf), reads=["H"], writes=["HBm"])
            P.op("dve", lambda e: e.tensor_scalar(out=t["HBm"][:, 0, :], in0=f["H"][:, nT, :], scalar1=t["m0"][:, 0:1], scalar2=None, op0=ALU.mult), reads=["H", "m0", "HBm"], writes=["HBm"])
            P.op("dve", lambda e: e.tensor_scalar(out=t["nHB"][:], in0=t["HBm"][:], scalar1=-1.0, scalar2=None, op0=ALU.mult), reads=["HBm"], writes=["nHB"])
            P.op("dve", lambda e: e.tensor_scalar(out=f["Ht"][:], in0=f["H"][:, 0, :], scalar1=t["m0"][:, 0:1], scalar2=None, op0=ALU.mult), reads=["H", "m0"], writes=["Ht"])
            P.op("dve", lambda e: e.scalar_tensor_tensor(out=t["P40"][:], in0=f["H"][:, nT, :], scalar=t["m0"][:, 1:2], in1=f["Ht"][:], op0=ALU.mult, op1=ALU.add), reads=["H", "m0", "Ht"], writes=["P40"])
            P.barrier()
        with ExitStack() as e3:
            g = _tiles(e3, nc, [("zh", (128, 6, L), BF16), ("u1", (128, L), F32), ("u2", (128, L), F32),
                                ("x0c", (128, 2, BG, L), BF16), ("sT", (128, 2, BG, L), BF16), ("stm", (128, nT, BG * 256), BF16),
                                ("Y", (128, nS, BG * 256), BF16), ("wfa", (128, nT, 128), BF16), ("wfb", (128, nT, 128), BF16),
                                ("wiv", (128, nS, W), BF16), ("p1", (128, BG * 256), F32), ("p2", (128, BG * 256), F32),
                                ("tmp", (128, W), F32), ("yo0", (128, W), BF16), ("yo1", (128, W), BF16)])
            pstb = _psum(e3, nc, "hpst", [128, 8, 128], BF16)
            psA = _psum(e3, nc, "hpA", [128, 512], F32)
            psB = _psum(e3, nc, "hpB", [128, 512], F32)
            psI = [_psum(e3, nc, "hpI%d" % i, [128, 512], F32) for i in range(4)]
            wf_v = hc["Wf"].rearrange("(t p) s -> p t s", p=128)
            wi_v = hc["Winv"].rearrange("(s p) t -> p s t", p=128)
            for g0 in range(0, NB, BG):
                for bb in range(BG):
                    b = g0 + bb
                    rds = [("zfd", l, b, x) for x in (0, 512, 1024, 1536, 2048)]
                    for c in range(6):
                        P.dma(lambda e, c=c, b=b: e.dma_start(out=g["zh"][:, c, :], in_=k.zf[ZF_ZH + c * 128:ZF_ZH + (c + 1) * 128, b * TB + off:b * TB + off + L]), reads=rds, writes=["zh"])

                    def conv3(c, dst_ap, dkey):
                        P.op("dve", lambda e: e.tensor_scalar(out=dst_ap, in0=g["zh"][:, c, :], scalar1=t["wc"][:, c, 1:2], scalar2=t["bc"][:, c:c + 1], op0=ALU.mult, op1=ALU.add),
                             reads=["zh", "wc", "bc"], writes=[dkey])
                        P.op("dve", lambda e: e.scalar_tensor_tensor(out=dst_ap[:, 1:L], in0=g["zh"][:, c, 0:L - 1], scalar=t["wc"][:, c, 0:1], in1=dst_ap[:, 1:L], op0=ALU.mult, op1=ALU.add),
                             reads=["zh", "wc", dkey], writes=[dkey])
                        P.op("dve", lambda e: e.scalar_tensor_tensor(out=dst_ap[:, 0:L - 1], in0=g["zh"][:, c, 1:L], scalar=t["wc"][:, c, 2:3], in1=dst_ap[:, 0:L - 1], op0=ALU.mult, op1=ALU.add),
                             reads=["zh", "wc", dkey], writes=[dkey])
                    for c in range(2):
                        conv3(c, g["u1"][:, :], "u1")
                        P.op("act", lambda e, c=c, bb=bb: e.copy(out=g["x0c"][:, c, bb, :], in_=g["u1"][:]), reads=["u1"], writes=["x0c"])
                        conv3(2 + c, g["u1"][:, :], "u1")
                        conv3(4 + c, g["u2"][:, :], "u2")
                        P.op("dve", lambda e, c=c, bb=bb: e.tensor_tensor(out=g["sT"][:, c, bb, :], in0=g["u1"][:], in1=g["u2"][:], op=ALU.mult), reads=["u1", "u2"], writes=["sT"])
                    for c in range(2):
                        for t4 in range(0, nT, 8):
                            n8 = min(8, nT - t4)
                            for i in range(n8):
                                P.op("pe", lambda e, c=c, bb=bb, i=i, t4=t4: e.transpose(out=pstb[:, i, :], in_=g["sT"][:, c, bb, (t4 + i) * 128:(t4 + i + 1) * 128], identity=t["idb"][:]),
                                     reads=["sT", "idb"], writes=["hpst"])
                            P.op("act", lambda e, c=c, bb=bb, t4=t4, n8=n8: e.copy(out=g["stm"][:, t4:t4 + n8, bb * 256 + c * 128:bb * 256 + (c + 1) * 128], in_=pstb[:, 0:n8, :]),
                                 reads=["hpst"], writes=["stm"])
                for j in range(nT):
                    P.dma(lambda e, j=j: e.dma_start(out=g["wfa"][:], in_=wf_v[:, :, j * 128:(j + 1) * 128]), writes=["wfa"])
                    P.dma(lambda e, j=j: e.dma_start(out=g["wfb"][:], in_=wf_v[:, :, (nT + j) * 128:(nT + j + 1) * 128]), writes=["wfb"])
                    for tt in range(nT):
                        P.op("pe", lambda e, tt=tt: e.matmul(psA[:, 0:BG * 256], lhsT=g["wfa"][:, tt, :], rhs=g["stm"][:, tt, :], start=(tt == 0), stop=(tt == nT - 1)),
                             reads=["wfa", "stm"], writes=["hpA"], skip_self=(tt > 0))
                    for tt in range(nT):
                        P.op("pe", lambda e, tt=tt: e.matmul(psB[:, 0:BG * 256], lhsT=g["wfb"][:, tt, :], rhs=g["stm"][:, tt, :], start=(tt == 0), stop=(tt == nT - 1)),
                             reads=["wfb", "stm"], writes=["hpB"], skip_self=(tt > 0))
                    A3 = psA[:, 0:BG * 256].rearrange("p (b c) -> p b c", b=BG)
                    B3 = psB[:, 0:BG * 256].rearrange("p (b c) -> p b c", b=BG)
                    p1 = g["p1"][:].rearrange("p (b c) -> p b c", b=BG)
                    p2 = g["p2"][:].rearrange("p (b c) -> p b c", b=BG)

                    def bc3(tab):
                        return tab.unsqueeze(1).to_broadcast([128, BG, 256])
                    P4 = t["P40"][:, :] if j == 0 else t["HA"][:, j, :]
                    P.op("dve", lambda e, j=j: e.tensor_tensor(out=p1, in0=A3, in1=bc3(t["HA"][:, j, :]), op=ALU.mult), reads=["hpA", "HA"], writes=["p1"])
                    P.op("dve", lambda e, j=j: e.tensor_tensor(out=p2, in0=B3, in1=bc3(t["nHB"][:, j, :]), op=ALU.mult), reads=["hpB", "nHB"], writes=["p2"])
                    P.op("pool", lambda e, j=j: e.tensor_tensor(out=g["Y"][:, j, :], in0=g["p1"][:], in1=g["p2"][:], op=ALU.add), reads=["p1", "p2"], writes=["Y"])
                    P.op("dve", lambda e, j=j: e.tensor_tensor(out=p1, in0=A3, in1=bc3(t["HBm"][:, j, :]), op=ALU.mult), reads=["hpA", "HBm", "Y"], writes=["p1"])
                    P.op("dve", lambda e, j=j, P4=P4: e.tensor_tensor(out=p2, in0=B3, in1=bc3(P4), op=ALU.mult), reads=["hpB", "HA", "P40", "Y"], writes=["p2"])
                    P.op("pool", lambda e, j=j: e.tensor_tensor(out=g["Y"][:, nT + j, :], in0=g["p1"][:], in1=g["p2"][:], op=ALU.add), reads=["p1", "p2"], writes=["Y"])
                for tb in range(L // W):
                    P.dma(lambda e, tb=tb: e.dma_start(out=g["wiv"][:], in_=wi_v[:, :, tb * W:(tb + 1) * W]), writes=["wiv"])
                    for bb in range(BG):
                        for c in range(2):
                            ps = psI[bb * 2 + c]
                            pk = "hpI%d" % (bb * 2 + c)
                            for sc in range(nS):
                                P.op("pe", lambda e, ps=ps, sc=sc, bb=bb, c=c: e.matmul(ps[:, 0:W], lhsT=g["Y"][:, sc, bb * 256 + c * 128:bb * 256 + (c + 1) * 128], rhs=g["wiv"][:, sc, :],
                                                                                  start=(sc == 0), stop=(sc == nS - 1)), reads=["Y", "wiv"], writes=[pk], skip_self=(sc > 0))
                            yo = g["yo%d" % (k.yc % 2)]
                            yk = "yo%d" % (k.yc % 2)
                            k.yc += 1
                            P.op("dve", lambda e, ps=ps, bb=bb, c=c, tb=tb: e.scalar_tensor_tensor(out=g["tmp"][:], in0=g["sT"][:, c, bb, tb * W:(tb + 1) * W], scalar=t["dcol"][:, c:c + 1], in1=ps[:, 0:W],
                                                                                             op0=ALU.mult, op1=ALU.add), reads=["sT", "dcol", pk], writes=["tmp"])
                            P.op("dve", lambda e, bb=bb, c=c, tb=tb, yo=yo: e.tensor_tensor(out=yo[:], in0=g["tmp"][:], in1=g["x0c"][:, c, bb, tb * W:(tb + 1) * W], op=ALU.mult),
                                 reads=["tmp", "x0c"], writes=[yk])
                            b = g0 + bb
                            P.dma(lambda e, yo=yo, b=b, c=c, tb=tb: e.dma_start(out=k.yT[512 + c * 128:512 + (c + 1) * 128, b * TB + off + tb * W:b * TB + off + (tb + 1) * W], in_=yo[:]),
                                  reads=[yk], writes=[("yTd", l, 512, c, b, off, tb)], dkey=("hyo", yk))
            P.barrier()
        P.emit()


def hyena_consts(L):
    n = L
    t_lin = np.linspace(0.0, 1.0, n, dtype=np.float32)
    bands = np.linspace(1e-4, 15.0, 16, dtype=np.float32)
    w = (2.0 * np.float32(math.pi) * np.arange(n, dtype=np.float32) / np.float32(n)).astype(np.float32)
    z = np.concatenate([t_lin[:, None], np.cos(bands[None, :] * w[:, None]), -np.sin(bands[None, :] * w[:, None])], axis=-1).astype(np.float32)
    ridx = (n - np.arange(n)) % n
    out = {}
    out["zf"] = np.ascontiguousarray(z.T)
    out["zr"] = np.ascontiguousarray(z[ridx].T)
    tf = -t_lin
    tr = -t_lin[ridx]
    out["tf"] = np.ascontiguousarray(tf.reshape(n // 128, 128).T)
    out["tr"] = np.ascontiguousarray(tr.reshape(n // 128, 128).T)
    max_decay = math.log(1e-2) / 0.3
    min_decay = math.log(1e-2) / 1.5
    deltas = np.abs(np.linspace(min_decay, max_decay, 256, dtype=np.float32))
    out["dl"] = np.broadcast_to(deltas[None, :], (128, 256)).astype(np.float32).copy()
    N = 2 * n
    tt = np.arange(n, dtype=np.float64)[:, None]
    kk = np.arange(n, dtype=np.float64)[None, :]
    ang = 2.0 * np.pi * ((tt * kk) % N) / N
    C = np.cos(ang)
    S = np.sin(ang)
    S[:, 0] = (-1.0) ** np.arange(n)
    Wf = np.concatenate([C, S], axis=1)
    out["Wf"] = Wf.astype(ml_dtypes.bfloat16)
    out["Winv"] = np.ascontiguousarray(Wf.T).astype(ml_dtypes.bfloat16)
    cw = np.full(N, 2.0 / N)
    cw[0] = 1.0 / N
    cw[n] = 1.0 / N
    sgn = np.tile((-1.0) ** np.arange(n), 2)
    sgn[n] = 1.0
    out["csl"] = np.ascontiguousarray(cw.reshape(N // 128, 128).T).astype(np.float32)
    out["csh"] = np.ascontiguousarray((cw * sgn).reshape(N // 128, 128).T).astype(np.float32)
    return out


def stage_outproj(k, l):
    nc, P, NB = k.nc, k.P, k.NB
    ntile = 18 if l < DEPTH - 1 else 16
    with ExitStack() as es:
        A, B = load_mod_cols(k, es, l, 1)
        t = _tiles(es, nc, [("idf", (128, 128), F32), ("idb", (128, 128), BF16), ("wst0", (128, D), F32), ("wst1", (128, D), F32), ("gng", (128, 8), F32), ("wout", (128, 8, D), BF16), ("wrf", (128, 8, 36), F32), ("wr", (128, 8, 36), BF16), ("ones", (128, 1), BF16), ("g1b", (128, D), F32), ("ohrun", (128, 32), F32), ("iota32", (128, 32), F32), ("LTf", (128, 128), F32), ("ONESf", (128, 128), F32), ("jbv", (128, 128), F32), ("A2row", (128, D), F32), ("B2row", (128, D), F32), ("g2row", (128, D), F32), ("cnt", (128, 32), F32), ("nf", (128, 32), F32), ("ni", (128, 32), mybir.dt.int32), ("dd", (128, 32), F32), ("up", (128, 32), F32), ("pend", (128, 32), F32), ("zero32", (128, 32), F32), ("cmp", (128, MAXBLK, 32), F32), ("bexp", (128, MAXBLK), F32)])
        ts = []
        for s_ in range(2):
            d_ = dict(t)
            d_.update(_tiles(es, nc, [("yt", (128, 8, 128), BF16), ("ysq", (128, 8, 128), BF16), ("r", (128, 4), F32), ("m", (128, D), F32), ("xt0", (128, D), F32), ("junk0", (128, D), BF16), ("ss0", (128, 1), F32), ("rstd0", (128, 1), F32), ("xn0", (128, D), BF16), ("fT", (128, 8, 128), BF16), ("lg", (128, 36), F32), ("s1", (128, 8), F32), ("s2", (128, 8), F32), ("s3", (128, 8), F32), ("mg", (128, 4), F32), ("pr", (128, 4, 8), F32), ("es", (128, 8), F32), ("m1", (128, 8), F32), ("m2", (128, 8), F32), ("e2", (128, 8), F32), ("cw8", (128, 8), F32), ("cw", (128, 4, 8), F32), ("oh1", (128, 4, 8), F32), ("oh2", (128, 4, 8), F32), ("ohs", (128, 32), F32), ("ohp", (128, 32), F32), ("rt", (128, 8), F32), ("ft1", (128, D), F32), ("ftm", (128, D), BF16)]))
            d_["pst"] = _psum(es, nc, "pst", [128, 8, 128], BF16)
            d_["pm"] = [_psum(es, nc, "pm%d" % i, [128, 512], F32) for i in range(2)]
            d_["misc"] = _psum(es, nc, "misc", [128, 512], F32)
            ts.append(d_)
        for nm, src in (("iota32", k.c_iota32), ("LTf", k.c_LT), ("ONESf", k.c_ONES), ("jbv", k.c_jbv)):
            P.dma(lambda e, nm=nm, src=src: e.dma_start(out=t[nm][:], in_=src), writes=[nm])
        P.op("pool", lambda e: e.memset(t["ohrun"][:], 0.0), writes=["ohrun"])
        P.op("pool", lambda e: e.memset(t["zero32"][:], 0.0), writes=["zero32"])
        for d_ in ts:
            P.op("pool", lambda e, d_=d_: e.memset(d_["rt"][:], 0.0), writes=["rt_" + str(ts.index(d_))])
        P.dma(lambda e: e.dma_start(out=t["g2row"][:], in_=k.norm2_g[l].partition_broadcast(128)), writes=["g2row"])
        P.dma(lambda e: e.dma_start(out=t["idf"][:], in_=k.identf), writes=["idf"])
        P.op("dve", lambda e: e.tensor_copy(out=t["idb"][:], in_=t["idf"][:]), reads=["idf"], writes=["idb"])
        P.op("pool", lambda e: e.memset(t["ones"][:], 1.0), writes=["ones"])
        P.dma(lambda e: e.dma_start(out=t["gng"][:], in_=k.group_norm_g[l].rearrange("(c p) -> p c", p=128)), writes=["gng"])
        for kk in range(8):
            ws = t["wst%d" % (kk % 2)]
            wk = "wst%d" % (kk % 2)
            P.dma(lambda e, kk=kk, ws=ws: e.dma_start(out=ws[:], in_=k.w_out[l][kk * 128:(kk + 1) * 128, :]), writes=[wk])
            P.op("dve", lambda e, kk=kk, ws=ws: e.tensor_scalar(out=t["wout"][:, kk, :], in0=ws[:], scalar1=t["gng"][:, kk:kk + 1], scalar2=None, op0=ALU.mult), reads=[wk, "gng"], writes=["wout"])
        P.dma(lambda e: e.dma_start(out=t["wrf"][:, :, 0:4], in_=k.moe_w_group[l].rearrange("(c p) n -> p c n", p=128)), writes=["wrf"])
        P.dma(lambda e: e.dma_start(out=t["wrf"][:, :, 4:36], in_=k.moe_w_expert[l].rearrange("(c p) n -> p c n", p=128)), writes=["wrf"])
        P.op("dve", lambda e: e.tensor_copy(out=t["wr"][:], in_=t["wrf"][:]), reads=["wrf"], writes=["wr"])
        yT_v = k.yT.rearrange("(c p) t -> p c t", p=128)
        fT_v = k.fT.rearrange("(c p) t -> p c t", p=128)
        def tile_body(t, b, i, other=None):
            P = k.P
            if True:
                t0 = b * TB + i * 128
                row = b if i < 16 else NB
                P.dma(lambda e, t0=t0: e.dma_start(out=t["yt"][:], in_=yT_v[:, :, t0:t0 + 128]), writes=["yt"])
                P.dma(lambda e, b=b, i=i: e.dma_start(out=t["xt0"][:], in_=xsrc(k, l, b, i)), writes=["xt0"])
                P.op("pool", lambda e: e.tensor_tensor(out=t["ysq"][:], in0=t["yt"][:], in1=t["yt"][:], op=ALU.mult), reads=["yt"], writes=["ysq"])
                for g in range(4):
                    for kc in range(2):
                        P.op("pe", lambda e, g=g, kc=kc: e.matmul(t["misc"][:, g:g + 1], lhsT=t["ysq"][:, 2 * g + kc, :], rhs=t["ones"][:], start=(kc == 0), stop=(kc == 1)), reads=["ysq", "ones"], writes=["misc"],
                             skip_self=(kc > 0))
                P.op("act", lambda e: e.activation(out=t["r"][:], in_=t["misc"][:, 0:4], func=AF.Sqrt, scale=1.0 / 256, bias=EPS), reads=["misc"], writes=["r"])
                P.op("dve", lambda e: e.reciprocal(out=t["r"][:], in_=t["r"][:]), reads=["r"], writes=["r"])
                for half in range(2):
                    for g in range(4):
                        ps = t["pm"][g % 2]
                        pk = "pm%d" % (g % 2)
                        for kc in range(2):
                            P.op("pe", lambda e, ps=ps, g=g, kc=kc, half=half: e.matmul(ps[:, :], lhsT=t["yt"][:, 2 * g + kc, :], rhs=t["wout"][:, 2 * g + kc, half * 512:(half + 1) * 512], start=(kc == 0), stop=(kc == 1)),
                                 reads=["yt", "wout"], writes=[pk], skip_self=(kc > 0))
                        if g == 0:
                            P.op("dve", lambda e, ps=ps, half=half: e.tensor_scalar(out=t["m"][:, half * 512:(half + 1) * 512], in0=ps[:, :], scalar1=t["r"][:, 0:1], scalar2=None, op0=ALU.mult), reads=[pk, "r"], writes=["m"])
                        else:
                            P.op("dve", lambda e, ps=ps, half=half, g=g: e.scalar_tensor_tensor(out=t["m"][:, half * 512:(half + 1) * 512], in0=ps[:, :], scalar=t["r"][:, g:g + 1], in1=t["m"][:, half * 512:(half + 1) * 512],
                                                                                             op0=ALU.mult, op1=ALU.add), reads=[pk, "r", "m"], writes=["m"])
                P.op("pool", lambda e: e.tensor_tensor(out=t["m"][:], in0=t["m"][:], in1=t["g1b"][:], op=ALU.mult), reads=["m", "g1b"], writes=["m"])
                P.op("dve", lambda e: e.tensor_tensor(out=t["xt0"][:], in0=t["xt0"][:], in1=t["m"][:], op=ALU.add), reads=["xt0", "m"], writes=["xt0"])
                P.dma(lambda e, t0=t0: e.dma_start(out=k.x1[t0:t0 + 128, :], in_=t["xt0"][:]), reads=["xt0"], writes=[("x1d", t0)], dkey="x1st")
                norm_mod_T(k, t, t["pst"], t["xt0"][:], "xt0", A, B, row, t["fT"], "fT", 0, "0")
                P.dma(lambda e, t0=t0: e.dma_start(out=fT_v[:, :, t0:t0 + 128], in_=t["fT"][:]), reads=["fT"], writes=[("fTd", t0)], dkey="fTst")
                for kk in range(8):
                    P.op("pe", lambda e, kk=kk: e.matmul(t["misc"][:, 64:100], lhsT=t["fT"][:, kk, :], rhs=t["wr"][:, kk, :], start=(kk == 0), stop=(kk == 7)), reads=["fT", "wr"], writes=["misc"], skip_self=(kk > 0))
                P.op("act", lambda e: e.copy(out=t["lg"][:], in_=t["misc"][:, 64:100]), reads=["misc"], writes=["lg"])
                gl = t["lg"][:, 0:4]
                el = t["lg"][:, 4:36].rearrange("p (g e) -> p g e", g=4)
                s1, s2, s3 = t["s1"], t["s2"], t["s3"]
                P.op("dve", lambda e: e.tensor_reduce(out=s1[:, 0:1], in_=gl, axis=AX.X, op=ALU.max), reads=["lg"], writes=["s1"])
                P.op("dve", lambda e: e.tensor_scalar(out=s1[:, 1:2], in0=s1[:, 0:1], scalar1=-1.0, scalar2=None, op0=ALU.mult), reads=["s1"], writes=["s1"])
                P.op("act", lambda e: e.activation(out=t["mg"][:], in_=gl, func=AF.Exp, bias=s1[:, 1:2], accum_out=s1[:, 2:3]), reads=["lg", "s1"], writes=["mg", "s1"])
                P.op("dve", lambda e: e.reciprocal(out=s1[:, 3:4], in_=s1[:, 2:3]), reads=["s1"], writes=["s1"])
                P.op("dve", lambda e: e.tensor_scalar(out=t["mg"][:], in0=gl, scalar1=s1[:, 0:1], scalar2=None, op0=ALU.is_equal), reads=["lg", "s1", "mg"], writes=["mg"])
                P.op("dve", lambda e: e.tensor_tensor(out=t["pr"][:], in0=el, in1=t["mg"][:].unsqueeze(2).to_broadcast([128, 4, 8]), op=ALU.mult), reads=["lg", "mg"], writes=["pr"])
                P.op("dve", lambda e: e.tensor_reduce(out=t["es"][:], in_=t["pr"][:].rearrange("p g e -> p e g"), axis=AX.X, op=ALU.add), reads=["pr"], writes=["es"])
                P.op("dve", lambda e: e.tensor_reduce(out=s2[:, 0:1], in_=t["es"][:], axis=AX.X, op=ALU.max), reads=["es"], writes=["s2"])
                P.op("dve", lambda e: e.tensor_scalar(out=t["m1"][:], in0=t["es"][:], scalar1=s2[:, 0:1], scalar2=None, op0=ALU.is_equal), reads=["es", "s2"], writes=["m1"])
                P.op("dve", lambda e: e.scalar_tensor_tensor(out=t["e2"][:], in0=t["m1"][:], scalar=-1e30, in1=t["es"][:], op0=ALU.mult, op1=ALU.add), reads=["m1", "es"], writes=["e2"])
                P.op("dve", lambda e: e.tensor_reduce(out=s2[:, 1:2], in_=t["e2"][:], axis=AX.X, op=ALU.max), reads=["e2"], writes=["s2"])
                P.op("dve", lambda e: e.tensor_scalar(out=t["m2"][:], in0=t["e2"][:], scalar1=s2[:, 1:2], scalar2=None, op0=ALU.is_equal), reads=["e2", "s2"], writes=["m2"])
                P.op("dve", lambda e: e.tensor_tensor(out=s2[:, 2:3], in0=s2[:, 1:2], in1=s2[:, 0:1], op=ALU.subtract), reads=["s2"], writes=["s2"])
                P.op("act", lambda e: e.activation(out=s2[:, 3:4], in_=s2[:, 2:3], func=AF.Exp), reads=["s2"], writes=["s2"])
                P.op("dve", lambda e: e.tensor_scalar(out=s3[:, 0:1], in0=s2[:, 3:4], scalar1=1.0, scalar2=None, op0=ALU.add), reads=["s2"], writes=["s3"])
                P.op("dve", lambda e: e.reciprocal(out=s3[:, 1:2], in_=s3[:, 0:1]), reads=["s3"], writes=["s3"])
                P.op("dve", lambda e: e.tensor_tensor(out=s3[:, 2:3], in0=s3[:, 1:2], in1=s1[:, 3:4], op=ALU.mult), reads=["s3", "s1"], writes=["s3"])
                P.op("dve", lambda e: e.tensor_tensor(out=s3[:, 3:4], in0=s3[:, 2:3], in1=s2[:, 3:4], op=ALU.mult), reads=["s3", "s2"], writes=["s3"])
                P.op("dve", lambda e: e.tensor_scalar(out=t["cw8"][:], in0=t["m1"][:], scalar1=s3[:, 2:3], scalar2=None, op0=ALU.mult), reads=["m1", "s3"], writes=["cw8"])
                P.op("dve", lambda e: e.scalar_tensor_tensor(out=t["cw8"][:], in0=t["m2"][:], scalar=s3[:, 3:4], in1=t["cw8"][:], op0=ALU.mult, op1=ALU.add), reads=["m2", "s3", "cw8"], writes=["cw8"])
                P.op("dve", lambda e: e.tensor_tensor(out=t["cw"][:], in0=t["mg"][:].unsqueeze(2).to_broadcast([128, 4, 8]), in1=t["cw8"][:].unsqueeze(1).to_broadcast([128, 4, 8]), op=ALU.mult),
                     reads=["mg", "cw8"], writes=["cw"])
                P.dma(lambda e, t0=t0: e.dma_start(out=k.cw[t0:t0 + 128, :], in_=t["cw"][:].rearrange("p g e -> p (g e)")), reads=["cw"], writes=[("cwd", t0)], dkey="cwst")
                mgb = t["mg"][:].unsqueeze(2).to_broadcast([128, 4, 8])
                P.op("dve", lambda e: e.tensor_tensor(out=t["oh1"][:], in0=mgb, in1=t["m1"][:].unsqueeze(1).to_broadcast([128, 4, 8]), op=ALU.mult), reads=["mg", "m1"], writes=["oh1"])
                P.op("dve", lambda e: e.tensor_tensor(out=t["oh2"][:], in0=mgb, in1=t["m2"][:].unsqueeze(1).to_broadcast([128, 4, 8]), op=ALU.mult), reads=["mg", "m2"], writes=["oh2"])
                oh1f = t["oh1"][:].rearrange("p g e -> p (g e)")
                oh2f = t["oh2"][:].rearrange("p g e -> p (g e)")
                P.op("dve", lambda e: e.tensor_tensor(out=t["ohs"][:], in0=oh1f, in1=oh2f, op=ALU.add), reads=["oh1", "oh2"], writes=["ohs"])
                P.op("pe", lambda e: e.matmul(t["misc"][:, 128:160], lhsT=t["LTf"][:], rhs=t["ohs"][:], start=True, stop=False), reads=["LTf", "ohs"], writes=["misc"])
                P.op("pe", lambda e: e.matmul(t["misc"][:, 128:160], lhsT=t["ONESf"][:], rhs=t["ohrun"][:], start=False, stop=(other is None)), reads=["ONESf", "ohrun"], writes=["misc"], skip_self=True)
                if other is not None:
                    P.op("pe", lambda e: e.matmul(t["misc"][:, 128:160], lhsT=t["ONESf"][:], rhs=other["ohs"][:], start=False, stop=True), reads=["ONESf", "ohs_0"], writes=["misc"], skip_self=True)
                for j, ohf, okey in ((0, oh1f, "oh1"), (1, oh2f, "oh2")):
                    P.op("dve", lambda e, ohf=ohf: e.tensor_tensor(out=t["ohp"][:], in0=t["misc"][:, 128:160], in1=ohf, op=ALU.mult), reads=["misc", okey], writes=["ohp"])
                    P.op("dve", lambda e, j=j: e.tensor_reduce(out=t["rt"][:, 2 + j:3 + j], in_=t["ohp"][:], axis=AX.X, op=ALU.add), reads=["ohp"], writes=["rt"])
                    P.op("dve", lambda e, ohf=ohf: e.tensor_tensor(out=t["ohp"][:], in0=t["iota32"][:], in1=ohf, op=ALU.mult), reads=["iota32", okey, "rt"], writes=["ohp"])
                    P.op("dve", lambda e, j=j: e.tensor_reduce(out=t["rt"][:, j:j + 1], in_=t["ohp"][:], axis=AX.X, op=ALU.add), reads=["ohp"], writes=["rt"])
                P.op("dve", lambda e: e.tensor_copy(out=t["rt"][:, 4:6], in_=s3[:, 2:4]), reads=["s3"], writes=["rt"])
                P.op("dve", lambda e: e.tensor_tensor(out=t["ohrun"][:], in0=t["ohrun"][:], in1=t["ohs"][:], op=ALU.add), reads=["ohrun", "ohs"], writes=["ohrun"])
                P.dma(lambda e, t0=t0: e.dma_start(out=k.rt[t0:t0 + 128, :], in_=t["rt"][:]), reads=["rt"], writes=[("rtd", t0)], dkey="rtst")
                P.op("pool", lambda e: e.tensor_tensor(out=t["ft1"][:], in0=t["xn0"][:], in1=t["A2row"][:], op=ALU.mult), reads=["xn0", "A2row"], writes=["ft1"])
                P.op("pool", lambda e: e.tensor_tensor(out=t["ftm"][:], in0=t["ft1"][:], in1=t["B2row"][:], op=ALU.add), reads=["ft1", "B2row"], writes=["ftm"])
                P.dma(lambda e, t0=t0: e.dma_start(out=k.ftm[t0:t0 + 128, :], in_=t["ftm"][:]), reads=["ftm"], writes=[("ftmd", t0)], dkey="ftmst")

        LOCALK = set(['yt', 'ysq', 'r', 'm', 'xt0', 'junk0', 'ss0', 'rstd0', 'xn0', 'fT', 'lg', 's1', 's2', 's3', 'mg', 'pr', 'es', 'm1', 'm2', 'e2', 'cw8', 'cw', 'oh1', 'oh2', 'ohs', 'ohp', 'rt', 'ft1', 'ftm']) | {"pst", "pm0", "pm1", "misc", "x1st", "fTst", "cwst", "rtst", "ftmst"}
        for b in range(NB):
            for i0 in range(0, ntile, 2):
                if i0 == 0 or i0 == 16:
                    row = b if i0 < 16 else NB
                    P.dma(lambda e, row=row: e.dma_start(out=t["g1b"][:], in_=k.modrow[l][row, 2 * D:3 * D].partition_broadcast(128)), reads=[("modrow", l)], writes=["g1b"])
                    P.dma(lambda e, row=row: e.dma_start(out=t["B2row"][:], in_=k.modrow[l][row, 3 * D:4 * D].partition_broadcast(128)), reads=[("modrow", l)], writes=["B2row"])
                    P.dma(lambda e, row=row: e.dma_start(out=t["A2row"][:], in_=k.modrow[l][row, 4 * D:5 * D].partition_broadcast(128)), reads=[("modrow", l)], writes=["A2row"])
                    P.op("dve", lambda e: e.scalar_tensor_tensor(out=t["A2row"][:], in0=t["A2row"][:], scalar=1.0, in1=t["g2row"][:], op0=ALU.add, op1=ALU.mult), reads=["A2row", "g2row"], writes=["A2row"])
                recs = []
                for s_ in range(2):
                    if i0 + s_ < ntile:
                        rec = Rec("_%d" % s_, LOCALK)
                        k.P = rec
                        tile_body(ts[s_], b, i0 + s_, other=(ts[0] if s_ == 1 else None))
                        recs.append(rec)
                k.P = P
                replay(P, recs)
        NBLK = k.nblk[l]
        P.op("pe", lambda e: e.matmul(ts[0]["misc"][:, 128:160], lhsT=t["ONESf"][:], rhs=t["ohrun"][:], start=True, stop=True), reads=["ONESf", "ohrun"], writes=["misc_0"])
        P.op("act", lambda e: e.copy(out=t["cnt"][:], in_=ts[0]["misc"][:, 128:160]), reads=["misc_0"], writes=["cnt"])
        P.op("dve", lambda e: e.tensor_scalar(out=t["nf"][:], in0=t["cnt"][:], scalar1=1.0 / MOE_BS, scalar2=None, op0=ALU.mult), reads=["cnt"], writes=["nf"])
        P.op("dve", lambda e: e.tensor_copy(out=t["ni"][:], in_=t["nf"][:]), reads=["nf"], writes=["ni"])
        P.op("dve", lambda e: e.tensor_copy(out=t["nf"][:], in_=t["ni"][:]), reads=["ni"], writes=["nf"])
        P.op("dve", lambda e: e.scalar_tensor_tensor(out=t["dd"][:], in0=t["nf"][:], scalar=float(MOE_BS), in1=t["cnt"][:], op0=ALU.mult, op1=ALU.subtract), reads=["nf", "cnt"], writes=["dd"])
        P.op("dve", lambda e: e.tensor_scalar(out=t["up"][:], in0=t["dd"][:], scalar1=0.0, scalar2=None, op0=ALU.is_lt), reads=["dd"], writes=["up"])
        P.op("dve", lambda e: e.tensor_tensor(out=t["nf"][:], in0=t["nf"][:], in1=t["up"][:], op=ALU.add), reads=["nf", "up"], writes=["nf"])
        P.op("dve", lambda e: e.scalar_tensor_tensor(out=t["dd"][:], in0=t["up"][:], scalar=float(MOE_BS), in1=t["dd"][:], op0=ALU.mult, op1=ALU.add), reads=["up", "dd"], writes=["dd"])
        P.op("dve", lambda e: e.tensor_scalar(out=t["up"][:], in0=t["dd"][:], scalar1=float(MOE_BS), scalar2=None, op0=ALU.is_ge), reads=["dd"], writes=["up"])
        P.op("dve", lambda e: e.tensor_tensor(out=t["nf"][:], in0=t["nf"][:], in1=t["up"][:], op=ALU.subtract), reads=["nf", "up"], writes=["nf"])
        P.op("dve", lambda e: e.tensor_scalar(out=t["nf"][:], in0=t["nf"][:], scalar1=float(MOE_BS), scalar2=None, op0=ALU.mult), reads=["nf"], writes=["nf"])
        P.op("dve", lambda e: e.tensor_tensor_scan(out=t["pend"][:], data0=t["nf"][:], data1=t["zero32"][:], initial=0.0, op0=ALU.add, op1=ALU.add), reads=["nf", "zero32"], writes=["pend"])
        P.op("dve", lambda e: e.tensor_tensor(out=t["nf"][:], in0=t["pend"][:], in1=t["nf"][:], op=ALU.subtract), reads=["pend", "nf"], writes=["nf"])
        P.dma(lambda e: e.dma_start(out=k.pstart[l], in_=t["nf"][:]), reads=["nf"], writes=[("pstart", l)], dkey="pstst")
        P.op("dve", lambda e: e.tensor_tensor(out=t["cmp"][:, 0:NBLK, :], in0=t["pend"][:].unsqueeze(1).to_broadcast([128, NBLK, 32]),
                                              in1=t["jbv"][:, 0:NBLK].unsqueeze(2).to_broadcast([128, NBLK, 32]), op=ALU.is_le), reads=["pend", "jbv"], writes=["cmp"])
        P.op("dve", lambda e: e.tensor_reduce(out=t["bexp"][:, 0:NBLK], in_=t["cmp"][:, 0:NBLK, :], axis=AX.X, op=ALU.add), reads=["cmp"], writes=["bexp"])
        P.op("dve", lambda e: e.tensor_scalar(out=t["bexp"][:, 0:NBLK], in0=t["bexp"][:, 0:NBLK], scalar1=31.0, scalar2=None, op0=ALU.min), reads=["bexp"], writes=["bexp"])
        P.dma(lambda e: e.dma_start(out=k.bexp[l][:, 0:NBLK], in_=t["bexp"][:, 0:NBLK]), reads=["bexp"], writes=[("bexp", l)], dkey="bexst")
        P.barrier()
        P.emit()


MOE_BS = 512
MAXBLK = 72
I32 = mybir.dt.int32


def stage_moe_sparse(k, l):
    nc, P, NB = k.nc, k.P, k.NB
    last = (l == DEPTH - 1)
    ntile = 16 if last else 18
    NBLK = k.nblk[l]
    NSLOT = NBLK * MOE_BS
    with ExitStack() as es:
        t = _tiles(es, nc, [("idf", (128, 128), F32), ("idb", (128, 128), BF16), ("zeros", (128, 4, D), BF16),
                            ("pstart", (128, 32), F32), ("bexp", (128, MAXBLK), F32), ("iota32", (128, 32), F32), ("iotaA", (128, 8), F32), ("iotaB", (128, 4), F32),
                            ("e1k", (128, MAXBLK), F32), ("ixf", (128, MAXBLK, 8), F32), ("ix1", (128, MAXBLK, 8), I32), ("ix2", (128, MAXBLK, 4), I32),
                            ("rtt", (128, 8), F32), ("ohq", (128, 32), F32), ("dsf", (128, 2), F32),
                            ("dest", (128, NB * 18, 2), I32), ("wgt", (128, NB * 18, 2), F32),
                            ("ft0", (128, D), BF16), ("ft1", (128, D), BF16),
                            ("stA0", (128, 8, 512), F32), ("stA1", (128, 8, 512), F32), ("stB0", (128, 8, 512), F32), ("stB1", (128, 8, 512), F32),
                            ("stC0", (128, 4, D), F32), ("stC1", (128, 4, D), F32),
                            ("w1b", (128, 8, 512), BF16), ("w3b", (128, 8, 512), BF16), ("w2b", (128, 4, D), BF16),
                            ("xb0", (128, 4, D), BF16), ("xb1", (128, 4, D), BF16), ("xT", (128, 8, 512), BF16),
                            ("s1", (128, 512), F32), ("act", (128, 4, 512), BF16), ("yb0", (128, 4, D), BF16), ("yb1", (128, 4, D), BF16),
                            ("y1", (128, D), BF16), ("y2", (128, D), BF16), ("acc", (128, D), F32), ("g2b", (128, D), F32), ("xt", (128, D), F32)])
        pstb = _psum(es, nc, "mpst", [128, 8, 128], BF16)
        p1 = [_psum(es, nc, "mp1_%d" % i, [128, 512], F32) for i in range(2)]
        p3 = [_psum(es, nc, "mp3_%d" % i, [128, 512], F32) for i in range(2)]
        py = [_psum(es, nc, "mpy_%d" % i, [128, 512], F32) for i in range(2)]
        P.dma(lambda e: e.dma_start(out=t["idf"][:], in_=k.identf), writes=["idf"])
        P.op("dve", lambda e: e.tensor_copy(out=t["idb"][:], in_=t["idf"][:]), reads=["idf"], writes=["idb"])
        P.op("dve", lambda e: e.memset(t["zeros"][:], 0.0), writes=["zeros"])
        for nm, src in (("iota32", k.c_iota32), ("iotaA", k.c_iotaA), ("iotaB", k.c_iotaB), ("pstart", k.pstart[l])):
            P.dma(lambda e, nm=nm, src=src: e.dma_start(out=t[nm][:], in_=src), writes=[nm])
        P.dma(lambda e: e.dma_start(out=t["bexp"][:, 0:NBLK], in_=k.bexp[l][:, 0:NBLK]), writes=["bexp"])
        xs_v = k.xs.rearrange("(n p) d -> p n d", p=128)
        ys_v = k.ys.rearrange("(n p) d -> p n d", p=128)
        for n0 in range(0, NSLOT // 128, 4):
            P.dma(lambda e, n0=n0: e.dma_start(out=xs_v[:, n0:n0 + 4, :], in_=t["zeros"][:]), reads=["zeros"], writes=["xs"], dkey="xszero")
            P.dma(lambda e, n0=n0: e.dma_start(out=ys_v[:, n0:n0 + 4, :], in_=t["zeros"][:]), reads=["zeros"], writes=["ys"], dkey="yszero")
        P.op("dve", lambda e: e.tensor_scalar(out=t["e1k"][:, 0:NBLK], in0=t["bexp"][:, 0:NBLK], scalar1=1024.0, scalar2=float(l * 32 * 1024), op0=ALU.mult, op1=ALU.add), reads=["bexp"], writes=["e1k"])
        P.op("dve", lambda e: e.tensor_tensor(out=t["ixf"][:, 0:NBLK, :], in0=t["e1k"][:, 0:NBLK].unsqueeze(2).to_broadcast([128, NBLK, 8]),
                                              in1=t["iotaA"][:].unsqueeze(1).to_broadcast([128, NBLK, 8]), op=ALU.add), reads=["e1k", "iotaA"], writes=["ixf"])
        P.op("dve", lambda e: e.tensor_copy(out=t["ix1"][:, 0:NBLK, :], in_=t["ixf"][:, 0:NBLK, :]), reads=["ixf"], writes=["ix1"])
        P.op("dve", lambda e: e.tensor_scalar(out=t["e1k"][:, 0:NBLK], in0=t["bexp"][:, 0:NBLK], scalar1=512.0, scalar2=float(l * 32 * 512), op0=ALU.mult, op1=ALU.add), reads=["bexp", "ixf"], writes=["e1k"])
        P.op("dve", lambda e: e.tensor_tensor(out=t["ixf"][:, 0:NBLK, 0:4], in0=t["e1k"][:, 0:NBLK].unsqueeze(2).to_broadcast([128, NBLK, 4]),
                                              in1=t["iotaB"][:].unsqueeze(1).to_broadcast([128, NBLK, 4]), op=ALU.add), reads=["e1k", "iotaB", "ix1"], writes=["ixf"])
        P.op("dve", lambda e: e.tensor_copy(out=t["ix2"][:, 0:NBLK, :], in_=t["ixf"][:, 0:NBLK, 0:4]), reads=["ixf"], writes=["ix2"])
        tl = []
        for b in range(NB):
            for i in range(ntile):
                tl.append((b, i))
        for n, (b, i) in enumerate(tl):
            t0 = b * TB + i * 128
            ft = t["ft%d" % (n % 2)]
            fk = "ft%d" % (n % 2)
            P.dma(lambda e, t0=t0: e.dma_start(out=t["rtt"][:], in_=k.rt[t0:t0 + 128, :]), writes=["rtt"])
            P.dma(lambda e, t0=t0, ft=ft: e.dma_start(out=ft[:], in_=k.ftm[t0:t0 + 128, :]), writes=[fk])
            for j in range(2):
                P.op("dve", lambda e, j=j: e.tensor_scalar(out=t["ohq"][:], in0=t["iota32"][:], scalar1=t["rtt"][:, j:j + 1], scalar2=None, op0=ALU.is_equal), reads=["iota32", "rtt"], writes=["ohq"])
                P.op("dve", lambda e: e.tensor_tensor(out=t["ohq"][:], in0=t["ohq"][:], in1=t["pstart"][:], op=ALU.mult), reads=["ohq", "pstart"], writes=["ohq"])
                P.op("dve", lambda e, j=j: e.tensor_reduce(out=t["dsf"][:, j:j + 1], in_=t["ohq"][:], axis=AX.X, op=ALU.add), reads=["ohq"], writes=["dsf"])
            P.op("dve", lambda e: e.tensor_tensor(out=t["dsf"][:], in0=t["dsf"][:], in1=t["rtt"][:, 2:4], op=ALU.add), reads=["dsf", "rtt"], writes=["dsf"])
            P.op("dve", lambda e, n=n: e.tensor_copy(out=t["dest"][:, n, :], in_=t["dsf"][:]), reads=["dsf"], writes=[("dest", n)])
            P.op("dve", lambda e, n=n: e.tensor_copy(out=t["wgt"][:, n, :], in_=t["rtt"][:, 4:6]), reads=["rtt"], writes=[("wgt", n)])
            for j in range(2):
                P.dma(lambda e, n=n, j=j, ft=ft: e.indirect_dma_start(out=k.xs[:, :], out_offset=bass.IndirectOffsetOnAxis(ap=t["dest"][:, n, j:j + 1], axis=0), in_=ft[:, :], in_offset=None),
                      reads=[fk, ("dest", n), "xs"], writes=[("xsw", n, j)], q="pool", dkey=("sw_sc" if STRICT_SCATTER else ("sw_sc", j, n % 4)))
        xs_dep = [("xsw", n, j) for n in range(len(tl)) for j in range(2)]
        w1_rows = k.moe_w1.rearrange("l e r n -> (l e r) n")
        w3_rows = k.moe_w3.rearrange("l e r n -> (l e r) n")
        w2_rows = k.moe_w2.rearrange("l e r n -> (l e r) n")
        for jb in range(NBLK):
            sa, sb_, sc_ = t["stA%d" % (jb % 2)], t["stB%d" % (jb % 2)], t["stC%d" % (jb % 2)]
            ka, kb, kc = "stA%d" % (jb % 2), "stB%d" % (jb % 2), "stC%d" % (jb % 2)
            for kk in range(8):
                P.dma(lambda e, jb=jb, kk=kk, sa=sa: e.indirect_dma_start(out=sa[:, kk, :], out_offset=None, in_=w1_rows[:, :], in_offset=bass.IndirectOffsetOnAxis(ap=t["ix1"][:, jb, kk:kk + 1], axis=0)), reads=["ix1"], writes=[ka], q="pool", dkey=("sw_w", ka, kk))
                P.dma(lambda e, jb=jb, kk=kk, sb_=sb_: e.indirect_dma_start(out=sb_[:, kk, :], out_offset=None, in_=w3_rows[:, :], in_offset=bass.IndirectOffsetOnAxis(ap=t["ix1"][:, jb, kk:kk + 1], axis=0)), reads=["ix1"], writes=[kb], q="pool", dkey=("sw_w", kb, kk))
            for c in range(4):
                P.dma(lambda e, jb=jb, c=c, sc_=sc_: e.indirect_dma_start(out=sc_[:, c, :], out_offset=None, in_=w2_rows[:, :], in_offset=bass.IndirectOffsetOnAxis(ap=t["ix2"][:, jb, c:c + 1], axis=0)), reads=["ix2"], writes=[kc], q="pool", dkey=("sw_w", kc, c))
            P.op("act", lambda e, sa=sa: e.copy(out=t["w1b"][:], in_=sa[:]), reads=[ka], writes=["w1b"])
            P.op("act", lambda e, sb_=sb_: e.copy(out=t["w3b"][:], in_=sb_[:]), reads=[kb], writes=["w3b"])
            P.op("dve", lambda e, sc_=sc_: e.tensor_copy(out=t["w2b"][:], in_=sc_[:]), reads=[kc], writes=["w2b"])
            xb = t["xb%d" % (jb % 2)]
            xk = "xb%d" % (jb % 2)
            P.dma(lambda e, jb=jb, xb=xb: e.dma_start(out=xb[:], in_=xs_v[:, jb * 4:(jb + 1) * 4, :]), reads=["xs"] + xs_dep, writes=[xk])
            for sidx in range(4):
                for kk in range(8):
                    P.op("pe", lambda e, sidx=sidx, kk=kk, xb=xb: e.transpose(out=pstb[:, kk, :], in_=xb[:, sidx, kk * 128:(kk + 1) * 128], identity=t["idb"][:]), reads=[xk, "idb"], writes=["mpst"])
                P.op("dve", lambda e, sidx=sidx: e.tensor_copy(out=t["xT"][:, :, sidx * 128:(sidx + 1) * 128], in_=pstb[:, :, :]), reads=["mpst"], writes=["xT"])
            W = MOE_BS
            for ffc in range(4):
                pa, pb = p1[ffc % 2], p3[ffc % 2]
                kpa, kpb = "mp1_%d" % (ffc % 2), "mp3_%d" % (ffc % 2)
                for kk in range(8):
                    P.op("pe", lambda e, pa=pa, kk=kk, ffc=ffc: e.matmul(pa[:, 0:W], lhsT=t["w1b"][:, kk, ffc * 128:(ffc + 1) * 128], rhs=t["xT"][:, kk, :], start=(kk == 0), stop=(kk == 7)),
                         reads=["w1b", "xT"], writes=[kpa], skip_self=(kk > 0))
                for kk in range(8):
                    P.op("pe", lambda e, pb=pb, kk=kk, ffc=ffc: e.matmul(pb[:, 0:W], lhsT=t["w3b"][:, kk, ffc * 128:(ffc + 1) * 128], rhs=t["xT"][:, kk, :], start=(kk == 0), stop=(kk == 7)),
                         reads=["w3b", "xT"], writes=[kpb], skip_self=(kk > 0))
                P.op("act", lambda e, pa=pa: e.activation(out=t["s1"][:, 0:W], in_=pa[:, 0:W], func=AF.Silu), reads=[kpa], writes=["s1"])
                P.op("dve", lambda e, pb=pb, ffc=ffc: e.tensor_tensor(out=t["act"][:, ffc, 0:W], in0=pb[:, 0:W], in1=t["s1"][:, 0:W], op=ALU.mult), reads=[kpb, "s1"], writes=["act"])
            yb = t["yb%d" % (jb % 2)]
            yk = "yb%d" % (jb % 2)
            for sidx in range(4):
                for half in range(2):
                    ps = py[half]
                    pk = "mpy_%d" % half
                    for ffc in range(4):
                        P.op("pe", lambda e, ps=ps, ffc=ffc, sidx=sidx, half=half: e.matmul(ps[:, :], lhsT=t["act"][:, ffc, sidx * 128:(sidx + 1) * 128], rhs=t["w2b"][:, ffc, half * 512:(half + 1) * 512],
                                                                                      start=(ffc == 0), stop=(ffc == 3)), reads=["act", "w2b"], writes=[pk], skip_self=(ffc > 0))
                    if half == 0:
                        P.op("act", lambda e, ps=ps, sidx=sidx, yb=yb: e.copy(out=yb[:, sidx, 0:512], in_=ps[:, :]), reads=[pk], writes=[yk])
                    else:
                        P.op("dve", lambda e, ps=ps, sidx=sidx, yb=yb: e.tensor_copy(out=yb[:, sidx, 512:1024], in_=ps[:, :]), reads=[pk], writes=[yk])
            P.dma(lambda e, jb=jb, yb=yb: e.dma_start(out=ys_v[:, jb * 4:(jb + 1) * 4, :], in_=yb[:]), reads=[yk, "ys"], writes=[("ysw", jb)], dkey=("ysst", jb % 2))
        ys_dep = [("ysw", jb) for jb in range(NBLK)]
        for n, (b, i) in enumerate(tl):
            t0 = b * TB + i * 128
            row = b if i < 16 else NB
            if i == 0 or i == 16:
                P.dma(lambda e, row=row: e.dma_start(out=t["g2b"][:], in_=k.modrow[l][row, 5 * D:6 * D].partition_broadcast(128)), writes=["g2b"])
            for j, yn in ((0, "y1"), (1, "y2")):
                P.dma(lambda e, n=n, j=j, yn=yn: e.indirect_dma_start(out=t[yn][:, :], out_offset=None, in_=k.ys[:, :], in_offset=bass.IndirectOffsetOnAxis(ap=t["dest"][:, n, j:j + 1], axis=0)), reads=[("dest", n), "ys"] + ys_dep, writes=[yn], q="pool", dkey=("sw_y", yn))
            P.dma(lambda e, t0=t0: e.dma_start(out=t["xt"][:], in_=k.x1[t0:t0 + 128, :]), writes=["xt"])
            P.op("dve", lambda e, n=n: e.tensor_scalar(out=t["acc"][:], in0=t["y1"][:], scalar1=t["wgt"][:, n, 0:1], scalar2=None, op0=ALU.mult), reads=["y1", ("wgt", n)], writes=["acc"])
            P.op("dve", lambda e, n=n: e.scalar_tensor_tensor(out=t["acc"][:], in0=t["y2"][:], scalar=t["wgt"][:, n, 1:2], in1=t["acc"][:], op0=ALU.mult, op1=ALU.add), reads=["y2", ("wgt", n), "acc"], writes=["acc"])
            P.op("dve", lambda e: e.tensor_tensor(out=t["acc"][:], in0=t["acc"][:], in1=t["g2b"][:], op=ALU.mult), reads=["acc", "g2b"], writes=["acc"])
            P.op("dve", lambda e: e.tensor_tensor(out=t["xt"][:], in0=t["xt"][:], in1=t["acc"][:], op=ALU.add), reads=["xt", "acc"], writes=["xt"])
            if last:
                P.dma(lambda e, b=b, i=i: e.dma_start(out=k.out[b, i * 128:(i + 1) * 128, :], in_=t["xt"][:]), reads=["xt"], writes=[("outd", b, i)], dkey="outst")
            else:
                P.dma(lambda e, t0=t0: e.dma_start(out=k.x2[t0:t0 + 128, :], in_=t["xt"][:]), reads=["xt"], writes=[("x2d", t0)], dkey="outst")
        P.barrier()
        P.emit()


def stage_moe(k, l):
    nc, P, NB = k.nc, k.P, k.NB
    last = (l == DEPTH - 1)
    ntile = 16 if last else 18
    blocks = BLK5[:4] if last else BLK5
    with ExitStack() as es:
        t = _tiles(es, nc, [("fTb", (128, 8, TB), BF16), ("acc", (128, 18, D), F32), ("cwb", (128, 18, 32), F32),
                            ("st0", (128, 8, 512), F32), ("st1", (128, 8, 512), F32),
                            ("w1b", (128, 8, 512), BF16), ("w3b", (128, 8, 512), BF16), ("w2b", (128, 4, D), BF16),
                            ("s1", (128, 512), F32), ("act", (128, 4, 512), BF16), ("g2b", (128, D), F32), ("xt", (128, D), F32)])
        p1 = [_psum(es, nc, "mp1_%d" % i, [128, 512], F32) for i in range(2)]
        p3 = [_psum(es, nc, "mp3_%d" % i, [128, 512], F32) for i in range(2)]
        py = [_psum(es, nc, "mpy_%d" % i, [128, 512], F32) for i in range(2)]
        fT_v = k.fT.rearrange("(c p) t -> p c t", p=128)
        cw_v = k.cw.rearrange("(n p) e -> p n e", p=128)
        sc = 0
        for b in range(NB):
            nb_tok = ntile * 128
            for c in range(8):
                P.dma(lambda e, b=b, c=c: e.dma_start(out=t["fTb"][:, c, 0:nb_tok], in_=fT_v[:, c, b * TB:b * TB + nb_tok]), writes=["fTb"])
            P.dma(lambda e, b=b: e.dma_start(out=t["cwb"][:, 0:ntile, :], in_=cw_v[:, b * 18:b * 18 + ntile, :]), writes=["cwb"])
            for ex in range(32):
                for nm, src, dst in (("w1", k.moe_w1, "w1b"), ("w3", k.moe_w3, "w3b")):
                    st = t["st%d" % (sc % 2)]
                    sk = "st%d" % (sc % 2)
                    sc += 1
                    P.dma(lambda e, st=st, src=src, ex=ex: e.dma_start(out=st[:], in_=src[l][ex].rearrange("(c p) n -> p c n", p=128)), writes=[sk])
                    P.op("pool", lambda e, st=st, dst=dst: e.tensor_copy(out=t[dst][:], in_=st[:]), reads=[sk], writes=[dst])
                st = t["st%d" % (sc % 2)]
                sk = "st%d" % (sc % 2)
                sc += 1
                stv = st[:].rearrange("p a b -> p (a b)").rearrange("p (c n) -> p c n", c=4)
                P.dma(lambda e, stv=stv, ex=ex: e.dma_start(out=stv, in_=k.moe_w2[l][ex].rearrange("(c p) n -> p c n", p=128)), writes=[sk])
                P.op("pool", lambda e, stv=stv: e.tensor_copy(out=t["w2b"][:], in_=stv), reads=[sk], writes=["w2b"])
                for (g0, W) in blocks:
                    for ffc in range(4):
                        pa, pb = p1[ffc % 2], p3[ffc % 2]
                        ka, kb = "mp1_%d" % (ffc % 2), "mp3_%d" % (ffc % 2)
                        for kk in range(8):
                            P.op("pe", lambda e, pa=pa, kk=kk, ffc=ffc, g0=g0, W=W: e.matmul(pa[:, 0:W], lhsT=t["w1b"][:, kk, ffc * 128:(ffc + 1) * 128], rhs=t["fTb"][:, kk, g0:g0 + W], start=(kk == 0), stop=(kk == 7)),
                                 reads=["w1b", "fTb"], writes=[ka], skip_self=(kk > 0))
                        for kk in range(8):
                            P.op("pe", lambda e, pb=pb, kk=kk, ffc=ffc, g0=g0, W=W: e.matmul(pb[:, 0:W], lhsT=t["w3b"][:, kk, ffc * 128:(ffc + 1) * 128], rhs=t["fTb"][:, kk, g0:g0 + W], start=(kk == 0), stop=(kk == 7)),
                                 reads=["w3b", "fTb"], writes=[kb], skip_self=(kk > 0))
                        P.op("act", lambda e, pa=pa, W=W: e.activation(out=t["s1"][:, 0:W], in_=pa[:, 0:W], func=AF.Silu), reads=[ka], writes=["s1"])
                        P.op("dve", lambda e, pb=pb, W=W, ffc=ffc: e.tensor_tensor(out=t["act"][:, ffc, 0:W], in0=pb[:, 0:W], in1=t["s1"][:, 0:W], op=ALU.mult), reads=[kb, "s1"], writes=["act"])
                    for sidx in range(W // 128):
                        ti = g0 // 128 + sidx
                        for half in range(2):
                            ps = py[half]
                            pk = "mpy_%d" % half
                            for ffc in range(4):
                                P.op("pe", lambda e, ps=ps, ffc=ffc, sidx=sidx, half=half: e.matmul(ps[:, :], lhsT=t["act"][:, ffc, sidx * 128:(sidx + 1) * 128], rhs=t["w2b"][:, ffc, half * 512:(half + 1) * 512],
                                                                                              start=(ffc == 0), stop=(ffc == 3)), reads=["act", "w2b"], writes=[pk], skip_self=(ffc > 0))
                            dst = t["acc"][:, ti, half * 512:(half + 1) * 512]
                            if ex == 0:
                                P.op("dve", lambda e, ps=ps, dst=dst, ti=ti, ex=ex: e.tensor_scalar(out=dst, in0=ps[:, :], scalar1=t["cwb"][:, ti, ex:ex + 1], scalar2=None, op0=ALU.mult), reads=[pk, "cwb"], writes=["acc"])
                            else:
                                P.op("dve", lambda e, ps=ps, dst=dst, ti=ti, ex=ex: e.scalar_tensor_tensor(out=dst, in0=ps[:, :], scalar=t["cwb"][:, ti, ex:ex + 1], in1=dst, op0=ALU.mult, op1=ALU.add),
                                     reads=[pk, "cwb", "acc"], writes=["acc"])
            for i in range(ntile):
                t0 = b * TB + i * 128
                row = b if i < 16 else NB
                if i == 0 or i == 16:
                    P.dma(lambda e, row=row: e.dma_start(out=t["g2b"][:], in_=k.modrow[l][row, 5 * D:6 * D].partition_broadcast(128)), writes=["g2b"])
                P.dma(lambda e, t0=t0: e.dma_start(out=t["xt"][:], in_=k.x1[t0:t0 + 128, :]), writes=["xt"])
                P.op("pool", lambda e, i=i: e.tensor_tensor(out=t["acc"][:, i, :], in0=t["acc"][:, i, :], in1=t["g2b"][:], op=ALU.mult), reads=["acc", "g2b"], writes=["acc"])
                P.op("dve", lambda e, i=i: e.tensor_tensor(out=t["xt"][:], in0=t["xt"][:], in1=t["acc"][:, i, :], op=ALU.add), reads=["xt", "acc"], writes=["xt"])
                if last:
                    P.dma(lambda e, b=b, i=i: e.dma_start(out=k.out[b, i * 128:(i + 1) * 128, :], in_=t["xt"][:]), reads=["xt"], writes=[("outd", b, i)], dkey="outst")
                else:
                    P.dma(lambda e, t0=t0: e.dma_start(out=k.x2[t0:t0 + 128, :], in_=t["xt"][:]), reads=["xt"], writes=[("x2d", t0)], dkey="outst")
        P.barrier()
        P.emit()


def build_program(NB, n_stages=99, dbg=()):
    nc = bass.Bass("TRN2", target_bir_lowering=False)
    k = K()
    k.nc, k.NB = nc, NB
    k.P = Prog(nc)
    T = NB * TB
    k.T = T

    def din(name, shape, dt=F32):
        return nc.dram_tensor(name, list(shape), dt, kind="ExternalInput").ap()

    def dscr(name, shape, dt, out=False):
        return nc.dram_tensor(name, list(shape), dt, kind=("ExternalOutput" if out else "Internal")).ap()

    k.x = din("x", (NB, SEQ, D)); k.ctx = din("ctx", (NB, NCTX, D)); k.c = din("c", (NB, D)); k.c_ctx = din("c_ctx", (D,))
    k.w_mod = din("w_mod", (DEPTH, D, 6 * D)); k.b_mod = din("b_mod", (DEPTH, 6 * D))
    k.norm1_g = din("norm1_g", (DEPTH, D)); k.norm2_g = din("norm2_g", (DEPTH, D))
    k.w_in = din("w_in", (DEPTH, D, INC))
    k.identf = din("identf", (128, 128))
    k.mla_q_norm_g = din("mla_q_norm_g", (DEPTH, 192)); k.mla_w_uq = din("mla_w_uq", (DEPTH, 192, 384))
    k.mla_kv_norm_g = din("mla_kv_norm_g", (DEPTH, 128)); k.mla_w_ukv = din("mla_w_ukv", (DEPTH, 128, 512))
    k.mla_qn_g = din("mla_qn_g", (DEPTH, 96)); k.mla_kn_g = din("mla_kn_g", (DEPTH, 96))
    k.ret_log_gamma = din("ret_log_gamma", (DEPTH, 2, 4)); k.ret_norm_g = din("ret_norm_g", (DEPTH, 256))
    k.lru_conv_w = din("lru_conv_w", (DEPTH, 4, 256)); k.lru_conv_b = din("lru_conv_b", (DEPTH, 256))
    k.lru_wa = din("lru_wa", (DEPTH, 2, 4, 64, 64)); k.lru_ba = din("lru_ba", (DEPTH, 2, 256))
    k.lru_wx = din("lru_wx", (DEPTH, 2, 4, 64, 64)); k.lru_bx = din("lru_bx", (DEPTH, 2, 256)); k.lru_lambda = din("lru_lambda", (DEPTH, 2, 256))
    k.hy_conv_w = din("hy_conv_w", (DEPTH, 3, 768)); k.hy_conv_b = din("hy_conv_b", (DEPTH, 768))
    k.hy_w1 = din("hy_w1", (DEPTH, 33, 64)); k.hy_b1 = din("hy_b1", (DEPTH, 64)); k.hy_w2 = din("hy_w2", (DEPTH, 64, 64)); k.hy_b2 = din("hy_b2", (DEPTH, 64))
    k.hy_w3 = din("hy_w3", (DEPTH, 64, 512)); k.hy_freq = din("hy_freq", (DEPTH, 64)); k.hy_d = din("hy_d", (DEPTH, 256))
    k.c_m0 = din("c_m0", (128, 2))
    k.hc = {}
    for Lh in (SEQ, NCTX):
        k.hc[Lh] = {"zf": din("hz_f%d" % Lh, (33, Lh)), "zr": din("hz_r%d" % Lh, (33, Lh)), "tf": din("ht_f%d" % Lh, (128, Lh // 128)), "tr": din("ht_r%d" % Lh, (128, Lh // 128)),
                    "dl": din("h_dl%d" % Lh, (128, 256)), "Wf": din("h_Wf%d" % Lh, (Lh, 2 * Lh), BF16), "Winv": din("h_Wi%d" % Lh, (2 * Lh, Lh), BF16),
                    "csl": din("h_csl%d" % Lh, (128, 2 * Lh // 128)), "csh": din("h_csh%d" % Lh, (128, 2 * Lh // 128))}
    k.group_norm_g = din("group_norm_g", (DEPTH, D)); k.w_out = din("w_out", (DEPTH, D, D))
    k.moe_w_group = din("moe_w_group", (DEPTH, D, 4)); k.moe_w_expert = din("moe_w_expert", (DEPTH, D, 32))
    k.moe_w1 = din("moe_w1", (DEPTH, 32, D, 512)); k.moe_w3 = din("moe_w3", (DEPTH, 32, D, 512)); k.moe_w2 = din("moe_w2", (DEPTH, 32, 512, D))
    k.x1 = dscr("x1", (T, D), F32, out=("x1" in dbg)); k.fT = dscr("fT", (D, T), BF16); k.cw = dscr("cw", (T, 32), F32, out=("cw" in dbg))
    k.out = nc.dram_tensor("out", [NB, SEQ, D], F32, kind="ExternalOutput").ap()
    k.c_iota32 = din("c_iota32", (128, 32)); k.c_LT = din("c_LT", (128, 128)); k.c_ONES = din("c_ONES", (128, 128)); k.c_jbv = din("c_jbv", (128, 128))
    k.c_iotaA = din("c_iotaA", (128, 8)); k.c_iotaB = din("c_iotaB", (128, 4))
    k.nblk = [-(-(2 * NB * nt_ * 128) // MOE_BS) + 32 for nt_ in (18, 16)]
    assert max(k.nblk) <= MAXBLK
    k.rt = dscr("rt", (T, 8), F32); k.ftm = dscr("ftm", (T, D), BF16)
    k.pstart = dscr("pstart", (DEPTH, 128, 32), F32); k.bexp = dscr("bexp", (DEPTH, 128, MAXBLK), F32)
    k.xs = dscr("xs", (max(k.nblk) * MOE_BS, D), BF16); k.ys = dscr("ys", (max(k.nblk) * MOE_BS, D), BF16)
    k.rope_cos = din("rope_cos", (SEQ, 16)); k.rope_sin = din("rope_sin", (SEQ, 16))
    k.c_rel0 = din("c_rel0", (128, 128)); k.c_mge = din("c_mge", (128, 128)); k.c_mle = din("c_mle", (128, 128)); k.c_dvals = din("c_dvals", (128, 18))
    k.yT = dscr("yT", (D, T), BF16, out=("yT" in dbg))
    k.cc = 0; k.oc = 0; k.yc = 0
    k.modrow = dscr("modrow", (DEPTH, NB + 1, 6 * D), F32, out=("modrow" in dbg))
    k.zt = dscr("zt", (T, ZT_W), BF16, out=("zt" in dbg))
    k.zf = dscr("zf", (ZF_ROWS, T), BF16, out=("zf" in dbg))
    k.x2 = dscr("x2", (T, D), F32)
    with nc.allow_low_precision("bf16 matmul operands, fp32 accumulation"), nc.allow_non_contiguous_dma("small strided loads"):
        stage_mod(k)
        stages = []
        for l in range(DEPTH):
            stages += [lambda l=l: stage_inproj(k, l), lambda l=l: stage_mla(k, l), lambda l=l: stage_ret(k, l), lambda l=l: stage_lru(k, l),
                       lambda l=l: stage_hyena(k, l, SEQ, 0)]
            if l < DEPTH - 1:
                stages.append(lambda l=l: stage_hyena(k, l, NCTX, SEQ))
            stages += [lambda l=l: stage_outproj(k, l), lambda l=l: (stage_moe_sparse(k, l) if SPARSE_MOE else stage_moe(k, l))]
        for si, f in enumerate(stages[:max(0, n_stages - 1)]):
            with nc.named_scope("st%02d" % si):
                f()
    return nc, k


def host_consts():
    c = {"identf": np.eye(128, dtype=np.float32)}
    rows = SEQ // 64
    row = np.repeat(np.arange(rows), 64).astype(np.float32)
    col = np.tile(np.arange(64), rows).astype(np.float32)
    inv_freq = (10000.0 ** (-np.arange(8, dtype=np.float32) / 8)).astype(np.float32)
    ang = np.stack([row[:, None] * inv_freq, col[:, None] * inv_freq], axis=1).astype(np.float32)
    c["rope_cos"] = np.cos(ang).reshape(SEQ, 16).astype(np.float32)
    c["rope_sin"] = np.sin(ang).reshape(SEQ, 16).astype(np.float32)
    jl = np.arange(128, dtype=np.float32)[:, None]
    cc = np.arange(128, dtype=np.float32)[None, :]
    c["c_rel0"] = (cc - jl).astype(np.float32)
    c["c_mge"] = (cc >= jl).astype(np.float32)
    c["c_mle"] = (cc <= jl).astype(np.float32)
    m0 = np.ones((128, 2), np.float32); m0[0, 0] = 0.0; m0[:, 1] = 1.0 - m0[:, 0]
    c["c_m0"] = m0
    names = {"zf": "hz_f", "zr": "hz_r", "tf": "ht_f", "tr": "ht_r", "dl": "h_dl", "Wf": "h_Wf", "Winv": "h_Wi", "csl": "h_csl", "csh": "h_csh"}
    for Lh in (SEQ, NCTX):
        hcn = hyena_consts(Lh)
        for kk2, v in hcn.items():
            c[names[kk2] + str(Lh)] = v
    c["c_iota32"] = np.broadcast_to(np.arange(32, dtype=np.float32)[None, :], (128, 32)).copy()
    tt_ = np.arange(128)
    c["c_LT"] = (tt_[:, None] < tt_[None, :]).astype(np.float32)
    c["c_ONES"] = np.ones((128, 128), np.float32)
    c["c_jbv"] = np.broadcast_to((512.0 * np.arange(128, dtype=np.float32))[None, :], (128, 128)).copy()
    c["c_iotaA"] = (np.arange(8, dtype=np.float32)[None, :] * 128 + np.arange(128, dtype=np.float32)[:, None]).astype(np.float32)
    c["c_iotaB"] = (np.arange(4, dtype=np.float32)[None, :] * 128 + np.arange(128, dtype=np.float32)[:, None]).astype(np.float32)
    c["c_dvals"] = np.broadcast_to(128.0 * np.arange(18, dtype=np.float32)[None, :], (128, 18)).astype(np.float32).copy()
    return c


IN_NAMES = ["w_mod", "b_mod", "norm1_g", "norm2_g", "w_in", "mla_q_norm_g", "mla_w_uq", "mla_kv_norm_g", "mla_w_ukv", "mla_qn_g", "mla_kn_g",
            "ret_log_gamma", "ret_norm_g", "lru_conv_w", "lru_conv_b", "lru_wa", "lru_ba", "lru_wx", "lru_bx", "lru_lambda",
            "hy_conv_w", "hy_conv_b", "hy_w1", "hy_b1", "hy_w2", "hy_b2", "hy_w3", "hy_freq", "hy_d",
            "group_norm_g", "w_out", "moe_w_group", "moe_w_expert", "moe_w1", "moe_w3", "moe_w2"]


def make_in_map(inp, b0, NB, consts=None):
    consts = consts if consts is not None else host_consts()
    m = {"x": np.ascontiguousarray(inp["x"][b0:b0 + NB]), "ctx": np.ascontiguousarray(inp["ctx"][b0:b0 + NB]),
         "c": np.ascontiguousarray(inp["c"][b0:b0 + NB]), "c_ctx": np.asarray(inp["c_ctx"])}
    for n in IN_NAMES:
        m[n] = np.asarray(inp[n])
    m.update(consts)
    return m


_CACHE = {}


def kernel(**inputs):
    NB = 4
    n_cores = 8
    if "nc" not in _CACHE:
        _CACHE["nc"] = build_program(NB)[0]
        _CACHE["consts"] = host_consts()
    nc = _CACHE["nc"]
    in_maps = [make_in_map(inputs, i * NB, NB, _CACHE["consts"]) for i in range(n_cores)]
    res = run_bass_kernel_spmd(nc, in_maps, core_ids=list(range(n_cores)))
    return np.concatenate([np.asarray(r["out"]) for r in res.results], axis=0).astype(np.float32)
```

```python
import math
from contextlib import ExitStack
import numpy as np
import ml_dtypes
import concourse.bass as bass
import concourse.mybir as mybir
from concourse.bass_utils import run_bass_kernel_spmd

F32 = mybir.dt.float32
BF16 = mybir.dt.bfloat16
AF = mybir.ActivationFunctionType
ALU = mybir.AluOpType
AX = mybir.AxisListType
ENGS = ("pe", "act", "dve", "pool", "sp")

D = 1024
SEQ = 2048
NCTX = 256
TB = SEQ + NCTX
DEPTH = 2
EPS = 1e-6
INC = 2656
SPARSE_MOE = True
STRICT_SCATTER = False


class Prog:
    def __init__(self, nc):
        self.nc = nc
        self.q = {e: [] for e in ENGS}
        self.cnt = {e: 0 for e in ENGS}
        self.known = {e: {} for e in ENGS}
        self.w = {}
        self.r = {}
        self.dsem = {}
        self.semobj = {}
        for e in ENGS:
            self.semobj[("c", e)] = nc.alloc_semaphore(name="c_" + e)
        self.out_waits = {}
        self.n_ins = 0
        self.n_wait = 0
        self.free_hw = []
        self.free_sw = []
        self.nsem = 0

    def _deps(self, e, reads, writes, skip_self):
        deps = {}
        for k in reads:
            for s, v in self.w.get(k, {}).items():
                if deps.get(s, -1) < v:
                    deps[s] = v
        for k in writes:
            for d in (self.w.get(k, {}), self.r.get(k, {})):
                for s, v in d.items():
                    if deps.get(s, -1) < v:
                        deps[s] = v
        waits = []
        kn = self.known[e]
        for s, v in deps.items():
            if skip_self and s == ("c", e):
                continue
            if kn.get(s, 0) >= v:
                continue
            kn[s] = v
            waits.append((s, v))
        return waits

    def _update(self, me, reads, writes):
        s, v = me
        for k in writes:
            if self.r.get(k):
                self.w[k] = {s: v}
                self.r[k] = {}
            else:
                self.w.setdefault(k, {})[s] = v
        for k in reads:
            self.r.setdefault(k, {})[s] = v

    def op(self, e, fn, reads=(), writes=(), skip_self=False):
        waits = self._deps(e, reads, writes, skip_self)
        self.cnt[e] += 1
        self._update((("c", e), self.cnt[e]), reads, writes)
        self.q[e].append((waits, fn, (("c", e), 1)))
        self.n_ins += 1
        self.n_wait += len(waits)

    def dma(self, fn, reads=(), writes=(), dkey=None, q="sp", is_output=False):
        if dkey is None:
            dkey = writes[0]
        waits = self._deps(q, reads, writes, False)
        if dkey not in self.dsem:
            pool = self.free_sw if q == "pool" else self.free_hw
            if pool:
                ent = pool.pop()
            else:
                self.nsem += 1
                ent = [("d", self.nsem), 0, q == "pool"]
                self.semobj[ent[0]] = self.nc.alloc_semaphore(name="d%d" % self.nsem)
            self.dsem[dkey] = ent
        ent = self.dsem[dkey]
        assert ent[2] == (q == "pool"), "DMA semaphore shared between software and hardware DGE: %r" % (dkey,)
        if ent[2] and ent[1] > 0 and self.known[q].get(ent[0], 0) < ent[1]:
            self.known[q][ent[0]] = ent[1]
            waits.append((ent[0], ent[1]))
        ent[1] += 16
        self._update((ent[0], ent[1]), reads, writes)
        self.q[q].append((waits, fn, (ent[0], 16)))
        if is_output:
            self.out_waits[ent[0]] = ent[1]
        self.n_ins += 1
        self.n_wait += len(waits)

    def barrier(self):
        allv = {("c", e): self.cnt[e] for e in ENGS if self.cnt[e] > 0}
        for ent in list(self.dsem.values()) + self.free_hw + self.free_sw:
            if ent[1] > 0:
                allv[ent[0]] = ent[1]
        for e in ENGS:
            kn = self.known[e]
            waits = []
            for s, v in allv.items():
                if kn.get(s, 0) < v:
                    kn[s] = v
                    waits.append((s, v))
            if waits:
                self.q[e].append((waits, None, None))
        self.w = {}
        self.r = {}
        for ent in self.dsem.values():
            (self.free_sw if ent[2] else self.free_hw).append(ent)
        self.dsem = {}

    def emit(self):
        nc = self.nc
        final = list(self.out_waits.items())
        with nc.Block() as block:
            def run(e):
                def body(eng):
                    for waits, fn, inc in self.q[e]:
                        for s, v in waits:
                            eng.wait_ge(self.semobj[s], v)
                        if fn is not None:
                            fn(eng).then_inc(self.semobj[inc[0]], inc[1])
                    if e == "sp":
                        for s, v in final:
                            eng.wait_ge(self.semobj[s], v)
                return body
            block.tensor(run("pe"))
            block.scalar(run("act"))
            block.vector(run("dve"))
            block.gpsimd(run("pool"))
            block.sync(run("sp"))
        self.q = {e: [] for e in ENGS}
        self.out_waits = {}


class Rec:
    def __init__(self, sfx, local):
        self.ops, self.sfx, self.local = [], sfx, local

    def _k(self, keys):
        return [(x + self.sfx) if (isinstance(x, str) and x in self.local) else x for x in keys]

    def op(self, e, fn, reads=(), writes=(), skip_self=False):
        self.ops.append((0, e, fn, self._k(reads), self._k(writes), skip_self))

    def dma(self, fn, reads=(), writes=(), dkey=None, q="sp", is_output=False):
        if dkey is None:
            dkey = writes[0]
        self.ops.append((1, fn, self._k(reads), self._k(writes), self._k([dkey])[0], q, is_output))


def replay(P, recs):
    idx = [0] * len(recs)
    live = True
    while live:
        live = False
        for j, r in enumerate(recs):
            if idx[j] < len(r.ops):
                o = r.ops[idx[j]]
                idx[j] += 1
                live = True
                if o[0] == 0:
                    P.op(o[1], o[2], reads=o[3], writes=o[4], skip_self=o[5])
                else:
                    P.dma(o[1], reads=o[2], writes=o[3], dkey=o[4], q=o[5], is_output=o[6])


class K:
    pass


_UID = [0]


def _u(name):
    _UID[0] += 1
    return "%s_%d" % (name, _UID[0])


def _tiles(es, nc, specs):
    out = {}
    for name, shape, dt in specs:
        out[name] = es.enter_context(nc.sbuf_tensor(_u(name), list(shape), dt))
    return out


def _psum(es, nc, name, shape, dt):
    return es.enter_context(nc.psum_tensor(_u(name), list(shape), dt))


def stage_mod(k):
    nc, P, NB = k.nc, k.P, k.NB
    R = NB + 1
    with ExitStack() as es:
        t = _tiles(es, nc, [("crow", (R, D), F32), ("srow", (R, D), F32), ("scT", (128, 8, R), F32),
                            ("wm0", (128, 8, 512), F32), ("wm1", (128, 8, 512), F32),
                            ("brow", (R, 6 * D), F32), ("mrow", (R, 6 * D), F32), ("idf", (128, 128), F32)])
        pt = _psum(es, nc, "pt0", [128, 512], F32)
        pm = [_psum(es, nc, "pm%d" % i, [128, 512], F32) for i in range(2)]
        P.dma(lambda e: e.dma_start(out=t["idf"][:], in_=k.identf), writes=["idf"])
        P.dma(lambda e: e.dma_start(out=t["crow"][0:NB, :], in_=k.c), writes=["crow"])
        P.dma(lambda e: e.dma_start(out=t["crow"][NB:R, :], in_=k.c_ctx.rearrange("(o d) -> o d", o=1)), writes=["crow"])
        P.op("act", lambda e: e.activation(out=t["srow"][:], in_=t["crow"][:], func=AF.Silu), reads=["crow"], writes=["srow"])
        for kk in range(8):
            P.op("pe", lambda e, kk=kk: e.transpose(out=pt[:, 0:R], in_=t["srow"][:, kk * 128:(kk + 1) * 128], identity=t["idf"][0:R, 0:R]),
                 reads=["srow", "idf"], writes=["pt"])
            P.op("dve", lambda e, kk=kk: e.tensor_copy(out=t["scT"][:, kk, :], in_=pt[:, 0:R]), reads=["pt"], writes=["scT"])
        for l in range(DEPTH):
            P.dma(lambda e, l=l: e.dma_start(out=t["brow"][:], in_=k.b_mod[l].partition_broadcast(R)), writes=["brow"])
            for n in range(12):
                wt = t["wm%d" % (n % 2)]
                wk = "wm%d" % (n % 2)
                P.dma(lambda e, l=l, n=n, wt=wt: e.dma_start(out=wt[:], in_=k.w_mod[l][:, n * 512:(n + 1) * 512].rearrange("(k p) n -> p k n", p=128)),
                      writes=[wk], q="sp")
                ps = pm[n % 2]
                pk = "pm%d" % (n % 2)
                for kk in range(8):
                    P.op("pe", lambda e, kk=kk, wt=wt, ps=ps: e.matmul(ps[0:R, :], lhsT=t["scT"][:, kk, :], rhs=wt[:, kk, :], start=(kk == 0), stop=(kk == 7)),
                         reads=["scT", wk], writes=[pk], skip_self=(kk > 0))
                P.op("dve", lambda e, n=n, ps=ps: e.tensor_tensor(out=t["mrow"][:, n * 512:(n + 1) * 512], in0=ps[0:R, :], in1=t["brow"][:, n * 512:(n + 1) * 512], op=ALU.add),
                     reads=[pk, "brow"], writes=["mrow"])
            P.dma(lambda e, l=l: e.dma_start(out=k.modrow[l], in_=t["mrow"][:]), reads=["mrow"], writes=[("modrow", l)])
        P.barrier()
        P.emit()


def load_mod_cols(k, es, l, which):
    nc, P, NB = k.nc, k.P, k.NB
    R = NB + 1
    A = es.enter_context(nc.sbuf_tensor(_u("modA"), [128, 8, R], F32))
    B = es.enter_context(nc.sbuf_tensor(_u("modB"), [128, 8, R], F32))
    with ExitStack() as e2:
        t = _tiles(e2, nc, [("mr", (R + 1, 2 * D), F32), ("gT", (128, 8, R + 1), F32), ("idf2", (128, 128), F32)])
        pt = _psum(e2, nc, "ptm", [128, 512], F32)
        base = 0 if which == 0 else 3 * D
        g = k.norm1_g if which == 0 else k.norm2_g
        P.dma(lambda e: e.dma_start(out=t["idf2"][:], in_=k.identf), writes=["idf2"])
        P.dma(lambda e: e.dma_start(out=t["mr"][0:R, :], in_=k.modrow[l][:, base:base + 2 * D]), reads=[("modrow", l)], writes=["mr"])
        P.dma(lambda e: e.dma_start(out=t["mr"][R:R + 1, 0:D], in_=g[l].rearrange("(o d) -> o d", o=1)), writes=["mr"])
        P.dma(lambda e: e.dma_start(out=t["mr"][R:R + 1, D:2 * D], in_=g[l].rearrange("(o d) -> o d", o=1)), writes=["mr"])
        for half, dst in ((0, B), (1, A)):
            for kk in range(8):
                c0 = half * D + kk * 128
                P.op("pe", lambda e, c0=c0: e.transpose(out=pt[:, 0:R + 1], in_=t["mr"][:, c0:c0 + 128], identity=t["idf2"][0:R + 1, 0:R + 1]),
                     reads=["mr", "idf2"], writes=["ptm"])
                if half == 0:
                    P.op("dve", lambda e, kk=kk: e.tensor_copy(out=B[:, kk, :], in_=pt[:, 0:R]), reads=["ptm"], writes=["modB"])
                else:
                    P.op("dve", lambda e, kk=kk: e.tensor_copy(out=t["gT"][:, kk, :], in_=pt[:, 0:R + 1]), reads=["ptm"], writes=["gT"])
                    P.op("dve", lambda e, kk=kk: e.tensor_scalar(out=A[:, kk, :], in0=t["gT"][:, kk, 0:R], scalar1=1.0, scalar2=t["gT"][:, kk, R:R + 1],
                                                               op0=ALU.add, op1=ALU.mult), reads=["gT"], writes=["modA"])
        P.barrier()
    return A, B


def xsrc(k, l, b, i):
    if l == 0:
        if i < 16:
            return k.x[b, i * 128:(i + 1) * 128, :]
        return k.ctx[b, (i - 16) * 128:(i - 15) * 128, :]
    t0 = b * TB + i * 128
    return k.x2[t0:t0 + 128, :]


def norm_mod_T(k, t, pst, xt_ap, xkey, A, B, b, dstT, dkeyT, col0, tag):
    P = k.P
    ss, rstd, xn = t["ss" + tag], t["rstd" + tag], t["xn" + tag]
    P.op("act", lambda e: e.activation(out=t["junk" + tag][:], in_=xt_ap, func=AF.Square, accum_out=ss[:]), reads=[xkey], writes=["junk" + tag, "ss" + tag])
    P.op("act", lambda e: e.activation(out=ss[:], in_=ss[:], func=AF.Sqrt, scale=1.0 / D, bias=EPS), reads=["ss" + tag], writes=["ss" + tag])
    P.op("dve", lambda e: e.reciprocal(out=rstd[:], in_=ss[:]), reads=["ss" + tag], writes=["rstd" + tag])
    P.op("dve", lambda e: e.tensor_scalar(out=xn[:], in0=xt_ap, scalar1=rstd[:], scalar2=None, op0=ALU.mult), reads=[xkey, "rstd" + tag], writes=["xn" + tag])
    for kk in range(8):
        P.op("pe", lambda e, kk=kk: e.transpose(out=pst[:, kk, :], in_=xn[:, kk * 128:(kk + 1) * 128], identity=t["idb"][:]),
             reads=["xn" + tag, "idb"], writes=["pst"])
    for kk in range(8):
        eng = "act" if kk % 2 == 0 else "dve"
        if eng == "act":
            P.op("act", lambda e, kk=kk: e.activation(out=dstT[:, kk, col0:col0 + 128], in_=pst[:, kk, :], func=AF.Identity,
                                                     scale=A[:, kk, b:b + 1], bias=B[:, kk, b:b + 1]), reads=["pst", "modA", "modB"], writes=[dkeyT])
        else:
            P.op("dve", lambda e, kk=kk: e.tensor_scalar(out=dstT[:, kk, col0:col0 + 128], in0=pst[:, kk, :], scalar1=A[:, kk, b:b + 1], scalar2=B[:, kk, b:b + 1],
                                                        op0=ALU.mult, op1=ALU.add), reads=["pst", "modA", "modB"], writes=[dkeyT])


TM_BLOCKS = ((0, 352, 0), (608, 512, 352), (1120, 256, 864))
ZT_W = 1120
FM_COLS = [352, 480, 608, 736] + [1376 + 128 * i for i in range(6)] + [2144, 2272, 2400, 2528]
ZF_ROWS = 128 * len(FM_COLS)
ZF_RQ, ZF_RK, ZF_ZH, ZF_LX, ZF_LG = 0, 256, 512, 1280, 1536


def stage_inproj(k, l):
    nc, P, NB = k.nc, k.P, k.NB
    with ExitStack() as es:
        A, B = load_mod_cols(k, es, l, 0)
        t = _tiles(es, nc, [("win", (128, 8, INC), BF16), ("wst0", (128, INC), F32), ("wst1", (128, INC), F32),
                            ("idf", (128, 128), F32), ("idb", (128, 128), BF16),
                            ("xt0", (128, D), F32), ("xt1", (128, D), F32),
                            ("junk0", (128, D), BF16), ("junk1", (128, D), BF16),
                            ("ss0", (128, 1), F32), ("ss1", (128, 1), F32), ("rstd0", (128, 1), F32), ("rstd1", (128, 1), F32),
                            ("xn0", (128, D), BF16), ("xn1", (128, D), BF16),
                            ("aT0", (128, 8, 512), BF16), ("aT1", (128, 8, 512), BF16),
                            ("zf0", (128, 14, 512), BF16), ("zf1", (128, 14, 512), BF16),
                            ("zt0", (128, ZT_W), BF16), ("zt1", (128, ZT_W), BF16)])
        pf = [_psum(es, nc, "pf%d" % i, [128, 512], F32) for i in range(3)]
        psl = [(_psum(es, nc, "pst", [128, 8, 128], BF16), _psum(es, nc, "pq", [128, 512], F32)) for _ in range(2)]
        P.dma(lambda e: e.dma_start(out=t["idf"][:], in_=k.identf), writes=["idf"])
        P.op("dve", lambda e: e.tensor_copy(out=t["idb"][:], in_=t["idf"][:]), reads=["idf"], writes=["idb"])
        for kk in range(8):
            ws = t["wst%d" % (kk % 2)]
            wk = "wst%d" % (kk % 2)
            P.dma(lambda e, kk=kk, ws=ws: e.dma_start(out=ws[:], in_=k.w_in[l][kk * 128:(kk + 1) * 128, :]), writes=[wk], q="sp")
            P.op("pool", lambda e, kk=kk, ws=ws: e.tensor_copy(out=t["win"][:, kk, :], in_=ws[:]), reads=[wk], writes=["win"])
        zf_v = k.zf.rearrange("(c p) t -> p c t", p=128)
        gi = 0
        ti = 0
        for b in range(NB):
            for (g0, W) in ((0, 512), (512, 512), (1024, 512), (1536, 512), (2048, 256)):
                aT = t["aT%d" % (gi % 2)]
                ak = "aT%d" % (gi % 2)
                nt = W // 128
                def tile_chain(j, ti, pst, pq):
                    P = k.P
                    tag = str(ti % 2)
                    i = g0 // 128 + j
                    P.dma(lambda e, b=b, i=i, tag=tag: e.dma_start(out=t["xt" + tag][:], in_=xsrc(k, l, b, i)),
                          reads=([("x2", b, i)] if l > 0 else []), writes=["xt" + tag], q="sp")
                    norm_mod_T(k, t, pst, t["xt" + tag][:], "xt" + tag, A, B, (b if i < 16 else NB), aT, (ak, j), j * 128, tag)
                    zt = t["zt" + tag]
                    for bi, (c0, w, d0) in enumerate(TM_BLOCKS):
                        for kk in range(8):
                            P.op("pe", lambda e, kk=kk, c0=c0, w=w, j=j, bi=bi, aT=aT: e.matmul(pq[:, 0:w], lhsT=aT[:, kk, j * 128:(j + 1) * 128], rhs=t["win"][:, kk, c0:c0 + w],
                                                                                  start=(kk == 0), stop=(kk == 7)),
                                 reads=[(ak, j), "win"], writes=["pq"], skip_self=(kk > 0))
                        if bi == 1:
                            P.op("act", lambda e, zt=zt: e.activation(out=zt[:, 352:608], in_=pq[:, 0:256], func=AF.Copy, scale=0.125), reads=["pq"], writes=["zt" + tag])
                            P.op("dve", lambda e, zt=zt: e.tensor_copy(out=zt[:, 608:864], in_=pq[:, 256:512]), reads=["pq"], writes=["zt" + tag])
                        elif bi == 0:
                            P.op("act", lambda e, zt=zt: e.copy(out=zt[:, 0:352], in_=pq[:, 0:352]), reads=["pq"], writes=["zt" + tag])
                        else:
                            P.op("dve", lambda e, zt=zt: e.tensor_copy(out=zt[:, 864:1120], in_=pq[:, 0:256]), reads=["pq"], writes=["zt" + tag])
                    t0 = b * TB + g0 + j * 128
                    P.dma(lambda e, zt=zt, t0=t0: e.dma_start(out=k.zt[t0:t0 + 128, :], in_=zt[:]), reads=["zt" + tag], writes=[("ztd", l, b, i)], dkey=("ztd", tag))

                for j0 in range(0, nt, 2):
                    recs = []
                    for s_ in range(2):
                        rec = Rec("_%d" % s_, {"pst", "pq"})
                        k.P = rec
                        tile_chain(j0 + s_, ti, psl[s_][0], psl[s_][1])
                        ti += 1
                        recs.append(rec)
                    k.P = P
                    replay(P, recs)
                zfs = t["zf%d" % (gi % 2)]
                zk = "zf%d" % (gi % 2)
                for ci, c0 in enumerate(FM_COLS):
                    ps = pf[ci % 3]
                    for kk in range(8):
                        P.op("pe", lambda e, kk=kk, c0=c0, ps=ps, aT=aT, W=W: e.matmul(ps[:, 0:W], lhsT=t["win"][:, kk, c0:c0 + 128], rhs=aT[:, kk, 0:W], start=(kk == 0), stop=(kk == 7)),
                             reads=[(ak, jj) for jj in range(nt)] + ["win"], writes=[("pf", ci % 3)], skip_self=(kk > 0))
                    if ci in (2, 3):
                        P.op("act", lambda e, ci=ci, ps=ps, W=W, zfs=zfs: e.activation(out=zfs[:, ci, 0:W], in_=ps[:, 0:W], func=AF.Copy, scale=0.125), reads=[("pf", ci % 3)], writes=[zk])
                    elif ci % 2 == 0:
                        P.op("act", lambda e, ci=ci, ps=ps, W=W, zfs=zfs: e.copy(out=zfs[:, ci, 0:W], in_=ps[:, 0:W]), reads=[("pf", ci % 3)], writes=[zk])
                    else:
                        P.op("dve", lambda e, ci=ci, ps=ps, W=W, zfs=zfs: e.tensor_copy(out=zfs[:, ci, 0:W], in_=ps[:, 0:W]), reads=[("pf", ci % 3)], writes=[zk])
                t0 = b * TB + g0
                for ci in range(len(FM_COLS)):
                    P.dma(lambda e, zfs=zfs, t0=t0, W=W, ci=ci: e.dma_start(out=zf_v[:, ci, t0:t0 + W], in_=zfs[:, ci, 0:W]), reads=[zk], writes=[("zfd", l, b, g0)],
                          dkey=("zfd", gi % 2))
                gi += 1
        P.barrier()
        P.emit()


def attn_core(k, pfx, QT_h, KT_h, V_h, q0, W, key_tiles, transform, ps_s, ps_o, pt, dv, rkeys, okey):
    P = k.P
    nt = W // 128
    for n, kt in enumerate(key_tiles):
        sb = k.cc % 2
        k.cc += 1
        P.op("pe", lambda e, kt=kt, sb=sb: e.matmul(ps_s[sb][:, 0:W], lhsT=KT_h[:, kt * 128:(kt + 1) * 128], rhs=QT_h[:, q0:q0 + W], start=True, stop=True),
             reads=rkeys, writes=[pfx + "ps_s%d" % sb])
        transform(kt, ps_s[sb], pfx + "ps_s%d" % sb, pt[sb], pfx + "pt%d" % sb)
        for sidx in range(nt):
            P.op("pe", lambda e, kt=kt, sb=sb, sidx=sidx, n=n: e.matmul(ps_o[sidx][:, 0:dv], lhsT=pt[sb][:, sidx * 128:(sidx + 1) * 128], rhs=V_h(kt),
                                                                  start=(n == 0), stop=(n == len(key_tiles) - 1)),
                 reads=[pfx + "pt%d" % sb] + rkeys, writes=[okey + str(sidx)], skip_self=(n > 0))


def rms_rstd(k, t, src_ap, srckey, junk, jkey, ss, sskey, n):
    P = k.P
    P.op("act", lambda e: e.activation(out=junk, in_=src_ap, func=AF.Square, accum_out=ss), reads=[srckey], writes=[jkey, sskey])
    P.op("act", lambda e: e.activation(out=ss, in_=ss, func=AF.Sqrt, scale=1.0 / n, bias=EPS), reads=[sskey], writes=[sskey])
    P.op("dve", lambda e: e.reciprocal(out=ss, in_=ss), reads=[sskey], writes=[sskey])


def transpose_to(k, t, pstile, pskey, src_blocks, dst_ap, dstkey, srckeys, npart, eng="act"):
    P = k.P
    for i, blk in enumerate(src_blocks):
        w = blk.shape[-1]
        P.op("pe", lambda e, i=i, blk=blk, w=w: e.transpose(out=pstile[0:w, i, :], in_=blk, identity=t["idb"][:]), reads=srckeys + ["idb"], writes=[pskey])
    n = len(src_blocks)
    if eng == "act":
        P.op("act", lambda e: e.copy(out=dst_ap, in_=pstile[0:npart, 0:n, :]), reads=[pskey], writes=[dstkey])
    else:
        P.op("dve", lambda e: e.tensor_copy(out=dst_ap, in_=pstile[0:npart, 0:n, :]), reads=[pskey], writes=[dstkey])


def store_yT(k, t, ytm, ykey, nt, row0, tcol0, pstile, pskey, l):
    P = k.P
    W = nt * 128
    yts = t["yts%d" % (k.yc % 2)]
    ytk = "yts%d" % (k.yc % 2)
    k.yc += 1
    for c in range(2):
        for sidx in range(nt):
            P.op("pe", lambda e, c=c, sidx=sidx: e.transpose(out=pstile[:, sidx, :], in_=ytm[:, sidx, c * 128:(c + 1) * 128], identity=t["idb"][:]),
                 reads=[ykey, "idb"], writes=[pskey])
        P.op("act", lambda e, c=c: e.copy(out=yts[:, c, 0:W], in_=pstile[:, 0:nt, :]), reads=[pskey], writes=[ytk])
        P.dma(lambda e, c=c: e.dma_start(out=k.yT[row0 + c * 128:row0 + (c + 1) * 128, tcol0:tcol0 + W], in_=yts[:, c, 0:W]), reads=[ytk],
              writes=[("yTd", l, row0, c, tcol0)], dkey=("yTd", ytk))


def stage_mla(k, l):
    nc, P, NB = k.nc, k.P, k.NB
    with ExitStack() as es:
        t = _tiles(es, nc, [("idf", (128, 128), F32), ("idb", (128, 128), BF16), ("wuqf", (128, 2, 384), F32), ("wuq", (128, 2, 384), BF16), ("gq", (128, 2), F32), ("wukvf", (128, 512), F32), ("wukv", (128, 512), BF16), ("gkv", (128, 1), F32), ("qng", (128, 96), F32), ("kng", (128, 96), F32), ("cos", (128, 16, 16), F32), ("sin", (128, 16, 16), F32), ("QT", (96, 4, TB), BF16), ("KT", (96, 4, TB), BF16), ("V", (128, 18, 4, 65), BF16), ("pt0", (128, 512), BF16), ("pt1", (128, 512), BF16), ("rec", (128, 4), F32), ("ytm0", (128, 4, 256), BF16), ("ytm1", (128, 4, 256), BF16), ("yts0", (128, 2, 512), BF16), ("yts1", (128, 2, 512), BF16)])
        ps_s = [_psum(es, nc, "pss%d" % i, [128, 512], F32) for i in range(2)]
        ts = []
        for s_ in range(2):
            d_ = dict(t)
            d_.update(_tiles(es, nc, [("junk", (128, 384), F32), ("ssq", (128, 1), F32), ("ssk", (128, 1), F32), ("cqn", (128, 192), BF16), ("ckvn", (128, 128), BF16), ("cqT", (128, 2, 128), BF16), ("ckvT", (128, 1, 128), BF16), ("sq", (128, 4, 96), F32), ("ssh", (128, 4), F32), ("qn", (128, 4, 96), F32), ("kfull", (128, 4, 96), F32), ("qf", (128, 4, 96), BF16), ("r1", (128, 4, 2, 8), F32), ("r2", (128, 4, 2, 8), F32), ("zin", (128, 352), BF16)]))
            d_["pstb"] = _psum(es, nc, "pstb", [128, 8, 128], BF16)
            d_["pskv"] = ps_s[s_]
            d_["pskey"] = "mla_ps_s%d" % s_
            ts.append(d_)
        pstb = ts[0]["pstb"]
        ps_o = [_psum(es, nc, "pso%d" % i, [128, 512], F32) for i in range(4)]
        P.dma(lambda e: e.dma_start(out=t["idf"][:], in_=k.identf), writes=["idf"])
        P.op("dve", lambda e: e.tensor_copy(out=t["idb"][:], in_=t["idf"][:]), reads=["idf"], writes=["idb"])
        P.op("pool", lambda e: e.memset(t["wuqf"][:], 0.0), writes=["wuqf"])
        P.op("pool", lambda e: e.memset(t["gq"][:], 0.0), writes=["gq"])
        P.dma(lambda e: e.dma_start(out=t["wuqf"][:, 0, :], in_=k.mla_w_uq[l][0:128, :]), writes=["wuqf"])
        P.dma(lambda e: e.dma_start(out=t["wuqf"][0:64, 1, :], in_=k.mla_w_uq[l][128:192, :]), writes=["wuqf"])
        P.dma(lambda e: e.dma_start(out=t["gq"][:, 0:1], in_=k.mla_q_norm_g[l][0:128].rearrange("(p o) -> p o", o=1)), writes=["gq"])
        P.dma(lambda e: e.dma_start(out=t["gq"][0:64, 1:2], in_=k.mla_q_norm_g[l][128:192].rearrange("(p o) -> p o", o=1)), writes=["gq"])
        for c in range(2):
            P.op("dve", lambda e, c=c: e.tensor_scalar(out=t["wuq"][:, c, :], in0=t["wuqf"][:, c, :], scalar1=t["gq"][:, c:c + 1], scalar2=None, op0=ALU.mult),
                 reads=["wuqf", "gq"], writes=["wuq"])
        P.dma(lambda e: e.dma_start(out=t["wukvf"][:], in_=k.mla_w_ukv[l]), writes=["wukvf"])
        P.dma(lambda e: e.dma_start(out=t["gkv"][:], in_=k.mla_kv_norm_g[l].rearrange("(p o) -> p o", o=1)), writes=["gkv"])
        P.op("dve", lambda e: e.tensor_scalar(out=t["wukv"][:], in0=t["wukvf"][:], scalar1=t["gkv"][:, 0:1], scalar2=None, op0=ALU.mult), reads=["wukvf", "gkv"], writes=["wukv"])
        P.dma(lambda e: e.dma_start(out=t["qng"][:], in_=k.mla_qn_g[l].partition_broadcast(128)), writes=["qng"])
        P.dma(lambda e: e.dma_start(out=t["kng"][:], in_=k.mla_kn_g[l].partition_broadcast(128)), writes=["kng"])
        P.op("dve", lambda e: e.tensor_scalar(out=t["qng"][:], in0=t["qng"][:], scalar1=96.0 ** -0.5, scalar2=None, op0=ALU.mult), reads=["qng"], writes=["qng"])
        P.dma(lambda e: e.dma_start(out=t["cos"][:], in_=k.rope_cos.rearrange("(n p) f -> p n f", p=128)), writes=["cos"])
        P.dma(lambda e: e.dma_start(out=t["sin"][:], in_=k.rope_sin.rearrange("(n p) f -> p n f", p=128)), writes=["sin"])
        P.op("pool", lambda e: e.memset(t["V"][:], 1.0), writes=["V"])

        def prep_tile(t, b, i):
            P = k.P
            pstb, pskv, pskey = t["pstb"], t["pskv"], t["pskey"]
            psq = pskv
            def head_norm_rope(src_ps_or_sb, srckey, gains, dstT, dstkey, i, col0, is_lat):
                P.op("act", lambda e: e.activation(out=t["sq"][:], in_=src_ps_or_sb, func=AF.Square), reads=[srckey], writes=["sq"])
                P.op("dve", lambda e: e.tensor_reduce(out=t["ssh"][:], in_=t["sq"][:], axis=AX.X, op=ALU.add), reads=["sq"], writes=["ssh"])
                P.op("act", lambda e: e.activation(out=t["ssh"][:], in_=t["ssh"][:], func=AF.Sqrt, scale=1.0 / 96, bias=EPS), reads=["ssh"], writes=["ssh"])
                P.op("dve", lambda e: e.reciprocal(out=t["ssh"][:], in_=t["ssh"][:]), reads=["ssh"], writes=["ssh"])
                P.op("dve", lambda e: e.tensor_tensor(out=t["qn"][:], in0=src_ps_or_sb, in1=t["ssh"][:].unsqueeze(2).to_broadcast([128, 4, 96]), op=ALU.mult),
                     reads=[srckey, "ssh"], writes=["qn"])
                if is_lat:
                    P.op("dve", lambda e: e.tensor_tensor(out=t["qn"][:], in0=t["qn"][:], in1=gains[:].unsqueeze(1).to_broadcast([128, 4, 96]), op=ALU.mult),
                         reads=["qn", "qng", "kng"], writes=["qn"])
                    P.op("act", lambda e: e.copy(out=t["qf"][:, :, 0:64], in_=t["qn"][:, :, 0:64]), reads=["qn"], writes=["qf"])
                    rv = t["qn"][:, :, 64:96].rearrange("p h (a b f) -> p h a b f", a=2, b=2)
                    ov = t["qf"][:, :, 64:96].rearrange("p h (a b f) -> p h a b f", a=2, b=2)
                    x1, x2 = rv[:, :, :, 0, :], rv[:, :, :, 1, :]
                    cs = t["cos"][:, i, :].rearrange("p (a f) -> p a f", a=2).unsqueeze(1).to_broadcast([128, 4, 2, 8])
                    sn = t["sin"][:, i, :].rearrange("p (a f) -> p a f", a=2).unsqueeze(1).to_broadcast([128, 4, 2, 8])
                    P.op("dve", lambda e: e.tensor_tensor(out=t["r1"][:], in0=x1, in1=cs, op=ALU.mult), reads=["qn", "cos"], writes=["r1"])
                    P.op("dve", lambda e: e.tensor_tensor(out=t["r2"][:], in0=x2, in1=sn, op=ALU.mult), reads=["qn", "sin"], writes=["r2"])
                    P.op("dve", lambda e: e.tensor_tensor(out=ov[:, :, :, 0, :], in0=t["r1"][:], in1=t["r2"][:], op=ALU.subtract), reads=["r1", "r2"], writes=["qf"])
                    P.op("dve", lambda e: e.tensor_tensor(out=t["r1"][:], in0=x2, in1=cs, op=ALU.mult), reads=["qn", "cos", "qf"], writes=["r1"])
                    P.op("dve", lambda e: e.tensor_tensor(out=t["r2"][:], in0=x1, in1=sn, op=ALU.mult), reads=["qn", "sin", "qf"], writes=["r2"])
                    P.op("dve", lambda e: e.tensor_tensor(out=ov[:, :, :, 1, :], in0=t["r1"][:], in1=t["r2"][:], op=ALU.add), reads=["r1", "r2"], writes=["qf"])
                else:
                    P.op("dve", lambda e: e.tensor_tensor(out=t["qf"][:], in0=t["qn"][:], in1=gains[:].unsqueeze(1).to_broadcast([128, 4, 96]), op=ALU.mult),
                         reads=["qn", "qng", "kng"], writes=["qf"])
                transpose_to(k, t, pstb, "pstb", [t["qf"][:, h, :] for h in range(4)], dstT[0:96, :, col0:col0 + 128], dstkey, ["qf"], 96)

            zin = t["zin"]
            zk = "zin"
            t0 = b * TB + i * 128
            is_lat = i < 16
            P.dma(lambda e, zin=zin, t0=t0: e.dma_start(out=zin[:], in_=k.zt[t0:t0 + 128, 0:352]), reads=[("ztd", l, b, i)], writes=[zk])
            rms_rstd(k, t, zin[:, 0:192], zk, t["junk"][:, 0:192], "junk", t["ssq"][:], "ssq", 192)
            P.op("dve", lambda e, zin=zin: e.tensor_scalar(out=t["cqn"][:], in0=zin[:, 0:192], scalar1=t["ssq"][:, 0:1], scalar2=None, op0=ALU.mult), reads=[zk, "ssq"], writes=["cqn"])
            P.op("pe", lambda e: e.transpose(out=pstb[:, 0, :], in_=t["cqn"][:, 0:128], identity=t["idb"][:]), reads=["cqn", "idb"], writes=["pstb"])
            P.op("pe", lambda e: e.transpose(out=pstb[0:64, 1, :], in_=t["cqn"][:, 128:192], identity=t["idb"][:]), reads=["cqn", "idb"], writes=["pstb"])
            P.op("act", lambda e: e.copy(out=t["cqT"][:, 0, :], in_=pstb[:, 0, :]), reads=["pstb"], writes=["cqT"])
            P.op("act", lambda e: e.copy(out=t["cqT"][0:64, 1, :], in_=pstb[0:64, 1, :]), reads=["pstb"], writes=["cqT"])
            P.op("pe", lambda e: e.matmul(psq[:, 0:384], lhsT=t["cqT"][:, 0, :], rhs=t["wuq"][:, 0, :], start=True, stop=False), reads=["cqT", "wuq"], writes=[pskey])
            P.op("pe", lambda e: e.matmul(psq[:, 0:384], lhsT=t["cqT"][0:64, 1, :], rhs=t["wuq"][0:64, 1, :], start=False, stop=True), reads=["cqT", "wuq"], writes=[pskey], skip_self=True)
            head_norm_rope(psq[:, 0:384].rearrange("p (h d) -> p h d", h=4), pskey, t["qng"], t["QT"], "QT", i, i * 128, is_lat)
            rms_rstd(k, t, zin[:, 192:320], zk, t["junk"][:, 0:128], "junk", t["ssk"][:], "ssk", 128)
            P.op("dve", lambda e, zin=zin: e.tensor_scalar(out=t["ckvn"][:], in0=zin[:, 192:320], scalar1=t["ssk"][:, 0:1], scalar2=None, op0=ALU.mult), reads=[zk, "ssk"], writes=["ckvn"])
            transpose_to(k, t, pstb, "pstb", [t["ckvn"][:, :]], t["ckvT"][:, :, :], "ckvT", ["ckvn"], 128)
            P.op("pe", lambda e: e.matmul(pskv[:, :], lhsT=t["ckvT"][:, 0, :], rhs=t["wukv"][:], start=True, stop=True), reads=["ckvT", "wukv"], writes=[pskey])
            kvv = pskv[:, :].rearrange("p (h d) -> p h d", h=4)
            P.op("act", lambda e, kvv=kvv: e.copy(out=t["kfull"][:, :, 0:64], in_=kvv[:, :, 0:64]), reads=[pskey], writes=["kfull"])
            P.op("dve", lambda e, zin=zin: e.tensor_copy(out=t["kfull"][:, :, 64:96], in_=zin[:, 320:352].unsqueeze(1).to_broadcast([128, 4, 32])), reads=[zk], writes=["kfull"])
            P.op("act", lambda e, kvv=kvv, i=i: e.copy(out=t["V"][:, i, :, 0:64], in_=kvv[:, :, 64:128]), reads=[pskey], writes=["V"])
            head_norm_rope(t["kfull"][:], "kfull", t["kng"], t["KT"], "KT", i, i * 128, is_lat)

        LOCALK = set(['zin', 'junk', 'ssq', 'ssk', 'cqn', 'ckvn', 'cqT', 'ckvT', 'sq', 'ssh', 'qn', 'kfull', 'qf', 'r1', 'r2']) | {"pstb"}
        for b in range(NB):
            for i0 in range(0, 18, 2):
                recs = []
                for s_ in range(2):
                    rec = Rec("_%d" % s_, LOCALK)
                    k.P = rec
                    prep_tile(ts[s_], b, i0 + s_)
                    recs.append(rec)
                k.P = P
                replay(P, recs)
            qblocks = [(0, 512, list(range(18))), (512, 512, list(range(18))), (1024, 512, list(range(18))), (1536, 512, list(range(18)))]
            if l < DEPTH - 1:
                qblocks.append((2048, 256, [16, 17]))
            for (q0, W, kts) in qblocks:
                nt = W // 128
                ytm = t["ytm%d" % (k.yc % 2)]
                ykey = "ytm%d" % (k.yc % 2)
                for h in range(4):
                    okey = "mla_pso"

                    def tf(kt, pss, psk, ptile, ptk, W=W):
                        P.op("act", lambda e: e.activation(out=ptile[:, 0:W], in_=pss[:, 0:W], func=AF.Exp), reads=[psk], writes=[ptk])
                    attn_core(k, "mla_", t["QT"][0:96, h, :], t["KT"][0:96, h, :], lambda kt, h=h: t["V"][:, kt, h, :], q0, W, kts, tf,
                              ps_s, ps_o, [t["pt0"], t["pt1"]], 65, ["QT", "KT", "V"], okey)
                    for sidx in range(nt):
                        P.op("dve", lambda e, sidx=sidx: e.reciprocal(out=t["rec"][:, sidx:sidx + 1], in_=ps_o[sidx][:, 64:65]), reads=[okey + str(sidx)], writes=["rec"])
                        P.op("dve", lambda e, sidx=sidx, h=h, ytm=ytm: e.tensor_scalar(out=ytm[:, sidx, h * 64:(h + 1) * 64], in0=ps_o[sidx][:, 0:64],
                                                                                     scalar1=t["rec"][:, sidx:sidx + 1], scalar2=None, op0=ALU.mult),
                             reads=[okey + str(sidx), "rec"], writes=[ykey])
                store_yT(k, t, ytm, ykey, nt, 0, b * TB + q0, pstb, "pstb_0", l)
        P.barrier()
        P.emit()


def stage_ret(k, l):
    nc, P, NB = k.nc, k.P, k.NB
    with ExitStack() as es:
        t = _tiles(es, nc, [("idf", (128, 128), F32), ("idb", (128, 128), BF16),
                            ("rel0", (128, 128), F32), ("mge", (128, 128), F32), ("mle", (128, 128), F32), ("dvals", (128, 18), F32),
                            ("lg", (128, 8), F32), ("nlg", (128, 8), F32), ("biasF", (128, 4, 18), F32), ("biasB", (128, 4, 18), F32),
                            ("e1", (128, 128), F32), ("e2", (128, 128), F32),
                            ("arr", (128, 4, 35, 128), BF16), ("Cx", (128, 4, 2, 16, 128), BF16),
                            ("QTr", (128, 2, TB), BF16), ("KTr", (128, 2, TB), BF16), ("Vr", (128, 18, 256), BF16),
                            ("pt0", (128, 512), BF16), ("pt1", (128, 512), BF16),
                            ("yo", (128, 4, 256), F32), ("sq", (128, 4, 256), F32), ("ssh", (128, 16), F32),
                            ("rng", (128, 256), F32), ("zg0", (128, 4, 256), BF16), ("zg1", (128, 4, 256), BF16), ("gs", (128, 4, 256), F32),
                            ("ytm0", (128, 4, 256), BF16), ("ytm1", (128, 4, 256), BF16),
                            ("yts0", (128, 2, 512), BF16), ("yts1", (128, 2, 512), BF16)])
        pstb = _psum(es, nc, "pstb", [128, 8, 128], BF16)
        ps_s = [_psum(es, nc, "pss%d" % i, [128, 512], F32) for i in range(2)]
        ps_o = [_psum(es, nc, "pso%d" % i, [128, 512], F32) for i in range(4)]
        P.dma(lambda e: e.dma_start(out=t["idf"][:], in_=k.identf), writes=["idf"])
        P.op("dve", lambda e: e.tensor_copy(out=t["idb"][:], in_=t["idf"][:]), reads=["idf"], writes=["idb"])
        for nm, src in (("rel0", k.c_rel0), ("mge", k.c_mge), ("mle", k.c_mle), ("dvals", k.c_dvals)):
            P.dma(lambda e, nm=nm, src=src: e.dma_start(out=t[nm][:], in_=src), writes=[nm])
        P.dma(lambda e: e.dma_start(out=t["lg"][:], in_=k.ret_log_gamma[l].rearrange("a h -> (a h)").partition_broadcast(128)), writes=["lg"])
        P.dma(lambda e: e.dma_start(out=t["rng"][:], in_=k.ret_norm_g[l].partition_broadcast(128)), writes=["rng"])
        P.op("dve", lambda e: e.tensor_scalar(out=t["nlg"][:], in0=t["lg"][:], scalar1=-1.0, scalar2=None, op0=ALU.mult), reads=["lg"], writes=["nlg"])
        for h in range(4):
            P.op("dve", lambda e, h=h: e.tensor_scalar(out=t["biasF"][:, h, :], in0=t["dvals"][:], scalar1=t["lg"][:, h:h + 1], scalar2=None, op0=ALU.mult),
                 reads=["dvals", "lg"], writes=["biasF"])
            P.op("dve", lambda e, h=h: e.tensor_scalar(out=t["biasB"][:, h, :], in0=t["dvals"][:], scalar1=t["lg"][:, 4 + h:5 + h], scalar2=None, op0=ALU.mult),
                 reads=["dvals", "lg"], writes=["biasB"])
        for h in range(4):
            for d in range(1, 18):
                P.op("act", lambda e, h=h, d=d: e.activation(out=t["arr"][:, h, 17 + d, :], in_=t["rel0"][:], func=AF.Exp, scale=t["lg"][:, h:h + 1], bias=t["biasF"][:, h, d:d + 1]),
                     reads=["rel0", "lg", "biasF"], writes=["arr"])
                P.op("act", lambda e, h=h, d=d: e.activation(out=t["arr"][:, h, 17 - d, :], in_=t["rel0"][:], func=AF.Exp, scale=t["nlg"][:, 4 + h:5 + h], bias=t["biasB"][:, h, d:d + 1]),
                     reads=["rel0", "nlg", "biasB"], writes=["arr"])
            P.op("act", lambda e, h=h: e.activation(out=t["e1"][:], in_=t["rel0"][:], func=AF.Exp, scale=t["lg"][:, h:h + 1]), reads=["rel0", "lg"], writes=["e1"])
            P.op("act", lambda e, h=h: e.activation(out=t["e2"][:], in_=t["rel0"][:], func=AF.Exp, scale=t["nlg"][:, 4 + h:5 + h]), reads=["rel0", "nlg"], writes=["e2"])
            P.op("dve", lambda e: e.tensor_tensor(out=t["e1"][:], in0=t["e1"][:], in1=t["mge"][:], op=ALU.mult), reads=["e1", "mge"], writes=["e1"])
            P.op("dve", lambda e: e.tensor_tensor(out=t["e2"][:], in0=t["e2"][:], in1=t["mle"][:], op=ALU.mult), reads=["e2", "mle"], writes=["e2"])
            P.op("dve", lambda e, h=h: e.tensor_tensor(out=t["arr"][:, h, 17, :], in0=t["e1"][:], in1=t["e2"][:], op=ALU.add), reads=["e1", "e2"], writes=["arr"])
            for mi in range(2):
                for qi in range(16):
                    P.op("pool", lambda e, h=h, mi=mi, qi=qi: e.tensor_tensor(out=t["Cx"][:, h, mi, qi, :], in0=t["arr"][:, h, 17 + qi - mi + 2, :],
                                                                              in1=t["arr"][:, h, 17 + qi - 16 - mi, :], op=ALU.add), reads=["arr"], writes=["Cx"])
        zt_v = k.zt.rearrange("(n p) c -> p n c", p=128)
        for b in range(NB):
            for c in range(2):
                P.dma(lambda e, c=c, b=b: e.dma_start(out=t["QTr"][:, c, :], in_=k.zf[ZF_RQ + c * 128:ZF_RQ + (c + 1) * 128, b * TB:(b + 1) * TB]),
                      reads=[("zfd", l, b, g0) for g0 in (0, 512, 1024, 1536, 2048)], writes=["QTr"])
                P.dma(lambda e, c=c, b=b: e.dma_start(out=t["KTr"][:, c, :], in_=k.zf[ZF_RK + c * 128:ZF_RK + (c + 1) * 128, b * TB:(b + 1) * TB]),
                      reads=[("zfd", l, b, g0) for g0 in (0, 512, 1024, 1536, 2048)], writes=["KTr"])
            P.dma(lambda e, b=b: e.dma_start(out=t["Vr"][:], in_=zt_v[:, b * 18:(b + 1) * 18, 608:864]), reads=[("ztd", l, b, i) for i in range(18)], writes=["Vr"])
            qblocks = [(0, 512, list(range(18))), (512, 512, list(range(18))), (1024, 512, list(range(18))), (1536, 512, list(range(18)))]
            if l < DEPTH - 1:
                qblocks.append((2048, 256, [16, 17]))
            for (q0, W, kts) in qblocks:
                nt = W // 128
                qi0 = q0 // 128
                zg = t["zg%d" % (k.yc % 2)]
                zgk = "zg%d" % (k.yc % 2)
                ytm = t["ytm%d" % (k.yc % 2)]
                ykey = "ytm%d" % (k.yc % 2)
                P.dma(lambda e, zg=zg, b=b, qi0=qi0, nt=nt: e.dma_start(out=zg[:, 0:nt, :], in_=zt_v[:, b * 18 + qi0:b * 18 + qi0 + nt, 864:1120]),
                      reads=[("ztd", l, b, i) for i in range(qi0, qi0 + nt)], writes=[zgk])
                for h in range(4):
                    okey = "ret_pso"
                    c, p0 = h // 2, 64 * (h % 2)

                    def tf(kt, pss, psk, ptile, ptk, W=W, h=h, qi0=qi0, nt=nt):
                        if kt < 16 and qi0 < 16:
                            mv = t["arr"][:, h, qi0 - kt + 17:qi0 - kt + 17 + nt, :]
                        elif kt >= 16 and qi0 < 16:
                            mv = t["Cx"][:, h, kt - 16, qi0:qi0 + nt, :]
                        else:
                            mv = t["arr"][:, h, qi0 - kt + 17:qi0 - kt + 17 + nt, :]
                        P.op("dve", lambda e: e.tensor_tensor(out=ptile[:, 0:W].rearrange("p (n c) -> p n c", c=128), in0=pss[:, 0:W].rearrange("p (n c) -> p n c", c=128),
                                                              in1=mv, op=ALU.mult), reads=[psk, "arr", "Cx"], writes=[ptk])
                    attn_core(k, "ret_", t["QTr"][p0:p0 + 64, c, :], t["KTr"][p0:p0 + 64, c, :], lambda kt, h=h: t["Vr"][:, kt, h * 64:(h + 1) * 64], q0, W, kts, tf,
                              ps_s, ps_o, [t["pt0"], t["pt1"]], 64, ["QTr", "KTr", "Vr"], okey)
                    for sidx in range(nt):
                        P.op("act", lambda e, sidx=sidx, h=h: e.copy(out=t["yo"][:, sidx, h * 64:(h + 1) * 64], in_=ps_o[sidx][:, 0:64]), reads=[okey + str(sidx)], writes=["yo"])
                P.op("act", lambda e, nt=nt: e.activation(out=t["sq"][:, 0:nt, :], in_=t["yo"][:, 0:nt, :], func=AF.Square), reads=["yo"], writes=["sq"])
                P.op("dve", lambda e, nt=nt: e.tensor_reduce(out=t["ssh"][:, 0:nt * 4], in_=t["sq"][:, 0:nt, :].rearrange("p n (h d) -> p (n h) d", h=4), axis=AX.X, op=ALU.add),
                     reads=["sq"], writes=["ssh"])
                P.op("act", lambda e, nt=nt: e.activation(out=t["ssh"][:, 0:nt * 4], in_=t["ssh"][:, 0:nt * 4], func=AF.Sqrt, scale=1.0 / 64, bias=EPS), reads=["ssh"], writes=["ssh"])
                P.op("dve", lambda e, nt=nt: e.reciprocal(out=t["ssh"][:, 0:nt * 4], in_=t["ssh"][:, 0:nt * 4]), reads=["ssh"], writes=["ssh"])
                P.op("dve", lambda e, nt=nt: e.tensor_tensor(out=t["yo"][:, 0:nt, :].rearrange("p n (h d) -> p (n h) d", h=4), in0=t["yo"][:, 0:nt, :].rearrange("p n (h d) -> p (n h) d", h=4),
                                                             in1=t["ssh"][:, 0:nt * 4].unsqueeze(2).to_broadcast([128, nt * 4, 64]), op=ALU.mult), reads=["yo", "ssh"], writes=["yo"])
                P.op("dve", lambda e, nt=nt: e.tensor_tensor(out=t["yo"][:, 0:nt, :], in0=t["yo"][:, 0:nt, :], in1=t["rng"][:].unsqueeze(1).to_broadcast([128, nt, 256]), op=ALU.mult),
                     reads=["yo", "rng"], writes=["yo"])
                P.op("act", lambda e, nt=nt, zg=zg: e.activation(out=t["gs"][:, 0:nt, :], in_=zg[:, 0:nt, :], func=AF.Silu), reads=[zgk], writes=["gs"])
                P.op("dve", lambda e, nt=nt, ytm=ytm: e.tensor_tensor(out=ytm[:, 0:nt, :], in0=t["yo"][:, 0:nt, :], in1=t["gs"][:, 0:nt, :], op=ALU.mult), reads=["yo", "gs"], writes=[ykey])
                store_yT(k, t, ytm, ykey, nt, 256, b * TB + q0, pstb, "pstb", l)
        P.barrier()
        P.emit()


BLK5 = ((0, 512), (512, 512), (1024, 512), (1536, 512), (2048, 256))


def stage_lru(k, l):
    nc, P, NB = k.nc, k.P, k.NB
    with ExitStack() as es:
        t = _tiles(es, nc, [("lx", (128, TB), BF16), ("lgz", (128, TB), BF16), ("xc", (128, TB), F32), ("xcb", (128, TB), BF16),
                            ("wconv", (128, 2, 4), F32), ("bconv", (128, 2), F32), ("wst", (128, 128), F32),
                            ("Wbd", (128, 8, 128), BF16), ("bgate", (128, 8), F32), ("lam", (128, 4), F32), ("coef", (128, 4), F32), ("coef2", (128, 4), F32),
                            ("rg", (128, 512), F32), ("ig", (128, 512), F32), ("a2", (128, 512), F32),
                            ("af", (128, TB), F32), ("bf", (128, TB), F32), ("hf", (128, TB), F32), ("hb", (128, TB), F32),
                            ("g1", (128, TB), F32), ("g2", (128, TB), F32), ("yo", (128, TB), BF16)])
        psg = [_psum(es, nc, "psg%d" % i, [128, 512], F32) for i in range(2)]
        for c in range(2):
            for j in range(4):
                P.dma(lambda e, c=c, j=j: e.dma_start(out=t["wconv"][:, c, j:j + 1], in_=k.lru_conv_w[l][j, c * 128:(c + 1) * 128].rearrange("(p o) -> p o", o=1)), writes=["wconv"])
        P.dma(lambda e: e.dma_start(out=t["bconv"][:], in_=k.lru_conv_b[l].rearrange("(c p) -> p c", p=128)), writes=["bconv"])
        for d in range(2):
            P.dma(lambda e, d=d: e.dma_start(out=t["lam"][:, d * 2:d * 2 + 2], in_=k.lru_lambda[l][d].rearrange("(c p) -> p c", p=128)), writes=["lam"])
        for d in range(2):
            for gi, (wsrc, bsrc) in enumerate(((k.lru_wa, k.lru_ba), (k.lru_wx, k.lru_bx))):
                P.dma(lambda e, d=d, gi=gi, bsrc=bsrc: e.dma_start(out=t["bgate"][:, (d * 2 + gi) * 2:(d * 2 + gi) * 2 + 2], in_=bsrc[l][d].rearrange("(c p) -> p c", p=128)), writes=["bgate"])
                for c in range(2):
                    P.op("pool", lambda e: e.memset(t["wst"][:], 0.0), writes=["wst"])
                    for j in range(2):
                        P.dma(lambda e, d=d, c=c, j=j, wsrc=wsrc: e.dma_start(out=t["wst"][64 * j:64 * j + 64, 64 * j:64 * j + 64], in_=wsrc[l][d][2 * c + j]), writes=["wst"])
                    P.op("dve", lambda e, d=d, gi=gi, c=c: e.tensor_copy(out=t["Wbd"][:, (d * 2 + gi) * 2 + c, :], in_=t["wst"][:]), reads=["wst"], writes=["Wbd"])
        P.op("act", lambda e: e.activation(out=t["coef"][:], in_=t["lam"][:], func=AF.Exp, scale=-1.0), reads=["lam"], writes=["coef"])
        P.op("act", lambda e: e.activation(out=t["coef"][:], in_=t["coef"][:], func=AF.Ln, bias=1.0), reads=["coef"], writes=["coef"])
        P.op("dve", lambda e: e.tensor_scalar(out=t["coef2"][:], in0=t["coef"][:], scalar1=-16.0, scalar2=None, op0=ALU.mult), reads=["coef"], writes=["coef2"])
        P.op("dve", lambda e: e.tensor_scalar(out=t["coef"][:], in0=t["coef"][:], scalar1=-8.0, scalar2=None, op0=ALU.mult), reads=["coef"], writes=["coef"])
        for b in range(NB):
            for c in range(2):
                rds = [("zfd", l, b, g0) for g0 in (0, 512, 1024, 1536, 2048)]
                P.dma(lambda e, b=b, c=c: e.dma_start(out=t["lx"][:], in_=k.zf[ZF_LX + c * 128:ZF_LX + (c + 1) * 128, b * TB:(b + 1) * TB]), reads=rds, writes=["lx"])
                P.dma(lambda e, b=b, c=c: e.dma_start(out=t["lgz"][:], in_=k.zf[ZF_LG + c * 128:ZF_LG + (c + 1) * 128, b * TB:(b + 1) * TB]), reads=rds, writes=["lgz"])
                for (r0, r1) in ((0, SEQ), (SEQ, TB)):
                    P.op("dve", lambda e, r0=r0, r1=r1, c=c: e.tensor_scalar(out=t["xc"][:, r0:r1], in0=t["lx"][:, r0:r1], scalar1=t["wconv"][:, c, 2:3], scalar2=t["bconv"][:, c:c + 1],
                                                                          op0=ALU.mult, op1=ALU.add), reads=["lx", "wconv", "bconv"], writes=["xc"])
                    for j, o in ((0, -2), (1, -1), (3, 1)):
                        a0, a1 = max(r0, r0 - o), min(r1, r1 - o)
                        P.op("dve", lambda e, a0=a0, a1=a1, o=o, j=j, c=c: e.scalar_tensor_tensor(out=t["xc"][:, a0:a1], in0=t["lx"][:, a0 + o:a1 + o], scalar=t["wconv"][:, c, j:j + 1],
                                                                                              in1=t["xc"][:, a0:a1], op0=ALU.mult, op1=ALU.add), reads=["lx", "wconv", "xc"], writes=["xc"])
                P.op("act", lambda e: e.copy(out=t["xcb"][:], in_=t["xc"][:]), reads=["xc"], writes=["xcb"])
                for d in range(2):
                    hd = t["hf"] if d == 0 else t["hb"]
                    hk = "hf" if d == 0 else "hb"
                    for (g0, W) in BLK5:
                        for gi, dst in ((0, "rg"), (1, "ig")):
                            ps = psg[gi]
                            P.op("pe", lambda e, ps=ps, d=d, gi=gi, c=c, g0=g0, W=W: e.matmul(ps[:, 0:W], lhsT=t["Wbd"][:, (d * 2 + gi) * 2 + c, :], rhs=t["xcb"][:, g0:g0 + W], start=True, stop=True),
                                 reads=["Wbd", "xcb"], writes=["psg%d" % gi])
                            P.op("act", lambda e, ps=ps, d=d, gi=gi, c=c, W=W, dst=dst: e.activation(out=t[dst][:, 0:W], in_=ps[:, 0:W], func=AF.Sigmoid,
                                                                                                   bias=t["bgate"][:, (d * 2 + gi) * 2 + c:(d * 2 + gi) * 2 + c + 1]),
                                 reads=["psg%d" % gi, "bgate"], writes=[dst])
                        ci = d * 2 + c
                        P.op("act", lambda e, g0=g0, W=W, ci=ci: e.activation(out=t["af"][:, g0:g0 + W], in_=t["rg"][:, 0:W], func=AF.Exp, scale=t["coef"][:, ci:ci + 1]), reads=["rg", "coef"], writes=["af"])
                        P.op("act", lambda e, W=W, ci=ci: e.activation(out=t["a2"][:, 0:W], in_=t["rg"][:, 0:W], func=AF.Exp, scale=t["coef2"][:, ci:ci + 1]), reads=["rg", "coef2"], writes=["a2"])
                        P.op("act", lambda e, W=W: e.activation(out=t["a2"][:, 0:W], in_=t["a2"][:, 0:W], func=AF.Sqrt, scale=-1.0, bias=1.0), reads=["a2"], writes=["a2"])
                        P.op("dve", lambda e, g0=g0, W=W: e.tensor_tensor(out=t["ig"][:, 0:W], in0=t["ig"][:, 0:W], in1=t["xc"][:, g0:g0 + W], op=ALU.mult), reads=["ig", "xc"], writes=["ig"])
                        P.op("dve", lambda e, g0=g0, W=W: e.tensor_tensor(out=t["bf"][:, g0:g0 + W], in0=t["ig"][:, 0:W], in1=t["a2"][:, 0:W], op=ALU.mult), reads=["ig", "a2"], writes=["bf"])
                    if d == 0:
                        P.op("dve", lambda e, hd=hd: e.tensor_tensor_scan(out=hd[:, SEQ:TB], data0=t["af"][:, SEQ:TB], data1=t["bf"][:, SEQ:TB], initial=0.0, op0=ALU.mult, op1=ALU.add),
                             reads=["af", "bf"], writes=[hk])
                        P.op("dve", lambda e, hd=hd: e.tensor_tensor_scan(out=hd[:, 0:SEQ], data0=t["af"][:, 0:SEQ], data1=t["bf"][:, 0:SEQ], initial=hd[:, TB - 1:TB], op0=ALU.mult, op1=ALU.add),
                             reads=["af", "bf", hk], writes=[hk])
                    else:
                        P.op("dve", lambda e, hd=hd: e.tensor_tensor_scan(out=hd[:, SEQ:TB][:, ::-1], data0=t["af"][:, SEQ:TB][:, ::-1], data1=t["bf"][:, SEQ:TB][:, ::-1], initial=0.0,
                                                                         op0=ALU.mult, op1=ALU.add), reads=["af", "bf"], writes=[hk])
                        P.op("dve", lambda e, hd=hd: e.tensor_tensor_scan(out=hd[:, 0:SEQ][:, ::-1], data0=t["af"][:, 0:SEQ][:, ::-1], data1=t["bf"][:, 0:SEQ][:, ::-1], initial=hd[:, SEQ:SEQ + 1],
                                                                         op0=ALU.mult, op1=ALU.add), reads=["af", "bf", hk], writes=[hk])
                P.op("pool", lambda e: e.tensor_tensor(out=t["g1"][:], in0=t["lgz"][:], in1=t["lgz"][:], op=ALU.mult), reads=["lgz"], writes=["g1"])
                P.op("pool", lambda e: e.tensor_scalar(out=t["g1"][:], in0=t["g1"][:], scalar1=0.044715, scalar2=1.0, op0=ALU.mult, op1=ALU.add), reads=["g1"], writes=["g1"])
                P.op("pool", lambda e: e.tensor_tensor(out=t["g1"][:], in0=t["g1"][:], in1=t["lgz"][:], op=ALU.mult), reads=["g1", "lgz"], writes=["g1"])
                P.op("act", lambda e: e.activation(out=t["g2"][:], in_=t["g1"][:], func=AF.Sigmoid, scale=1.5957691216057308), reads=["g1"], writes=["g2"])
                P.op("pool", lambda e: e.tensor_tensor(out=t["g2"][:], in0=t["g2"][:], in1=t["lgz"][:], op=ALU.mult), reads=["g2", "lgz"], writes=["g2"])
                P.op("dve", lambda e: e.tensor_tensor(out=t["hf"][:], in0=t["hf"][:], in1=t["hb"][:], op=ALU.add), reads=["hf", "hb"], writes=["hf"])
                P.op("dve", lambda e: e.tensor_tensor(out=t["yo"][:], in0=t["hf"][:], in1=t["g2"][:], op=ALU.mult), reads=["hf", "g2"], writes=["yo"])
                P.dma(lambda e, b=b, c=c: e.dma_start(out=k.yT[768 + c * 128:768 + (c + 1) * 128, b * TB:(b + 1) * TB], in_=t["yo"][:]), reads=["yo"],
                      writes=[("yTd", l, 768, c, b)], dkey="yo_store")
        P.barrier()
        P.emit()


TWO_PI = 2.0 * math.pi


def stage_hyena(k, l, L, off):
    nc, P, NB = k.nc, k.P, k.NB
    hc = k.hc[L]
    nT = L // 128
    nS = 2 * nT
    W = min(512, L)
    BG = 2 if NB % 2 == 0 else 1
    with ExitStack() as es:
        t = _tiles(es, nc, [("idf", (128, 128), F32), ("idb", (128, 128), BF16),
                            ("HA", (128, nT, 256), BF16), ("HBm", (128, nT, 256), BF16), ("nHB", (128, nT, 256), BF16), ("P40", (128, 256), BF16),
                            ("m0", (128, 2), F32), ("wc", (128, 6, 3), F32), ("bc", (128, 6), F32), ("dcol", (128, 2), F32)])
        P.dma(lambda e: e.dma_start(out=t["idf"][:], in_=k.identf), writes=["idf"])
        P.op("dve", lambda e: e.tensor_copy(out=t["idb"][:], in_=t["idf"][:]), reads=["idf"], writes=["idb"])
        P.dma(lambda e: e.dma_start(out=t["m0"][:], in_=k.c_m0), writes=["m0"])
        for c in range(6):
            for j in range(3):
                P.dma(lambda e, c=c, j=j: e.dma_start(out=t["wc"][:, c, j:j + 1], in_=k.hy_conv_w[l][j, c * 128:(c + 1) * 128].rearrange("(p o) -> p o", o=1)), writes=["wc"])
        P.dma(lambda e: e.dma_start(out=t["bc"][:], in_=k.hy_conv_b[l].rearrange("(c p) -> p c", p=128)), writes=["bc"])
        P.dma(lambda e: e.dma_start(out=t["dcol"][:], in_=k.hy_d[l].rearrange("(c p) -> p c", p=128)), writes=["dcol"])
        with ExitStack() as e2:
            f = _tiles(e2, nc, [("w1", (33, 64), F32), ("w2", (64, 64), F32), ("w3", (64, 512), F32), ("fq", (64, 1), F32), ("b1", (64, 1), F32), ("b2", (64, 1), F32),
                                ("ze", (33, 2, L), F32), ("arg", (64, 512), F32), ("ni", (64, 512), mybir.dt.int32), ("nf", (64, 512), F32), ("npi", (64, 1), F32), ("h1", (64, 512), F32), ("h2", (64, 2, L), F32),
                                ("dl", (128, 256), F32), ("tneg", (128, 2, nT), F32), ("win", (128, 256), F32), ("g", (128, 2, nT, 256), BF16),
                                ("csl", (128, nS), F32), ("csh", (128, nS), F32), ("wf0", (128, nT, 128), BF16), ("wf1", (128, nT, 128), BF16),
                                ("Ht", (128, 256), F32), ("H", (128, nS, 256), F32)])
            pf = [_psum(e2, nc, "hpf%d" % i, [128, 512], F32) for i in range(4)]
            for nm, src in (("w1", k.hy_w1[l]), ("w2", k.hy_w2[l]), ("w3", k.hy_w3[l]), ("dl", hc["dl"]), ("csl", hc["csl"]), ("csh", hc["csh"])):
                P.dma(lambda e, nm=nm, src=src: e.dma_start(out=f[nm][:], in_=src), writes=[nm])
            for nm, src in (("fq", k.hy_freq[l]), ("b1", k.hy_b1[l]), ("b2", k.hy_b2[l])):
                P.dma(lambda e, nm=nm, src=src: e.dma_start(out=f[nm][:], in_=src.rearrange("(p o) -> p o", o=1)), writes=[nm])
            for gi, nm in enumerate(("zf", "zr")):
                P.dma(lambda e, gi=gi, nm=nm: e.dma_start(out=f["ze"][:, gi, :], in_=hc[nm]), writes=["ze"])
            for gi, nm in enumerate(("tf", "tr")):
                P.dma(lambda e, gi=gi, nm=nm: e.dma_start(out=f["tneg"][:, gi, :], in_=hc[nm]), writes=["tneg"])
            P.op("pool", lambda e: e.memset(f["npi"][:], -math.pi), writes=["npi"])
            P.op("dve", lambda e: e.tensor_tensor(out=f["b1"][:], in0=f["b1"][:], in1=f["fq"][:], op=ALU.mult), reads=["b1", "fq"], writes=["b1"])
            P.op("dve", lambda e: e.tensor_tensor(out=f["b2"][:], in0=f["b2"][:], in1=f["fq"][:], op=ALU.mult), reads=["b2", "fq"], writes=["b2"])

            def sin_layer(ps, pk, bias, dst_ap, dkey):
                P.op("dve", lambda e: e.tensor_scalar(out=f["arg"][:, 0:W], in0=ps[0:64, 0:W], scalar1=f["fq"][:, 0:1], scalar2=bias[:, 0:1], op0=ALU.mult, op1=ALU.add),
                     reads=[pk, "fq", "b1", "b2"], writes=["arg"])
                P.op("dve", lambda e: e.tensor_scalar(out=f["arg"][:, 0:W], in0=f["arg"][:, 0:W], scalar1=1.0 / TWO_PI, scalar2=4.5, op0=ALU.mult, op1=ALU.add), reads=["arg"], writes=["arg"])
                P.op("dve", lambda e: e.tensor_copy(out=f["ni"][:, 0:W], in_=f["arg"][:, 0:W]), reads=["arg"], writes=["ni"])
                P.op("dve", lambda e: e.tensor_copy(out=f["nf"][:, 0:W], in_=f["ni"][:, 0:W]), reads=["ni"], writes=["nf"])
                P.op("dve", lambda e: e.tensor_tensor(out=f["arg"][:, 0:W], in0=f["arg"][:, 0:W], in1=f["nf"][:, 0:W], op=ALU.subtract), reads=["arg", "nf"], writes=["arg"])
                P.op("dve", lambda e: e.tensor_scalar(out=f["nf"][:, 0:W], in0=f["arg"][:, 0:W], scalar1=0.0, scalar2=None, op0=ALU.is_lt), reads=["arg", "nf"], writes=["nf"])
                P.op("dve", lambda e: e.tensor_tensor(out=f["arg"][:, 0:W], in0=f["arg"][:, 0:W], in1=f["nf"][:, 0:W], op=ALU.add), reads=["arg", "nf"], writes=["arg"])
                P.op("act", lambda e: e.activation(out=dst_ap, in_=f["arg"][:, 0:W], func=AF.Sin, scale=TWO_PI, bias=f["npi"][:, 0:1]), reads=["arg", "npi"], writes=[dkey])

            for gi in range(2):
                for cb in range(L // W):
                    P.op("pe", lambda e, gi=gi, cb=cb: e.matmul(pf[0][0:64, 0:W], lhsT=f["w1"][:], rhs=f["ze"][:, gi, cb * W:(cb + 1) * W], start=True, stop=True), reads=["w1", "ze"], writes=["hpf0"])
                    sin_layer(pf[0], "hpf0", f["b1"], f["h1"][:, 0:W], "h1")
                    P.op("pe", lambda e: e.matmul(pf[1][0:64, 0:W], lhsT=f["w2"][:], rhs=f["h1"][:, 0:W], start=True, stop=True), reads=["w2", "h1"], writes=["hpf1"])
                    sin_layer(pf[1], "hpf1", f["b2"], f["h2"][:, gi, cb * W:(cb + 1) * W], "h2")
                for tt in range(nT):
                    P.op("pe", lambda e, gi=gi, tt=tt: e.matmul(pf[2][:, :], lhsT=f["h2"][:, gi, tt * 128:(tt + 1) * 128], rhs=f["w3"][:], start=True, stop=True), reads=["h2", "w3"], writes=["hpf2"])
                    P.op("act", lambda e, gi=gi, tt=tt: e.activation(out=f["win"][:], in_=f["dl"][:], func=AF.Exp, scale=f["tneg"][:, gi, tt:tt + 1]), reads=["dl", "tneg"], writes=["win"])
                    P.op("dve", lambda e, gi=gi, tt=tt: e.tensor_tensor(out=f["g"][:, gi, tt, :], in0=pf[2][:, gi * 256:(gi + 1) * 256], in1=f["win"][:], op=ALU.mult), reads=["hpf2", "win"], writes=["g"])
            P.op("dve", lambda e: e.memset(f["g"][0:1, 1, 0, :], 0.0), reads=["g"], writes=["g"])
            for j in range(nS):
                wf = f["wf%d" % (j % 2)]
                wk = "wf%d" % (j % 2)
                P.dma(lambda e, j=j, wf=wf: e.dma_start(out=wf[:], in_=hc["Wf"].rearrange("(t p) s -> p t s", p=128)[:, :, j * 128:(j + 1) * 128]), writes=[wk])
                for gi in range(2):
                    for tt in range(nT):
                        P.op("pe", lambda e, gi=gi, tt=tt, wf=wf: e.matmul(pf[gi][:, 0:256], lhsT=wf[:, tt, :], rhs=f["g"][:, gi, tt, :], start=(tt == 0), stop=(tt == nT - 1)),
                             reads=[wk, "g"], writes=["hpf%d" % gi], skip_self=(tt > 0))
                P.op("dve", lambda e, j=j: e.tensor_scalar(out=f["Ht"][:], in0=pf[0][:, 0:256], scalar1=f["csl"][:, j:j + 1], scalar2=None, op0=ALU.mult), reads=["hpf0", "csl"], writes=["Ht"])
                P.op("dve", lambda e, j=j: e.scalar_tensor_tensor(out=f["H"][:, j, :], in0=pf[1][:, 0:256], scalar=f["csh"][:, j:j + 1], in1=f["Ht"][:], op0=ALU.mult, op1=ALU.add),
                     reads=["hpf1", "csh", "Ht"], writes=["H"])
            HAf, HBf = f["H"][:, 0:nT, :], f["H"][:, nT:nS, :]
            P.op("act", lambda e: e.copy(out=t["HA"][:], in_=HAf), reads=["H"], writes=["HA"])
            P.op("act", lambda e: e.copy(out=t["HBm"][:], in_=HBf), reads=["H"], writes=["HBm"])
            P.op("dve", lambda e: e.tensor_scalar(out=t["HBm"][:, 0, :], in0=f["H"][:, nT, :], scalar1=t["m0"][:, 0:1], scalar2=None, op0=ALU.mult), reads=["H", "m0", "HBm"], writes=["HBm"])
            P.op("dve", lambda e: e.tensor_scalar(out=t["nHB"][:], in0=t["HBm"][:], scalar1=-1.0, scalar2=None, op0=ALU.mult), reads=["HBm"], writes=["nHB"])
            P.op("dve", lambda e: e.tensor_scalar(out=f["Ht"][:], in0=f["H"][:, 0, :], scalar1=t["m0"][:, 0:1], scalar2=None, op0=ALU.mult), reads=["H", "m0"], writes=["Ht"])
            P.op("dve", lambda e: e.scalar_tensor_tensor(out=t["P40"][:], in0=f["H"][:, nT, :], scalar=t["m0"][:, 1:2], in1=f["Ht"][:], op0=ALU.mult, op1=ALU.add), reads=["H", "m0", "Ht"], writes=["P40"])
            P.barrier()
        with ExitStack() as e3:
            g = _tiles(e3, nc, [("zh", (128, 6, L), BF16), ("u1", (128, L), F32), ("u2", (128, L), F32),
                                ("x0c", (128, 2, BG, L), BF16), ("sT", (128, 2, BG, L), BF16), ("stm", (128, nT, BG * 256), BF16),
                                ("Y", (128, nS, BG * 256), BF16), ("wfa", (128, nT, 128), BF16), ("wfb", (128, nT, 128), BF16),
                                ("wiv", (128, nS, W), BF16), ("p1", (128, BG * 256), F32), ("p2", (128, BG * 256), F32),
                                ("tmp", (128, W), F32), ("yo0", (128, W), BF16), ("yo1", (128, W), BF16)])
            pstb = _psum(e3, nc, "hpst", [128, 8, 128], BF16)
            psA = _psum(e3, nc, "hpA", [128, 512], F32)
            psB = _psum(e3, nc, "hpB", [128, 512], F32)
            psI = [_psum(e3, nc, "hpI%d" % i, [128, 512], F32) for i in range(4)]
            wf_v = hc["Wf"].rearrange("(t p) s -> p t s", p=128)
            wi_v = hc["Winv"].rearrange("(s p) t -> p s t", p=128)
            for g0 in range(0, NB, BG):
                for bb in range(BG):
                    b = g0 + bb
                    rds = [("zfd", l, b, x) for x in (0, 512, 1024, 1536, 2048)]
                    for c in range(6):
                        P.dma(lambda e, c=c, b=b: e.dma_start(out=g["zh"][:, c, :], in_=k.zf[ZF_ZH + c * 128:ZF_ZH + (c + 1) * 128, b * TB + off:b * TB + off + L]), reads=rds, writes=["zh"])

                    def conv3(c, dst_ap, dkey):
                        P.op("dve", lambda e: e.tensor_scalar(out=dst_ap, in0=g["zh"][:, c, :], scalar1=t["wc"][:, c, 1:2], scalar2=t["bc"][:, c:c + 1], op0=ALU.mult, op1=ALU.add),
                             reads=["zh", "wc", "bc"], writes=[dkey])
                        P.op("dve", lambda e: e.scalar_tensor_tensor(out=dst_ap[:, 1:L], in0=g["zh"][:, c, 0:L - 1], scalar=t["wc"][:, c, 0:1], in1=dst_ap[:, 1:L], op0=ALU.mult, op1=ALU.add),
                             reads=["zh", "wc", dkey], writes=[dkey])
                        P.op("dve", lambda e: e.scalar_tensor_tensor(out=dst_ap[:, 0:L - 1], in0=g["zh"][:, c, 1:L], scalar=t["wc"][:, c, 2:3], in1=dst_ap[:, 0:L - 1], op0=ALU.mult, op1=ALU.add),
                             reads=["zh", "wc", dkey], writes=[dkey])
                    for c in range(2):
                        conv3(c, g["u1"][:, :], "u1")
                        P.op("act", lambda e, c=c, bb=bb: e.copy(out=g["x0c"][:, c, bb, :], in_=g["u1"][:]), reads=["u1"], writes=["x0c"])
                        conv3(2 + c, g["u1"][:, :], "u1")
                        conv3(4 + c, g["u2"][:, :], "u2")
                        P.op("dve", lambda e, c=c, bb=bb: e.tensor_tensor(out=g["sT"][:, c, bb, :], in0=g["u1"][:], in1=g["u2"][:], op=ALU.mult), reads=["u1", "u2"], writes=["sT"])
                    for c in range(2):
                        for t4 in range(0, nT, 8):
                            n8 = min(8, nT - t4)
                            for i in range(n8):
                                P.op("pe", lambda e, c=c, bb=bb, i=i, t4=t4: e.transpose(out=pstb[:, i, :], in_=g["sT"][:, c, bb, (t4 + i) * 128:(t4 + i + 1) * 128], identity=t["idb"][:]),
                                     reads=["sT", "idb"], writes=["hpst"])
                            P.op("act", lambda e, c=c, bb=bb, t4=t4, n8=n8: e.copy(out=g["stm"][:, t4:t4 + n8, bb * 256 + c * 128:bb * 256 + (c + 1) * 128], in_=pstb[:, 0:n8, :]),
                                 reads=["hpst"], writes=["stm"])
                for j in range(nT):
                    P.dma(lambda e, j=j: e.dma_start(out=g["wfa"][:], in_=wf_v[:, :, j * 128:(j + 1) * 128]), writes=["wfa"])
                    P.dma(lambda e, j=j: e.dma_start(out=g["wfb"][:], in_=wf_v[:, :, (nT + j) * 128:(nT + j + 1) * 128]), writes=["wfb"])
                    for tt in range(nT):
                        P.op("pe", lambda e, tt=tt: e.matmul(psA[:, 0:BG * 256], lhsT=g["wfa"][:, tt, :], rhs=g["stm"][:, tt, :], start=(tt == 0), stop=(tt == nT - 1)),
                             reads=["wfa", "stm"], writes=["hpA"], skip_self=(tt > 0))
                    for tt in range(nT):
                        P.op("pe", lambda e, tt=tt: e.matmul(psB[:, 0:BG * 256], lhsT=g["wfb"][:, tt, :], rhs=g["stm"][:, tt, :], start=(tt == 0), stop=(tt == nT - 1)),
                             reads=["wfb", "stm"], writes=["hpB"], skip_self=(tt > 0))
                    A3 = psA[:, 0:BG * 256].rearrange("p (b c) -> p b c", b=BG)
                    B3 = psB[:, 0:BG * 256].rearrange("p (b c) -> p b c", b=BG)
                    p1 = g["p1"][:].rearrange("p (b c) -> p b c", b=BG)
                    p2 = g["p2"][:].rearrange("p (b c) -> p b c", b=BG)

                    def bc3(tab):
                        return tab.unsqueeze(1).to_broadcast([128, BG, 256])
                    P4 = t["P40"][:, :] if j == 0 else t["HA"][:, j, :]
                    P.op("dve", lambda e, j=j: e.tensor_tensor(out=p1, in0=A3, in1=bc3(t["HA"][:, j, :]), op=ALU.mult), reads=["hpA", "HA"], writes=["p1"])
                    P.op("dve", lambda e, j=j: e.tensor_tensor(out=p2, in0=B3, in1=bc3(t["nHB"][:, j, :]), op=ALU.mult), reads=["hpB", "nHB"], writes=["p2"])
                    P.op("pool", lambda e, j=j: e.tensor_tensor(out=g["Y"][:, j, :], in0=g["p1"][:], in1=g["p2"][:], op=ALU.add), reads=["p1", "p2"], writes=["Y"])
                    P.op("dve", lambda e, j=j: e.tensor_tensor(out=p1, in0=A3, in1=bc3(t["HBm"][:, j, :]), op=ALU.mult), reads=["hpA", "HBm", "Y"], writes=["p1"])
                    P.op("dve", lambda e, j=j, P4=P4: e.tensor_tensor(out=p2, in0=B3, in1=bc3(P4), op=ALU.mult), reads=["hpB", "HA", "P40", "Y"], writes=["p2"])
                    P.op("pool", lambda e, j=j: e.tensor_tensor(out=g["Y"][:, nT + j, :], in0=g["p1"][:], in1=g["p2"][:], op=ALU.add), reads=["p1", "p2"], writes=["Y"])
                for tb in range(L // W):
                    P.dma(lambda e, tb=tb: e.dma_start(out=g["wiv"][:], in_=wi_v[:, :, tb * W:(tb + 1) * W]), writes=["wiv"])
                    for bb in range(BG):
                        for c in range(2):
                            ps = psI[bb * 2 + c]
                            pk = "hpI%d" % (bb * 2 + c)
                            for sc in range(nS):
                                P.op("pe", lambda e, ps=ps, sc=sc, bb=bb, c=c: e.matmul(ps[:, 0:W], lhsT=g["Y"][:, sc, bb * 256 + c * 128:bb * 256 + (c + 1) * 128], rhs=g["wiv"][:, sc, :],
                                                                                  start=(sc == 0), stop=(sc == nS - 1)), reads=["Y", "wiv"], writes=[pk], skip_self=(sc > 0))
                            yo = g["yo%d" % (k.yc % 2)]
                            yk = "yo%d" % (k.yc % 2)
                            k.yc += 1
                            P.op("dve", lambda e, ps=ps, bb=bb, c=c, tb=tb: e.scalar_tensor_tensor(out=g["tmp"][:], in0=g["sT"][:, c, bb, tb * W:(tb + 1) * W], scalar=t["dcol"][:, c:c + 1], in1=ps[:, 0:W],
                                                                                             op0=ALU.mult, op1=ALU.add), reads=["sT", "dcol", pk], writes=["tmp"])
                            P.op("dve", lambda e, bb=bb, c=c, tb=tb, yo=yo: e.tensor_tensor(out=yo[:], in0=g["tmp"][:], in1=g["x0c"][:, c, bb, tb * W:(tb + 1) * W], op=ALU.mult),
                                 reads=["tmp", "x0c"], writes=[yk])
                            b = g0 + bb
                            P.dma(lambda e, yo=yo, b=b, c=c, tb=tb: e.dma_start(out=k.yT[512 + c * 128:512 + (c + 1) * 128, b * TB + off + tb * W:b * TB + off + (tb + 1) * W], in_=yo[:]),
                                  reads=[yk], writes=[("yTd", l, 512, c, b, off, tb)], dkey=("hyo", yk))
            P.barrier()
        P.emit()


def hyena_consts(L):
    n = L
    t_lin = np.linspace(0.0, 1.0, n, dtype=np.float32)
    bands = np.linspace(1e-4, 15.0, 16, dtype=np.float32)
    w = (2.0 * np.float32(math.pi) * np.arange(n, dtype=np.float32) / np.float32(n)).astype(np.float32)
    z = np.concatenate([t_lin[:, None], np.cos(bands[None, :] * w[:, None]), -np.sin(bands[None, :] * w[:, None])], axis=-1).astype(np.float32)
    ridx = (n - np.arange(n)) % n
    out = {}
    out["zf"] = np.ascontiguousarray(z.T)
    out["zr"] = np.ascontiguousarray(z[ridx].T)
    tf = -t_lin
    tr = -t_lin[ridx]
    out["tf"] = np.ascontiguousarray(tf.reshape(n // 128, 128).T)
    out["tr"] = np.ascontiguousarray(tr.reshape(n // 128, 128).T)
    max_decay = math.log(1e-2) / 0.3
    min_decay = math.log(1e-2) / 1.5
    deltas = np.abs(np.linspace(min_decay, max_decay, 256, dtype=np.float32))
    out["dl"] = np.broadcast_to(deltas[None, :], (128, 256)).astype(np.float32).copy()
    N = 2 * n
    tt = np.arange(n, dtype=np.float64)[:, None]
    kk = np.arange(n, dtype=np.float64)[None, :]
    ang = 2.0 * np.pi * ((tt * kk) % N) / N
    C = np.cos(ang)
    S = np.sin(ang)
    S[:, 0] = (-1.0) ** np.arange(n)
    Wf = np.concatenate([C, S], axis=1)
    out["Wf"] = Wf.astype(ml_dtypes.bfloat16)
    out["Winv"] = np.ascontiguousarray(Wf.T).astype(ml_dtypes.bfloat16)
    cw = np.full(N, 2.0 / N)
    cw[0] = 1.0 / N
    cw[n] = 1.0 / N
    sgn = np.tile((-1.0) ** np.arange(n), 2)
    sgn[n] = 1.0
    out["csl"] = np.ascontiguousarray(cw.reshape(N // 128, 128).T).astype(np.float32)
    out["csh"] = np.ascontiguousarray((cw * sgn).reshape(N // 128, 128).T).astype(np.float32)
    return out


def stage_outproj(k, l):
    nc, P, NB = k.nc, k.P, k.NB
    ntile = 18 if l < DEPTH - 1 else 16
    with ExitStack() as es:
        A, B = load_mod_cols(k, es, l, 1)
        t = _tiles(es, nc, [("idf", (128, 128), F32), ("idb", (128, 128), BF16), ("wst0", (128, D), F32), ("wst1", (128, D), F32), ("gng", (128, 8), F32), ("wout", (128, 8, D), BF16), ("wrf", (128, 8, 36), F32), ("wr", (128, 8, 36), BF16), ("ones", (128, 1), BF16), ("g1b", (128, D), F32), ("ohrun", (128, 32), F32), ("iota32", (128, 32), F32), ("LTf", (128, 128), F32), ("ONESf", (128, 128), F32), ("jbv", (128, 128), F32), ("A2row", (128, D), F32), ("B2row", (128, D), F32), ("g2row", (128, D), F32), ("cnt", (128, 32), F32), ("nf", (128, 32), F32), ("ni", (128, 32), mybir.dt.int32), ("dd", (128, 32), F32), ("up", (128, 32), F32), ("pend", (128, 32), F32), ("zero32", (128, 32), F32), ("cmp", (128, MAXBLK, 32), F32), ("bexp", (128, MAXBLK), F32)])
        ts = []
        for s_ in range(2):
            d_ = dict(t)
            d_.update(_tiles(es, nc, [("yt", (128, 8, 128), BF16), ("ysq", (128, 8, 128), BF16), ("r", (128, 4), F32), ("m", (128, D), F32), ("xt0", (128, D), F32), ("junk0", (128, D), BF16), ("ss0", (128, 1), F32), ("rstd0", (128, 1), F32), ("xn0", (128, D), BF16), ("fT", (128, 8, 128), BF16), ("lg", (128, 36), F32), ("s1", (128, 8), F32), ("s2", (128, 8), F32), ("s3", (128, 8), F32), ("mg", (128, 4), F32), ("pr", (128, 4, 8), F32), ("es", (128, 8), F32), ("m1", (128, 8), F32), ("m2", (128, 8), F32), ("e2", (128, 8), F32), ("cw8", (128, 8), F32), ("cw", (128, 4, 8), F32), ("oh1", (128, 4, 8), F32), ("oh2", (128, 4, 8), F32), ("ohs", (128, 32), F32), ("ohp", (128, 32), F32), ("rt", (128, 8), F32), ("ft1", (128, D), F32), ("ftm", (128, D), BF16)]))
            d_["pst"] = _psum(es, nc, "pst", [128, 8, 128], BF16)
            d_["pm"] = [_psum(es, nc, "pm%d" % i, [128, 512], F32) for i in range(2)]
            d_["misc"] = _psum(es, nc, "misc", [128, 512], F32)
            ts.append(d_)
        for nm, src in (("iota32", k.c_iota32), ("LTf", k.c_LT), ("ONESf", k.c_ONES), ("jbv", k.c_jbv)):
            P.dma(lambda e, nm=nm, src=src: e.dma_start(out=t[nm][:], in_=src), writes=[nm])
        P.op("pool", lambda e: e.memset(t["ohrun"][:], 0.0), writes=["ohrun"])
        P.op("pool", lambda e: e.memset(t["zero32"][:], 0.0), writes=["zero32"])
        for d_ in ts:
            P.op("pool", lambda e, d_=d_: e.memset(d_["rt"][:], 0.0), writes=["rt_" + str(ts.index(d_))])
        P.dma(lambda e: e.dma_start(out=t["g2row"][:], in_=k.norm2_g[l].partition_broadcast(128)), writes=["g2row"])
        P.dma(lambda e: e.dma_start(out=t["idf"][:], in_=k.identf), writes=["idf"])
        P.op("dve", lambda e: e.tensor_copy(out=t["idb"][:], in_=t["idf"][:]), reads=["idf"], writes=["idb"])
        P.op("pool", lambda e: e.memset(t["ones"][:], 1.0), writes=["ones"])
        P.dma(lambda e: e.dma_start(out=t["gng"][:], in_=k.group_norm_g[l].rearrange("(c p) -> p c", p=128)), writes=["gng"])
        for kk in range(8):
            ws = t["wst%d" % (kk % 2)]
            wk = "wst%d" % (kk % 2)
            P.dma(lambda e, kk=kk, ws=ws: e.dma_start(out=ws[:], in_=k.w_out[l][kk * 128:(kk + 1) * 128, :]), writes=[wk])
            P.op("dve", lambda e, kk=kk, ws=ws: e.tensor_scalar(out=t["wout"][:, kk, :], in0=ws[:], scalar1=t["gng"][:, kk:kk + 1], scalar2=None, op0=ALU.mult), reads=[wk, "gng"], writes=["wout"])
        P.dma(lambda e: e.dma_start(out=t["wrf"][:, :, 0:4], in_=k.moe_w_group[l].rearrange("(c p) n -> p c n", p=128)), writes=["wrf"])
        P.dma(lambda e: e.dma_start(out=t["wrf"][:, :, 4:36], in_=k.moe_w_expert[l].rearrange("(c p) n -> p c n", p=128)), writes=["wrf"])
        P.op("dve", lambda e: e.tensor_copy(out=t["wr"][:], in_=t["wrf"][:]), reads=["wrf"], writes=["wr"])
        yT_v = k.yT.rearrange("(c p) t -> p c t", p=128)
        fT_v = k.fT.rearrange("(c p) t -> p c t", p=128)
        def tile_body(t, b, i, other=None):
            P = k.P
            if True:
                t0 = b * TB + i * 128
                row = b if i < 16 else NB
                P.dma(lambda e, t0=t0: e.dma_start(out=t["yt"][:], in_=yT_v[:, :, t0:t0 + 128]), writes=["yt"])
                P.dma(lambda e, b=b, i=i: e.dma_start(out=t["xt0"][:], in_=xsrc(k, l, b, i)), writes=["xt0"])
                P.op("pool", lambda e: e.tensor_tensor(out=t["ysq"][:], in0=t["yt"][:], in1=t["yt"][:], op=ALU.mult), reads=["yt"], writes=["ysq"])
                for g in range(4):
                    for kc in range(2):
                        P.op("pe", lambda e, g=g, kc=kc: e.matmul(t["misc"][:, g:g + 1], lhsT=t["ysq"][:, 2 * g + kc, :], rhs=t["ones"][:], start=(kc == 0), stop=(kc == 1)), reads=["ysq", "ones"], writes=["misc"],
                             skip_self=(kc > 0))
                P.op("act", lambda e: e.activation(out=t["r"][:], in_=t["misc"][:, 0:4], func=AF.Sqrt, scale=1.0 / 256, bias=EPS), reads=["misc"], writes=["r"])
                P.op("dve", lambda e: e.reciprocal(out=t["r"][:], in_=t["r"][:]), reads=["r"], writes=["r"])
                for half in range(2):
                    for g in range(4):
                        ps = t["pm"][g % 2]
                        pk = "pm%d" % (g % 2)
                        for kc in range(2):
                            P.op("pe", lambda e, ps=ps, g=g, kc=kc, half=half: e.matmul(ps[:, :], lhsT=t["yt"][:, 2 * g + kc, :], rhs=t["wout"][:, 2 * g + kc, half * 512:(half + 1) * 512], start=(kc == 0), stop=(kc == 1)),
                                 reads=["yt", "wout"], writes=[pk], skip_self=(kc > 0))
                        if g == 0:
                            P.op("dve", lambda e, ps=ps, half=half: e.tensor_scalar(out=t["m"][:, half * 512:(half + 1) * 512], in0=ps[:, :], scalar1=t["r"][:, 0:1], scalar2=None, op0=ALU.mult), reads=[pk, "r"], writes=["m"])
                        else:
                            P.op("dve", lambda e, ps=ps, half=half, g=g: e.scalar_tensor_tensor(out=t["m"][:, half * 512:(half + 1) * 512], in0=ps[:, :], scalar=t["r"][:, g:g + 1], in1=t["m"][:, half * 512:(half + 1) * 512],
                                                                                             op0=ALU.mult, op1=ALU.add), reads=[pk, "r", "m"], writes=["m"])
                P.op("pool", lambda e: e.tensor_tensor(out=t["m"][:], in0=t["m"][:], in1=t["g1b"][:], op=ALU.mult), reads=["m", "g1b"], writes=["m"])
                P.op("dve", lambda e: e.tensor_tensor(out=t["xt0"][:], in0=t["xt0"][:], in1=t["m"][:], op=ALU.add), reads=["xt0", "m"], writes=["xt0"])
                P.dma(lambda e, t0=t0: e.dma_start(out=k.x1[t0:t0 + 128, :], in_=t["xt0"][:]), reads=["xt0"], writes=[("x1d", t0)], dkey="x1st")
                norm_mod_T(k, t, t["pst"], t["xt0"][:], "xt0", A, B, row, t["fT"], "fT", 0, "0")
                P.dma(lambda e, t0=t0: e.dma_start(out=fT_v[:, :, t0:t0 + 128], in_=t["fT"][:]), reads=["fT"], writes=[("fTd", t0)], dkey="fTst")
                for kk in range(8):
                    P.op("pe", lambda e, kk=kk: e.matmul(t["misc"][:, 64:100], lhsT=t["fT"][:, kk, :], rhs=t["wr"][:, kk, :], start=(kk == 0), stop=(kk == 7)), reads=["fT", "wr"], writes=["misc"], skip_self=(kk > 0))
                P.op("act", lambda e: e.copy(out=t["lg"][:], in_=t["misc"][:, 64:100]), reads=["misc"], writes=["lg"])
                gl = t["lg"][:, 0:4]
                el = t["lg"][:, 4:36].rearrange("p (g e) -> p g e", g=4)
                s1, s2, s3 = t["s1"], t["s2"], t["s3"]
                P.op("dve", lambda e: e.tensor_reduce(out=s1[:, 0:1], in_=gl, axis=AX.X, op=ALU.max), reads=["lg"], writes=["s1"])
                P.op("dve", lambda e: e.tensor_scalar(out=s1[:, 1:2], in0=s1[:, 0:1], scalar1=-1.0, scalar2=None, op0=ALU.mult), reads=["s1"], writes=["s1"])
                P.op("act", lambda e: e.activation(out=t["mg"][:], in_=gl, func=AF.Exp, bias=s1[:, 1:2], accum_out=s1[:, 2:3]), reads=["lg", "s1"], writes=["mg", "s1"])
                P.op("dve", lambda e: e.reciprocal(out=s1[:, 3:4], in_=s1[:, 2:3]), reads=["s1"], writes=["s1"])
                P.op("dve", lambda e: e.tensor_scalar(out=t["mg"][:], in0=gl, scalar1=s1[:, 0:1], scalar2=None, op0=ALU.is_equal), reads=["lg", "s1", "mg"], writes=["mg"])
                P.op("dve", lambda e: e.tensor_tensor(out=t["pr"][:], in0=el, in1=t["mg"][:].unsqueeze(2).to_broadcast([128, 4, 8]), op=ALU.mult), reads=["lg", "mg"], writes=["pr"])
                P.op("dve", lambda e: e.tensor_reduce(out=t["es"][:], in_=t["pr"][:].rearrange("p g e -> p e g"), axis=AX.X, op=ALU.add), reads=["pr"], writes=["es"])
                P.op("dve", lambda e: e.tensor_reduce(out=s2[:, 0:1], in_=t["es"][:], axis=AX.X, op=ALU.max), reads=["es"], writes=["s2"])
                P.op("dve", lambda e: e.tensor_scalar(out=t["m1"][:], in0=t["es"][:], scalar1=s2[:, 0:1], scalar2=None, op0=ALU.is_equal), reads=["es", "s2"], writes=["m1"])
                P.op("dve", lambda e: e.scalar_tensor_tensor(out=t["e2"][:], in0=t["m1"][:], scalar=-1e30, in1=t["es"][:], op0=ALU.mult, op1=ALU.add), reads=["m1", "es"], writes=["e2"])
                P.op("dve", lambda e: e.tensor_reduce(out=s2[:, 1:2], in_=t["e2"][:], axis=AX.X, op=ALU.max), reads=["e2"], writes=["s2"])
                P.op("dve", lambda e: e.tensor_scalar(out=t["m2"][:], in0=t["e2"][:], scalar1=s2[:, 1:2], scalar2=None, op0=ALU.is_equal), reads=["e2", "s2"], writes=["m2"])
                P.op("dve", lambda e: e.tensor_tensor(out=s2[:, 2:3], in0=s2[:, 1:2], in1=s2[:, 0:1], op=ALU.subtract), reads=["s2"], writes=["s2"])
                P.op("act", lambda e: e.activation(out=s2[:, 3:4], in_=s2[:, 2:3], func=AF.Exp), reads=["s2"], writes=["s2"])
                P.op("dve", lambda e: e.tensor_scalar(out=s3[:, 0:1], in0=s2[:, 3:4], scalar1=1.0, scalar2=None, op0=ALU.add), reads=["s2"], writes=["s3"])
                P.op("dve", lambda e: e.reciprocal(out=s3[:, 1:2], in_=s3[:, 0:1]), reads=["s3"], writes=["s3"])
                P.op("dve", lambda e: e.tensor_tensor(out=s3[:, 2:3], in0=s3[:, 1:2], in1=s1[:, 3:4], op=ALU.mult), reads=["s3", "s1"], writes=["s3"])
                P.op("dve", lambda e: e.tensor_tensor(out=s3[:, 3:4], in0=s3[:, 2:3], in1=s2[:, 3:4], op=ALU.mult), reads=["s3", "s2"], writes=["s3"])
                P.op("dve", lambda e: e.tensor_scalar(out=t["cw8"][:], in0=t["m1"][:], scalar1=s3[:, 2:3], scalar2=None, op0=ALU.mult), reads=["m1", "s3"], writes=["cw8"])
                P.op("dve", lambda e: e.scalar_tensor_tensor(out=t["cw8"][:], in0=t["m2"][:], scalar=s3[:, 3:4], in1=t["cw8"][:], op0=ALU.mult, op1=ALU.add), reads=["m2", "s3", "cw8"], writes=["cw8"])
                P.op("dve", lambda e: e.tensor_tensor(out=t["cw"][:], in0=t["mg"][:].unsqueeze(2).to_broadcast([128, 4, 8]), in1=t["cw8"][:].unsqueeze(1).to_broadcast([128, 4, 8]), op=ALU.mult),
                     reads=["mg", "cw8"], writes=["cw"])
                P.dma(lambda e, t0=t0: e.dma_start(out=k.cw[t0:t0 + 128, :], in_=t["cw"][:].rearrange("p g e -> p (g e)")), reads=["cw"], writes=[("cwd", t0)], dkey="cwst")
                mgb = t["mg"][:].unsqueeze(2).to_broadcast([128, 4, 8])
                P.op("dve", lambda e: e.tensor_tensor(out=t["oh1"][:], in0=mgb, in1=t["m1"][:].unsqueeze(1).to_broadcast([128, 4, 8]), op=ALU.mult), reads=["mg", "m1"], writes=["oh1"])
                P.op("dve", lambda e: e.tensor_tensor(out=t["oh2"][:], in0=mgb, in1=t["m2"][:].unsqueeze(1).to_broadcast([128, 4, 8]), op=ALU.mult), reads=["mg", "m2"], writes=["oh2"])
                oh1f = t["oh1"][:].rearrange("p g e -> p (g e)")
                oh2f = t["oh2"][:].rearrange("p g e -> p (g e)")
                P.op("dve", lambda e: e.tensor_tensor(out=t["ohs"][:], in0=oh1f, in1=oh2f, op=ALU.add), reads=["oh1", "oh2"], writes=["ohs"])
                P.op("pe", lambda e: e.matmul(t["misc"][:, 128:160], lhsT=t["LTf"][:], rhs=t["ohs"][:], start=True, stop=False), reads=["LTf", "ohs"], writes=["misc"])
                P.op("pe", lambda e: e.matmul(t["misc"][:, 128:160], lhsT=t["ONESf"][:], rhs=t["ohrun"][:], start=False, stop=(other is None)), reads=["ONESf", "ohrun"], writes=["misc"], skip_self=True)
                if other is not None:
                    P.op("pe", lambda e: e.matmul(t["misc"][:, 128:160], lhsT=t["ONESf"][:], rhs=other["ohs"][:], start=False, stop=True), reads=["ONESf", "ohs_0"], writes=["misc"], skip_self=True)
                for j, ohf, okey in ((0, oh1f, "oh1"), (1, oh2f, "oh2")):
                    P.op("dve", lambda e, ohf=ohf: e.tensor_tensor(out=t["ohp"][:], in0=t["misc"][:, 128:160], in1=ohf, op=ALU.mult), reads=["misc", okey], writes=["ohp"])
                    P.op("dve", lambda e, j=j: e.tensor_reduce(out=t["rt"][:, 2 + j:3 + j], in_=t["ohp"][:], axis=AX.X, op=ALU.add), reads=["ohp"], writes=["rt"])
                    P.op("dve", lambda e, ohf=ohf: e.tensor_tensor(out=t["ohp"][:], in0=t["iota32"][:], in1=ohf, op=ALU.mult), reads=["iota32", okey, "rt"], writes=["ohp"])
                    P.op("dve", lambda e, j=j: e.tensor_reduce(out=t["rt"][:, j:j + 1], in_=t["ohp"][:], axis=AX.X, op=ALU.add), reads=["ohp"], writes=["rt"])
                P.op("dve", lambda e: e.tensor_copy(out=t["rt"][:, 4:6], in_=s3[:, 2:4]), reads=["s3"], writes=["rt"])
                P.op("dve", lambda e: e.tensor_tensor(out=t["ohrun"][:], in0=t["ohrun"][:], in1=t["ohs"][:], op=ALU.add), reads=["ohrun", "ohs"], writes=["ohrun"])
                P.dma(lambda e, t0=t0: e.dma_start(out=k.rt[t0:t0 + 128, :], in_=t["rt"][:]), reads=["rt"], writes=[("rtd", t0)], dkey="rtst")
                P.op("pool", lambda e: e.tensor_tensor(out=t["ft1"][:], in0=t["xn0"][:], in1=t["A2row"][:], op=ALU.mult), reads=["xn0", "A2row"], writes=["ft1"])
                P.op("pool", lambda e: e.tensor_tensor(out=t["ftm"][:], in0=t["ft1"][:], in1=t["B2row"][:], op=ALU.add), reads=["ft1", "B2row"], writes=["ftm"])
                P.dma(lambda e, t0=t0: e.dma_start(out=k.ftm[t0:t0 + 128, :], in_=t["ftm"][:]), reads=["ftm"], writes=[("ftmd", t0)], dkey="ftmst")

        LOCALK = set(['yt', 'ysq', 'r', 'm', 'xt0', 'junk0', 'ss0', 'rstd0', 'xn0', 'fT', 'lg', 's1', 's2', 's3', 'mg', 'pr', 'es', 'm1', 'm2', 'e2', 'cw8', 'cw', 'oh1', 'oh2', 'ohs', 'ohp', 'rt', 'ft1', 'ftm']) | {"pst", "pm0", "pm1", "misc", "x1st", "fTst", "cwst", "rtst", "ftmst"}
        for b in range(NB):
            for i0 in range(0, ntile, 2):
                if i0 == 0 or i0 == 16:
                    row = b if i0 < 16 else NB
                    P.dma(lambda e, row=row: e.dma_start(out=t["g1b"][:], in_=k.modrow[l][row, 2 * D:3 * D].partition_broadcast(128)), reads=[("modrow", l)], writes=["g1b"])
                    P.dma(lambda e, row=row: e.dma_start(out=t["B2row"][:], in_=k.modrow[l][row, 3 * D:4 * D].partition_broadcast(128)), reads=[("modrow", l)], writes=["B2row"])
                    P.dma(lambda e, row=row: e.dma_start(out=t["A2row"][:], in_=k.modrow[l][row, 4 * D:5 * D].partition_broadcast(128)), reads=[("modrow", l)], writes=["A2row"])
                    P.op("dve", lambda e: e.scalar_tensor_tensor(out=t["A2row"][:], in0=t["A2row"][:], scalar=1.0, in1=t["g2row"][:], op0=ALU.add, op1=ALU.mult), reads=["A2row", "g2row"], writes=["A2row"])
                recs = []
                for s_ in range(2):
                    if i0 + s_ < ntile:
                        rec = Rec("_%d" % s_, LOCALK)
                        k.P = rec
                        tile_body(ts[s_], b, i0 + s_, other=(ts[0] if s_ == 1 else None))
                        recs.append(rec)
                k.P = P
                replay(P, recs)
        NBLK = k.nblk[l]
        P.op("pe", lambda e: e.matmul(ts[0]["misc"][:, 128:160], lhsT=t["ONESf"][:], rhs=t["ohrun"][:], start=True, stop=True), reads=["ONESf", "ohrun"], writes=["misc_0"])
        P.op("act", lambda e: e.copy(out=t["cnt"][:], in_=ts[0]["misc"][:, 128:160]), reads=["misc_0"], writes=["cnt"])
        P.op("dve", lambda e: e.tensor_scalar(out=t["nf"][:], in0=t["cnt"][:], scalar1=1.0 / MOE_BS, scalar2=None, op0=ALU.mult), reads=["cnt"], writes=["nf"])
        P.op("dve", lambda e: e.tensor_copy(out=t["ni"][:], in_=t["nf"][:]), reads=["nf"], writes=["ni"])
        P.op("dve", lambda e: e.tensor_copy(out=t["nf"][:], in_=t["ni"][:]), reads=["ni"], writes=["nf"])
        P.op("dve", lambda e: e.scalar_tensor_tensor(out=t["dd"][:], in0=t["nf"][:], scalar=float(MOE_BS), in1=t["cnt"][:], op0=ALU.mult, op1=ALU.subtract), reads=["nf", "cnt"], writes=["dd"])
        P.op("dve", lambda e: e.tensor_scalar(out=t["up"][:], in0=t["dd"][:], scalar1=0.0, scalar2=None, op0=ALU.is_lt), reads=["dd"], writes=["up"])
        P.op("dve", lambda e: e.tensor_tensor(out=t["nf"][:], in0=t["nf"][:], in1=t["up"][:], op=ALU.add), reads=["nf", "up"], writes=["nf"])
        P.op("dve", lambda e: e.scalar_tensor_tensor(out=t["dd"][:], in0=t["up"][:], scalar=float(MOE_BS), in1=t["dd"][:], op0=ALU.mult, op1=ALU.add), reads=["up", "dd"], writes=["dd"])
        P.op("dve", lambda e: e.tensor_scalar(out=t["up"][:], in0=t["dd"][:], scalar1=float(MOE_BS), scalar2=None, op0=ALU.is_ge), reads=["dd"], writes=["up"])
        P.op("dve", lambda e: e.tensor_tensor(out=t["nf"][:], in0=t["nf"][:], in1=t["up"][:], op=ALU.subtract), reads=["nf", "up"], writes=["nf"])
        P.op("dve", lambda e: e.tensor_scalar(out=t["nf"][:], in0=t["nf"][:], scalar1=float(MOE_BS), scalar2=None, op0=ALU.mult), reads=["nf"], writes=["nf"])
        P.op("dve", lambda e: e.tensor_tensor_scan(out=t["pend"][:], data0=t["nf"][:], data1=t["zero32"][:], initial=0.0, op0=ALU.add, op1=ALU.add), reads=["nf", "zero32"], writes=["pend"])
        P.op("dve", lambda e: e.tensor_tensor(out=t["nf"][:], in0=t["pend"][:], in1=t["nf"][:], op=ALU.subtract), reads=["pend", "nf"], writes=["nf"])
        P.dma(lambda e: e.dma_start(out=k.pstart[l], in_=t["nf"][:]), reads=["nf"], writes=[("pstart", l)], dkey="pstst")
        P.op("dve", lambda e: e.tensor_tensor(out=t["cmp"][:, 0:NBLK, :], in0=t["pend"][:].unsqueeze(1).to_broadcast([128, NBLK, 32]),
                                              in1=t["jbv"][:, 0:NBLK].unsqueeze(2).to_broadcast([128, NBLK, 32]), op=ALU.is_le), reads=["pend", "jbv"], writes=["cmp"])
        P.op("dve", lambda e: e.tensor_reduce(out=t["bexp"][:, 0:NBLK], in_=t["cmp"][:, 0:NBLK, :], axis=AX.X, op=ALU.add), reads=["cmp"], writes=["bexp"])
        P.op("dve", lambda e: e.tensor_scalar(out=t["bexp"][:, 0:NBLK], in0=t["bexp"][:, 0:NBLK], scalar1=31.0, scalar2=None, op0=ALU.min), reads=["bexp"], writes=["bexp"])
        P.dma(lambda e: e.dma_start(out=k.bexp[l][:, 0:NBLK], in_=t["bexp"][:, 0:NBLK]), reads=["bexp"], writes=[("bexp", l)], dkey="bexst")
        P.barrier()
        P.emit()


MOE_BS = 512
MAXBLK = 72
I32 = mybir.dt.int32


def stage_moe_sparse(k, l):
    nc, P, NB = k.nc, k.P, k.NB
    last = (l == DEPTH - 1)
    ntile = 16 if last else 18
    NBLK = k.nblk[l]
    NSLOT = NBLK * MOE_BS
    with ExitStack() as es:
        t = _tiles(es, nc, [("idf", (128, 128), F32), ("idb", (128, 128), BF16), ("zeros", (128, 4, D), BF16),
                            ("pstart", (128, 32), F32), ("bexp", (128, MAXBLK), F32), ("iota32", (128, 32), F32), ("iotaA", (128, 8), F32), ("iotaB", (128, 4), F32),
                            ("e1k", (128, MAXBLK), F32), ("ixf", (128, MAXBLK, 8), F32), ("ix1", (128, MAXBLK, 8), I32), ("ix2", (128, MAXBLK, 4), I32),
                            ("rtt", (128, 8), F32), ("ohq", (128, 32), F32), ("dsf", (128, 2), F32),
                            ("dest", (128, NB * 18, 2), I32), ("wgt", (128, NB * 18, 2), F32),
                            ("ft0", (128, D), BF16), ("ft1", (128, D), BF16),
                            ("stA0", (128, 8, 512), F32), ("stA1", (128, 8, 512), F32), ("stB0", (128, 8, 512), F32), ("stB1", (128, 8, 512), F32),
                            ("stC0", (128, 4, D), F32), ("stC1", (128, 4, D), F32),
                            ("w1b", (128, 8, 512), BF16), ("w3b", (128, 8, 512), BF16), ("w2b", (128, 4, D), BF16),
                            ("xb0", (128, 4, D), BF16), ("xb1", (128, 4, D), BF16), ("xT", (128, 8, 512), BF16),
                            ("s1", (128, 512), F32), ("act", (128, 4, 512), BF16), ("yb0", (128, 4, D), BF16), ("yb1", (128, 4, D), BF16),
                            ("y1", (128, D), BF16), ("y2", (128, D), BF16), ("acc", (128, D), F32), ("g2b", (128, D), F32), ("xt", (128, D), F32)])
        pstb = _psum(es, nc, "mpst", [128, 8, 128], BF16)
        p1 = [_psum(es, nc, "mp1_%d" % i, [128, 512], F32) for i in range(2)]
        p3 = [_psum(es, nc, "mp3_%d" % i, [128, 512], F32) for i in range(2)]
        py = [_psum(es, nc, "mpy_%d" % i, [128, 512], F32) for i in range(2)]
        P.dma(lambda e: e.dma_start(out=t["idf"][:], in_=k.identf), writes=["idf"])
        P.op("dve", lambda e: e.tensor_copy(out=t["idb"][:], in_=t["idf"][:]), reads=["idf"], writes=["idb"])
        P.op("dve", lambda e: e.memset(t["zeros"][:], 0.0), writes=["zeros"])
        for nm, src in (("iota32", k.c_iota32), ("iotaA", k.c_iotaA), ("iotaB", k.c_iotaB), ("pstart", k.pstart[l])):
            P.dma(lambda e, nm=nm, src=src: e.dma_start(out=t[nm][:], in_=src), writes=[nm])
        P.dma(lambda e: e.dma_start(out=t["bexp"][:, 0:NBLK], in_=k.bexp[l][:, 0:NBLK]), writes=["bexp"])
        xs_v = k.xs.rearrange("(n p) d -> p n d", p=128)
        ys_v = k.ys.rearrange("(n p) d -> p n d", p=128)
        for n0 in range(0, NSLOT // 128, 4):
            P.dma(lambda e, n0=n0: e.dma_start(out=xs_v[:, n0:n0 + 4, :], in_=t["zeros"][:]), reads=["zeros"], writes=["xs"], dkey="xszero")
            P.dma(lambda e, n0=n0: e.dma_start(out=ys_v[:, n0:n0 + 4, :], in_=t["zeros"][:]), reads=["zeros"], writes=["ys"], dkey="yszero")
        P.op("dve", lambda e: e.tensor_scalar(out=t["e1k"][:, 0:NBLK], in0=t["bexp"][:, 0:NBLK], scalar1=1024.0, scalar2=float(l * 32 * 1024), op0=ALU.mult, op1=ALU.add), reads=["bexp"], writes=["e1k"])
        P.op("dve", lambda e: e.tensor_tensor(out=t["ixf"][:, 0:NBLK, :], in0=t["e1k"][:, 0:NBLK].unsqueeze(2).to_broadcast([128, NBLK, 8]),
                                              in1=t["iotaA"][:].unsqueeze(1).to_broadcast([128, NBLK, 8]), op=ALU.add), reads=["e1k", "iotaA"], writes=["ixf"])
        P.op("dve", lambda e: e.tensor_copy(out=t["ix1"][:, 0:NBLK, :], in_=t["ixf"][:, 0:NBLK, :]), reads=["ixf"], writes=["ix1"])
        P.op("dve", lambda e: e.tensor_scalar(out=t["e1k"][:, 0:NBLK], in0=t["bexp"][:, 0:NBLK], scalar1=512.0, scalar2=float(l * 32 * 512), op0=ALU.mult, op1=ALU.add), reads=["bexp", "ixf"], writes=["e1k"])
        P.op("dve", lambda e: e.tensor_tensor(out=t["ixf"][:, 0:NBLK, 0:4], in0=t["e1k"][:, 0:NBLK].unsqueeze(2).to_broadcast([128, NBLK, 4]),
                                              in1=t["iotaB"][:].unsqueeze(1).to_broadcast([128, NBLK, 4]), op=ALU.add), reads=["e1k", "iotaB", "ix1"], writes=["ixf"])
        P.op("dve", lambda e: e.tensor_copy(out=t["ix2"][:, 0:NBLK, :], in_=t["ixf"][:, 0:NBLK, 0:4]), reads=["ixf"], writes=["ix2"])
        tl = []
        for b in range(NB):
            for i in range(ntile):
                tl.append((b, i))
        for n, (b, i) in enumerate(tl):
            t0 = b * TB + i * 128
            ft = t["ft%d" % (n % 2)]
            fk = "ft%d" % (n % 2)
            P.dma(lambda e, t0=t0: e.dma_start(out=t["rtt"][:], in_=k.rt[t0:t0 + 128, :]), writes=["rtt"])
            P.dma(lambda e, t0=t0, ft=ft: e.dma_start(out=ft[:], in_=k.ftm[t0:t0 + 128, :]), writes=[fk])
            for j in range(2):
                P.op("dve", lambda e, j=j: e.tensor_scalar(out=t["ohq"][:], in0=t["iota32"][:], scalar1=t["rtt"][:, j:j + 1], scalar2=None, op0=ALU.is_equal), reads=["iota32", "rtt"], writes=["ohq"])
                P.op("dve", lambda e: e.tensor_tensor(out=t["ohq"][:], in0=t["ohq"][:], in1=t["pstart"][:], op=ALU.mult), reads=["ohq", "pstart"], writes=["ohq"])
                P.op("dve", lambda e, j=j: e.tensor_reduce(out=t["dsf"][:, j:j + 1], in_=t["ohq"][:], axis=AX.X, op=ALU.add), reads=["ohq"], writes=["dsf"])
            P.op("dve", lambda e: e.tensor_tensor(out=t["dsf"][:], in0=t["dsf"][:], in1=t["rtt"][:, 2:4], op=ALU.add), reads=["dsf", "rtt"], writes=["dsf"])
            P.op("dve", lambda e, n=n: e.tensor_copy(out=t["dest"][:, n, :], in_=t["dsf"][:]), reads=["dsf"], writes=[("dest", n)])
            P.op("dve", lambda e, n=n: e.tensor_copy(out=t["wgt"][:, n, :], in_=t["rtt"][:, 4:6]), reads=["rtt"], writes=[("wgt", n)])
            for j in range(2):
                P.dma(lambda e, n=n, j=j, ft=ft: e.indirect_dma_start(out=k.xs[:, :], out_offset=bass.IndirectOffsetOnAxis(ap=t["dest"][:, n, j:j + 1], axis=0), in_=ft[:, :], in_offset=None),
                      reads=[fk, ("dest", n), "xs"], writes=[("xsw", n, j)], q="pool", dkey=("sw_sc" if STRICT_SCATTER else ("sw_sc", j, n % 4)))
        xs_dep = [("xsw", n, j) for n in range(len(tl)) for j in range(2)]
        w1_rows = k.moe_w1.rearrange("l e r n -> (l e r) n")
        w3_rows = k.moe_w3.rearrange("l e r n -> (l e r) n")
        w2_rows = k.moe_w2.rearrange("l e r n -> (l e r) n")
        for jb in range(NBLK):
            sa, sb_, sc_ = t["stA%d" % (jb % 2)], t["stB%d" % (jb % 2)], t["stC%d" % (jb % 2)]
            ka, kb, kc = "stA%d" % (jb % 2), "stB%d" % (jb % 2), "stC%d" % (jb % 2)
            for kk in range(8):
                P.dma(lambda e, jb=jb, kk=kk, sa=sa: e.indirect_dma_start(out=sa[:, kk, :], out_offset=None, in_=w1_rows[:, :], in_offset=bass.IndirectOffsetOnAxis(ap=t["ix1"][:, jb, kk:kk + 1], axis=0)), reads=["ix1"], writes=[ka], q="pool", dkey=("sw_w", ka, kk))
                P.dma(lambda e, jb=jb, kk=kk, sb_=sb_: e.indirect_dma_start(out=sb_[:, kk, :], out_offset=None, in_=w3_rows[:, :], in_offset=bass.IndirectOffsetOnAxis(ap=t["ix1"][:, jb, kk:kk + 1], axis=0)), reads=["ix1"], writes=[kb], q="pool", dkey=("sw_w", kb, kk))
            for c in range(4):
                P.dma(lambda e, jb=jb, c=c, sc_=sc_: e.indirect_dma_start(out=sc_[:, c, :], out_offset=None, in_=w2_rows[:, :], in_offset=bass.IndirectOffsetOnAxis(ap=t["ix2"][:, jb, c:c + 1], axis=0)), reads=["ix2"], writes=[kc], q="pool", dkey=("sw_w", kc, c))
            P.op("act", lambda e, sa=sa: e.copy(out=t["w1b"][:], in_=sa[:]), reads=[ka], writes=["w1b"])
            P.op("act", lambda e, sb_=sb_: e.copy(out=t["w3b"][:], in_=sb_[:]), reads=[kb], writes=["w3b"])
            P.op("dve", lambda e, sc_=sc_: e.tensor_copy(out=t["w2b"][:], in_=sc_[:]), reads=[kc], writes=["w2b"])
            xb = t["xb%d" % (jb % 2)]
            xk = "xb%d" % (jb % 2)
            P.dma(lambda e, jb=jb, xb=xb: e.dma_start(out=xb[:], in_=xs_v[:, jb * 4:(jb + 1) * 4, :]), reads=["xs"] + xs_dep, writes=[xk])
            for sidx in range(4):
                for kk in range(8):
                    P.op("pe", lambda e, sidx=sidx, kk=kk, xb=xb: e.transpose(out=pstb[:, kk, :], in_=xb[:, sidx, kk * 128:(kk + 1) * 128], identity=t["idb"][:]), reads=[xk, "idb"], writes=["mpst"])
                P.op("dve", lambda e, sidx=sidx: e.tensor_copy(out=t["xT"][:, :, sidx * 128:(sidx + 1) * 128], in_=pstb[:, :, :]), reads=["mpst"], writes=["xT"])
            W = MOE_BS
            for ffc in range(4):
                pa, pb = p1[ffc % 2], p3[ffc % 2]
                kpa, kpb = "mp1_%d" % (ffc % 2), "mp3_%d" % (ffc % 2)
                for kk in range(8):
                    P.op("pe", lambda e, pa=pa, kk=kk, ffc=ffc: e.matmul(pa[:, 0:W], lhsT=t["w1b"][:, kk, ffc * 128:(ffc + 1) * 128], rhs=t["xT"][:, kk, :], start=(kk == 0), stop=(kk == 7)),
                         reads=["w1b", "xT"], writes=[kpa], skip_self=(kk > 0))
                for kk in range(8):
                    P.op("pe", lambda e, pb=pb, kk=kk, ffc=ffc: e.matmul(pb[:, 0:W], lhsT=t["w3b"][:, kk, ffc * 128:(ffc + 1) * 128], rhs=t["xT"][:, kk, :], start=(kk == 0), stop=(kk == 7)),
                         reads=["w3b", "xT"], writes=[kpb], skip_self=(kk > 0))
                P.op("act", lambda e, pa=pa: e.activation(out=t["s1"][:, 0:W], in_=pa[:, 0:W], func=AF.Silu), reads=[kpa], writes=["s1"])
                P.op("dve", lambda e, pb=pb, ffc=ffc: e.tensor_tensor(out=t["act"][:, ffc, 0:W], in0=pb[:, 0:W], in1=t["s1"][:, 0:W], op=ALU.mult), reads=[kpb, "s1"], writes=["act"])
            yb = t["yb%d" % (jb % 2)]
            yk = "yb%d" % (jb % 2)
            for sidx in range(4):
                for half in range(2):
                    ps = py[half]
                    pk = "mpy_%d" % half
                    for ffc in range(4):
                        P.op("pe", lambda e, ps=ps, ffc=ffc, sidx=sidx, half=half: e.matmul(ps[:, :], lhsT=t["act"][:, ffc, sidx * 128:(sidx + 1) * 128], rhs=t["w2b"][:, ffc, half * 512:(half + 1) * 512],
                                                                                      start=(ffc == 0), stop=(ffc == 3)), reads=["act", "w2b"], writes=[pk], skip_self=(ffc > 0))
                    if half == 0:
                        P.op("act", lambda e, ps=ps, sidx=sidx, yb=yb: e.copy(out=yb[:, sidx, 0:512], in_=ps[:, :]), reads=[pk], writes=[yk])
                    else:
                        P.op("dve", lambda e, ps=ps, sidx=sidx, yb=yb: e.tensor_copy(out=yb[:, sidx, 512:1024], in_=ps[:, :]), reads=[pk], writes=[yk])
            P.dma(lambda e, jb=jb, yb=yb: e.dma_start(out=ys_v[:, jb * 4:(jb + 1) * 4, :], in_=yb[:]), reads=[yk, "ys"], writes=[("ysw", jb)], dkey=("ysst", jb % 2))
        ys_dep = [("ysw", jb) for jb in range(NBLK)]
        for n, (b, i) in enumerate(tl):
            t0 = b * TB + i * 128
            row = b if i < 16 else NB
            if i == 0 or i == 16:
                P.dma(lambda e, row=row: e.dma_start(out=t["g2b"][:], in_=k.modrow[l][row, 5 * D:6 * D].partition_broadcast(128)), writes=["g2b"])
            for j, yn in ((0, "y1"), (1, "y2")):
                P.dma(lambda e, n=n, j=j, yn=yn: e.indirect_dma_start(out=t[yn][:, :], out_offset=None, in_=k.ys[:, :], in_offset=bass.IndirectOffsetOnAxis(ap=t["dest"][:, n, j:j + 1], axis=0)), reads=[("dest", n), "ys"] + ys_dep, writes=[yn], q="pool", dkey=("sw_y", yn))
            P.dma(lambda e, t0=t0: e.dma_start(out=t["xt"][:], in_=k.x1[t0:t0 + 128, :]), writes=["xt"])
            P.op("dve", lambda e, n=n: e.tensor_scalar(out=t["acc"][:], in0=t["y1"][:], scalar1=t["wgt"][:, n, 0:1], scalar2=None, op0=ALU.mult), reads=["y1", ("wgt", n)], writes=["acc"])
            P.op("dve", lambda e, n=n: e.scalar_tensor_tensor(out=t["acc"][:], in0=t["y2"][:], scalar=t["wgt"][:, n, 1:2], in1=t["acc"][:], op0=ALU.mult, op1=ALU.add), reads=["y2", ("wgt", n), "acc"], writes=["acc"])
            P.op("dve", lambda e: e.tensor_tensor(out=t["acc"][:], in0=t["acc"][:], in1=t["g2b"][:], op=ALU.mult), reads=["acc", "g2b"], writes=["acc"])
            P.op("dve", lambda e: e.tensor_tensor(out=t["xt"][:], in0=t["xt"][:], in1=t["acc"][:], op=ALU.add), reads=["xt", "acc"], writes=["xt"])
            if last:
                P.dma(lambda e, b=b, i=i: e.dma_start(out=k.out[b, i * 128:(i + 1) * 128, :], in_=t["xt"][:]), reads=["xt"], writes=[("outd", b, i)], dkey="outst")
            else:
                P.dma(lambda e, t0=t0: e.dma_start(out=k.x2[t0:t0 + 128, :], in_=t["xt"][:]), reads=["xt"], writes=[("x2d", t0)], dkey="outst")
        P.barrier()
        P.emit()


def stage_moe(k, l):
    nc, P, NB = k.nc, k.P, k.NB
    last = (l == DEPTH - 1)
    ntile = 16 if last else 18
    blocks = BLK5[:4] if last else BLK5
    with ExitStack() as es:
        t = _tiles(es, nc, [("fTb", (128, 8, TB), BF16), ("acc", (128, 18, D), F32), ("cwb", (128, 18, 32), F32),
                            ("st0", (128, 8, 512), F32), ("st1", (128, 8, 512), F32),
                            ("w1b", (128, 8, 512), BF16), ("w3b", (128, 8, 512), BF16), ("w2b", (128, 4, D), BF16),
                            ("s1", (128, 512), F32), ("act", (128, 4, 512), BF16), ("g2b", (128, D), F32), ("xt", (128, D), F32)])
        p1 = [_psum(es, nc, "mp1_%d" % i, [128, 512], F32) for i in range(2)]
        p3 = [_psum(es, nc, "mp3_%d" % i, [128, 512], F32) for i in range(2)]
        py = [_psum(es, nc, "mpy_%d" % i, [128, 512], F32) for i in range(2)]
        fT_v = k.fT.rearrange("(c p) t -> p c t", p=128)
        cw_v = k.cw.rearrange("(n p) e -> p n e", p=128)
        sc = 0
        for b in range(NB):
            nb_tok = ntile * 128
            for c in range(8):
                P.dma(lambda e, b=b, c=c: e.dma_start(out=t["fTb"][:, c, 0:nb_tok], in_=fT_v[:, c, b * TB:b * TB + nb_tok]), writes=["fTb"])
            P.dma(lambda e, b=b: e.dma_start(out=t["cwb"][:, 0:ntile, :], in_=cw_v[:, b * 18:b * 18 + ntile, :]), writes=["cwb"])
            for ex in range(32):
                for nm, src, dst in (("w1", k.moe_w1, "w1b"), ("w3", k.moe_w3, "w3b")):
                    st = t["st%d" % (sc % 2)]
                    sk = "st%d" % (sc % 2)
                    sc += 1
                    P.dma(lambda e, st=st, src=src, ex=ex: e.dma_start(out=st[:], in_=src[l][ex].rearrange("(c p) n -> p c n", p=128)), writes=[sk])
                    P.op("pool", lambda e, st=st, dst=dst: e.tensor_copy(out=t[dst][:], in_=st[:]), reads=[sk], writes=[dst])
                st = t["st%d" % (sc % 2)]
                sk = "st%d" % (sc % 2)
                sc += 1
                stv = st[:].rearrange("p a b -> p (a b)").rearrange("p (c n) -> p c n", c=4)
                P.dma(lambda e, stv=stv, ex=ex: e.dma_start(out=stv, in_=k.moe_w2[l][ex].rearrange("(c p) n -> p c n", p=128)), writes=[sk])
                P.op("pool", lambda e, stv=stv: e.tensor_copy(out=t["w2b"][:], in_=stv), reads=[sk], writes=["w2b"])
                for (g0, W) in blocks:
                    for ffc in range(4):
                        pa, pb = p1[ffc % 2], p3[ffc % 2]
                        ka, kb = "mp1_%d" % (ffc % 2), "mp3_%d" % (ffc % 2)
                        for kk in range(8):
                            P.op("pe", lambda e, pa=pa, kk=kk, ffc=ffc, g0=g0, W=W: e.matmul(pa[:, 0:W], lhsT=t["w1b"][:, kk, ffc * 128:(ffc + 1) * 128], rhs=t["fTb"][:, kk, g0:g0 + W], start=(kk == 0), stop=(kk == 7)),
                                 reads=["w1b", "fTb"], writes=[ka], skip_self=(kk > 0))
                        for kk in range(8):
                            P.op("pe", lambda e, pb=pb, kk=kk, ffc=ffc, g0=g0, W=W: e.matmul(pb[:, 0:W], lhsT=t["w3b"][:, kk, ffc * 128:(ffc + 1) * 128], rhs=t["fTb"][:, kk, g0:g0 + W], start=(kk == 0), stop=(kk == 7)),
                                 reads=["w3b", "fTb"], writes=[kb], skip_self=(kk > 0))
                        P.op("act", lambda e, pa=pa, W=W: e.activation(out=t["s1"][:, 0:W], in_=pa[:, 0:W], func=AF.Silu), reads=[ka], writes=["s1"])
                        P.op("dve", lambda e, pb=pb, W=W, ffc=ffc: e.tensor_tensor(out=t["act"][:, ffc, 0:W], in0=pb[:, 0:W], in1=t["s1"][:, 0:W], op=ALU.mult), reads=[kb, "s1"], writes=["act"])
                    for sidx in range(W // 128):
                        ti = g0 // 128 + sidx
                        for half in range(2):
                            ps = py[half]
                            pk = "mpy_%d" % half
                            for ffc in range(4):
                                P.op("pe", lambda e, ps=ps, ffc=ffc, sidx=sidx, half=half: e.matmul(ps[:, :], lhsT=t["act"][:, ffc, sidx * 128:(sidx + 1) * 128], rhs=t["w2b"][:, ffc, half * 512:(half + 1) * 512],
                                                                                              start=(ffc == 0), stop=(ffc == 3)), reads=["act", "w2b"], writes=[pk], skip_self=(ffc > 0))
                            dst = t["acc"][:, ti, half * 512:(half + 1) * 512]
                            if ex == 0:
                                P.op("dve", lambda e, ps=ps, dst=dst, ti=ti, ex=ex: e.tensor_scalar(out=dst, in0=ps[:, :], scalar1=t["cwb"][:, ti, ex:ex + 1], scalar2=None, op0=ALU.mult), reads=[pk, "cwb"], writes=["acc"])
                            else:
                                P.op("dve", lambda e, ps=ps, dst=dst, ti=ti, ex=ex: e.scalar_tensor_tensor(out=dst, in0=ps[:, :], scalar=t["cwb"][:, ti, ex:ex + 1], in1=dst, op0=ALU.mult, op1=ALU.add),
                                     reads=[pk, "cwb", "acc"], writes=["acc"])
            for i in range(ntile):
                t0 = b * TB + i * 128
                row = b if i < 16 else NB
                if i == 0 or i == 16:
                    P.dma(lambda e, row=row: e.dma_start(out=t["g2b"][:], in_=k.modrow[l][row, 5 * D:6 * D].partition_broadcast(128)), writes=["g2b"])
                P.dma(lambda e, t0=t0: e.dma_start(out=t["xt"][:], in_=k.x1[t0:t0 + 128, :]), writes=["xt"])
                P.op("pool", lambda e, i=i: e.tensor_tensor(out=t["acc"][:, i, :], in0=t["acc"][:, i, :], in1=t["g2b"][:], op=ALU.mult), reads=["acc", "g2b"], writes=["acc"])
                P.op("dve", lambda e, i=i: e.tensor_tensor(out=t["xt"][:], in0=t["xt"][:], in1=t["acc"][:, i, :], op=ALU.add), reads=["xt", "acc"], writes=["xt"])
                if last:
                    P.dma(lambda e, b=b, i=i: e.dma_start(out=k.out[b, i * 128:(i + 1) * 128, :], in_=t["xt"][:]), reads=["xt"], writes=[("outd", b, i)], dkey="outst")
                else:
                    P.dma(lambda e, t0=t0: e.dma_start(out=k.x2[t0:t0 + 128, :], in_=t["xt"][:]), reads=["xt"], writes=[("x2d", t0)], dkey="outst")
        P.barrier()
        P.emit()


def build_program(NB, n_stages=99, dbg=()):
    nc = bass.Bass("TRN2", target_bir_lowering=False)
    k = K()
    k.nc, k.NB = nc, NB
    k.P = Prog(nc)
    T = NB * TB
    k.T = T

    def din(name, shape, dt=F32):
        return nc.dram_tensor(name, list(shape), dt, kind="ExternalInput").ap()

    def dscr(name, shape, dt, out=False):
        return nc.dram_tensor(name, list(shape), dt, kind=("ExternalOutput" if out else "Internal")).ap()

    k.x = din("x", (NB, SEQ, D)); k.ctx = din("ctx", (NB, NCTX, D)); k.c = din("c", (NB, D)); k.c_ctx = din("c_ctx", (D,))
    k.w_mod = din("w_mod", (DEPTH, D, 6 * D)); k.b_mod = din("b_mod", (DEPTH, 6 * D))
    k.norm1_g = din("norm1_g", (DEPTH, D)); k.norm2_g = din("norm2_g", (DEPTH, D))
    k.w_in = din("w_in", (DEPTH, D, INC))
    k.identf = din("identf", (128, 128))
    k.mla_q_norm_g = din("mla_q_norm_g", (DEPTH, 192)); k.mla_w_uq = din("mla_w_uq", (DEPTH, 192, 384))
    k.mla_kv_norm_g = din("mla_kv_norm_g", (DEPTH, 128)); k.mla_w_ukv = din("mla_w_ukv", (DEPTH, 128, 512))
    k.mla_qn_g = din("mla_qn_g", (DEPTH, 96)); k.mla_kn_g = din("mla_kn_g", (DEPTH, 96))
    k.ret_log_gamma = din("ret_log_gamma", (DEPTH, 2, 4)); k.ret_norm_g = din("ret_norm_g", (DEPTH, 256))
    k.lru_conv_w = din("lru_conv_w", (DEPTH, 4, 256)); k.lru_conv_b = din("lru_conv_b", (DEPTH, 256))
    k.lru_wa = din("lru_wa", (DEPTH, 2, 4, 64, 64)); k.lru_ba = din("lru_ba", (DEPTH, 2, 256))
    k.lru_wx = din("lru_wx", (DEPTH, 2, 4, 64, 64)); k.lru_bx = din("lru_bx", (DEPTH, 2, 256)); k.lru_lambda = din("lru_lambda", (DEPTH, 2, 256))
    k.hy_conv_w = din("hy_conv_w", (DEPTH, 3, 768)); k.hy_conv_b = din("hy_conv_b", (DEPTH, 768))
    k.hy_w1 = din("hy_w1", (DEPTH, 33, 64)); k.hy_b1 = din("hy_b1", (DEPTH, 64)); k.hy_w2 = din("hy_w2", (DEPTH, 64, 64)); k.hy_b2 = din("hy_b2", (DEPTH, 64))
    k.hy_w3 = din("hy_w3", (DEPTH, 64, 512)); k.hy_freq = din("hy_freq", (DEPTH, 64)); k.hy_d = din("hy_d", (DEPTH, 256))
    k.c_m0 = din("c_m0", (128, 2))
    k.hc = {}
    for Lh in (SEQ, NCTX):
        k.hc[Lh] = {"zf": din("hz_f%d" % Lh, (33, Lh)), "zr": din("hz_r%d" % Lh, (33, Lh)), "tf": din("ht_f%d" % Lh, (128, Lh // 128)), "tr": din("ht_r%d" % Lh, (128, Lh // 128)),
                    "dl": din("h_dl%d" % Lh, (128, 256)), "Wf": din("h_Wf%d" % Lh, (Lh, 2 * Lh), BF16), "Winv": din("h_Wi%d" % Lh, (2 * Lh, Lh), BF16),
                    "csl": din("h_csl%d" % Lh, (128, 2 * Lh // 128)), "csh": din("h_csh%d" % Lh, (128, 2 * Lh // 128))}
    k.group_norm_g = din("group_norm_g", (DEPTH, D)); k.w_out = din("w_out", (DEPTH, D, D))
    k.moe_w_group = din("moe_w_group", (DEPTH, D, 4)); k.moe_w_expert = din("moe_w_expert", (DEPTH, D, 32))
    k.moe_w1 = din("moe_w1", (DEPTH, 32, D, 512)); k.moe_w3 = din("moe_w3", (DEPTH, 32, D, 512)); k.moe_w2 = din("moe_w2", (DEPTH, 32, 512, D))
    k.x1 = dscr("x1", (T, D), F32, out=("x1" in dbg)); k.fT = dscr("fT", (D, T), BF16); k.cw = dscr("cw", (T, 32), F32, out=("cw" in dbg))
    k.out = nc.dram_tensor("out", [NB, SEQ, D], F32, kind="ExternalOutput").ap()
    k.c_iota32 = din("c_iota32", (128, 32)); k.c_LT = din("c_LT", (128, 128)); k.c_ONES = din("c_ONES", (128, 128)); k.c_jbv = din("c_jbv", (128, 128))
    k.c_iotaA = din("c_iotaA", (128, 8)); k.c_iotaB = din("c_iotaB", (128, 4))
    k.nblk = [-(-(2 * NB * nt_ * 128) // MOE_BS) + 32 for nt_ in (18, 16)]
    assert max(k.nblk) <= MAXBLK
    k.rt = dscr("rt", (T, 8), F32); k.ftm = dscr("ftm", (T, D), BF16)
    k.pstart = dscr("pstart", (DEPTH, 128, 32), F32); k.bexp = dscr("bexp", (DEPTH, 128, MAXBLK), F32)
    k.xs = dscr("xs", (max(k.nblk) * MOE_BS, D), BF16); k.ys = dscr("ys", (max(k.nblk) * MOE_BS, D), BF16)
    k.rope_cos = din("rope_cos", (SEQ, 16)); k.rope_sin = din("rope_sin", (SEQ, 16))
    k.c_rel0 = din("c_rel0", (128, 128)); k.c_mge = din("c_mge", (128, 128)); k.c_mle = din("c_mle", (128, 128)); k.c_dvals = din("c_dvals", (128, 18))
    k.yT = dscr("yT", (D, T), BF16, out=("yT" in dbg))
    k.cc = 0; k.oc = 0; k.yc = 0
    k.modrow = dscr("modrow", (DEPTH, NB + 1, 6 * D), F32, out=("modrow" in dbg))
    k.zt = dscr("zt", (T, ZT_W), BF16, out=("zt" in dbg))
    k.zf = dscr("zf", (ZF_ROWS, T), BF16, out=("zf" in dbg))
    k.x2 = dscr("x2", (T, D), F32)
    with nc.allow_low_precision("bf16 matmul operands, fp32 accumulation"), nc.allow_non_contiguous_dma("small strided loads"):
        stage_mod(k)
        stages = []
        for l in range(DEPTH):
            stages += [lambda l=l: stage_inproj(k, l), lambda l=l: stage_mla(k, l), lambda l=l: stage_ret(k, l), lambda l=l: stage_lru(k, l),
                       lambda l=l: stage_hyena(k, l, SEQ, 0)]
            if l < DEPTH - 1:
                stages.append(lambda l=l: stage_hyena(k, l, NCTX, SEQ))
            stages += [lambda l=l: stage_outproj(k, l), lambda l=l: (stage_moe_sparse(k, l) if SPARSE_MOE else stage_moe(k, l))]
        for si, f in enumerate(stages[:max(0, n_stages - 1)]):
            with nc.named_scope("st%02d" % si):
                f()
    return nc, k


def host_consts():
    c = {"identf": np.eye(128, dtype=np.float32)}
    rows = SEQ // 64
    row = np.repeat(np.arange(rows), 64).astype(np.float32)
    col = np.tile(np.arange(64), rows).astype(np.float32)
    inv_freq = (10000.0 ** (-np.arange(8, dtype=np.float32) / 8)).astype(np.float32)
    ang = np.stack([row[:, None] * inv_freq, col[:, None] * inv_freq], axis=1).astype(np.float32)
    c["rope_cos"] = np.cos(ang).reshape(SEQ, 16).astype(np.float32)
    c["rope_sin"] = np.sin(ang).reshape(SEQ, 16).astype(np.float32)
    jl = np.arange(128, dtype=np.float32)[:, None]
    cc = np.arange(128, dtype=np.float32)[None, :]
    c["c_rel0"] = (cc - jl).astype(np.float32)
    c["c_mge"] = (cc >= jl).astype(np.float32)
    c["c_mle"] = (cc <= jl).astype(np.float32)
    m0 = np.ones((128, 2), np.float32); m0[0, 0] = 0.0; m0[:, 1] = 1.0 - m0[:, 0]
    c["c_m0"] = m0
    names = {"zf": "hz_f", "zr": "hz_r", "tf": "ht_f", "tr": "ht_r", "dl": "h_dl", "Wf": "h_Wf", "Winv": "h_Wi", "csl": "h_csl", "csh": "h_csh"}
    for Lh in (SEQ, NCTX):
        hcn = hyena_consts(Lh)
        for kk2, v in hcn.items():
            c[names[kk2] + str(Lh)] = v
    c["c_iota32"] = np.broadcast_to(np.arange(32, dtype=np.float32)[None, :], (128, 32)).copy()
    tt_ = np.arange(128)
    c["c_LT"] = (tt_[:, None] < tt_[None, :]).astype(np.float32)
    c["c_ONES"] = np.ones((128, 128), np.float32)
    c["c_jbv"] = np.broadcast_to((512.0 * np.arange(128, dtype=np.float32))[None, :], (128, 128)).copy()
    c["c_iotaA"] = (np.arange(8, dtype=np.float32)[None, :] * 128 + np.arange(128, dtype=np.float32)[:, None]).astype(np.float32)
    c["c_iotaB"] = (np.arange(4, dtype=np.float32)[None, :] * 128 + np.arange(128, dtype=np.float32)[:, None]).astype(np.float32)
    c["c_dvals"] = np.broadcast_to(128.0 * np.arange(18, dtype=np.float32)[None, :], (128, 18)).astype(np.float32).copy()
    return c


IN_NAMES = ["w_mod", "b_mod", "norm1_g", "norm2_g", "w_in", "mla_q_norm_g", "mla_w_uq", "mla_kv_norm_g", "mla_w_ukv", "mla_qn_g", "mla_kn_g",
            "ret_log_gamma", "ret_norm_g", "lru_conv_w", "lru_conv_b", "lru_wa", "lru_ba", "lru_wx", "lru_bx", "lru_lambda",
            "hy_conv_w", "hy_conv_b", "hy_w1", "hy_b1", "hy_w2", "hy_b2", "hy_w3", "hy_freq", "hy_d",
            "group_norm_g", "w_out", "moe_w_group", "moe_w_expert", "moe_w1", "moe_w3", "moe_w2"]


def make_in_map(inp, b0, NB, consts=None):
    consts = consts if consts is not None else host_consts()
    m = {"x": np.ascontiguousarray(inp["x"][b0:b0 + NB]), "ctx": np.ascontiguousarray(inp["ctx"][b0:b0 + NB]),
         "c": np.ascontiguousarray(inp["c"][b0:b0 + NB]), "c_ctx": np.asarray(inp["c_ctx"])}
    for n in IN_NAMES:
        m[n] = np.asarray(inp[n])
    m.update(consts)
    return m


_CACHE = {}


def kernel(**inputs):
    NB = 4
    n_cores = 8
    if "nc" not in _CACHE:
        _CACHE["nc"] = build_program(NB)[0]
        _CACHE["consts"] = host_consts()
    nc = _CACHE["nc"]
    in_maps = [make_in_map(inputs, i * NB, NB, _CACHE["consts"]) for i in range(n_cores)]
    res = run_bass_kernel_spmd(nc, in_maps, core_ids=list(range(n_cores)))
    return np.concatenate([np.asarray(r["out"]) for r in res.results], axis=0).astype(np.float32)
```

```python
import math
from contextlib import ExitStack
import numpy as np
import ml_dtypes
import concourse.bass as bass
import concourse.mybir as mybir
from concourse.bass_utils import run_bass_kernel_spmd

F32 = mybir.dt.float32
BF16 = mybir.dt.bfloat16
AF = mybir.ActivationFunctionType
ALU = mybir.AluOpType
AX = mybir.AxisListType
ENGS = ("pe", "act", "dve", "pool", "sp")

D = 1024
SEQ = 2048
NCTX = 256
TB = SEQ + NCTX
DEPTH = 2
EPS = 1e-6
INC = 2656
SPARSE_MOE = True
STRICT_SCATTER = False


class Prog:
    def __init__(self, nc):
        self.nc = nc
        self.q = {e: [] for e in ENGS}
        self.cnt = {e: 0 for e in ENGS}
        self.known = {e: {} for e in ENGS}
        self.w = {}
        self.r = {}
        self.dsem = {}
        self.semobj = {}
        for e in ENGS:
            self.semobj[("c", e)] = nc.alloc_semaphore(name="c_" + e)
        self.out_waits = {}
        self.n_ins = 0
        self.n_wait = 0
        self.free_hw = []
        self.free_sw = []
        self.nsem = 0

    def _deps(self, e, reads, writes, skip_self):
        deps = {}
        for k in reads:
            for s, v in self.w.get(k, {}).items():
                if deps.get(s, -1) < v:
                    deps[s] = v
        for k in writes:
            for d in (self.w.get(k, {}), self.r.get(k, {})):
                for s, v in d.items():
                    if deps.get(s, -1) < v:
                        deps[s] = v
        waits = []
        kn = self.known[e]
        for s, v in deps.items():
            if skip_self and s == ("c", e):
                continue
            if kn.get(s, 0) >= v:
                continue
            kn[s] = v
            waits.append((s, v))
        return waits

    def _update(self, me, reads, writes):
        s, v = me
        for k in writes:
            if self.r.get(k):
                self.w[k] = {s: v}
                self.r[k] = {}
            else:
                self.w.setdefault(k, {})[s] = v
        for k in reads:
            self.r.setdefault(k, {})[s] = v

    def op(self, e, fn, reads=(), writes=(), skip_self=False):
        waits = self._deps(e, reads, writes, skip_self)
        self.cnt[e] += 1
        self._update((("c", e), self.cnt[e]), reads, writes)
        self.q[e].append((waits, fn, (("c", e), 1)))
        self.n_ins += 1
        self.n_wait += len(waits)

    def dma(self, fn, reads=(), writes=(), dkey=None, q="sp", is_output=False):
        if dkey is None:
            dkey = writes[0]
        waits = self._deps(q, reads, writes, False)
        if dkey not in self.dsem:
            pool = self.free_sw if q == "pool" else self.free_hw
            if pool:
                ent = pool.pop()
            else:
                self.nsem += 1
                ent = [("d", self.nsem), 0, q == "pool"]
                self.semobj[ent[0]] = self.nc.alloc_semaphore(name="d%d" % self.nsem)
            self.dsem[dkey] = ent
        ent = self.dsem[dkey]
        assert ent[2] == (q == "pool"), "DMA semaphore shared between software and hardware DGE: %r" % (dkey,)
        if ent[2] and ent[1] > 0 and self.known[q].get(ent[0], 0) < ent[1]:
            self.known[q][ent[0]] = ent[1]
            waits.append((ent[0], ent[1]))
        ent[1] += 16
        self._update((ent[0], ent[1]), reads, writes)
        self.q[q].append((waits, fn, (ent[0], 16)))
        if is_output:
            self.out_waits[ent[0]] = ent[1]
        self.n_ins += 1
        self.n_wait += len(waits)

    def barrier(self):
        allv = {("c", e): self.cnt[e] for e in ENGS if self.cnt[e] > 0}
        for ent in list(self.dsem.values()) + self.free_hw + self.free_sw:
            if ent[1] > 0:
                allv[ent[0]] = ent[1]
        for e in ENGS:
            kn = self.known[e]
            waits = []
            for s, v in allv.items():
                if kn.get(s, 0) < v:
                    kn[s] = v
                    waits.append((s, v))
            if waits:
                self.q[e].append((waits, None, None))
        self.w = {}
        self.r = {}
        for ent in self.dsem.values():
            (self.free_sw if ent[2] else self.free_hw).append(ent)
        self.dsem = {}

    def emit(self):
        nc = self.nc
        final = list(self.out_waits.items())
        with nc.Block() as block:
            def run(e):
                def body(eng):
                    for waits, fn, inc in self.q[e]:
                        for s, v in waits:
                            eng.wait_ge(self.semobj[s], v)
                        if fn is not None:
                            fn(eng).then_inc(self.semobj[inc[0]], inc[1])
                    if e == "sp":
                        for s, v in final:
                            eng.wait_ge(self.semobj[s], v)
                return body
            block.tensor(run("pe"))
            block.scalar(run("act"))
            block.vector(run("dve"))
            block.gpsimd(run("pool"))
            block.sync(run("sp"))
        self.q = {e: [] for e in ENGS}
        self.out_waits = {}


class Rec:
    def __init__(self, sfx, local):
        self.ops, self.sfx, self.local = [], sfx, local

    def _k(self, keys):
        return [(x + self.sfx) if (isinstance(x, str) and x in self.local) else x for x in keys]

    def op(self, e, fn, reads=(), writes=(), skip_self=False):
        self.ops.append((0, e, fn, self._k(reads), self._k(writes), skip_self))

    def dma(self, fn, reads=(), writes=(), dkey=None, q="sp", is_output=False):
        if dkey is None:
            dkey = writes[0]
        self.ops.append((1, fn, self._k(reads), self._k(writes), self._k([dkey])[0], q, is_output))


def replay(P, recs):
    idx = [0] * len(recs)
    live = True
    while live:
        live = False
        for j, r in enumerate(recs):
            if idx[j] < len(r.ops):
                o = r.ops[idx[j]]
                idx[j] += 1
                live = True
                if o[0] == 0:
                    P.op(o[1], o[2], reads=o[3], writes=o[4], skip_self=o[5])
                else:
                    P.dma(o[1], reads=o[2], writes=o[3], dkey=o[4], q=o[5], is_output=o[6])


class K:
    pass


_UID = [0]


def _u(name):
    _UID[0] += 1
    return "%s_%d" % (name, _UID[0])


def _tiles(es, nc, specs):
    out = {}
    for name, shape, dt in specs:
        out[name] = es.enter_context(nc.sbuf_tensor(_u(name), list(shape), dt))
    return out


def _psum(es, nc, name, shape, dt):
    return es.enter_context(nc.psum_tensor(_u(name), list(shape), dt))


def stage_mod(k):
    nc, P, NB = k.nc, k.P, k.NB
    R = NB + 1
    with ExitStack() as es:
        t = _tiles(es, nc, [("crow", (R, D), F32), ("srow", (R, D), F32), ("scT", (128, 8, R), F32),
                            ("wm0", (128, 8, 512), F32), ("wm1", (128, 8, 512), F32),
                            ("brow", (R, 6 * D), F32), ("mrow", (R, 6 * D), F32), ("idf", (128, 128), F32)])
        pt = _psum(es, nc, "pt0", [128, 512], F32)
        pm = [_psum(es, nc, "pm%d" % i, [128, 512], F32) for i in range(2)]
        P.dma(lambda e: e.dma_start(out=t["idf"][:], in_=k.identf), writes=["idf"])
        P.dma(lambda e: e.dma_start(out=t["crow"][0:NB, :], in_=k.c), writes=["crow"])
        P.dma(lambda e: e.dma_start(out=t["crow"][NB:R, :], in_=k.c_ctx.rearrange("(o d) -> o d", o=1)), writes=["crow"])
        P.op("act", lambda e: e.activation(out=t["srow"][:], in_=t["crow"][:], func=AF.Silu), reads=["crow"], writes=["srow"])
        for kk in range(8):
            P.op("pe", lambda e, kk=kk: e.transpose(out=pt[:, 0:R], in_=t["srow"][:, kk * 128:(kk + 1) * 128], identity=t["idf"][0:R, 0:R]),
                 reads=["srow", "idf"], writes=["pt"])
            P.op("dve", lambda e, kk=kk: e.tensor_copy(out=t["scT"][:, kk, :], in_=pt[:, 0:R]), reads=["pt"], writes=["scT"])
        for l in range(DEPTH):
            P.dma(lambda e, l=l: e.dma_start(out=t["brow"][:], in_=k.b_mod[l].partition_broadcast(R)), writes=["brow"])
            for n in range(12):
                wt = t["wm%d" % (n % 2)]
                wk = "wm%d" % (n % 2)
                P.dma(lambda e, l=l, n=n, wt=wt: e.dma_start(out=wt[:], in_=k.w_mod[l][:, n * 512:(n + 1) * 512].rearrange("(k p) n -> p k n", p=128)),
                      writes=[wk], q="sp")
                ps = pm[n % 2]
                pk = "pm%d" % (n % 2)
                for kk in range(8):
                    P.op("pe", lambda e, kk=kk, wt=wt, ps=ps: e.matmul(ps[0:R, :], lhsT=t["scT"][:, kk, :], rhs=wt[:, kk, :], start=(kk == 0), stop=(kk == 7)),
                         reads=["scT", wk], writes=[pk], skip_self=(kk > 0))
                P.op("dve", lambda e, n=n, ps=ps: e.tensor_tensor(out=t["mrow"][:, n * 512:(n + 1) * 512], in0=ps[0:R, :], in1=t["brow"][:, n * 512:(n + 1) * 512], op=ALU.add),
                     reads=[pk, "brow"], writes=["mrow"])
            P.dma(lambda e, l=l: e.dma_start(out=k.modrow[l], in_=t["mrow"][:]), reads=["mrow"], writes=[("modrow", l)])
        P.barrier()
        P.emit()


def load_mod_cols(k, es, l, which):
    nc, P, NB = k.nc, k.P, k.NB
    R = NB + 1
    A = es.enter_context(nc.sbuf_tensor(_u("modA"), [128, 8, R], F32))
    B = es.enter_context(nc.sbuf_tensor(_u("modB"), [128, 8, R], F32))
    with ExitStack() as e2:
        t = _tiles(e2, nc, [("mr", (R + 1, 2 * D), F32), ("gT", (128, 8, R + 1), F32), ("idf2", (128, 128), F32)])
        pt = _psum(e2, nc, "ptm", [128, 512], F32)
        base = 0 if which == 0 else 3 * D
        g = k.norm1_g if which == 0 else k.norm2_g
        P.dma(lambda e: e.dma_start(out=t["idf2"][:], in_=k.identf), writes=["idf2"])
        P.dma(lambda e: e.dma_start(out=t["mr"][0:R, :], in_=k.modrow[l][:, base:base + 2 * D]), reads=[("modrow", l)], writes=["mr"])
        P.dma(lambda e: e.dma_start(out=t["mr"][R:R + 1, 0:D], in_=g[l].rearrange("(o d) -> o d", o=1)), writes=["mr"])
        P.dma(lambda e: e.dma_start(out=t["mr"][R:R + 1, D:2 * D], in_=g[l].rearrange("(o d) -> o d", o=1)), writes=["mr"])
        for half, dst in ((0, B), (1, A)):
            for kk in range(8):
                c0 = half * D + kk * 128
                P.op("pe", lambda e, c0=c0: e.transpose(out=pt[:, 0:R + 1], in_=t["mr"][:, c0:c0 + 128], identity=t["idf2"][0:R + 1, 0:R + 1]),
                     reads=["mr", "idf2"], writes=["ptm"])
                if half == 0:
                    P.op("dve", lambda e, kk=kk: e.tensor_copy(out=B[:, kk, :], in_=pt[:, 0:R]), reads=["ptm"], writes=["modB"])
                else:
                    P.op("dve", lambda e, kk=kk: e.tensor_copy(out=t["gT"][:, kk, :], in_=pt[:, 0:R + 1]), reads=["ptm"], writes=["gT"])
                    P.op("dve", lambda e, kk=kk: e.tensor_scalar(out=A[:, kk, :], in0=t["gT"][:, kk, 0:R], scalar1=1.0, scalar2=t["gT"][:, kk, R:R + 1],
                                                               op0=ALU.add, op1=ALU.mult), reads=["gT"], writes=["modA"])
        P.barrier()
    return A, B


def xsrc(k, l, b, i):
    if l == 0:
        if i < 16:
            return k.x[b, i * 128:(i + 1) * 128, :]
        return k.ctx[b, (i - 16) * 128:(i - 15) * 128, :]
    t0 = b * TB + i * 128
    return k.x2[t0:t0 + 128, :]


def norm_mod_T(k, t, pst, xt_ap, xkey, A, B, b, dstT, dkeyT, col0, tag):
    P = k.P
    ss, rstd, xn = t["ss" + tag], t["rstd" + tag], t["xn" + tag]
    P.op("act", lambda e: e.activation(out=t["junk" + tag][:], in_=xt_ap, func=AF.Square, accum_out=ss[:]), reads=[xkey], writes=["junk" + tag, "ss" + tag])
    P.op("act", lambda e: e.activation(out=ss[:], in_=ss[:], func=AF.Sqrt, scale=1.0 / D, bias=EPS), reads=["ss" + tag], writes=["ss" + tag])
    P.op("dve", lambda e: e.reciprocal(out=rstd[:], in_=ss[:]), reads=["ss" + tag], writes=["rstd" + tag])
    P.op("dve", lambda e: e.tensor_scalar(out=xn[:], in0=xt_ap, scalar1=rstd[:], scalar2=None, op0=ALU.mult), reads=[xkey, "rstd" + tag], writes=["xn" + tag])
    for kk in range(8):
        P.op("pe", lambda e, kk=kk: e.transpose(out=pst[:, kk, :], in_=xn[:, kk * 128:(kk + 1) * 128], identity=t["idb"][:]),
             reads=["xn" + tag, "idb"], writes=["pst"])
    for kk in range(8):
        eng = "act" if kk % 2 == 0 else "dve"
        if eng == "act":
            P.op("act", lambda e, kk=kk: e.activation(out=dstT[:, kk, col0:col0 + 128], in_=pst[:, kk, :], func=AF.Identity,
                                                     scale=A[:, kk, b:b + 1], bias=B[:, kk, b:b + 1]), reads=["pst", "modA", "modB"], writes=[dkeyT])
        else:
            P.op("dve", lambda e, kk=kk: e.tensor_scalar(out=dstT[:, kk, col0:col0 + 128], in0=pst[:, kk, :], scalar1=A[:, kk, b:b + 1], scalar2=B[:, kk, b:b + 1],
                                                        op0=ALU.mult, op1=ALU.add), reads=["pst", "modA", "modB"], writes=[dkeyT])


TM_BLOCKS = ((0, 352, 0), (608, 512, 352), (1120, 256, 864))
ZT_W = 1120
FM_COLS = [352, 480, 608, 736] + [1376 + 128 * i for i in range(6)] + [2144, 2272, 2400, 2528]
ZF_ROWS = 128 * len(FM_COLS)
ZF_RQ, ZF_RK, ZF_ZH, ZF_LX, ZF_LG = 0, 256, 512, 1280, 1536


def stage_inproj(k, l):
    nc, P, NB = k.nc, k.P, k.NB
    with ExitStack() as es:
        A, B = load_mod_cols(k, es, l, 0)
        t = _tiles(es, nc, [("win", (128, 8, INC), BF16), ("wst0", (128, INC), F32), ("wst1", (128, INC), F32),
                            ("idf", (128, 128), F32), ("idb", (128, 128), BF16),
                            ("xt0", (128, D), F32), ("xt1", (128, D), F32),
                            ("junk0", (128, D), BF16), ("junk1", (128, D), BF16),
                            ("ss0", (128, 1), F32), ("ss1", (128, 1), F32), ("rstd0", (128, 1), F32), ("rstd1", (128, 1), F32),
                            ("xn0", (128, D), BF16), ("xn1", (128, D), BF16),
                            ("aT0", (128, 8, 512), BF16), ("aT1", (128, 8, 512), BF16),
                            ("zf0", (128, 14, 512), BF16), ("zf1", (128, 14, 512), BF16),
                            ("zt0", (128, ZT_W), BF16), ("zt1", (128, ZT_W), BF16)])
        pf = [_psum(es, nc, "pf%d" % i, [128, 512], F32) for i in range(3)]
        psl = [(_psum(es, nc, "pst", [128, 8, 128], BF16), _psum(es, nc, "pq", [128, 512], F32)) for _ in range(2)]
        P.dma(lambda e: e.dma_start(out=t["idf"][:], in_=k.identf), writes=["idf"])
        P.op("dve", lambda e: e.tensor_copy(out=t["idb"][:], in_=t["idf"][:]), reads=["idf"], writes=["idb"])
        for kk in range(8):
            ws = t["wst%d" % (kk % 2)]
            wk = "wst%d" % (kk % 2)
            P.dma(lambda e, kk=kk, ws=ws: e.dma_start(out=ws[:], in_=k.w_in[l][kk * 128:(kk + 1) * 128, :]), writes=[wk], q="sp")
            P.op("pool", lambda e, kk=kk, ws=ws: e.tensor_copy(out=t["win"][:, kk, :], in_=ws[:]), reads=[wk], writes=["win"])
        zf_v = k.zf.rearrange("(c p) t -> p c t", p=128)
        gi = 0
        ti = 0
        for b in range(NB):
            for (g0, W) in ((0, 512), (512, 512), (1024, 512), (1536, 512), (2048, 256)):
                aT = t["aT%d" % (gi % 2)]
                ak = "aT%d" % (gi % 2)
                nt = W // 128
                def tile_chain(j, ti, pst, pq):
                    P = k.P
                    tag = str(ti % 2)
                    i = g0 // 128 + j
                    P.dma(lambda e, b=b, i=i, tag=tag: e.dma_start(out=t["xt" + tag][:], in_=xsrc(k, l, b, i)),
                          reads=([("x2", b, i)] if l > 0 else []), writes=["xt" + tag], q="sp")
                    norm_mod_T(k, t, pst, t["xt" + tag][:], "xt" + tag, A, B, (b if i < 16 else NB), aT, (ak, j), j * 128, tag)
                    zt = t["zt" + tag]
                    for bi, (c0, w, d0) in enumerate(TM_BLOCKS):
                        for kk in range(8):
                            P.op("pe", lambda e, kk=kk, c0=c0, w=w, j=j, bi=bi, aT=aT: e.matmul(pq[:, 0:w], lhsT=aT[:, kk, j * 128:(j + 1) * 128], rhs=t["win"][:, kk, c0:c0 + w],
                                                                                  start=(kk == 0), stop=(kk == 7)),
                                 reads=[(ak, j), "win"], writes=["pq"], skip_self=(kk > 0))
                        if bi == 1:
                            P.op("act", lambda e, zt=zt: e.activation(out=zt[:, 352:608], in_=pq[:, 0:256], func=AF.Copy, scale=0.125), reads=["pq"], writes=["zt" + tag])
                            P.op("dve", lambda e, zt=zt: e.tensor_copy(out=zt[:, 608:864], in_=pq[:, 256:512]), reads=["pq"], writes=["zt" + tag])
                        elif bi == 0:
                            P.op("act", lambda e, zt=zt: e.copy(out=zt[:, 0:352], in_=pq[:, 0:352]), reads=["pq"], writes=["zt" + tag])
                        else:
                            P.op("dve", lambda e, zt=zt: e.tensor_copy(out=zt[:, 864:1120], in_=pq[:, 0:256]), reads=["pq"], writes=["zt" + tag])
                    t0 = b * TB + g0 + j * 128
                    P.dma(lambda e, zt=zt, t0=t0: e.dma_start(out=k.zt[t0:t0 + 128, :], in_=zt[:]), reads=["zt" + tag], writes=[("ztd", l, b, i)], dkey=("ztd", tag))

                for j0 in range(0, nt, 2):
                    recs = []
                    for s_ in range(2):
                        rec = Rec("_%d" % s_, {"pst", "pq"})
                        k.P = rec
                        tile_chain(j0 + s_, ti, psl[s_][0], psl[s_][1])
                        ti += 1
                        recs.append(rec)
                    k.P = P
                    replay(P, recs)
                zfs = t["zf%d" % (gi % 2)]
                zk = "zf%d" % (gi % 2)
                for ci, c0 in enumerate(FM_COLS):
                    ps = pf[ci % 3]
                    for kk in range(8):
                        P.op("pe", lambda e, kk=kk, c0=c0, ps=ps, aT=aT, W=W: e.matmul(ps[:, 0:W], lhsT=t["win"][:, kk, c0:c0 + 128], rhs=aT[:, kk, 0:W], start=(kk == 0), stop=(kk == 7)),
                             reads=[(ak, jj) for jj in range(nt)] + ["win"], writes=[("pf", ci % 3)], skip_self=(kk > 0))
                    if ci in (2, 3):
                        P.op("act", lambda e, ci=ci, ps=ps, W=W, zfs=zfs: e.activation(out=zfs[:, ci, 0:W], in_=ps[:, 0:W], func=AF.Copy, scale=0.125), reads=[("pf", ci % 3)], writes=[zk])
                    elif ci % 2 == 0:
                        P.op("act", lambda e, ci=ci, ps=ps, W=W, zfs=zfs: e.copy(out=zfs[:, ci, 0:W], in_=ps[:, 0:W]), reads=[("pf", ci % 3)], writes=[zk])
                    else:
                        P.op("dve", lambda e, ci=ci, ps=ps, W=W, zfs=zfs: e.tensor_copy(out=zfs[:, ci, 0:W], in_=ps[:, 0:W]), reads=[("pf", ci % 3)], writes=[zk])
                t0 = b * TB + g0
                for ci in range(len(FM_COLS)):
                    P.dma(lambda e, zfs=zfs, t0=t0, W=W, ci=ci: e.dma_start(out=zf_v[:, ci, t0:t0 + W], in_=zfs[:, ci, 0:W]), reads=[zk], writes=[("zfd", l, b, g0)],
                          dkey=("zfd", gi % 2))
                gi += 1
        P.barrier()
        P.emit()


def attn_core(k, pfx, QT_h, KT_h, V_h, q0, W, key_tiles, transform, ps_s, ps_o, pt, dv, rkeys, okey):
    P = k.P
    nt = W // 128
    for n, kt in enumerate(key_tiles):
        sb = k.cc % 2
        k.cc += 1
        P.op("pe", lambda e, kt=kt, sb=sb: e.matmul(ps_s[sb][:, 0:W], lhsT=KT_h[:, kt * 128:(kt + 1) * 128], rhs=QT_h[:, q0:q0 + W], start=True, stop=True),
             reads=rkeys, writes=[pfx + "ps_s%d" % sb])
        transform(kt, ps_s[sb], pfx + "ps_s%d" % sb, pt[sb], pfx + "pt%d" % sb)
        for sidx in range(nt):
            P.op("pe", lambda e, kt=kt, sb=sb, sidx=sidx, n=n: e.matmul(ps_o[sidx][:, 0:dv], lhsT=pt[sb][:, sidx * 128:(sidx + 1) * 128], rhs=V_h(kt),
                                                                  start=(n == 0), stop=(n == len(key_tiles) - 1)),
                 reads=[pfx + "pt%d" % sb] + rkeys, writes=[okey + str(sidx)], skip_self=(n > 0))


def rms_rstd(k, t, src_ap, srckey, junk, jkey, ss, sskey, n):
    P = k.P
    P.op("act", lambda e: e.activation(out=junk, in_=src_ap, func=AF.Square, accum_out=ss), reads=[srckey], writes=[jkey, sskey])
    P.op("act", lambda e: e.activation(out=ss, in_=ss, func=AF.Sqrt, scale=1.0 / n, bias=EPS), reads=[sskey], writes=[sskey])
    P.op("dve", lambda e: e.reciprocal(out=ss, in_=ss), reads=[sskey], writes=[sskey])


def transpose_to(k, t, pstile, pskey, src_blocks, dst_ap, dstkey, srckeys, npart, eng="act"):
    P = k.P
    for i, blk in enumerate(src_blocks):
        w = blk.shape[-1]
        P.op("pe", lambda e, i=i, blk=blk, w=w: e.transpose(out=pstile[0:w, i, :], in_=blk, identity=t["idb"][:]), reads=srckeys + ["idb"], writes=[pskey])
    n = len(src_blocks)
    if eng == "act":
        P.op("act", lambda e: e.copy(out=dst_ap, in_=pstile[0:npart, 0:n, :]), reads=[pskey], writes=[dstkey])
    else:
        P.op("dve", lambda e: e.tensor_copy(out=dst_ap, in_=pstile[0:npart, 0:n, :]), reads=[pskey], writes=[dstkey])


def store_yT(k, t, ytm, ykey, nt, row0, tcol0, pstile, pskey, l):
    P = k.P
    W = nt * 128
    yts = t["yts%d" % (k.yc % 2)]
    ytk = "yts%d" % (k.yc % 2)
    k.yc += 1
    for c in range(2):
        for sidx in range(nt):
            P.op("pe", lambda e, c=c, sidx=sidx: e.transpose(out=pstile[:, sidx, :], in_=ytm[:, sidx, c * 128:(c + 1) * 128], identity=t["idb"][:]),
                 reads=[ykey, "idb"], writes=[pskey])
        P.op("act", lambda e, c=c: e.copy(out=yts[:, c, 0:W], in_=pstile[:, 0:nt, :]), reads=[pskey], writes=[ytk])
        P.dma(lambda e, c=c: e.dma_start(out=k.yT[row0 + c * 128:row0 + (c + 1) * 128, tcol0:tcol0 + W], in_=yts[:, c, 0:W]), reads=[ytk],
              writes=[("yTd", l, row0, c, tcol0)], dkey=("yTd", ytk))


def stage_mla(k, l):
    nc, P, NB = k.nc, k.P, k.NB
    with ExitStack() as es:
        t = _tiles(es, nc, [("idf", (128, 128), F32), ("idb", (128, 128), BF16), ("wuqf", (128, 2, 384), F32), ("wuq", (128, 2, 384), BF16), ("gq", (128, 2), F32), ("wukvf", (128, 512), F32), ("wukv", (128, 512), BF16), ("gkv", (128, 1), F32), ("qng", (128, 96), F32), ("kng", (128, 96), F32), ("cos", (128, 16, 16), F32), ("sin", (128, 16, 16), F32), ("QT", (96, 4, TB), BF16), ("KT", (96, 4, TB), BF16), ("V", (128, 18, 4, 65), BF16), ("pt0", (128, 512), BF16), ("pt1", (128, 512), BF16), ("rec", (128, 4), F32), ("ytm0", (128, 4, 256), BF16), ("ytm1", (128, 4, 256), BF16), ("yts0", (128, 2, 512), BF16), ("yts1", (128, 2, 512), BF16)])
        ps_s = [_psum(es, nc, "pss%d" % i, [128, 512], F32) for i in range(2)]
        ts = []
        for s_ in range(2):
            d_ = dict(t)
            d_.update(_tiles(es, nc, [("junk", (128, 384), F32), ("ssq", (128, 1), F32), ("ssk", (128, 1), F32), ("cqn", (128, 192), BF16), ("ckvn", (128, 128), BF16), ("cqT", (128, 2, 128), BF16), ("ckvT", (128, 1, 128), BF16), ("sq", (128, 4, 96), F32), ("ssh", (128, 4), F32), ("qn", (128, 4, 96), F32), ("kfull", (128, 4, 96), F32), ("qf", (128, 4, 96), BF16), ("r1", (128, 4, 2, 8), F32), ("r2", (128, 4, 2, 8), F32), ("zin", (128, 352), BF16)]))
            d_["pstb"] = _psum(es, nc, "pstb", [128, 8, 128], BF16)
            d_["pskv"] = ps_s[s_]
            d_["pskey"] = "mla_ps_s%d" % s_
            ts.append(d_)
        pstb = ts[0]["pstb"]
        ps_o = [_psum(es, nc, "pso%d" % i, [128, 512], F32) for i in range(4)]
        P.dma(lambda e: e.dma_start(out=t["idf"][:], in_=k.identf), writes=["idf"])
        P.op("dve", lambda e: e.tensor_copy(out=t["idb"][:], in_=t["idf"][:]), reads=["idf"], writes=["idb"])
        P.op("pool", lambda e: e.memset(t["wuqf"][:], 0.0), writes=["wuqf"])
        P.op("pool", lambda e: e.memset(t["gq"][:], 0.0), writes=["gq"])
        P.dma(lambda e: e.dma_start(out=t["wuqf"][:, 0, :], in_=k.mla_w_uq[l][0:128, :]), writes=["wuqf"])
        P.dma(lambda e: e.dma_start(out=t["wuqf"][0:64, 1, :], in_=k.mla_w_uq[l][128:192, :]), writes=["wuqf"])
        P.dma(lambda e: e.dma_start(out=t["gq"][:, 0:1], in_=k.mla_q_norm_g[l][0:128].rearrange("(p o) -> p o", o=1)), writes=["gq"])
        P.dma(lambda e: e.dma_start(out=t["gq"][0:64, 1:2], in_=k.mla_q_norm_g[l][128:192].rearrange("(p o) -> p o", o=1)), writes=["gq"])
        for c in range(2):
            P.op("dve", lambda e, c=c: e.tensor_scalar(out=t["wuq"][:, c, :], in0=t["wuqf"][:, c, :], scalar1=t["gq"][:, c:c + 1], scalar2=None, op0=ALU.mult),
                 reads=["wuqf", "gq"], writes=["wuq"])
        P.dma(lambda e: e.dma_start(out=t["wukvf"][:], in_=k.mla_w_ukv[l]), writes=["wukvf"])
        P.dma(lambda e: e.dma_start(out=t["gkv"][:], in_=k.mla_kv_norm_g[l].rearrange("(p o) -> p o", o=1)), writes=["gkv"])
        P.op("dve", lambda e: e.tensor_scalar(out=t["wukv"][:], in0=t["wukvf"][:], scalar1=t["gkv"][:, 0:1], scalar2=None, op0=ALU.mult), reads=["wukvf", "gkv"], writes=["wukv"])
        P.dma(lambda e: e.dma_start(out=t["qng"][:], in_=k.mla_qn_g[l].partition_broadcast(128)), writes=["qng"])
        P.dma(lambda e: e.dma_start(out=t["kng"][:], in_=k.mla_kn_g[l].partition_broadcast(128)), writes=["kng"])
        P.op("dve", lambda e: e.tensor_scalar(out=t["qng"][:], in0=t["qng"][:], scalar1=96.0 ** -0.5, scalar2=None, op0=ALU.mult), reads=["qng"], writes=["qng"])
        P.dma(lambda e: e.dma_start(out=t["cos"][:], in_=k.rope_cos.rearrange("(n p) f -> p n f", p=128)), writes=["cos"])
        P.dma(lambda e: e.dma_start(out=t["sin"][:], in_=k.rope_sin.rearrange("(n p) f -> p n f", p=128)), writes=["sin"])
        P.op("pool", lambda e: e.memset(t["V"][:], 1.0), writes=["V"])

        def prep_tile(t, b, i):
            P = k.P
            pstb, pskv, pskey = t["pstb"], t["pskv"], t["pskey"]
            psq = pskv
            def head_norm_rope(src_ps_or_sb, srckey, gains, dstT, dstkey, i, col0, is_lat):
                P.op("act", lambda e: e.activation(out=t["sq"][:], in_=src_ps_or_sb, func=AF.Square), reads=[srckey], writes=["sq"])
                P.op("dve", lambda e: e.tensor_reduce(out=t["ssh"][:], in_=t["sq"][:], axis=AX.X, op=ALU.add), reads=["sq"], writes=["ssh"])
                P.op("act", lambda e: e.activation(out=t["ssh"][:], in_=t["ssh"][:], func=AF.Sqrt, scale=1.0 / 96, bias=EPS), reads=["ssh"], writes=["ssh"])
                P.op("dve", lambda e: e.reciprocal(out=t["ssh"][:], in_=t["ssh"][:]), reads=["ssh"], writes=["ssh"])
                P.op("dve", lambda e: e.tensor_tensor(out=t["qn"][:], in0=src_ps_or_sb, in1=t["ssh"][:].unsqueeze(2).to_broadcast([128, 4, 96]), op=ALU.mult),
                     reads=[srckey, "ssh"], writes=["qn"])
                if is_lat:
                    P.op("dve", lambda e: e.tensor_tensor(out=t["qn"][:], in0=t["qn"][:], in1=gains[:].unsqueeze(1).to_broadcast([128, 4, 96]), op=ALU.mult),
                         reads=["qn", "qng", "kng"], writes=["qn"])
                    P.op("act", lambda e: e.copy(out=t["qf"][:, :, 0:64], in_=t["qn"][:, :, 0:64]), reads=["qn"], writes=["qf"])
                    rv = t["qn"][:, :, 64:96].rearrange("p h (a b f) -> p h a b f", a=2, b=2)
                    ov = t["qf"][:, :, 64:96].rearrange("p h (a b f) -> p h a b f", a=2, b=2)
                    x1, x2 = rv[:, :, :, 0, :], rv[:, :, :, 1, :]
                    cs = t["cos"][:, i, :].rearrange("p (a f) -> p a f", a=2).unsqueeze(1).to_broadcast([128, 4, 2, 8])
                    sn = t["sin"][:, i, :].rearrange("p (a f) -> p a f", a=2).unsqueeze(1).to_broadcast([128, 4, 2, 8])
                    P.op("dve", lambda e: e.tensor_tensor(out=t["r1"][:], in0=x1, in1=cs, op=ALU.mult), reads=["qn", "cos"], writes=["r1"])
                    P.op("dve", lambda e: e.tensor_tensor(out=t["r2"][:], in0=x2, in1=sn, op=ALU.mult), reads=["qn", "sin"], writes=["r2"])
                    P.op("dve", lambda e: e.tensor_tensor(out=ov[:, :, :, 0, :], in0=t["r1"][:], in1=t["r2"][:], op=ALU.subtract), reads=["r1", "r2"], writes=["qf"])
                    P.op("dve", lambda e: e.tensor_tensor(out=t["r1"][:], in0=x2, in1=cs, op=ALU.mult), reads=["qn", "cos", "qf"], writes=["r1"])
                    P.op("dve", lambda e: e.tensor_tensor(out=t["r2"][:], in0=x1, in1=sn, op=ALU.mult), reads=["qn", "sin", "qf"], writes=["r2"])
                    P.op("dve", lambda e: e.tensor_tensor(out=ov[:, :, :, 1, :], in0=t["r1"][:], in1=t["r2"][:], op=ALU.add), reads=["r1", "r2"], writes=["qf"])
                else:
                    P.op("dve", lambda e: e.tensor_tensor(out=t["qf"][:], in0=t["qn"][:], in1=gains[:].unsqueeze(1).to_broadcast([128, 4, 96]), op=ALU.mult),
                         reads=["qn", "qng", "kng"], writes=["qf"])
                transpose_to(k, t, pstb, "pstb", [t["qf"][:, h, :] for h in range(4)], dstT[0:96, :, col0:col0 + 128], dstkey, ["qf"], 96)

            zin = t["zin"]
            zk = "zin"
            t0 = b * TB + i * 128
            is_lat = i < 16
            P.dma(lambda e, zin=zin, t0=t0: e.dma_start(out=zin[:], in_=k.zt[t0:t0 + 128, 0:352]), reads=[("ztd", l, b, i)], writes=[zk])
            rms_rstd(k, t, zin[:, 0:192], zk, t["junk"][:, 0:192], "junk", t["ssq"][:], "ssq", 192)
            P.op("dve", lambda e, zin=zin: e.tensor_scalar(out=t["cqn"][:], in0=zin[:, 0:192], scalar1=t["ssq"][:, 0:1], scalar2=None, op0=ALU.mult), reads=[zk, "ssq"], writes=["cqn"])
            P.op("pe", lambda e: e.transpose(out=pstb[:, 0, :], in_=t["cqn"][:, 0:128], identity=t["idb"][:]), reads=["cqn", "idb"], writes=["pstb"])
            P.op("pe", lambda e: e.transpose(out=pstb[0:64, 1, :], in_=t["cqn"][:, 128:192], identity=t["idb"][:]), reads=["cqn", "idb"], writes=["pstb"])
            P.op("act", lambda e: e.copy(out=t["cqT"][:, 0, :], in_=pstb[:, 0, :]), reads=["pstb"], writes=["cqT"])
            P.op("act", lambda e: e.copy(out=t["cqT"][0:64, 1, :], in_=pstb[0:64, 1, :]), reads=["pstb"], writes=["cqT"])
            P.op("pe", lambda e: e.matmul(psq[:, 0:384], lhsT=t["cqT"][:, 0, :], rhs=t["wuq"][:, 0, :], start=True, stop=False), reads=["cqT", "wuq"], writes=[pskey])
            P.op("pe", lambda e: e.matmul(psq[:, 0:384], lhsT=t["cqT"][0:64, 1, :], rhs=t["wuq"][0:64, 1, :], start=False, stop=True), reads=["cqT", "wuq"], writes=[pskey], skip_self=True)
            head_norm_rope(psq[:, 0:384].rearrange("p (h d) -> p h d", h=4), pskey, t["qng"], t["QT"], "QT", i, i * 128, is_lat)
            rms_rstd(k, t, zin[:, 192:320], zk, t["junk"][:, 0:128], "junk", t["ssk"][:], "ssk", 128)
            P.op("dve", lambda e, zin=zin: e.tensor_scalar(out=t["ckvn"][:], in0=zin[:, 192:320], scalar1=t["ssk"][:, 0:1], scalar2=None, op0=ALU.mult), reads=[zk, "ssk"], writes=["ckvn"])
            transpose_to(k, t, pstb, "pstb", [t["ckvn"][:, :]], t["ckvT"][:, :, :], "ckvT", ["ckvn"], 128)
            P.op("pe", lambda e: e.matmul(pskv[:, :], lhsT=t["ckvT"][:, 0, :], rhs=t["wukv"][:], start=True, stop=True), reads=["ckvT", "wukv"], writes=[pskey])
            kvv = pskv[:, :].rearrange("p (h d) -> p h d", h=4)
            P.op("act", lambda e, kvv=kvv: e.copy(out=t["kfull"][:, :, 0:64], in_=kvv[:, :, 0:64]), reads=[pskey], writes=["kfull"])
            P.op("dve", lambda e, zin=zin: e.tensor_copy(out=t["kfull"][:, :, 64:96], in_=zin[:, 320:352].unsqueeze(1).to_broadcast([128, 4, 32])), reads=[zk], writes=["kfull"])
            P.op("act", lambda e, kvv=kvv, i=i: e.copy(out=t["V"][:, i, :, 0:64], in_=kvv[:, :, 64:128]), reads=[pskey], writes=["V"])
            head_norm_rope(t["kfull"][:], "kfull", t["kng"], t["KT"], "KT", i, i * 128, is_lat)

        LOCALK = set(['zin', 'junk', 'ssq', 'ssk', 'cqn', 'ckvn', 'cqT', 'ckvT', 'sq', 'ssh', 'qn', 'kfull', 'qf', 'r1', 'r2']) | {"pstb"}
        for b in range(NB):
            for i0 in range(0, 18, 2):
                recs = []
                for s_ in range(2):
                    rec = Rec("_%d" % s_, LOCALK)
                    k.P = rec
                    prep_tile(ts[s_], b, i0 + s_)
                    recs.append(rec)
                k.P = P
                replay(P, recs)
            qblocks = [(0, 512, list(range(18))), (512, 512, list(range(18))), (1024, 512, list(range(18))), (1536, 512, list(range(18)))]
            if l < DEPTH - 1:
                qblocks.append((2048, 256, [16, 17]))
            for (q0, W, kts) in qblocks:
                nt = W // 128
                ytm = t["ytm%d" % (k.yc % 2)]
                ykey = "ytm%d" % (k.yc % 2)
                for h in range(4):
                    okey = "mla_pso"

                    def tf(kt, pss, psk, ptile, ptk, W=W):
                        P.op("act", lambda e: e.activation(out=ptile[:, 0:W], in_=pss[:, 0:W], func=AF.Exp), reads=[psk], writes=[ptk])
                    attn_core(k, "mla_", t["QT"][0:96, h, :], t["KT"][0:96, h, :], lambda kt, h=h: t["V"][:, kt, h, :], q0, W, kts, tf,
                              ps_s, ps_o, [t["pt0"], t["pt1"]], 65, ["QT", "KT", "V"], okey)
                    for sidx in range(nt):
                        P.op("dve", lambda e, sidx=sidx: e.reciprocal(out=t["rec"][:, sidx:sidx + 1], in_=ps_o[sidx][:, 64:65]), reads=[okey + str(sidx)], writes=["rec"])
                        P.op("dve", lambda e, sidx=sidx, h=h, ytm=ytm: e.tensor_scalar(out=ytm[:, sidx, h * 64:(h + 1) * 64], in0=ps_o[sidx][:, 0:64],
                                                                                     scalar1=t["rec"][:, sidx:sidx + 1], scalar2=None, op0=ALU.mult),
                             reads=[okey + str(sidx), "rec"], writes=[ykey])
                store_yT(k, t, ytm, ykey, nt, 0, b * TB + q0, pstb, "pstb_0", l)
        P.barrier()
        P.emit()


def stage_ret(k, l):
    nc, P, NB = k.nc, k.P, k.NB
    with ExitStack() as es:
        t = _tiles(es, nc, [("idf", (128, 128), F32), ("idb", (128, 128), BF16),
                            ("rel0", (128, 128), F32), ("mge", (128, 128), F32), ("mle", (128, 128), F32), ("dvals", (128, 18), F32),
                            ("lg", (128, 8), F32), ("nlg", (128, 8), F32), ("biasF", (128, 4, 18), F32), ("biasB", (128, 4, 18), F32),
                            ("e1", (128, 128), F32), ("e2", (128, 128), F32),
                            ("arr", (128, 4, 35, 128), BF16), ("Cx", (128, 4, 2, 16, 128), BF16),
                            ("QTr", (128, 2, TB), BF16), ("KTr", (128, 2, TB), BF16), ("Vr", (128, 18, 256), BF16),
                            ("pt0", (128, 512), BF16), ("pt1", (128, 512), BF16),
                            ("yo", (128, 4, 256), F32), ("sq", (128, 4, 256), F32), ("ssh", (128, 16), F32),
                            ("rng", (128, 256), F32), ("zg0", (128, 4, 256), BF16), ("zg1", (128, 4, 256), BF16), ("gs", (128, 4, 256), F32),
                            ("ytm0", (128, 4, 256), BF16), ("ytm1", (128, 4, 256), BF16),
                            ("yts0", (128, 2, 512), BF16), ("yts1", (128, 2, 512), BF16)])
        pstb = _psum(es, nc, "pstb", [128, 8, 128], BF16)
        ps_s = [_psum(es, nc, "pss%d" % i, [128, 512], F32) for i in range(2)]
        ps_o = [_psum(es, nc, "pso%d" % i, [128, 512], F32) for i in range(4)]
        P.dma(lambda e: e.dma_start(out=t["idf"][:], in_=k.identf), writes=["idf"])
        P.op("dve", lambda e: e.tensor_copy(out=t["idb"][:], in_=t["idf"][:]), reads=["idf"], writes=["idb"])
        for nm, src in (("rel0", k.c_rel0), ("mge", k.c_mge), ("mle", k.c_mle), ("dvals", k.c_dvals)):
            P.dma(lambda e, nm=nm, src=src: e.dma_start(out=t[nm][:], in_=src), writes=[nm])
        P.dma(lambda e: e.dma_start(out=t["lg"][:], in_=k.ret_log_gamma[l].rearrange("a h -> (a h)").partition_broadcast(128)), writes=["lg"])
        P.dma(lambda e: e.dma_start(out=t["rng"][:], in_=k.ret_norm_g[l].partition_broadcast(128)), writes=["rng"])
        P.op("dve", lambda e: e.tensor_scalar(out=t["nlg"][:], in0=t["lg"][:], scalar1=-1.0, scalar2=None, op0=ALU.mult), reads=["lg"], writes=["nlg"])
        for h in range(4):
            P.op("dve", lambda e, h=h: e.tensor_scalar(out=t["biasF"][:, h, :], in0=t["dvals"][:], scalar1=t["lg"][:, h:h + 1], scalar2=None, op0=ALU.mult),
                 reads=["dvals", "lg"], writes=["biasF"])
            P.op("dve", lambda e, h=h: e.tensor_scalar(out=t["biasB"][:, h, :], in0=t["dvals"][:], scalar1=t["lg"][:, 4 + h:5 + h], scalar2=None, op0=ALU.mult),
                 reads=["dvals", "lg"], writes=["biasB"])
        for h in range(4):
            for d in range(1, 18):
                P.op("act", lambda e, h=h, d=d: e.activation(out=t["arr"][:, h, 17 + d, :], in_=t["rel0"][:], func=AF.Exp, scale=t["lg"][:, h:h + 1], bias=t["biasF"][:, h, d:d + 1]),
                     reads=["rel0", "lg", "biasF"], writes=["arr"])
                P.op("act", lambda e, h=h, d=d: e.activation(out=t["arr"][:, h, 17 - d, :], in_=t["rel0"][:], func=AF.Exp, scale=t["nlg"][:, 4 + h:5 + h], bias=t["biasB"][:, h, d:d + 1]),
                     reads=["rel0", "nlg", "biasB"], writes=["arr"])
            P.op("act", lambda e, h=h: e.activation(out=t["e1"][:], in_=t["rel0"][:], func=AF.Exp, scale=t["lg"][:, h:h + 1]), reads=["rel0", "lg"], writes=["e1"])
            P.op("act", lambda e, h=h: e.activation(out=t["e2"][:], in_=t["rel0"][:], func=AF.Exp, scale=t["nlg"][:, 4 + h:5 + h]), reads=["rel0", "nlg"], writes=["e2"])
            P.op("dve", lambda e: e.tensor_tensor(out=t["e1"][:], in0=t["e1"][:], in1=t["mge"][:], op=ALU.mult), reads=["e1", "mge"], writes=["e1"])
            P.op("dve", lambda e: e.tensor_tensor(out=t["e2"][:], in0=t["e2"][:], in1=t["mle"][:], op=ALU.mult), reads=["e2", "mle"], writes=["e2"])
            P.op("dve", lambda e, h=h: e.tensor_tensor(out=t["arr"][:, h, 17, :], in0=t["e1"][:], in1=t["e2"][:], op=ALU.add), reads=["e1", "e2"], writes=["arr"])
            for mi in range(2):
                for qi in range(16):
                    P.op("pool", lambda e, h=h, mi=mi, qi=qi: e.tensor_tensor(out=t["Cx"][:, h, mi, qi, :], in0=t["arr"][:, h, 17 + qi - mi + 2, :],
                                                                              in1=t["arr"][:, h, 17 + qi - 16 - mi, :], op=ALU.add), reads=["arr"], writes=["Cx"])
        zt_v = k.zt.rearrange("(n p) c -> p n c", p=128)
        for b in range(NB):
            for c in range(2):
                P.dma(lambda e, c=c, b=b: e.dma_start(out=t["QTr"][:, c, :], in_=k.zf[ZF_RQ + c * 128:ZF_RQ + (c + 1) * 128, b * TB:(b + 1) * TB]),
                      reads=[("zfd", l, b, g0) for g0 in (0, 512, 1024, 1536, 2048)], writes=["QTr"])
                P.dma(lambda e, c=c, b=b: e.dma_start(out=t["KTr"][:, c, :], in_=k.zf[ZF_RK + c * 128:ZF_RK + (c + 1) * 128, b * TB:(b + 1) * TB]),
                      reads=[("zfd", l, b, g0) for g0 in (0, 512, 1024, 1536, 2048)], writes=["KTr"])
            P.dma(lambda e, b=b: e.dma_start(out=t["Vr"][:], in_=zt_v[:, b * 18:(b + 1) * 18, 608:864]), reads=[("ztd", l, b, i) for i in range(18)], writes=["Vr"])
            qblocks = [(0, 512, list(range(18))), (512, 512, list(range(18))), (1024, 512, list(range(18))), (1536, 512, list(range(18)))]
            if l < DEPTH - 1:
                qblocks.append((2048, 256, [16, 17]))
            for (q0, W, kts) in qblocks:
                nt = W // 128
                qi0 = q0 // 128
                zg = t["zg%d" % (k.yc % 2)]
                zgk = "zg%d" % (k.yc % 2)
                ytm = t["ytm%d" % (k.yc % 2)]
                ykey = "ytm%d" % (k.yc % 2)
                P.dma(lambda e, zg=zg, b=b, qi0=qi0, nt=nt: e.dma_start(out=zg[:, 0:nt, :], in_=zt_v[:, b * 18 + qi0:b * 18 + qi0 + nt, 864:1120]),
                      reads=[("ztd", l, b, i) for i in range(qi0, qi0 + nt)], writes=[zgk])
                for h in range(4):
                    okey = "ret_pso"
                    c, p0 = h // 2, 64 * (h % 2)

                    def tf(kt, pss, psk, ptile, ptk, W=W, h=h, qi0=qi0, nt=nt):
                        if kt < 16 and qi0 < 16:
                            mv = t["arr"][:, h, qi0 - kt + 17:qi0 - kt + 17 + nt, :]
                        elif kt >= 16 and qi0 < 16:
                            mv = t["Cx"][:, h, kt - 16, qi0:qi0 + nt, :]
                        else:
                            mv = t["arr"][:, h, qi0 - kt + 17:qi0 - kt + 17 + nt, :]
                        P.op("dve", lambda e: e.tensor_tensor(out=ptile[:, 0:W].rearrange("p (n c) -> p n c", c=128), in0=pss[:, 0:W].rearrange("p (n c) -> p n c", c=128),
                                                              in1=mv, op=ALU.mult), reads=[psk, "arr", "Cx"], writes=[ptk])
                    attn_core(k, "ret_", t["QTr"][p0:p0 + 64, c, :], t["KTr"][p0:p0 + 64, c, :], lambda kt, h=h: t["Vr"][:, kt, h * 64:(h + 1) * 64], q0, W, kts, tf,
                              ps_s, ps_o, [t["pt0"], t["pt1"]], 64, ["QTr", "KTr", "Vr"], okey)
                    for sidx in range(nt):
                        P.op("act", lambda e, sidx=sidx, h=h: e.copy(out=t["yo"][:, sidx, h * 64:(h + 1) * 64], in_=ps_o[sidx][:, 0:64]), reads=[okey + str(sidx)], writes=["yo"])
                P.op("act", lambda e, nt=nt: e.activation(out=t["sq"][:, 0:nt, :], in_=t["yo"][:, 0:nt, :], func=AF.Square), reads=["yo"], writes=["sq"])
                P.op("dve", lambda e, nt=nt: e.tensor_reduce(out=t["ssh"][:, 0:nt * 4], in_=t["sq"][:, 0:nt, :].rearrange("p n (h d) -> p (n h) d", h=4), axis=AX.X, op=ALU.add),
                     reads=["sq"], writes=["ssh"])
                P.op("act", lambda e, nt=nt: e.activation(out=t["ssh"][:, 0:nt * 4], in_=t["ssh"][:, 0:nt * 4], func=AF.Sqrt, scale=1.0 / 64, bias=EPS), reads=["ssh"], writes=["ssh"])
                P.op("dve", lambda e, nt=nt: e.reciprocal(out=t["ssh"][:, 0:nt * 4], in_=t["ssh"][:, 0:nt * 4]), reads=["ssh"], writes=["ssh"])
                P.op("dve", lambda e, nt=nt: e.tensor_tensor(out=t["yo"][:, 0:nt, :].rearrange("p n (h d) -> p (n h) d", h=4), in0=t["yo"][:, 0:nt, :].rearrange("p n (h d) -> p (n h) d", h=4),
                                                             in1=t["ssh"][:, 0:nt * 4].unsqueeze(2).to_broadcast([128, nt * 4, 64]), op=ALU.mult), reads=["yo", "ssh"], writes=["yo"])
                P.op("dve", lambda e, nt=nt: e.tensor_tensor(out=t["yo"][:, 0:nt, :], in0=t["yo"][:, 0:nt, :], in1=t["rng"][:].unsqueeze(1).to_broadcast([128, nt, 256]), op=ALU.mult),
                     reads=["yo", "rng"], writes=["yo"])
                P.op("act", lambda e, nt=nt, zg=zg: e.activation(out=t["gs"][:, 0:nt, :], in_=zg[:, 0:nt, :], func=AF.Silu), reads=[zgk], writes=["gs"])
                P.op("dve", lambda e, nt=nt, ytm=ytm: e.tensor_tensor(out=ytm[:, 0:nt, :], in0=t["yo"][:, 0:nt, :], in1=t["gs"][:, 0:nt, :], op=ALU.mult), reads=["yo", "gs"], writes=[ykey])
                store_yT(k, t, ytm, ykey, nt, 256, b * TB + q0, pstb, "pstb", l)
        P.barrier()
        P.emit()


BLK5 = ((0, 512), (512, 512), (1024, 512), (1536, 512), (2048, 256))


def stage_lru(k, l):
    nc, P, NB = k.nc, k.P, k.NB
    with ExitStack() as es:
        t = _tiles(es, nc, [("lx", (128, TB), BF16), ("lgz", (128, TB), BF16), ("xc", (128, TB), F32), ("xcb", (128, TB), BF16),
                            ("wconv", (128, 2, 4), F32), ("bconv", (128, 2), F32), ("wst", (128, 128), F32),
                            ("Wbd", (128, 8, 128), BF16), ("bgate", (128, 8), F32), ("lam", (128, 4), F32), ("coef", (128, 4), F32), ("coef2", (128, 4), F32),
                            ("rg", (128, 512), F32), ("ig", (128, 512), F32), ("a2", (128, 512), F32),
                            ("af", (128, TB), F32), ("bf", (128, TB), F32), ("hf", (128, TB), F32), ("hb", (128, TB), F32),
                            ("g1", (128, TB), F32), ("g2", (128, TB), F32), ("yo", (128, TB), BF16)])
        psg = [_psum(es, nc, "psg%d" % i, [128, 512], F32) for i in range(2)]
        for c in range(2):
            for j in range(4):
                P.dma(lambda e, c=c, j=j: e.dma_start(out=t["wconv"][:, c, j:j + 1], in_=k.lru_conv_w[l][j, c * 128:(c + 1) * 128].rearrange("(p o) -> p o", o=1)), writes=["wconv"])
        P.dma(lambda e: e.dma_start(out=t["bconv"][:], in_=k.lru_conv_b[l].rearrange("(c p) -> p c", p=128)), writes=["bconv"])
        for d in range(2):
            P.dma(lambda e, d=d: e.dma_start(out=t["lam"][:, d * 2:d * 2 + 2], in_=k.lru_lambda[l][d].rearrange("(c p) -> p c", p=128)), writes=["lam"])
        for d in range(2):
            for gi, (wsrc, bsrc) in enumerate(((k.lru_wa, k.lru_ba), (k.lru_wx, k.lru_bx))):
                P.dma(lambda e, d=d, gi=gi, bsrc=bsrc: e.dma_start(out=t["bgate"][:, (d * 2 + gi) * 2:(d * 2 + gi) * 2 + 2], in_=bsrc[l][d].rearrange("(c p) -> p c", p=128)), writes=["bgate"])
                for c in range(2):
                    P.op("pool", lambda e: e.memset(t["wst"][:], 0.0), writes=["wst"])
                    for j in range(2):
                        P.dma(lambda e, d=d, c=c, j=j, wsrc=wsrc: e.dma_start(out=t["wst"][64 * j:64 * j + 64, 64 * j:64 * j + 64], in_=wsrc[l][d][2 * c + j]), writes=["wst"])
                    P.op("dve", lambda e, d=d, gi=gi, c=c: e.tensor_copy(out=t["Wbd"][:, (d * 2 + gi) * 2 + c, :], in_=t["wst"][:]), reads=["wst"], writes=["Wbd"])
        P.op("act", lambda e: e.activation(out=t["coef"][:], in_=t["lam"][:], func=AF.Exp, scale=-1.0), reads=["lam"], writes=["coef"])
        P.op("act", lambda e: e.activation(out=t["coef"][:], in_=t["coef"][:], func=AF.Ln, bias=1.0), reads=["coef"], writes=["coef"])
        P.op("dve", lambda e: e.tensor_scalar(out=t["coef2"][:], in0=t["coef"][:], scalar1=-16.0, scalar2=None, op0=ALU.mult), reads=["coef"], writes=["coef2"])
        P.op("dve", lambda e: e.tensor_scalar(out=t["coef"][:], in0=t["coef"][:], scalar1=-8.0, scalar2=None, op0=ALU.mult), reads=["coef"], writes=["coef"])
        for b in range(NB):
            for c in range(2):
                rds = [("zfd", l, b, g0) for g0 in (0, 512, 1024, 1536, 2048)]
                P.dma(lambda e, b=b, c=c: e.dma_start(out=t["lx"][:], in_=k.zf[ZF_LX + c * 128:ZF_LX + (c + 1) * 128, b * TB:(b + 1) * TB]), reads=rds, writes=["lx"])
                P.dma(lambda e, b=b, c=c: e.dma_start(out=t["lgz"][:], in_=k.zf[ZF_LG + c * 128:ZF_LG + (c + 1) * 128, b * TB:(b + 1) * TB]), reads=rds, writes=["lgz"])
                for (r0, r1) in ((0, SEQ), (SEQ, TB)):
                    P.op("dve", lambda e, r0=r0, r1=r1, c=c: e.tensor_scalar(out=t["xc"][:, r0:r1], in0=t["lx"][:, r0:r1], scalar1=t["wconv"][:, c, 2:3], scalar2=t["bconv"][:, c:c + 1],
                                                                          op0=ALU.mult, op1=ALU.add), reads=["lx", "wconv", "bconv"], writes=["xc"])
                    for j, o in ((0, -2), (1, -1), (3, 1)):
                        a0, a1 = max(r0, r0 - o), min(r1, r1 - o)
                        P.op("dve", lambda e, a0=a0, a1=a1, o=o, j=j, c=c: e.scalar_tensor_tensor(out=t["xc"][:, a0:a1], in0=t["lx"][:, a0 + o:a1 + o], scalar=t["wconv"][:, c, j:j + 1],
                                                                                              in1=t["xc"][:, a0:a1], op0=ALU.mult, op1=ALU.add), reads=["lx", "wconv", "xc"], writes=["xc"])
                P.op("act", lambda e: e.copy(out=t["xcb"][:], in_=t["xc"][:]), reads=["xc"], writes=["xcb"])
                for d in range(2):
                    hd = t["hf"] if d == 0 else t["hb"]
                    hk = "hf" if d == 0 else "hb"
                    for (g0, W) in BLK5:
                        for gi, dst in ((0, "rg"), (1, "ig")):
                            ps = psg[gi]
                            P.op("pe", lambda e, ps=ps, d=d, gi=gi, c=c, g0=g0, W=W: e.matmul(ps[:, 0:W], lhsT=t["Wbd"][:, (d * 2 + gi) * 2 + c, :], rhs=t["xcb"][:, g0:g0 + W], start=True, stop=True),
                                 reads=["Wbd", "xcb"], writes=["psg%d" % gi])
                            P.op("act", lambda e, ps=ps, d=d, gi=gi, c=c, W=W, dst=dst: e.activation(out=t[dst][:, 0:W], in_=ps[:, 0:W], func=AF.Sigmoid,
                                                                                                   bias=t["bgate"][:, (d * 2 + gi) * 2 + c:(d * 2 + gi) * 2 + c + 1]),
                                 reads=["psg%d" % gi, "bgate"], writes=[dst])
                        ci = d * 2 + c
                        P.op("act", lambda e, g0=g0, W=W, ci=ci: e.activation(out=t["af"][:, g0:g0 + W], in_=t["rg"][:, 0:W], func=AF.Exp, scale=t["coef"][:, ci:ci + 1]), reads=["rg", "coef"], writes=["af"])
                        P.op("act", lambda e, W=W, ci=ci: e.activation(out=t["a2"][:, 0:W], in_=t["rg"][:, 0:W], func=AF.Exp, scale=t["coef2"][:, ci:ci + 1]), reads=["rg", "coef2"], writes=["a2"])
                        P.op("act", lambda e, W=W: e.activation(out=t["a2"][:, 0:W], in_=t["a2"][:, 0:W], func=AF.Sqrt, scale=-1.0, bias=1.0), reads=["a2"], writes=["a2"])
                        P.op("dve", lambda e, g0=g0, W=W: e.tensor_tensor(out=t["ig"][:, 0:W], in0=t["ig"][:, 0:W], in1=t["xc"][:, g0:g0 + W], op=ALU.mult), reads=["ig", "xc"], writes=["ig"])
                        P.op("dve", lambda e, g0=g0, W=W: e.tensor_tensor(out=t["bf"][:, g0:g0 + W], in0=t["ig"][:, 0:W], in1=t["a2"][:, 0:W], op=ALU.mult), reads=["ig", "a2"], writes=["bf"])
                    if d == 0:
                        P.op("dve", lambda e, hd=hd: e.tensor_tensor_scan(out=hd[:, SEQ:TB], data0=t["af"][:, SEQ:TB], data1=t["bf"][:, SEQ:TB], initial=0.0, op0=ALU.mult, op1=ALU.add),
                             reads=["af", "bf"], writes=[hk])
                        P.op("dve", lambda e, hd=hd: e.tensor_tensor_scan(out=hd[:, 0:SEQ], data0=t["af"][:, 0:SEQ], data1=t["bf"][:, 0:SEQ], initial=hd[:, TB - 1:TB], op0=ALU.mult, op1=ALU.add),
                             reads=["af", "bf", hk], writes=[hk])
                    else:
                        P.op("dve", lambda e, hd=hd: e.tensor_tensor_scan(out=hd[:, SEQ:TB][:, ::-1], data0=t["af"][:, SEQ:TB][:, ::-1], data1=t["bf"][:, SEQ:TB][:, ::-1], initial=0.0,
                                                                         op0=ALU.mult, op1=ALU.add), reads=["af", "bf"], writes=[hk])
                        P.op("dve", lambda e, hd=hd: e.tensor_tensor_scan(out=hd[:, 0:SEQ][:, ::-1], data0=t["af"][:, 0:SEQ][:, ::-1], data1=t["bf"][:, 0:SEQ][:, ::-1], initial=hd[:, SEQ:SEQ + 1],
                                                                         op0=ALU.mult, op1=ALU.add), reads=["af", "bf", hk], writes=[hk])
                P.op("pool", lambda e: e.tensor_tensor(out=t["g1"][:], in0=t["lgz"][:], in1=t["lgz"][:], op=ALU.mult), reads=["lgz"], writes=["g1"])
                P.op("pool", lambda e: e.tensor_scalar(out=t["g1"][:], in0=t["g1"][:], scalar1=0.044715, scalar2=1.0, op0=ALU.mult, op1=ALU.add), reads=["g1"], writes=["g1"])
                P.op("pool", lambda e: e.tensor_tensor(out=t["g1"][:], in0=t["g1"][:], in1=t["lgz"][:], op=ALU.mult), reads=["g1", "lgz"], writes=["g1"])
                P.op("act", lambda e: e.activation(out=t["g2"][:], in_=t["g1"][:], func=AF.Sigmoid, scale=1.5957691216057308), reads=["g1"], writes=["g2"])
                P.op("pool", lambda e: e.tensor_tensor(out=t["g2"][:], in0=t["g2"][:], in1=t["lgz"][:], op=ALU.mult), reads=["g2", "lgz"], writes=["g2"])
                P.op("dve", lambda e: e.tensor_tensor(out=t["hf"][:], in0=t["hf"][:], in1=t["hb"][:], op=ALU.add), reads=["hf", "hb"], writes=["hf"])
                P.op("dve", lambda e: e.tensor_tensor(out=t["yo"][:], in0=t["hf"][:], in1=t["g2"][:], op=ALU.mult), reads=["hf", "g2"], writes=["yo"])
                P.dma(lambda e, b=b, c=c: e.dma_start(out=k.yT[768 + c * 128:768 + (c + 1) * 128, b * TB:(b + 1) * TB], in_=t["yo"][:]), reads=["yo"],
                      writes=[("yTd", l, 768, c, b)], dkey="yo_store")
        P.barrier()
        P.emit()


TWO_PI = 2.0 * math.pi


def stage_hyena(k, l, L, off):
    nc, P, NB = k.nc, k.P, k.NB
    hc = k.hc[L]
    nT = L // 128
    nS = 2 * nT
    W = min(512, L)
    BG = 2 if NB % 2 == 0 else 1
    with ExitStack() as es:
        t = _tiles(es, nc, [("idf", (128, 128), F32), ("idb", (128, 128), BF16),
                            ("HA", (128, nT, 256), BF16), ("HBm", (128, nT, 256), BF16), ("nHB", (128, nT, 256), BF16), ("P40", (128, 256), BF16),
                            ("m0", (128, 2), F32), ("wc", (128, 6, 3), F32), ("bc", (128, 6), F32), ("dcol", (128, 2), F32)])
        P.dma(lambda e: e.dma_start(out=t["idf"][:], in_=k.identf), writes=["idf"])
        P.op("dve", lambda e: e.tensor_copy(out=t["idb"][:], in_=t["idf"][:]), reads=["idf"], writes=["idb"])
        P.dma(lambda e: e.dma_start(out=t["m0"][:], in_=k.c_m0), writes=["m0"])
        for c in range(6):
            for j in range(3):
                P.dma(lambda e, c=c, j=j: e.dma_start(out=t["wc"][:, c, j:j + 1], in_=k.hy_conv_w[l][j, c * 128:(c + 1) * 128].rearrange("(p o) -> p o", o=1)), writes=["wc"])
        P.dma(lambda e: e.dma_start(out=t["bc"][:], in_=k.hy_conv_b[l].rearrange("(c p) -> p c", p=128)), writes=["bc"])
        P.dma(lambda e: e.dma_start(out=t["dcol"][:], in_=k.hy_d[l].rearrange("(c p) -> p c", p=128)), writes=["dcol"])
        with ExitStack() as e2:
            f = _tiles(e2, nc, [("w1", (33, 64), F32), ("w2", (64, 64), F32), ("w3", (64, 512), F32), ("fq", (64, 1), F32), ("b1", (64, 1), F32), ("b2", (64, 1), F32),
                                ("ze", (33, 2, L), F32), ("arg", (64, 512), F32), ("ni", (64, 512), mybir.dt.int32), ("nf", (64, 512), F32), ("npi", (64, 1), F32), ("h1", (64, 512), F32), ("h2", (64, 2, L), F32),
                                ("dl", (128, 256), F32), ("tneg", (128, 2, nT), F32), ("win", (128, 256), F32), ("g", (128, 2, nT, 256), BF16),
                                ("csl", (128, nS), F32), ("csh", (128, nS), F32), ("wf0", (128, nT, 128), BF16), ("wf1", (128, nT, 128), BF16),
                                ("Ht", (128, 256), F32), ("H", (128, nS, 256), F32)])
            pf = [_psum(e2, nc, "hpf%d" % i, [128, 512], F32) for i in range(4)]
            for nm, src in (("w1", k.hy_w1[l]), ("w2", k.hy_w2[l]), ("w3", k.hy_w3[l]), ("dl", hc["dl"]), ("csl", hc["csl"]), ("csh", hc["csh"])):
                P.dma(lambda e, nm=nm, src=src: e.dma_start(out=f[nm][:], in_=src), writes=[nm])
            for nm, src in (("fq", k.hy_freq[l]), ("b1", k.hy_b1[l]), ("b2", k.hy_b2[l])):
                P.dma(lambda e, nm=nm, src=src: e.dma_start(out=f[nm][:], in_=src.rearrange("(p o) -> p o", o=1)), writes=[nm])
            for gi, nm in enumerate(("zf", "zr")):
                P.dma(lambda e, gi=gi, nm=nm: e.dma_start(out=f["ze"][:, gi, :], in_=hc[nm]), writes=["ze"])
            for gi, nm in enumerate(("tf", "tr")):
                P.dma(lambda e, gi=gi, nm=nm: e.dma_start(out=f["tneg"][:, gi, :], in_=hc[nm]), writes=["tneg"])
            P.op("pool", lambda e: e.memset(f["npi"][:], -math.pi), writes=["npi"])
            P.op("dve", lambda e: e.tensor_tensor(out=f["b1"][:], in0=f["b1"][:], in1=f["fq"][:], op=ALU.mult), reads=["b1", "fq"], writes=["b1"])
            P.op("dve", lambda e: e.tensor_tensor(out=f["b2"][:], in0=f["b2"][:], in1=f["fq"][:], op=ALU.mult), reads=["b2", "fq"], writes=["b2"])

            def sin_layer(ps, pk, bias, dst_ap, dkey):
                P.op("dve", lambda e: e.tensor_scalar(out=f["arg"][:, 0:W], in0=ps[0:64, 0:W], scalar1=f["fq"][:, 0:1], scalar2=bias[:, 0:1], op0=ALU.mult, op1=ALU.add),
                     reads=[pk, "fq", "b1", "b2"], writes=["arg"])
                P.op("dve", lambda e: e.tensor_scalar(out=f["arg"][:, 0:W], in0=f["arg"][:, 0:W], scalar1=1.0 / TWO_PI, scalar2=4.5, op0=ALU.mult, op1=ALU.add), reads=["arg"], writes=["arg"])
                P.op("dve", lambda e: e.tensor_copy(out=f["ni"][:, 0:W], in_=f["arg"][:, 0:W]), reads=["arg"], writes=["ni"])
                P.op("dve", lambda e: e.tensor_copy(out=f["nf"][:, 0:W], in_=f["ni"][:, 0:W]), reads=["ni"], writes=["nf"])
                P.op("dve", lambda e: e.tensor_tensor(out=f["arg"][:, 0:W], in0=f["arg"][:, 0:W], in1=f["nf"][:, 0:W], op=ALU.subtract), reads=["arg", "nf"], writes=["arg"])
                P.op("dve", lambda e: e.tensor_scalar(out=f["nf"][:, 0:W], in0=f["arg"][:, 0:W], scalar1=0.0, scalar2=None, op0=ALU.is_lt), reads=["arg", "nf"], writes=["nf"])
                P.op("dve", lambda e: e.tensor_tensor(out=f["arg"][:, 0:W], in0=f["arg"][:, 0:W], in1=f["nf"][:, 0:W], op=ALU.add), reads=["arg", "nf"], writes=["arg"])
                P.op("act", lambda e: e.activation(out=dst_ap, in_=f["arg"][:, 0:W], func=AF.Sin, scale=TWO_PI, bias=f["npi"][:, 0:1]), reads=["arg", "npi"], writes=[dkey])

            for gi in range(2):
                for cb in range(L // W):
                    P.op("pe", lambda e, gi=gi, cb=cb: e.matmul(pf[0][0:64, 0:W], lhsT=f["w1"][:], rhs=f["ze"][:, gi, cb * W:(cb + 1) * W], start=True, stop=True), reads=["w1", "ze"], writes=["hpf0"])
                    sin_layer(pf[0], "hpf0", f["b1"], f["h1"][:, 0:W], "h1")
                    P.op("pe", lambda e: e.matmul(pf[1][0:64, 0:W], lhsT=f["w2"][:], rhs=f["h1"][:, 0:W], start=True, stop=True), reads=["w2", "h1"], writes=["hpf1"])
                    sin_layer(pf[1], "hpf1", f["b2"], f["h2"][:, gi, cb * W:(cb + 1) * W], "h2")
                for tt in range(nT):
                    P.op("pe", lambda e, gi=gi, tt=tt: e.matmul(pf[2][:, :], lhsT=f["h2"][:, gi, tt * 128:(tt + 1) * 128], rhs=f["w3"][:], start=True, stop=True), reads=["h2", "w3"], writes=["hpf2"])
                    P.op("act", lambda e, gi=gi, tt=tt: e.activation(out=f["win"][:], in_=f["dl"][:], func=AF.Exp, scale=f["tneg"][:, gi, tt:tt + 1]), reads=["dl", "tneg"], writes=["win"])
                    P.op("dve", lambda e, gi=gi, tt=tt: e.tensor_tensor(out=f["g"][:, gi, tt, :], in0=pf[2][:, gi * 256:(gi + 1) * 256], in1=f["win"][:], op=ALU.mult), reads=["hpf2", "win"], writes=["g"])
            P.op("dve", lambda e: e.memset(f["g"][0:1, 1, 0, :], 0.0), reads=["g"], writes=["g"])
            for j in range(nS):
                wf = f["wf%d" % (j % 2)]
                wk = "wf%d" % (j % 2)
                P.dma(lambda e, j=j, wf=wf: e.dma_start(out=wf[:], in_=hc["Wf"].rearrange("(t p) s -> p t s", p=128)[:, :, j * 128:(j + 1) * 128]), writes=[wk])
                for gi in range(2):
                    for tt in range(nT):
                        P.op("pe", lambda e, gi=gi, tt=tt, wf=wf: e.matmul(pf[gi][:, 0:256], lhsT=wf[:, tt, :], rhs=f["g"][:, gi, tt, :], start=(tt == 0), stop=(tt == nT - 1)),
                             reads=[wk, "g"], writes=["hpf%d" % gi], skip_self=(tt > 0))
                P.op("dve", lambda e, j=j: e.tensor_scalar(out=f["Ht"][:], in0=pf[0][:, 0:256], scalar1=f["csl"][:, j:j + 1], scalar2=None, op0=ALU.mult), reads=["hpf0", "csl"], writes=["Ht"])
                P.op("dve", lambda e, j=j: e.scalar_tensor_tensor(out=f["H"][:, j, :], in0=pf[1][:, 0:256], scalar=f["csh"][:, j:j + 1], in1=f["Ht"][:], op0=ALU.mult, op1=ALU.add),
                     reads=["hpf1", "csh", "Ht"], writes=["H"])
            HAf, HBf = f["H"][:, 0:nT, :], f["H"][:, nT:nS, :]
            P.op("act", lambda e: e.copy(out=t["HA"][:], in_=HAf), reads=["H"], writes=["HA"])
            P.op("act", lambda e: e.copy(out=t["HBm"][:], in_=HBf), reads=["H"], writes=["HBm"])
            P.op("dve", lambda e: e.tensor_scalar(out=t["HBm"][:, 0, :], in0=f["H"][:, nT, :], scalar1=t["m0"][:, 0:1], scalar2=None, op0=ALU.mult), reads=["H", "m0", "HBm"], writes=["HBm"])
            P.op("dve", lambda e: e.tensor_scalar(out=t["nHB"][:], in0=t["HBm"][:], scalar1=-1.0, scalar2=None, op0=ALU.mult), reads=["HBm"], writes=["nHB"])
            P.op("dve", lambda e: e.tensor_scalar(out=f["Ht"][:], in0=f["H"][:, 0, :], scalar1=t["m0"][:, 0:1], scalar2=None, op0=ALU.mult), reads=["H", "m0"], writes=["Ht"])
            P.op("dve", lambda e: e.scalar_tensor_tensor(out=t["P40"][:], in0=f["H"][:, nT, :], scalar=t["m0"][:, 1:2], in1=f["Ht"][:], op0=ALU.mult, op1=ALU.add), reads=["H", "m0", "Ht"], writes=["P40"])
            P.barrier()
        with ExitStack() as e3:
            g = _tiles(e3, nc, [("zh", (128, 6, L), BF16), ("u1", (128, L), F32), ("u2", (128, L), F32),
                                ("x0c", (128, 2, BG, L), BF16), ("sT", (128, 2, BG, L), BF16), ("stm", (128, nT, BG * 256), BF16),
                                ("Y", (128, nS, BG * 256), BF16), ("wfa", (128, nT, 128), BF16), ("wfb", (128, nT, 128), BF16),
                                ("wiv", (128, nS, W), BF16), ("p1", (128, BG * 256), F32), ("p2", (128, BG * 256), F32),
                                ("tmp", (128, W), F32), ("yo0", (128, W), BF16), ("yo1", (128, W), BF16)])
            pstb = _psum(e3, nc, "hpst", [128, 8, 128], BF16)
            psA = _psum(e3, nc, "hpA", [128, 512], F32)
            psB = _psum(e3, nc, "hpB", [128, 512], F32)
            psI = [_psum(e3, nc, "hpI%d" % i, [128, 512], F32) for i in range(4)]
            wf_v = hc["Wf"].rearrange("(t p) s -> p t s", p=128)
            wi_v = hc["Winv"].rearrange("(s p) t -> p s t", p=128)
            for g0 in range(0, NB, BG):
                for bb in range(BG):
                    b = g0 + bb
                    rds = [("zfd", l, b, x) for x in (0, 512, 1024, 1536, 2048)]
                    for c in range(6):
                        P.dma(lambda e, c=c, b=b: e.dma_start(out=g["zh"][:, c, :], in_=k.zf[ZF_ZH + c * 128:ZF_ZH + (c + 1) * 128, b * TB + off:b * TB + off + L]), reads=rds, writes=["zh"])

                    def conv3(c, dst_ap, dkey):
                        P.op("dve", lambda e: e.tensor_scalar(out=dst_ap, in0=g["zh"][:, c, :], scalar1=t["wc"][:, c, 1:2], scalar2=t["bc"][:, c:c + 1], op0=ALU.mult, op1=ALU.add),
                             reads=["zh", "wc", "bc"], writes=[dkey])
                        P.op("dve", lambda e: e.scalar_tensor_tensor(out=dst_ap[:, 1:L], in0=g["zh"][:, c, 0:L - 1], scalar=t["wc"][:, c, 0:1], in1=dst_ap[:, 1:L], op0=ALU.mult, op1=ALU.add),
                             reads=["zh", "wc", dkey], writes=[dkey])
                        P.op("dve", lambda e: e.scalar_tensor_tensor(out=dst_ap[:, 0:L - 1], in0=g["zh"][:, c, 1:L], scalar=t["wc"][:, c, 2:3], in1=dst_ap[:, 0:L - 1], op0=ALU.mult, op1=ALU.add),
                             reads=["zh", "wc", dkey], writes=[dkey])
                    for c in range(2):
                        conv3(c, g["u1"][:, :], "u1")
                        P.op("act", lambda e, c=c, bb=bb: e.copy(out=g["x0c"][:, c, bb, :], in_=g["u1"][:]), reads=["u1"], writes=["x0c"])
                        conv3(2 + c, g["u1"][:, :], "u1")
                        conv3(4 + c, g["u2"][:, :], "u2")
                        P.op("dve", lambda e, c=c, bb=bb: e.tensor_tensor(out=g["sT"][:, c, bb, :], in0=g["u1"][:], in1=g["u2"][:], op=ALU.mult), reads=["u1", "u2"], writes=["sT"])
                    for c in range(2):
                        for t4 in range(0, nT, 8):
                            n8 = min(8, nT - t4)
                            for i in range(n8):
                                P.op("pe", lambda e, c=c, bb=bb, i=i, t4=t4: e.transpose(out=pstb[:, i, :], in_=g["sT"][:, c, bb, (t4 + i) * 128:(t4 + i + 1) * 128], identity=t["idb"][:]),
                                     reads=["sT", "idb"], writes=["hpst"])
                            P.op("act", lambda e, c=c, bb=bb, t4=t4, n8=n8: e.copy(out=g["stm"][:, t4:t4 + n8, bb * 256 + c * 128:bb * 256 + (c + 1) * 128], in_=pstb[:, 0:n8, :]),
                                 reads=["hpst"], writes=["stm"])
                for j in range(nT):
                    P.dma(lambda e, j=j: e.dma_start(out=g["wfa"][:], in_=wf_v[:, :, j * 128:(j + 1) * 128]), writes=["wfa"])
                    P.dma(lambda e, j=j: e.dma_start(out=g["wfb"][:], in_=wf_v[:, :, (nT + j) * 128:(nT + j + 1) * 128]), writes=["wfb"])
                    for tt in range(nT):
                        P.op("pe", lambda e, tt=tt: e.matmul(psA[:, 0:BG * 256], lhsT=g["wfa"][:, tt, :], rhs=g["stm"][:, tt, :], start=(tt == 0), stop=(tt == nT - 1)),
                             reads=["wfa", "stm"], writes=["hpA"], skip_self=(tt > 0))
                    for tt in range(nT):
                        P.op("pe", lambda e, tt=tt: e.matmul(psB[:, 0:BG * 256], lhsT=g["wfb"][:, tt, :], rhs=g["stm"][:, tt, :], start=(tt == 0), stop=(tt == nT - 1)),
                             reads=["wfb", "stm"], writes=["hpB"], skip_self=(tt > 0))
                    A3 = psA[:, 0:BG * 256].rearrange("p (b c) -> p b c", b=BG)
                    B3 = psB[:, 0:BG * 256].rearrange("p (b c) -> p b c", b=BG)
                    p1 = g["p1"][:].rearrange("p (b c) -> p b c", b=BG)
                    p2 = g["p2"][:].rearrange("p (b c) -> p b c", b=BG)

                    def bc3(tab):
                        return tab.unsqueeze(1).to_broadcast([128, BG, 256])
                    P4 = t["P40"][:, :] if j == 0 else t["HA"][:, j, :]
                    P.op("dve", lambda e, j=j: e.tensor_tensor(out=p1, in0=A3, in1=bc3(t["HA"][:, j, :]), op=ALU.mult), reads=["hpA", "HA"], writes=["p1"])
                    P.op("dve", lambda e, j=j: e.tensor_tensor(out=p2, in0=B3, in1=bc3(t["nHB"][:, j, :]), op=ALU.mult), reads=["hpB", "nHB"], writes=["p2"])
                    P.op("pool", lambda e, j=j: e.tensor_tensor(out=g["Y"][:, j, :], in0=g["p1"][:], in1=g["p2"][:], op=ALU.add), reads=["p1", "p2"], writes=["Y"])
                    P.op("dve", lambda e, j=j: e.tensor_tensor(out=p1, in0=A3, in1=bc3(t["HBm"][:, j, :]), op=ALU.mult), reads=["hpA", "HBm", "Y"], writes=["p1"])
                    P.op("dve", lambda e, j=j, P4=P4: e.tensor_tensor(out=p2, in0=B3, in1=bc3(P4), op=ALU.mult), reads=["hpB", "HA", "P40", "Y"], writes=["p2"])
                    P.op("pool", lambda e, j=j: e.tensor_tensor(out=g["Y"][:, nT + j, :], in0=g["p1"][:], in1=g["p2"][:], op=ALU.add), reads=["p1", "p2"], writes=["Y"])
                for tb in range(L // W):
                    P.dma(lambda e, tb=tb: e.dma_start(out=g["wiv"][:], in_=wi_v[:, :, tb * W:(tb + 1) * W]), writes=["wiv"])
                    for bb in range(BG):
                        for c in range(2):
                            ps = psI[bb * 2 + c]
                            pk = "hpI%d" % (bb * 2 + c)
                            for sc in range(nS):
                                P.op("pe", lambda e, ps=ps, sc=sc, bb=bb, c=c: e.matmul(ps[:, 0:W], lhsT=g["Y"][:, sc, bb * 256 + c * 128:bb * 256 + (c + 1) * 128], rhs=g["wiv"][:, sc, :],
                                                                                  start=(sc == 0), stop=(sc == nS - 1)), reads=["Y", "wiv"], writes=[pk], skip_self=(sc > 0))
                            yo = g["yo%d" % (k.yc % 2)]
                            yk = "yo%d" % (k.yc % 2)
                            k.yc += 1
                            P.op("dve", lambda e, ps=ps, bb=bb, c=c, tb=tb: e.scalar_tensor_tensor(out=g["tmp"][:], in0=g["sT"][:, c, bb, tb * W:(tb + 1) * W], scalar=t["dcol"][:, c:c + 1], in1=ps[:, 0:W],
                                                                                             op0=ALU.mult, op1=ALU.add), reads=["sT", "dcol", pk], writes=["tmp"])
                            P.op("dve", lambda e, bb=bb, c=c, tb=tb, yo=yo: e.tensor_tensor(out=yo[:], in0=g["tmp"][:], in1=g["x0c"][:, c, bb, tb * W:(tb + 1) * W], op=ALU.mult),
                                 reads=["tmp", "x0c"], writes=[yk])
                            b = g0 + bb
                            P.dma(lambda e, yo=yo, b=b, c=c, tb=tb: e.dma_start(out=k.yT[512 + c * 128:512 + (c + 1) * 128, b * TB + off + tb * W:b * TB + off + (tb + 1) * W], in_=yo[:]),
                                  reads=[yk], writes=[("yTd", l, 512, c, b, off, tb)], dkey=("hyo", yk))
            P.barrier()
        P.emit()


def hyena_consts(L):
    n = L
    t_lin = np.linspace(0.0, 1.0, n, dtype=np.float32)
    bands = np.linspace(1e-4, 15.0, 16, dtype=np.float32)
    w = (2.0 * np.float32(math.pi) * np.arange(n, dtype=np.float32) / np.float32(n)).astype(np.float32)
    z = np.concatenate([t_lin[:, None], np.cos(bands[None, :] * w[:, None]), -np.sin(bands[None, :] * w[:, None])], axis=-1).astype(np.float32)
    ridx = (n - np.arange(n)) % n
    out = {}
    out["zf"] = np.ascontiguousarray(z.T)
    out["zr"] = np.ascontiguousarray(z[ridx].T)
    tf = -t_lin
    tr = -t_lin[ridx]
    out["tf"] = np.ascontiguousarray(tf.reshape(n // 128, 128).T)
    out["tr"] = np.ascontiguousarray(tr.reshape(n // 128, 128).T)
    max_decay = math.log(1e-2) / 0.3
    min_decay = math.log(1e-2) / 1.5
    deltas = np.abs(np.linspace(min_decay, max_decay, 256, dtype=np.float32))
    out["dl"] = np.broadcast_to(deltas[None, :], (128, 256)).astype(np.float32).copy()
    N = 2 * n
    tt = np.arange(n, dtype=np.float64)[:, None]
    kk = np.arange(n, dtype=np.float64)[None, :]
    ang = 2.0 * np.pi * ((tt * kk) % N) / N
    C = np.cos(ang)
    S = np.sin(ang)
    S[:, 0] = (-1.0) ** np.arange(n)
    Wf = np.concatenate([C, S], axis=1)
    out["Wf"] = Wf.astype(ml_dtypes.bfloat16)
    out["Winv"] = np.ascontiguousarray(Wf.T).astype(ml_dtypes.bfloat16)
    cw = np.full(N, 2.0 / N)
    cw[0] = 1.0 / N
    cw[n] = 1.0 / N
    sgn = np.tile((-1.0) ** np.arange(n), 2)
    sgn[n] = 1.0
    out["csl"] = np.ascontiguousarray(cw.reshape(N // 128, 128).T).astype(np.float32)
    out["csh"] = np.ascontiguousarray((cw * sgn).reshape(N // 128, 128).T).astype(np.float32)
    return out


def stage_outproj(k, l):
    nc, P, NB = k.nc, k.P, k.NB
    ntile = 18 if l < DEPTH - 1 else 16
    with ExitStack() as es:
        A, B = load_mod_cols(k, es, l, 1)
        t = _tiles(es, nc, [("idf", (128, 128), F32), ("idb", (128, 128), BF16), ("wst0", (128, D), F32), ("wst1", (128, D), F32), ("gng", (128, 8), F32), ("wout", (128, 8, D), BF16), ("wrf", (128, 8, 36), F32), ("wr", (128, 8, 36), BF16), ("ones", (128, 1), BF16), ("g1b", (128, D), F32), ("ohrun", (128, 32), F32), ("iota32", (128, 32), F32), ("LTf", (128, 128), F32), ("ONESf", (128, 128), F32), ("jbv", (128, 128), F32), ("A2row", (128, D), F32), ("B2row", (128, D), F32), ("g2row", (128, D), F32), ("cnt", (128, 32), F32), ("nf", (128, 32), F32), ("ni", (128, 32), mybir.dt.int32), ("dd", (128, 32), F32), ("up", (128, 32), F32), ("pend", (128, 32), F32), ("zero32", (128, 32), F32), ("cmp", (128, MAXBLK, 32), F32), ("bexp", (128, MAXBLK), F32)])
        ts = []
        for s_ in range(2):
            d_ = dict(t)
            d_.update(_tiles(es, nc, [("yt", (128, 8, 128), BF16), ("ysq", (128, 8, 128), BF16), ("r", (128, 4), F32), ("m", (128, D), F32), ("xt0", (128, D), F32), ("junk0", (128, D), BF16), ("ss0", (128, 1), F32), ("rstd0", (128, 1), F32), ("xn0", (128, D), BF16), ("fT", (128, 8, 128), BF16), ("lg", (128, 36), F32), ("s1", (128, 8), F32), ("s2", (128, 8), F32), ("s3", (128, 8), F32), ("mg", (128, 4), F32), ("pr", (128, 4, 8), F32), ("es", (128, 8), F32), ("m1", (128, 8), F32), ("m2", (128, 8), F32), ("e2", (128, 8), F32), ("cw8", (128, 8), F32), ("cw", (128, 4, 8), F32), ("oh1", (128, 4, 8), F32), ("oh2", (128, 4, 8), F32), ("ohs", (128, 32), F32), ("ohp", (128, 32), F32), ("rt", (128, 8), F32), ("ft1", (128, D), F32), ("ftm", (128, D), BF16)]))
            d_["pst"] = _psum(es, nc, "pst", [128, 8, 128], BF16)
            d_["pm"] = [_psum(es, nc, "pm%d" % i, [128, 512], F32) for i in range(2)]
            d_["misc"] = _psum(es, nc, "misc", [128, 512], F32)
            ts.append(d_)
        for nm, src in (("iota32", k.c_iota32), ("LTf", k.c_LT), ("ONESf", k.c_ONES), ("jbv", k.c_jbv)):
            P.dma(lambda e, nm=nm, src=src: e.dma_start(out=t[nm][:], in_=src), writes=[nm])
        P.op("pool", lambda e: e.memset(t["ohrun"][:], 0.0), writes=["ohrun"])
        P.op("pool", lambda e: e.memset(t["zero32"][:], 0.0), writes=["zero32"])
        for d_ in ts:
            P.op("pool", lambda e, d_=d_: e.memset(d_["rt"][:], 0.0), writes=["rt_" + str(ts.index(d_))])
        P.dma(lambda e: e.dma_start(out=t["g2row"][:], in_=k.norm2_g[l].partition_broadcast(128)), writes=["g2row"])
        P.dma(lambda e: e.dma_start(out=t["idf"][:], in_=k.identf), writes=["idf"])
        P.op("dve", lambda e: e.tensor_copy(out=t["idb"][:], in_=t["idf"][:]), reads=["idf"], writes=["idb"])
        P.op("pool", lambda e: e.memset(t["ones"][:], 1.0), writes=["ones"])
        P.dma(lambda e: e.dma_start(out=t["gng"][:], in_=k.group_norm_g[l].rearrange("(c p) -> p c", p=128)), writes=["gng"])
        for kk in range(8):
            ws = t["wst%d" % (kk % 2)]
            wk = "wst%d" % (kk % 2)
            P.dma(lambda e, kk=kk, ws=ws: e.dma_start(out=ws[:], in_=k.w_out[l][kk * 128:(kk + 1) * 128, :]), writes=[wk])
            P.op("dve", lambda e, kk=kk, ws=ws: e.tensor_scalar(out=t["wout"][:, kk, :], in0=ws[:], scalar1=t["gng"][:, kk:kk + 1], scalar2=None, op0=ALU.mult), reads=[wk, "gng"], writes=["wout"])
        P.dma(lambda e: e.dma_start(out=t["wrf"][:, :, 0:4], in_=k.moe_w_group[l].rearrange("(c p) n -> p c n", p=128)), writes=["wrf"])
        P.dma(lambda e: e.dma_start(out=t["wrf"][:, :, 4:36], in_=k.moe_w_expert[l].rearrange("(c p) n -> p c n", p=128)), writes=["wrf"])
        P.op("dve", lambda e: e.tensor_copy(out=t["wr"][:], in_=t["wrf"][:]), reads=["wrf"], writes=["wr"])
        yT_v = k.yT.rearrange("(c p) t -> p c t", p=128)
        fT_v = k.fT.rearrange("(c p) t -> p c t", p=128)
        def tile_body(t, b, i, other=None):
            P = k.P
            if True:
                t0 = b * TB + i * 128
                row = b if i < 16 else NB
                P.dma(lambda e, t0=t0: e.dma_start(out=t["yt"][:], in_=yT_v[:, :, t0:t0 + 128]), writes=["yt"])
                P.dma(lambda e, b=b, i=i: e.dma_start(out=t["xt0"][:], in_=xsrc(k, l, b, i)), writes=["xt0"])
                P.op("pool", lambda e: e.tensor_tensor(out=t["ysq"][:], in0=t["yt"][:], in1=t["yt"][:], op=ALU.mult), reads=["yt"], writes=["ysq"])
                for g in range(4):
                    for kc in range(2):
                        P.op("pe", lambda e, g=g, kc=kc: e.matmul(t["misc"][:, g:g + 1], lhsT=t["ysq"][:, 2 * g + kc, :], rhs=t["ones"][:], start=(kc == 0), stop=(kc == 1)), reads=["ysq", "ones"], writes=["misc"],
                             skip_self=(kc > 0))
                P.op("act", lambda e: e.activation(out=t["r"][:], in_=t["misc"][:, 0:4], func=AF.Sqrt, scale=1.0 / 256, bias=EPS), reads=["misc"], writes=["r"])
                P.op("dve", lambda e: e.reciprocal(out=t["r"][:], in_=t["r"][:]), reads=["r"], writes=["r"])
                for half in range(2):
                    for g in range(4):
                        ps = t["pm"][g % 2]
                        pk = "pm%d" % (g % 2)
                        for kc in range(2):
                            P.op("pe", lambda e, ps=ps, g=g, kc=kc, half=half: e.matmul(ps[:, :], lhsT=t["yt"][:, 2 * g + kc, :], rhs=t["wout"][:, 2 * g + kc, half * 512:(half + 1) * 512], start=(kc == 0), stop=(kc == 1)),
                                 reads=["yt", "wout"], writes=[pk], skip_self=(kc > 0))
                        if g == 0:
                            P.op("dve", lambda e, ps=ps, half=half: e.tensor_scalar(out=t["m"][:, half * 512:(half + 1) * 512], in0=ps[:, :], scalar1=t["r"][:, 0:1], scalar2=None, op0=ALU.mult), reads=[pk, "r"], writes=["m"])
                        else:
                            P.op("dve", lambda e, ps=ps, half=half, g=g: e.scalar_tensor_tensor(out=t["m"][:, half * 512:(half + 1) * 512], in0=ps[:, :], scalar=t["r"][:, g:g + 1], in1=t["m"][:, half * 512:(half + 1) * 512],
                                                                                             op0=ALU.mult, op1=ALU.add), reads=[pk, "r", "m"], writes=["m"])
                P.op("pool", lambda e: e.tensor_tensor(out=t["m"][:], in0=t["m"][:], in1=t["g1b"][:], op=ALU.mult), reads=["m", "g1b"], writes=["m"])
                P.op("dve", lambda e: e.tensor_tensor(out=t["xt0"][:], in0=t["xt0"][:], in1=t["m"][:], op=ALU.add), reads=["xt0", "m"], writes=["xt0"])
                P.dma(lambda e, t0=t0: e.dma_start(out=k.x1[t0:t0 + 128, :], in_=t["xt0"][:]), reads=["xt0"], writes=[("x1d", t0)], dkey="x1st")
                norm_mod_T(k, t, t["pst"], t["xt0"][:], "xt0", A, B, row, t["fT"], "fT", 0, "0")
                P.dma(lambda e, t0=t0: e.dma_start(out=fT_v[:, :, t0:t0 + 128], in_=t["fT"][:]), reads=["fT"], writes=[("fTd", t0)], dkey="fTst")
                for kk in range(8):
                    P.op("pe", lambda e, kk=kk: e.matmul(t["misc"][:, 64:100], lhsT=t["fT"][:, kk, :], rhs=t["wr"][:, kk, :], start=(kk == 0), stop=(kk == 7)), reads=["fT", "wr"], writes=["misc"], skip_self=(kk > 0))
                P.op("act", lambda e: e.copy(out=t["lg"][:], in_=t["misc"][:, 64:100]), reads=["misc"], writes=["lg"])
                gl = t["lg"][:, 0:4]
                el = t["lg"][:, 4:36].rearrange("p (g e) -> p g e", g=4)
                s1, s2, s3 = t["s1"], t["s2"], t["s3"]
                P.op("dve", lambda e: e.tensor_reduce(out=s1[:, 0:1], in_=gl, axis=AX.X, op=ALU.max), reads=["lg"], writes=["s1"])
                P.op("dve", lambda e: e.tensor_scalar(out=s1[:, 1:2], in0=s1[:, 0:1], scalar1=-1.0, scalar2=None, op0=ALU.mult), reads=["s1"], writes=["s1"])
                P.op("act", lambda e: e.activation(out=t["mg"][:], in_=gl, func=AF.Exp, bias=s1[:, 1:2], accum_out=s1[:, 2:3]), reads=["lg", "s1"], writes=["mg", "s1"])
                P.op("dve", lambda e: e.reciprocal(out=s1[:, 3:4], in_=s1[:, 2:3]), reads=["s1"], writes=["s1"])
                P.op("dve", lambda e: e.tensor_scalar(out=t["mg"][:], in0=gl, scalar1=s1[:, 0:1], scalar2=None, op0=ALU.is_equal), reads=["lg", "s1", "mg"], writes=["mg"])
                P.op("dve", lambda e: e.tensor_tensor(out=t["pr"][:], in0=el, in1=t["mg"][:].unsqueeze(2).to_broadcast([128, 4, 8]), op=ALU.mult), reads=["lg", "mg"], writes=["pr"])
                P.op("dve", lambda e: e.tensor_reduce(out=t["es"][:], in_=t["pr"][:].rearrange("p g e -> p e g"), axis=AX.X, op=ALU.add), reads=["pr"], writes=["es"])
                P.op("dve", lambda e: e.tensor_reduce(out=s2[:, 0:1], in_=t["es"][:], axis=AX.X, op=ALU.max), reads=["es"], writes=["s2"])
                P.op("dve", lambda e: e.tensor_scalar(out=t["m1"][:], in0=t["es"][:], scalar1=s2[:, 0:1], scalar2=None, op0=ALU.is_equal), reads=["es", "s2"], writes=["m1"])
                P.op("dve", lambda e: e.scalar_tensor_tensor(out=t["e2"][:], in0=t["m1"][:], scalar=-1e30, in1=t["es"][:], op0=ALU.mult, op1=ALU.add), reads=["m1", "es"], writes=["e2"])
                P.op("dve", lambda e: e.tensor_reduce(out=s2[:, 1:2], in_=t["e2"][:], axis=AX.X, op=ALU.max), reads=["e2"], writes=["s2"])
                P.op("dve", lambda e: e.tensor_scalar(out=t["m2"][:], in0=t["e2"][:], scalar1=s2[:, 1:2], scalar2=None, op0=ALU.is_equal), reads=["e2", "s2"], writes=["m2"])
                P.op("dve", lambda e: e.tensor_tensor(out=s2[:, 2:3], in0=s2[:, 1:2], in1=s2[:, 0:1], op=ALU.subtract), reads=["s2"], writes=["s2"])
                P.op("act", lambda e: e.activation(out=s2[:, 3:4], in_=s2[:, 2:3], func=AF.Exp), reads=["s2"], writes=["s2"])
                P.op("dve", lambda e: e.tensor_scalar(out=s3[:, 0:1], in0=s2[:, 3:4], scalar1=1.0, scalar2=None, op0=ALU.add), reads=["s2"], writes=["s3"])
                P.op("dve", lambda e: e.reciprocal(out=s3[:, 1:2], in_=s3[:, 0:1]), reads=["s3"], writes=["s3"])
                P.op("dve", lambda e: e.tensor_tensor(out=s3[:, 2:3], in0=s3[:, 1:2], in1=s1[:, 3:4], op=ALU.mult), reads=["s3", "s1"], writes=["s3"])
                P.op("dve", lambda e: e.tensor_tensor(out=s3[:, 3:4], in0=s3[:, 2:3], in1=s2[:, 3:4], op=ALU.mult), reads=["s3", "s2"], writes=["s3"])
                P.op("dve", lambda e: e.tensor_scalar(out=t["cw8"][:], in0=t["m1"][:], scalar1=s3[:, 2:3], scalar2=None, op0=ALU.mult), reads=["m1", "s3"], writes=["cw8"])
                P.op("dve", lambda e: e.scalar_tensor_tensor(out=t["cw8"][:], in0=t["m2"][:], scalar=s3[:, 3:4], in1=t["cw8"][:], op0=ALU.mult, op1=ALU.add), reads=["m2", "s3", "cw8"], writes=["cw8"])
                P.op("dve", lambda e: e.tensor_tensor(out=t["cw"][:], in0=t["mg"][:].unsqueeze(2).to_broadcast([128, 4, 8]), in1=t["cw8"][:].unsqueeze(1).to_broadcast([128, 4, 8]), op=ALU.mult),
                     reads=["mg", "cw8"], writes=["cw"])
                P.dma(lambda e, t0=t0: e.dma_start(out=k.cw[t0:t0 + 128, :], in_=t["cw"][:].rearrange("p g e -> p (g e)")), reads=["cw"], writes=[("cwd", t0)], dkey="cwst")
                mgb = t["mg"][:].unsqueeze(2).to_broadcast([128, 4, 8])
                P.op("dve", lambda e: e.tensor_tensor(out=t["oh1"][:], in0=mgb, in1=t["m1"][:].unsqueeze(1).to_broadcast([128, 4, 8]), op=ALU.mult), reads=["mg", "m1"], writes=["oh1"])
                P.op("dve", lambda e: e.tensor_tensor(out=t["oh2"][:], in0=mgb, in1=t["m2"][:].unsqueeze(1).to_broadcast([128, 4, 8]), op=ALU.mult), reads=["mg", "m2"], writes=["oh2"])
                oh1f = t["oh1"][:].rearrange("p g e -> p (g e)")
                oh2f = t["oh2"][:].rearrange("p g e -> p (g e)")
                P.op("dve", lambda e: e.tensor_tensor(out=t["ohs"][:], in0=oh1f, in1=oh2f, op=ALU.add), reads=["oh1", "oh2"], writes=["ohs"])
                P.op("pe", lambda e: e.matmul(t["misc"][:, 128:160], lhsT=t["LTf"][:], rhs=t["ohs"][:], start=True, stop=False), reads=["LTf", "ohs"], writes=["misc"])
                P.op("pe", lambda e: e.matmul(t["misc"][:, 128:160], lhsT=t["ONESf"][:], rhs=t["ohrun"][:], start=False, stop=(other is None)), reads=["ONESf", "ohrun"], writes=["misc"], skip_self=True)
                if other is not None:
                    P.op("pe", lambda e: e.matmul(t["misc"][:, 128:160], lhsT=t["ONESf"][:], rhs=other["ohs"][:], start=False, stop=True), reads=["ONESf", "ohs_0"], writes=["misc"], skip_self=True)
                for j, ohf, okey in ((0, oh1f, "oh1"), (1, oh2f, "oh2")):
                    P.op("dve", lambda e, ohf=ohf: e.tensor_tensor(out=t["ohp"][:], in0=t["misc"][:, 128:160], in1=ohf, op=ALU.mult), reads=["misc", okey], writes=["ohp"])
                    P.op("dve", lambda e, j=j: e.tensor_reduce(out=t["rt"][:, 2 + j:3 + j], in_=t["ohp"][:], axis=AX.X, op=ALU.add), reads=["ohp"], writes=["rt"])
                    P.op("dve", lambda e, ohf=ohf: e.tensor_tensor(out=t["ohp"][:], in0=t["iota32"][:], in1=ohf, op=ALU.mult), reads=["iota32", okey, "rt"], writes=["ohp"])
                    P.op("dve", lambda e, j=j: e.tensor_reduce(out=t["rt"][:, j:j + 1], in_=t["ohp"][:], axis=AX.X, op=ALU.add), reads=["ohp"], writes=["rt"])
                P.op("dve", lambda e: e.tensor_copy(out=t["rt"][:, 4:6], in_=s3[:, 2:4]), reads=["s3"], writes=["rt"])
                P.op("dve", lambda e: e.tensor_tensor(out=t["ohrun"][:], in0=t["ohrun"][:], in1=t["ohs"][:], op=ALU.add), reads=["ohrun", "ohs"], writes=["ohrun"])
                P.dma(lambda e, t0=t0: e.dma_start(out=k.rt[t0:t0 + 128, :], in_=t["rt"][:]), reads=["rt"], writes=[("rtd", t0)], dkey="rtst")
                P.op("pool", lambda e: e.tensor_tensor(out=t["ft1"][:], in0=t["xn0"][:], in1=t["A2row"][:], op=ALU.mult), reads=["xn0", "A2row"], writes=["ft1"])
                P.op("pool", lambda e: e.tensor_tensor(out=t["ftm"][:], in0=t["ft1"][:], in1=t["B2row"][:], op=ALU.add), reads=["ft1", "B2row"], writes=["ftm"])
                P.dma(lambda e, t0=t0: e.dma_start(out=k.ftm[t0:t0 + 128, :], in_=t["ftm"][:]), reads=["ftm"], writes=[("ftmd", t0)], dkey="ftmst")

        LOCALK = set(['yt', 'ysq', 'r', 'm', 'xt0', 'junk0', 'ss0', 'rstd0', 'xn0', 'fT', 'lg', 's1', 's2', 's3', 'mg', 'pr', 'es', 'm1', 'm2', 'e2', 'cw8', 'cw', 'oh1', 'oh2', 'ohs', 'ohp', 'rt', 'ft1', 'ftm']) | {"pst", "pm0", "pm1", "misc", "x1st", "fTst", "cwst", "rtst", "ftmst"}
        for b in range(NB):
            for i0 in range(0, ntile, 2):
                if i0 == 0 or i0 == 16:
                    row = b if i0 < 16 else NB
                    P.dma(lambda e, row=row: e.dma_start(out=t["g1b"][:], in_=k.modrow[l][row, 2 * D:3 * D].partition_broadcast(128)), reads=[("modrow", l)], writes=["g1b"])
                    P.dma(lambda e, row=row: e.dma_start(out=t["B2row"][:], in_=k.modrow[l][row, 3 * D:4 * D].partition_broadcast(128)), reads=[("modrow", l)], writes=["B2row"])
                    P.dma(lambda e, row=row: e.dma_start(out=t["A2row"][:], in_=k.modrow[l][row, 4 * D:5 * D].partition_broadcast(128)), reads=[("modrow", l)], writes=["A2row"])
                    P.op("dve", lambda e: e.scalar_tensor_tensor(out=t["A2row"][:], in0=t["A2row"][:], scalar=1.0, in1=t["g2row"][:], op0=ALU.add, op1=ALU.mult), reads=["A2row", "g2row"], writes=["A2row"])
                recs = []
                for s_ in range(2):
                    if i0 + s_ < ntile:
                        rec = Rec("_%d" % s_, LOCALK)
                        k.P = rec
                        tile_body(ts[s_], b, i0 + s_, other=(ts[0] if s_ == 1 else None))
                        recs.append(rec)
                k.P = P
                replay(P, recs)
        NBLK = k.nblk[l]
        P.op("pe", lambda e: e.matmul(ts[0]["misc"][:, 128:160], lhsT=t["ONESf"][:], rhs=t["ohrun"][:], start=True, stop=True), reads=["ONESf", "ohrun"], writes=["misc_0"])
        P.op("act", lambda e: e.copy(out=t["cnt"][:], in_=ts[0]["misc"][:, 128:160]), reads=["misc_0"], writes=["cnt"])
        P.op("dve", lambda e: e.tensor_scalar(out=t["nf"][:], in0=t["cnt"][:], scalar1=1.0 / MOE_BS, scalar2=None, op0=ALU.mult), reads=["cnt"], writes=["nf"])
        P.op("dve", lambda e: e.tensor_copy(out=t["ni"][:], in_=t["nf"][:]), reads=["nf"], writes=["ni"])
        P.op("dve", lambda e: e.tensor_copy(out=t["nf"][:], in_=t["ni"][:]), reads=["ni"], writes=["nf"])
        P.op("dve", lambda e: e.scalar_tensor_tensor(out=t["dd"][:], in0=t["nf"][:], scalar=float(MOE_BS), in1=t["cnt"][:], op0=ALU.mult, op1=ALU.subtract), reads=["nf", "cnt"], writes=["dd"])
        P.op("dve", lambda e: e.tensor_scalar(out=t["up"][:], in0=t["dd"][:], scalar1=0.0, scalar2=None, op0=ALU.is_lt), reads=["dd"], writes=["up"])
        P.op("dve", lambda e: e.tensor_tensor(out=t["nf"][:], in0=t["nf"][:], in1=t["up"][:], op=ALU.add), reads=["nf", "up"], writes=["nf"])
        P.op("dve", lambda e: e.scalar_tensor_tensor(out=t["dd"][:], in0=t["up"][:], scalar=float(MOE_BS), in1=t["dd"][:], op0=ALU.mult, op1=ALU.add), reads=["up", "dd"], writes=["dd"])
        P.op("dve", lambda e: e.tensor_scalar(out=t["up"][:], in0=t["dd"][:], scalar1=float(MOE_BS), scalar2=None, op0=ALU.is_ge), reads=["dd"], writes=["up"])
        P.op("dve", lambda e: e.tensor_tensor(out=t["nf"][:], in0=t["nf"][:], in1=t["up"][:], op=ALU.subtract), reads=["nf", "up"], writes=["nf"])
        P.op("dve", lambda e: e.tensor_scalar(out=t["nf"][:], in0=t["nf"][:], scalar1=float(MOE_BS), scalar2=None, op0=ALU.mult), reads=["nf"], writes=["nf"])
        P.op("dve", lambda e: e.tensor_tensor_scan(out=t["pend"][:], data0=t["nf"][:], data1=t["zero32"][:], initial=0.0, op0=ALU.add, op1=ALU.add), reads=["nf", "zero32"], writes=["pend"])
        P.op("dve", lambda e: e.tensor_tensor(out=t["nf"][:], in0=t["pend"][:], in1=t["nf"][:], op=ALU.subtract), reads=["pend", "nf"], writes=["nf"])
        P.dma(lambda e: e.dma_start(out=k.pstart[l], in_=t["nf"][:]), reads=["nf"], writes=[("pstart", l)], dkey="pstst")
        P.op("dve", lambda e: e.tensor_tensor(out=t["cmp"][:, 0:NBLK, :], in0=t["pend"][:].unsqueeze(1).to_broadcast([128, NBLK, 32]),
                                              in1=t["jbv"][:, 0:NBLK].unsqueeze(2).to_broadcast([128, NBLK, 32]), op=ALU.is_le), reads=["pend", "jbv"], writes=["cmp"])
        P.op("dve", lambda e: e.tensor_reduce(out=t["bexp"][:, 0:NBLK], in_=t["cmp"][:, 0:NBLK, :], axis=AX.X, op=ALU.add), reads=["cmp"], writes=["bexp"])
        P.op("dve", lambda e: e.tensor_scalar(out=t["bexp"][:, 0:NBLK], in0=t["bexp"][:, 0:NBLK], scalar1=31.0, scalar2=None, op0=ALU.min), reads=["bexp"], writes=["bexp"])
        P.dma(lambda e: e.dma_start(out=k.bexp[l][:, 0:NBLK], in_=t["bexp"][:, 0:NBLK]), reads=["bexp"], writes=[("bexp", l)], dkey="bexst")
        P.barrier()
        P.emit()


MOE_BS = 512
MAXBLK = 72
I32 = mybir.dt.int32


def stage_moe_sparse(k, l):
    nc, P, NB = k.nc, k.P, k.NB
    last = (l == DEPTH - 1)
    ntile = 16 if last else 18
    NBLK = k.nblk[l]
    NSLOT = NBLK * MOE_BS
    with ExitStack() as es:
        t = _tiles(es, nc, [("idf", (128, 128), F32), ("idb", (128, 128), BF16), ("zeros", (128, 4, D), BF16),
                            ("pstart", (128, 32), F32), ("bexp", (128, MAXBLK), F32), ("iota32", (128, 32), F32), ("iotaA", (128, 8), F32), ("iotaB", (128, 4), F32),
                            ("e1k", (128, MAXBLK), F32), ("ixf", (128, MAXBLK, 8), F32), ("ix1", (128, MAXBLK, 8), I32), ("ix2", (128, MAXBLK, 4), I32),
                            ("rtt", (128, 8), F32), ("ohq", (128, 32), F32), ("dsf", (128, 2), F32),
                            ("dest", (128, NB * 18, 2), I32), ("wgt", (128, NB * 18, 2), F32),
                            ("ft0", (128, D), BF16), ("ft1", (128, D), BF16),
                            ("stA0", (128, 8, 512), F32), ("stA1", (128, 8, 512), F32), ("stB0", (128, 8, 512), F32), ("stB1", (128, 8, 512), F32),
                            ("stC0", (128, 4, D), F32), ("stC1", (128, 4, D), F32),
                            ("w1b", (128, 8, 512), BF16), ("w3b", (128, 8, 512), BF16), ("w2b", (128, 4, D), BF16),
                            ("xb0", (128, 4, D), BF16), ("xb1", (128, 4, D), BF16), ("xT", (128, 8, 512), BF16),
                            ("s1", (128, 512), F32), ("act", (128, 4, 512), BF16), ("yb0", (128, 4, D), BF16), ("yb1", (128, 4, D), BF16),
                            ("y1", (128, D), BF16), ("y2", (128, D), BF16), ("acc", (128, D), F32), ("g2b", (128, D), F32), ("xt", (128, D), F32)])
        pstb = _psum(es, nc, "mpst", [128, 8, 128], BF16)
        p1 = [_psum(es, nc, "mp1_%d" % i, [128, 512], F32) for i in range(2)]
        p3 = [_psum(es, nc, "mp3_%d" % i, [128, 512], F32) for i in range(2)]
        py = [_psum(es, nc, "mpy_%d" % i, [128, 512], F32) for i in range(2)]
        P.dma(lambda e: e.dma_start(out=t["idf"][:], in_=k.identf), writes=["idf"])
        P.op("dve", lambda e: e.tensor_copy(out=t["idb"][:], in_=t["idf"][:]), reads=["idf"], writes=["idb"])
        P.op("dve", lambda e: e.memset(t["zeros"][:], 0.0), writes=["zeros"])
        for nm, src in (("iota32", k.c_iota32), ("iotaA", k.c_iotaA), ("iotaB", k.c_iotaB), ("pstart", k.pstart[l])):
            P.dma(lambda e, nm=nm, src=src: e.dma_start(out=t[nm][:], in_=src), writes=[nm])
        P.dma(lambda e: e.dma_start(out=t["bexp"][:, 0:NBLK], in_=k.bexp[l][:, 0:NBLK]), writes=["bexp"])
        xs_v = k.xs.rearrange("(n p) d -> p n d", p=128)
        ys_v = k.ys.rearrange("(n p) d -> p n d", p=128)
        for n0 in range(0, NSLOT // 128, 4):
            P.dma(lambda e, n0=n0: e.dma_start(out=xs_v[:, n0:n0 + 4, :], in_=t["zeros"][:]), reads=["zeros"], writes=["xs"], dkey="xszero")
        P.op("dve", lambda e: e.tensor_scalar(out=t["e1k"][:, 0:NBLK], in0=t["bexp"][:, 0:NBLK], scalar1=1024.0, scalar2=float(l * 32 * 1024), op0=ALU.mult, op1=ALU.add), reads=["bexp"], writes=["e1k"])
        P.op("dve", lambda e: e.tensor_tensor(out=t["ixf"][:, 0:NBLK, :], in0=t["e1k"][:, 0:NBLK].unsqueeze(2).to_broadcast([128, NBLK, 8]),
                                              in1=t["iotaA"][:].unsqueeze(1).to_broadcast([128, NBLK, 8]), op=ALU.add), reads=["e1k", "iotaA"], writes=["ixf"])
        P.op("dve", lambda e: e.tensor_copy(out=t["ix1"][:, 0:NBLK, :], in_=t["ixf"][:, 0:NBLK, :]), reads=["ixf"], writes=["ix1"])
        P.op("dve", lambda e: e.tensor_scalar(out=t["e1k"][:, 0:NBLK], in0=t["bexp"][:, 0:NBLK], scalar1=512.0, scalar2=float(l * 32 * 512), op0=ALU.mult, op1=ALU.add), reads=["bexp", "ixf"], writes=["e1k"])
        P.op("dve", lambda e: e.tensor_tensor(out=t["ixf"][:, 0:NBLK, 0:4], in0=t["e1k"][:, 0:NBLK].unsqueeze(2).to_broadcast([128, NBLK, 4]),
                                              in1=t["iotaB"][:].unsqueeze(1).to_broadcast([128, NBLK, 4]), op=ALU.add), reads=["e1k", "iotaB", "ix1"], writes=["ixf"])
        P.op("dve", lambda e: e.tensor_copy(out=t["ix2"][:, 0:NBLK, :], in_=t["ixf"][:, 0:NBLK, 0:4]), reads=["ixf"], writes=["ix2"])
        tl = []
        for b in range(NB):
            for i in range(ntile):
                tl.append((b, i))
        for n, (b, i) in enumerate(tl):
            t0 = b * TB + i * 128
            ft = t["ft%d" % (n % 2)]
            fk = "ft%d" % (n % 2)
            P.dma(lambda e, t0=t0: e.dma_start(out=t["rtt"][:], in_=k.rt[t0:t0 + 128, :]), writes=["rtt"])
            P.dma(lambda e, t0=t0, ft=ft: e.dma_start(out=ft[:], in_=k.ftm[t0:t0 + 128, :]), writes=[fk])
            for j in range(2):
                P.op("dve", lambda e, j=j: e.tensor_scalar(out=t["ohq"][:], in0=t["iota32"][:], scalar1=t["rtt"][:, j:j + 1], scalar2=None, op0=ALU.is_equal), reads=["iota32", "rtt"], writes=["ohq"])
                P.op("dve", lambda e: e.tensor_tensor(out=t["ohq"][:], in0=t["ohq"][:], in1=t["pstart"][:], op=ALU.mult), reads=["ohq", "pstart"], writes=["ohq"])
                P.op("dve", lambda e, j=j: e.tensor_reduce(out=t["dsf"][:, j:j + 1], in_=t["ohq"][:], axis=AX.X, op=ALU.add), reads=["ohq"], writes=["dsf"])
            P.op("dve", lambda e: e.tensor_tensor(out=t["dsf"][:], in0=t["dsf"][:], in1=t["rtt"][:, 2:4], op=ALU.add), reads=["dsf", "rtt"], writes=["dsf"])
            P.op("dve", lambda e, n=n: e.tensor_copy(out=t["dest"][:, n, :], in_=t["dsf"][:]), reads=["dsf"], writes=[("dest", n)])
            P.op("dve", lambda e, n=n: e.tensor_copy(out=t["wgt"][:, n, :], in_=t["rtt"][:, 4:6]), reads=["rtt"], writes=[("wgt", n)])
            for j in range(2):
                P.dma(lambda e, n=n, j=j, ft=ft: e.indirect_dma_start(out=k.xs[:, :], out_offset=bass.IndirectOffsetOnAxis(ap=t["dest"][:, n, j:j + 1], axis=0), in_=ft[:, :], in_offset=None),
                      reads=[fk, ("dest", n), "xs"], writes=[("xsw", n, j)], q="pool", dkey=("sw_sc" if STRICT_SCATTER else ("sw_sc", j, n % 4)))
        xs_dep = [("xsw", n, j) for n in range(len(tl)) for j in range(2)]
        w1_rows = k.moe_w1.rearrange("l e r n -> (l e r) n")
        w3_rows = k.moe_w3.rearrange("l e r n -> (l e r) n")
        w2_rows = k.moe_w2.rearrange("l e r n -> (l e r) n")
        for jb in range(NBLK):
            sa, sb_, sc_ = t["stA%d" % (jb % 2)], t["stB%d" % (jb % 2)], t["stC%d" % (jb % 2)]
            ka, kb, kc = "stA%d" % (jb % 2), "stB%d" % (jb % 2), "stC%d" % (jb % 2)
            for kk in range(8):
                P.dma(lambda e, jb=jb, kk=kk, sa=sa: e.indirect_dma_start(out=sa[:, kk, :], out_offset=None, in_=w1_rows[:, :], in_offset=bass.IndirectOffsetOnAxis(ap=t["ix1"][:, jb, kk:kk + 1], axis=0)), reads=["ix1"], writes=[ka], q="pool", dkey=("sw_w", ka, kk))
                P.dma(lambda e, jb=jb, kk=kk, sb_=sb_: e.indirect_dma_start(out=sb_[:, kk, :], out_offset=None, in_=w3_rows[:, :], in_offset=bass.IndirectOffsetOnAxis(ap=t["ix1"][:, jb, kk:kk + 1], axis=0)), reads=["ix1"], writes=[kb], q="pool", dkey=("sw_w", kb, kk))
            for c in range(4):
                P.dma(lambda e, jb=jb, c=c, sc_=sc_: e.indirect_dma_start(out=sc_[:, c, :], out_offset=None, in_=w2_rows[:, :], in_offset=bass.IndirectOffsetOnAxis(ap=t["ix2"][:, jb, c:c + 1], axis=0)), reads=["ix2"], writes=[kc], q="pool", dkey=("sw_w", kc, c))
            P.op("act", lambda e, sa=sa: e.copy(out=t["w1b"][:], in_=sa[:]), reads=[ka], writes=["w1b"])
            P.op("act", lambda e, sb_=sb_: e.copy(out=t["w3b"][:], in_=sb_[:]), reads=[kb], writes=["w3b"])
            P.op("dve", lambda e, sc_=sc_: e.tensor_copy(out=t["w2b"][:], in_=sc_[:]), reads=[kc], writes=["w2b"])
            xb = t["xb%d" % (jb % 2)]
            xk = "xb%d" % (jb % 2)
            P.dma(lambda e, jb=jb, xb=xb: e.dma_start(out=xb[:], in_=xs_v[:, jb * 4:(jb + 1) * 4, :]), reads=["xs"] + xs_dep, writes=[xk])
            for sidx in range(4):
                for kk in range(8):
                    P.op("pe", lambda e, sidx=sidx, kk=kk, xb=xb: e.transpose(out=pstb[:, kk, :], in_=xb[:, sidx, kk * 128:(kk + 1) * 128], identity=t["idb"][:]), reads=[xk, "idb"], writes=["mpst"])
                P.op("dve", lambda e, sidx=sidx: e.tensor_copy(out=t["xT"][:, :, sidx * 128:(sidx + 1) * 128], in_=pstb[:, :, :]), reads=["mpst"], writes=["xT"])
            W = MOE_BS
            for ffc in range(4):
                pa, pb = p1[ffc % 2], p3[ffc % 2]
                kpa, kpb = "mp1_%d" % (ffc % 2), "mp3_%d" % (ffc % 2)
                for kk in range(8):
                    P.op("pe", lambda e, pa=pa, kk=kk, ffc=ffc: e.matmul(pa[:, 0:W], lhsT=t["w1b"][:, kk, ffc * 128:(ffc + 1) * 128], rhs=t["xT"][:, kk, :], start=(kk == 0), stop=(kk == 7)),
                         reads=["w1b", "xT"], writes=[kpa], skip_self=(kk > 0))
                for kk in range(8):
                    P.op("pe", lambda e, pb=pb, kk=kk, ffc=ffc: e.matmul(pb[:, 0:W], lhsT=t["w3b"][:, kk, ffc * 128:(ffc + 1) * 128], rhs=t["xT"][:, kk, :], start=(kk == 0), stop=(kk == 7)),
                         reads=["w3b", "xT"], writes=[kpb], skip_self=(kk > 0))
                P.op("act", lambda e, pa=pa: e.activation(out=t["s1"][:, 0:W], in_=pa[:, 0:W], func=AF.Silu), reads=[kpa], writes=["s1"])
                P.op("dve", lambda e, pb=pb, ffc=ffc: e.tensor_tensor(out=t["act"][:, ffc, 0:W], in0=pb[:, 0:W], in1=t["s1"][:, 0:W], op=ALU.mult), reads=[kpb, "s1"], writes=["act"])
            yb = t["yb%d" % (jb % 2)]
            yk = "yb%d" % (jb % 2)
            for sidx in range(4):
                for half in range(2):
                    ps = py[half]
                    pk = "mpy_%d" % half
                    for ffc in range(4):
                        P.op("pe", lambda e, ps=ps, ffc=ffc, sidx=sidx, half=half: e.matmul(ps[:, :], lhsT=t["act"][:, ffc, sidx * 128:(sidx + 1) * 128], rhs=t["w2b"][:, ffc, half * 512:(half + 1) * 512],
                                                                                      start=(ffc == 0), stop=(ffc == 3)), reads=["act", "w2b"], writes=[pk], skip_self=(ffc > 0))
                    if half == 0:
                        P.op("act", lambda e, ps=ps, sidx=sidx, yb=yb: e.copy(out=yb[:, sidx, 0:512], in_=ps[:, :]), reads=[pk], writes=[yk])
                    else:
                        P.op("dve", lambda e, ps=ps, sidx=sidx, yb=yb: e.tensor_copy(out=yb[:, sidx, 512:1024], in_=ps[:, :]), reads=[pk], writes=[yk])
            P.dma(lambda e, jb=jb, yb=yb: e.dma_start(out=ys_v[:, jb * 4:(jb + 1) * 4, :], in_=yb[:]), reads=[yk, "ys"], writes=[("ysw", jb)], dkey=("ysst", jb % 2))
        ys_dep = [("ysw", jb) for jb in range(NBLK)]
        for n, (b, i) in enumerate(tl):
            t0 = b * TB + i * 128
            row = b if i < 16 else NB
            if i == 0 or i == 16:
                P.dma(lambda e, row=row: e.dma_start(out=t["g2b"][:], in_=k.modrow[l][row, 5 * D:6 * D].partition_broadcast(128)), writes=["g2b"])
            for j, yn in ((0, "y1"), (1, "y2")):
                P.dma(lambda e, n=n, j=j, yn=yn: e.indirect_dma_start(out=t[yn][:, :], out_offset=None, in_=k.ys[:, :], in_offset=bass.IndirectOffsetOnAxis(ap=t["dest"][:, n, j:j + 1], axis=0)), reads=[("dest", n), "ys"] + ys_dep, writes=[yn], q="pool", dkey=("sw_y", yn))
            P.dma(lambda e, t0=t0: e.dma_start(out=t["xt"][:], in_=k.x1[t0:t0 + 128, :]), writes=["xt"])
            P.op("dve", lambda e, n=n: e.tensor_scalar(out=t["acc"][:], in0=t["y1"][:], scalar1=t["wgt"][:, n, 0:1], scalar2=None, op0=ALU.mult), reads=["y1", ("wgt", n)], writes=["acc"])
            P.op("dve", lambda e, n=n: e.scalar_tensor_tensor(out=t["acc"][:], in0=t["y2"][:], scalar=t["wgt"][:, n, 1:2], in1=t["acc"][:], op0=ALU.mult, op1=ALU.add), reads=["y2", ("wgt", n), "acc"], writes=["acc"])
            P.op("dve", lambda e: e.tensor_tensor(out=t["acc"][:], in0=t["acc"][:], in1=t["g2b"][:], op=ALU.mult), reads=["acc", "g2b"], writes=["acc"])
            P.op("dve", lambda e: e.tensor_tensor(out=t["xt"][:], in0=t["xt"][:], in1=t["acc"][:], op=ALU.add), reads=["xt", "acc"], writes=["xt"])
            if last:
                P.dma(lambda e, b=b, i=i: e.dma_start(out=k.out[b, i * 128:(i + 1) * 128, :], in_=t["xt"][:]), reads=["xt"], writes=[("outd", b, i)], dkey="outst")
            else:
                P.dma(lambda e, t0=t0: e.dma_start(out=k.x2[t0:t0 + 128, :], in_=t["xt"][:]), reads=["xt"], writes=[("x2d", t0)], dkey="outst")
        P.barrier()
        P.emit()


def stage_moe(k, l):
    nc, P, NB = k.nc, k.P, k.NB
    last = (l == DEPTH - 1)
    ntile = 16 if last else 18
    blocks = BLK5[:4] if last else BLK5
    with ExitStack() as es:
        t = _tiles(es, nc, [("fTb", (128, 8, TB), BF16), ("acc", (128, 18, D), F32), ("cwb", (128, 18, 32), F32),
                            ("st0", (128, 8, 512), F32), ("st1", (128, 8, 512), F32),
                            ("w1b", (128, 8, 512), BF16), ("w3b", (128, 8, 512), BF16), ("w2b", (128, 4, D), BF16),
                            ("s1", (128, 512), F32), ("act", (128, 4, 512), BF16), ("g2b", (128, D), F32), ("xt", (128, D), F32)])
        p1 = [_psum(es, nc, "mp1_%d" % i, [128, 512], F32) for i in range(2)]
        p3 = [_psum(es, nc, "mp3_%d" % i, [128, 512], F32) for i in range(2)]
        py = [_psum(es, nc, "mpy_%d" % i, [128, 512], F32) for i in range(2)]
        fT_v = k.fT.rearrange("(c p) t -> p c t", p=128)
        cw_v = k.cw.rearrange("(n p) e -> p n e", p=128)
        sc = 0
        for b in range(NB):
            nb_tok = ntile * 128
            for c in range(8):
                P.dma(lambda e, b=b, c=c: e.dma_start(out=t["fTb"][:, c, 0:nb_tok], in_=fT_v[:, c, b * TB:b * TB + nb_tok]), writes=["fTb"])
            P.dma(lambda e, b=b: e.dma_start(out=t["cwb"][:, 0:ntile, :], in_=cw_v[:, b * 18:b * 18 + ntile, :]), writes=["cwb"])
            for ex in range(32):
                for nm, src, dst in (("w1", k.moe_w1, "w1b"), ("w3", k.moe_w3, "w3b")):
                    st = t["st%d" % (sc % 2)]
                    sk = "st%d" % (sc % 2)
                    sc += 1
                    P.dma(lambda e, st=st, src=src, ex=ex: e.dma_start(out=st[:], in_=src[l][ex].rearrange("(c p) n -> p c n", p=128)), writes=[sk])
                    P.op("pool", lambda e, st=st, dst=dst: e.tensor_copy(out=t[dst][:], in_=st[:]), reads=[sk], writes=[dst])
                st = t["st%d" % (sc % 2)]
                sk = "st%d" % (sc % 2)
                sc += 1
                stv = st[:].rearrange("p a b -> p (a b)").rearrange("p (c n) -> p c n", c=4)
                P.dma(lambda e, stv=stv, ex=ex: e.dma_start(out=stv, in_=k.moe_w2[l][ex].rearrange("(c p) n -> p c n", p=128)), writes=[sk])
                P.op("pool", lambda e, stv=stv: e.tensor_copy(out=t["w2b"][:], in_=stv), reads=[sk], writes=["w2b"])
                for (g0, W) in blocks:
                    for ffc in range(4):
                        pa, pb = p1[ffc % 2], p3[ffc % 2]
                        ka, kb = "mp1_%d" % (ffc % 2), "mp3_%d" % (ffc % 2)
                        for kk in range(8):
                            P.op("pe", lambda e, pa=pa, kk=kk, ffc=ffc, g0=g0, W=W: e.matmul(pa[:, 0:W], lhsT=t["w1b"][:, kk, ffc * 128:(ffc + 1) * 128], rhs=t["fTb"][:, kk, g0:g0 + W], start=(kk == 0), stop=(kk == 7)),
                                 reads=["w1b", "fTb"], writes=[ka], skip_self=(kk > 0))
                        for kk in range(8):
                            P.op("pe", lambda e, pb=pb, kk=kk, ffc=ffc, g0=g0, W=W: e.matmul(pb[:, 0:W], lhsT=t["w3b"][:, kk, ffc * 128:(ffc + 1) * 128], rhs=t["fTb"][:, kk, g0:g0 + W], start=(kk == 0), stop=(kk == 7)),
                                 reads=["w3b", "fTb"], writes=[kb], skip_self=(kk > 0))
                        P.op("act", lambda e, pa=pa, W=W: e.activation(out=t["s1"][:, 0:W], in_=pa[:, 0:W], func=AF.Silu), reads=[ka], writes=["s1"])
                        P.op("dve", lambda e, pb=pb, W=W, ffc=ffc: e.tensor_tensor(out=t["act"][:, ffc, 0:W], in0=pb[:, 0:W], in1=t["s1"][:, 0:W], op=ALU.mult), reads=[kb, "s1"], writes=["act"])
                    for sidx in range(W // 128):
                        ti = g0 // 128 + sidx
                        for half in range(2):
                            ps = py[half]
                            pk = "mpy_%d" % half
                            for ffc in range(4):
                                P.op("pe", lambda e, ps=ps, ffc=ffc, sidx=sidx, half=half: e.matmul(ps[:, :], lhsT=t["act"][:, ffc, sidx * 128:(sidx + 1) * 128], rhs=t["w2b"][:, ffc, half * 512:(half + 1) * 512],
                                                                                              start=(ffc == 0), stop=(ffc == 3)), reads=["act", "w2b"], writes=[pk], skip_self=(ffc > 0))
                            dst = t["acc"][:, ti, half * 512:(half + 1) * 512]
                            if ex == 0:
                                P.op("dve", lambda e, ps=ps, dst=dst, ti=ti, ex=ex: e.tensor_scalar(out=dst, in0=ps[:, :], scalar1=t["cwb"][:, ti, ex:ex + 1], scalar2=None, op0=ALU.mult), reads=[pk, "cwb"], writes=["acc"])
                            else:
                                P.op("dve", lambda e, ps=ps, dst=dst, ti=ti, ex=ex: e.scalar_tensor_tensor(out=dst, in0=ps[:, :], scalar=t["cwb"][:, ti, ex:ex + 1], in1=dst, op0=ALU.mult, op1=ALU.add),
                                     reads=[pk, "cwb", "acc"], writes=["acc"])
            for i in range(ntile):
                t0 = b * TB + i * 128
                row = b if i < 16 else NB
                if i == 0 or i == 16:
                    P.dma(lambda e, row=row: e.dma_start(out=t["g2b"][:], in_=k.modrow[l][row, 5 * D:6 * D].partition_broadcast(128)), writes=["g2b"])
                P.dma(lambda e, t0=t0: e.dma_start(out=t["xt"][:], in_=k.x1[t0:t0 + 128, :]), writes=["xt"])
                P.op("pool", lambda e, i=i: e.tensor_tensor(out=t["acc"][:, i, :], in0=t["acc"][:, i, :], in1=t["g2b"][:], op=ALU.mult), reads=["acc", "g2b"], writes=["acc"])
                P.op("dve", lambda e, i=i: e.tensor_tensor(out=t["xt"][:], in0=t["xt"][:], in1=t["acc"][:, i, :], op=ALU.add), reads=["xt", "acc"], writes=["xt"])
                if last:
                    P.dma(lambda e, b=b, i=i: e.dma_start(out=k.out[b, i * 128:(i + 1) * 128, :], in_=t["xt"][:]), reads=["xt"], writes=[("outd", b, i)], dkey="outst")
                else:
                    P.dma(lambda e, t0=t0: e.dma_start(out=k.x2[t0:t0 + 128, :], in_=t["xt"][:]), reads=["xt"], writes=[("x2d", t0)], dkey="outst")
        P.barrier()
        P.emit()


def build_program(NB, n_stages=99, dbg=()):
    nc = bass.Bass("TRN2", target_bir_lowering=False)
    k = K()
    k.nc, k.NB = nc, NB
    k.P = Prog(nc)
    T = NB * TB
    k.T = T

    def din(name, shape, dt=F32):
        return nc.dram_tensor(name, list(shape), dt, kind="ExternalInput").ap()

    def dscr(name, shape, dt, out=False):
        return nc.dram_tensor(name, list(shape), dt, kind=("ExternalOutput" if out else "Internal")).ap()

    k.x = din("x", (NB, SEQ, D)); k.ctx = din("ctx", (NB, NCTX, D)); k.c = din("c", (NB, D)); k.c_ctx = din("c_ctx", (D,))
    k.w_mod = din("w_mod", (DEPTH, D, 6 * D)); k.b_mod = din("b_mod", (DEPTH, 6 * D))
    k.norm1_g = din("norm1_g", (DEPTH, D)); k.norm2_g = din("norm2_g", (DEPTH, D))
    k.w_in = din("w_in", (DEPTH, D, INC))
    k.identf = din("identf", (128, 128))
    k.mla_q_norm_g = din("mla_q_norm_g", (DEPTH, 192)); k.mla_w_uq = din("mla_w_uq", (DEPTH, 192, 384))
    k.mla_kv_norm_g = din("mla_kv_norm_g", (DEPTH, 128)); k.mla_w_ukv = din("mla_w_ukv", (DEPTH, 128, 512))
    k.mla_qn_g = din("mla_qn_g", (DEPTH, 96)); k.mla_kn_g = din("mla_kn_g", (DEPTH, 96))
    k.ret_log_gamma = din("ret_log_gamma", (DEPTH, 2, 4)); k.ret_norm_g = din("ret_norm_g", (DEPTH, 256))
    k.lru_conv_w = din("lru_conv_w", (DEPTH, 4, 256)); k.lru_conv_b = din("lru_conv_b", (DEPTH, 256))
    k.lru_wa = din("lru_wa", (DEPTH, 2, 4, 64, 64)); k.lru_ba = din("lru_ba", (DEPTH, 2, 256))
    k.lru_wx = din("lru_wx", (DEPTH, 2, 4, 64, 64)); k.lru_bx = din("lru_bx", (DEPTH, 2, 256)); k.lru_lambda = din("lru_lambda", (DEPTH, 2, 256))
    k.hy_conv_w = din("hy_conv_w", (DEPTH, 3, 768)); k.hy_conv_b = din("hy_conv_b", (DEPTH, 768))
    k.hy_w1 = din("hy_w1", (DEPTH, 33, 64)); k.hy_b1 = din("hy_b1", (DEPTH, 64)); k.hy_w2 = din("hy_w2", (DEPTH, 64, 64)); k.hy_b2 = din("hy_b2", (DEPTH, 64))
    k.hy_w3 = din("hy_w3", (DEPTH, 64, 512)); k.hy_freq = din("hy_freq", (DEPTH, 64)); k.hy_d = din("hy_d", (DEPTH, 256))
    k.c_m0 = din("c_m0", (128, 2))
    k.hc = {}
    for Lh in (SEQ, NCTX):
        k.hc[Lh] = {"zf": din("hz_f%d" % Lh, (33, Lh)), "zr": din("hz_r%d" % Lh, (33, Lh)), "tf": din("ht_f%d" % Lh, (128, Lh // 128)), "tr": din("ht_r%d" % Lh, (128, Lh // 128)),
                    "dl": din("h_dl%d" % Lh, (128, 256)), "Wf": din("h_Wf%d" % Lh, (Lh, 2 * Lh), BF16), "Winv": din("h_Wi%d" % Lh, (2 * Lh, Lh), BF16),
                    "csl": din("h_csl%d" % Lh, (128, 2 * Lh // 128)), "csh": din("h_csh%d" % Lh, (128, 2 * Lh // 128))}
    k.group_norm_g = din("group_norm_g", (DEPTH, D)); k.w_out = din("w_out", (DEPTH, D, D))
    k.moe_w_group = din("moe_w_group", (DEPTH, D, 4)); k.moe_w_expert = din("moe_w_expert", (DEPTH, D, 32))
    k.moe_w1 = din("moe_w1", (DEPTH, 32, D, 512)); k.moe_w3 = din("moe_w3", (DEPTH, 32, D, 512)); k.moe_w2 = din("moe_w2", (DEPTH, 32, 512, D))
    k.x1 = dscr("x1", (T, D), F32, out=("x1" in dbg)); k.fT = dscr("fT", (D, T), BF16); k.cw = dscr("cw", (T, 32), F32, out=("cw" in dbg))
    k.out = nc.dram_tensor("out", [NB, SEQ, D], F32, kind="ExternalOutput").ap()
    k.c_iota32 = din("c_iota32", (128, 32)); k.c_LT = din("c_LT", (128, 128)); k.c_ONES = din("c_ONES", (128, 128)); k.c_jbv = din("c_jbv", (128, 128))
    k.c_iotaA = din("c_iotaA", (128, 8)); k.c_iotaB = din("c_iotaB", (128, 4))
    k.nblk = [-(-(2 * NB * nt_ * 128) // MOE_BS) + 32 for nt_ in (18, 16)]
    assert max(k.nblk) <= MAXBLK
    k.rt = dscr("rt", (T, 8), F32); k.ftm = dscr("ftm", (T, D), BF16)
    k.pstart = dscr("pstart", (DEPTH, 128, 32), F32); k.bexp = dscr("bexp", (DEPTH, 128, MAXBLK), F32)
    k.xs = dscr("xs", (max(k.nblk) * MOE_BS, D), BF16); k.ys = dscr("ys", (max(k.nblk) * MOE_BS, D), BF16)
    k.rope_cos = din("rope_cos", (SEQ, 16)); k.rope_sin = din("rope_sin", (SEQ, 16))
    k.c_rel0 = din("c_rel0", (128, 128)); k.c_mge = din("c_mge", (128, 128)); k.c_mle = din("c_mle", (128, 128)); k.c_dvals = din("c_dvals", (128, 18))
    k.yT = dscr("yT", (D, T), BF16, out=("yT" in dbg))
    k.cc = 0; k.oc = 0; k.yc = 0
    k.modrow = dscr("modrow", (DEPTH, NB + 1, 6 * D), F32, out=("modrow" in dbg))
    k.zt = dscr("zt", (T, ZT_W), BF16, out=("zt" in dbg))
    k.zf = dscr("zf", (ZF_ROWS, T), BF16, out=("zf" in dbg))
    k.x2 = dscr("x2", (T, D), F32)
    with nc.allow_low_precision("bf16 matmul operands, fp32 accumulation"), nc.allow_non_contiguous_dma("small strided loads"):
        stage_mod(k)
        stages = []
        for l in range(DEPTH):
            stages += [lambda l=l: stage_inproj(k, l), lambda l=l: stage_mla(k, l), lambda l=l: stage_ret(k, l), lambda l=l: stage_lru(k, l),
                       lambda l=l: stage_hyena(k, l, SEQ, 0)]
            if l < DEPTH - 1:
                stages.append(lambda l=l: stage_hyena(k, l, NCTX, SEQ))
            stages += [lambda l=l: stage_outproj(k, l), lambda l=l: (stage_moe_sparse(k, l) if SPARSE_MOE else stage_moe(k, l))]
        for si, f in enumerate(stages[:max(0, n_stages - 1)]):
            with nc.named_scope("st%02d" % si):
                f()
    return nc, k


def host_consts():
    c = {"identf": np.eye(128, dtype=np.float32)}
    rows = SEQ // 64
    row = np.repeat(np.arange(rows), 64).astype(np.float32)
    col = np.tile(np.arange(64), rows).astype(np.float32)
    inv_freq = (10000.0 ** (-np.arange(8, dtype=np.float32) / 8)).astype(np.float32)
    ang = np.stack([row[:, None] * inv_freq, col[:, None] * inv_freq], axis=1).astype(np.float32)
    c["rope_cos"] = np.cos(ang).reshape(SEQ, 16).astype(np.float32)
    c["rope_sin"] = np.sin(ang).reshape(SEQ, 16).astype(np.float32)
    jl = np.arange(128, dtype=np.float32)[:, None]
    cc = np.arange(128, dtype=np.float32)[None, :]
    c["c_rel0"] = (cc - jl).astype(np.float32)
    c["c_mge"] = (cc >= jl).astype(np.float32)
    c["c_mle"] = (cc <= jl).astype(np.float32)
    m0 = np.ones((128, 2), np.float32); m0[0, 0] = 0.0; m0[:, 1] = 1.0 - m0[:, 0]
    c["c_m0"] = m0
    names = {"zf": "hz_f", "zr": "hz_r", "tf": "ht_f", "tr": "ht_r", "dl": "h_dl", "Wf": "h_Wf", "Winv": "h_Wi", "csl": "h_csl", "csh": "h_csh"}
    for Lh in (SEQ, NCTX):
        hcn = hyena_consts(Lh)
        for kk2, v in hcn.items():
            c[names[kk2] + str(Lh)] = v
    c["c_iota32"] = np.broadcast_to(np.arange(32, dtype=np.float32)[None, :], (128, 32)).copy()
    tt_ = np.arange(128)
    c["c_LT"] = (tt_[:, None] < tt_[None, :]).astype(np.float32)
    c["c_ONES"] = np.ones((128, 128), np.float32)
    c["c_jbv"] = np.broadcast_to((512.0 * np.arange(128, dtype=np.float32))[None, :], (128, 128)).copy()
    c["c_iotaA"] = (np.arange(8, dtype=np.float32)[None, :] * 128 + np.arange(128, dtype=np.float32)[:, None]).astype(np.float32)
    c["c_iotaB"] = (np.arange(4, dtype=np.float32)[None, :] * 128 + np.arange(128, dtype=np.float32)[:, None]).astype(np.float32)
    c["c_dvals"] = np.broadcast_to(128.0 * np.arange(18, dtype=np.float32)[None, :], (128, 18)).astype(np.float32).copy()
    return c


IN_NAMES = ["w_mod", "b_mod", "norm1_g", "norm2_g", "w_in", "mla_q_norm_g", "mla_w_uq", "mla_kv_norm_g", "mla_w_ukv", "mla_qn_g", "mla_kn_g",
            "ret_log_gamma", "ret_norm_g", "lru_conv_w", "lru_conv_b", "lru_wa", "lru_ba", "lru_wx", "lru_bx", "lru_lambda",
            "hy_conv_w", "hy_conv_b", "hy_w1", "hy_b1", "hy_w2", "hy_b2", "hy_w3", "hy_freq", "hy_d",
            "group_norm_g", "w_out", "moe_w_group", "moe_w_expert", "moe_w1", "moe_w3", "moe_w2"]


def make_in_map(inp, b0, NB, consts=None):
    consts = consts if consts is not None else host_consts()
    m = {"x": np.ascontiguousarray(inp["x"][b0:b0 + NB]), "ctx": np.ascontiguousarray(inp["ctx"][b0:b0 + NB]),
         "c": np.ascontiguousarray(inp["c"][b0:b0 + NB]), "c_ctx": np.asarray(inp["c_ctx"])}
    for n in IN_NAMES:
        m[n] = np.asarray(inp[n])
    m.update(consts)
    return m


_CACHE = {}


def kernel(**inputs):
    NB = 4
    n_cores = 8
    if "nc" not in _CACHE:
        _CACHE["nc"] = build_program(NB)[0]
        _CACHE["consts"] = host_consts()
    nc = _CACHE["nc"]
    in_maps = [make_in_map(inputs, i * NB, NB, _CACHE["consts"]) for i in range(n_cores)]
    res = run_bass_kernel_spmd(nc, in_maps, core_ids=list(range(n_cores)))
    return np.concatenate([np.asarray(r["out"]) for r in res.results], axis=0).astype(np.float32)
```
